# Optimizing a Trainium2 kernel written in Bass

```python
import math
import jax, jax.numpy as jnp
from jax import lax
import numpy as np

D_MODEL = 1024
BATCH = 8
SEQ = 4096
DEPTH = 4

HEAD_DIM = 64
RWKV_HEADS = 4
ATTN_HEADS = 8
RET_HEADS = 4
RWKV_DIM = RWKV_HEADS * HEAD_DIM
ATTN_DIM = ATTN_HEADS * HEAD_DIM
RET_DIM = RET_HEADS * HEAD_DIM
D_MIX = RWKV_DIM + ATTN_DIM + RET_DIM
DECAY_LORA = 64
ICL_LORA = 64
GATE_LORA = 128
RWKV_SPLITS = (RWKV_DIM, 2 * RWKV_DIM, 3 * RWKV_DIM, 3 * RWKV_DIM + DECAY_LORA, 3 * RWKV_DIM + DECAY_LORA + ICL_LORA)
RWKV_IN = 3 * RWKV_DIM + DECAY_LORA + ICL_LORA + GATE_LORA
ATTN_IN = 3 * ATTN_DIM
RET_IN = 4 * RET_DIM
N_IN = RWKV_IN + ATTN_IN + RET_IN
RWKV_GN_EPS = 64e-5
DECAY_SCALE = math.exp(-0.5)
DILATED_PATTERNS = ((128, 1), (512, 4), (2048, 16))
NUM_BUCKETS = 32
MAX_DISTANCE = 2048
ROPE_BASE = 10000.0
RET_CHUNK = 128
FF_DENSE = 2816
N_EXPERTS = 8
TOP_K = 2
FF_EXPERT = 3584
MOE_BLOCK = 256
LN_EPS = 1e-5
DEEPNORM_ALPHA = (2 * DEPTH) ** 0.25
DEEPNORM_BETA = (8 * DEPTH) ** -0.25
N_DENSE = (DEPTH + 1) // 2
N_MOE = DEPTH // 2

kernel_name = 'hybrid_rwkv7_dilated_retention_moe_deepnorm'


def layer_norm(x, g, b):
    xf = x.astype(jnp.float32)
    mu = xf.mean(-1, keepdims=True)
    var = jnp.square(xf - mu).mean(-1, keepdims=True)
    return ((xf - mu) * lax.rsqrt(var + LN_EPS) * g + b).astype(x.dtype)


def head_group_norm(y, g, b, eps):
    b_, s_, h, n = y.shape
    yf = y.astype(jnp.float32)
    mu = yf.mean(-1, keepdims=True)
    var = jnp.square(yf - mu).mean(-1, keepdims=True)
    return ((yf - mu) * lax.rsqrt(var + eps)).reshape(b_, s_, h * n) * g + b


def token_shift(p):
    return jnp.pad(p, ((0, 0), (1, 0), (0, 0)))[:, :-1]


def swiglu(x, wg, wu, wd):
    return (jax.nn.silu(x @ wg) * (x @ wu)) @ wd


def wkv7_scan(r, w, k, v, kap, a):
    def step(state, inp):
        r_t, w_t, k_t, v_t, kap_t, a_t = inp
        sa = jnp.einsum('bhvk,bhk->bhv', state, -kap_t)
        state = (state * w_t[:, :, None, :]
                 + jnp.einsum('bhv,bhk->bhvk', sa, kap_t * a_t)
                 + jnp.einsum('bhv,bhk->bhvk', v_t, k_t))
        return state, jnp.einsum('bhvk,bhk->bhv', state, r_t)
    b_, s_, h, n = r.shape
    s0 = jnp.zeros((b_, h, n, n), jnp.float32)
    xs = tuple(jnp.moveaxis(t, 1, 0) for t in (r, w, k, v, kap, a))
    _, ys = lax.scan(step, s0, xs)
    return jnp.moveaxis(ys, 0, 1)


def rwkv7_time_mix(p, mu, w0, w_up, a0, a_up, g_up, k_k, k_a, r_k, ln_g, ln_b):
    b_, s_, _ = p.shape
    p = p + (token_shift(p) - p) * mu
    r, k, v, xw, xa, xg = jnp.split(p, RWKV_SPLITS, axis=-1)
    decay = jnp.exp(-DECAY_SCALE * jax.nn.sigmoid((w0 + jnp.tanh(xw) @ w_up).astype(jnp.float32)))
    a = jax.nn.sigmoid((a0 + xa @ a_up).astype(jnp.float32))
    g = (jax.nn.sigmoid(xg) @ g_up).astype(jnp.float32)
    heads = lambda t: t.astype(jnp.float32).reshape(b_, s_, RWKV_HEADS, HEAD_DIM)
    hv = lambda t: t.astype(jnp.float32).reshape(RWKV_HEADS, HEAD_DIM)
    r, k, v, a, decay = heads(r), heads(k), heads(v), heads(a), heads(decay)
    kap = k * hv(k_k)
    kap = kap / jnp.maximum(jnp.sqrt(jnp.sum(kap * kap, -1, keepdims=True)), 1e-12)
    k = k * (1.0 + (a - 1.0) * hv(k_a))
    y = wkv7_scan(r, decay, k, v, kap, a)
    y = head_group_norm(y, ln_g, ln_b, RWKV_GN_EPS)
    bonus = (jnp.sum(r * k * hv(r_k), -1, keepdims=True) * v).reshape(b_, s_, RWKV_DIM)
    return (y + bonus) * g


def t5_bucket(dist):
    max_exact = NUM_BUCKETS // 2
    large = max_exact + (np.log(np.maximum(dist, max_exact) / max_exact)
                         / math.log(MAX_DISTANCE / max_exact) * (NUM_BUCKETS - max_exact)).astype(np.int32)
    return np.where(dist < max_exact, dist, np.minimum(large, NUM_BUCKETS - 1)).astype(np.int32)


def dilated_window_attention(q, k, v, rel_bias, window, dilation):
    b_, s_, h, dh = q.shape
    w = window // dilation
    l = s_ // dilation
    nb = -(-l // w)
    lp = nb * w

    def blocks(t):
        t = t.reshape(b_, l, dilation, h, dh)
        t = jnp.pad(t, ((0, 0), (0, lp - l), (0, 0), (0, 0), (0, 0)))
        return t.reshape(b_, nb, w, dilation, h, dh)

    qb, kb, vb = blocks(q), blocks(k), blocks(v)
    prev = lambda t: jnp.concatenate([jnp.zeros_like(t[:, :1]), t[:, :-1]], axis=1)
    kk = jnp.concatenate([prev(kb), kb], axis=2)
    vv = jnp.concatenate([prev(vb), vb], axis=2)
    i = np.arange(w)[:, None]
    j = np.arange(2 * w)[None, :]
    rel = i + w - j
    band = (rel >= 0) & (rel <= w)
    first = (np.arange(nb) == 0)[:, None, None]
    valid = band[None] & ~(first & (j < w)[None])
    bucket = t5_bucket(np.clip(rel, 0, None) * dilation)
    bias = jnp.transpose(rel_bias[bucket], (2, 0, 1)).astype(jnp.float32)
    s = jnp.einsum('bnidhe,bnjdhe->bndhij', qb, kk).astype(jnp.float32) * (dh ** -0.5) + bias
    s = jnp.where(valid[None, :, None, None], s, -jnp.inf)
    m = s.max(-1, keepdims=True)
    pr = jnp.exp(s - m)
    den = pr.sum(-1)
    o = jnp.einsum('bndhij,bnjdhe->bndhie', pr, vv.astype(jnp.float32)) / den[..., None]
    lse = m[..., 0] + jnp.log(den)
    o = jnp.transpose(o, (0, 1, 4, 2, 3, 5)).reshape(b_, lp, dilation, h, dh)[:, :l].reshape(b_, s_, h, dh)
    lse = jnp.transpose(lse, (0, 1, 4, 2, 3)).reshape(b_, lp, dilation, h)[:, :l].reshape(b_, s_, h)
    return o, lse


def dilated_attention_mixture(p, rel_bias):
    b_, s_, _ = p.shape
    q, k, v = (t.reshape(b_, s_, ATTN_HEADS, HEAD_DIM) for t in jnp.split(p, 3, axis=-1))
    outs, lses = [], []
    for window, dilation in DILATED_PATTERNS:
        o, lse = dilated_window_attention(q, k, v, rel_bias, window, dilation)
        outs.append(o)
        lses.append(lse)
    wts = jax.nn.softmax(jnp.stack(lses), axis=0)
    out = jnp.einsum('pbsh,pbshe->bshe', wts, jnp.stack(outs))
    return out.reshape(b_, s_, ATTN_DIM)


def rotary(x):
    s_, dh = x.shape[1], x.shape[-1]
    half = dh // 2
    inv = ROPE_BASE ** (-jnp.arange(half, dtype=jnp.float32) / half)
    ang = jnp.arange(s_, dtype=jnp.float32)[:, None] * inv
    cos, sin = jnp.cos(ang)[None, :, None, :], jnp.sin(ang)[None, :, None, :]
    x1, x2 = x[..., :half], x[..., half:]
    return jnp.concatenate([x1 * cos - x2 * sin, x1 * sin + x2 * cos], axis=-1)


def retention_chunkwise(q, k, v):
    b_, s_, h, dk = q.shape
    c = RET_CHUNK
    nc = s_ // c
    log_g = jnp.log1p(-jnp.exp2(-5.0 - jnp.arange(h, dtype=jnp.float32)))
    n = jnp.arange(c, dtype=jnp.float32)
    diff = n[:, None] - n[None, :]
    d_intra = jnp.where(diff >= 0, jnp.exp(log_g[:, None, None] * jnp.maximum(diff, 0.0)), 0.0)
    zeta = jnp.exp(log_g[:, None] * (c - 1 - n))
    xi = jnp.exp(log_g[:, None] * (n + 1))
    chunk_decay = jnp.exp(log_g * c)
    qc, kc, vc = (t.reshape(b_, nc, c, h, -1) for t in (q, k, v))
    sc = jnp.einsum('bcnhd,bcmhd->bchnm', qc, kc) * d_intra
    intra = jnp.einsum('bchnm,bcmhe->bcnhe', sc, vc)
    kv = jnp.einsum('bcmhd,bcmhe->bchde', kc * zeta.T[:, :, None], vc)

    def step(state, kv_c):
        return state * chunk_decay[None, :, None, None] + kv_c, state

    _, r_prev = lax.scan(step, jnp.zeros_like(kv[:, 0]), jnp.moveaxis(kv, 1, 0))
    cross = jnp.einsum('bcnhd,cbhde->bcnhe', qc * xi.T[:, :, None], r_prev)
    return (intra + cross).reshape(b_, s_, h, -1)


def retention_mixer(p, gn_g, gn_b):
    b_, s_, _ = p.shape
    q, k, v, g = jnp.split(p, 4, axis=-1)
    hd = lambda t: t.astype(jnp.float32).reshape(b_, s_, RET_HEADS, HEAD_DIM)
    q = rotary(hd(q))
    k = rotary(hd(k)) * (HEAD_DIM ** -0.5)
    y = retention_chunkwise(q, k, hd(v))
    y = head_group_norm(y, gn_g, gn_b, LN_EPS)
    return jax.nn.silu(g.astype(jnp.float32)) * y


def hybrid_mixer(x, w_in, w_out, mu, w0, w_up, a0, a_up, g_up, k_k, k_a, r_k, rln_g, rln_b,
                 gn_g, gn_b, rel_bias):
    p = x @ w_in
    p_rwkv, p_attn, p_ret = jnp.split(p, [RWKV_IN, RWKV_IN + ATTN_IN], axis=-1)
    y_a = rwkv7_time_mix(p_rwkv, mu, w0, w_up, a0, a_up, g_up, k_k, k_a, r_k, rln_g, rln_b)
    y_b = dilated_attention_mixture(p_attn, rel_bias)
    y_c = retention_mixer(p_ret, gn_g, gn_b)
    y = jnp.concatenate([y_a.astype(x.dtype), y_b.astype(x.dtype), y_c.astype(x.dtype)], axis=-1)
    return y @ w_out


def moe_ffn(x, router, wg, wu, wd):
    b_, s_, d = x.shape
    t = b_ * s_
    xt = x.reshape(t, d)
    logits = (xt @ router).astype(jnp.float32)
    top_val, top_idx = lax.top_k(logits, TOP_K)
    gates = jax.nn.softmax(top_val, axis=-1)
    e_flat = top_idx.reshape(-1)
    tok_flat = jnp.repeat(jnp.arange(t), TOP_K)
    g_flat = gates.reshape(-1)
    order = jnp.argsort(e_flat)
    e_sorted, tok_sorted, g_sorted = e_flat[order], tok_flat[order], g_flat[order]
    counts = jnp.bincount(e_flat, length=N_EXPERTS)
    starts = jnp.cumsum(counts) - counts
    padded = (counts + MOE_BLOCK - 1) // MOE_BLOCK * MOE_BLOCK
    pad_starts = jnp.cumsum(padded) - padded
    pad_ends = pad_starts + padded
    dest = pad_starts[e_sorted] + jnp.arange(t * TOP_K) - starts[e_sorted]
    rows = t * TOP_K + N_EXPERTS * MOE_BLOCK
    n_blk = rows // MOE_BLOCK
    buf = jnp.zeros((rows, d), x.dtype).at[dest].set(xt[tok_sorted])
    blk_e = jnp.minimum(jnp.searchsorted(pad_ends, jnp.arange(n_blk) * MOE_BLOCK, side='right'), N_EXPERTS - 1)

    def run(args):
        xb, e = args
        return swiglu(xb, wg[e], wu[e], wd[e])

    yb = lax.map(run, (buf.reshape(n_blk, MOE_BLOCK, d), blk_e)).reshape(rows, d)
    y_assign = yb[dest] * g_sorted[:, None].astype(yb.dtype)
    out = jnp.zeros((t, d), yb.dtype).at[tok_sorted].add(y_assign)
    return out.reshape(b_, s_, d)


def setup_inputs(seed: int = 0) -> dict:
    key = jax.random.key(seed)
    ks = jax.random.split(key, 27)
    f32 = jnp.float32
    nrm = lambda i, shape, scale: jax.random.normal(ks[i], shape, f32) * scale
    beta = DEEPNORM_BETA
    return {
        'x': nrm(0, (BATCH, SEQ, D_MODEL), 1.0),
        'w_in': nrm(1, (DEPTH, D_MODEL, N_IN), D_MODEL ** -0.5),
        'w_out': nrm(2, (DEPTH, D_MIX, D_MODEL), beta * D_MIX ** -0.5),
        'rwkv_mu': jax.random.uniform(ks[3], (DEPTH, RWKV_IN), f32),
        'rwkv_w0': jax.random.uniform(ks[4], (DEPTH, RWKV_DIM), f32, -4.0, 1.0),
        'rwkv_w_up': nrm(5, (DEPTH, DECAY_LORA, RWKV_DIM), 0.1),
        'rwkv_a0': nrm(6, (DEPTH, RWKV_DIM), 0.1),
        'rwkv_a_up': nrm(7, (DEPTH, ICL_LORA, RWKV_DIM), ICL_LORA ** -0.5),
        'rwkv_g_up': nrm(8, (DEPTH, GATE_LORA, RWKV_DIM), GATE_LORA ** -0.5),
        'rwkv_k_k': 0.85 + nrm(9, (DEPTH, RWKV_DIM), 0.05),
        'rwkv_k_a': 1.0 + nrm(10, (DEPTH, RWKV_DIM), 0.05),
        'rwkv_r_k': nrm(11, (DEPTH, RWKV_DIM), 0.1),
        'rwkv_ln_g': 1.0 + nrm(12, (DEPTH, RWKV_DIM), 0.02),
        'rwkv_ln_b': nrm(13, (DEPTH, RWKV_DIM), 0.02),
        'ret_gn_g': 1.0 + nrm(14, (DEPTH, RET_DIM), 0.02),
        'ret_gn_b': nrm(15, (DEPTH, RET_DIM), 0.02),
        'rel_bias': nrm(16, (NUM_BUCKETS, ATTN_HEADS), 0.2),
        'ln_g': 1.0 + nrm(17, (DEPTH, 2, D_MODEL), 0.02),
        'ln_b': nrm(18, (DEPTH, 2, D_MODEL), 0.02),
        'ffn_w_gate': nrm(19, (N_DENSE, D_MODEL, FF_DENSE), D_MODEL ** -0.5),
        'ffn_w_up': nrm(20, (N_DENSE, D_MODEL, FF_DENSE), D_MODEL ** -0.5),
        'ffn_w_down': nrm(21, (N_DENSE, FF_DENSE, D_MODEL), beta * FF_DENSE ** -0.5),
        'moe_router': nrm(22, (N_MOE, D_MODEL, N_EXPERTS), D_MODEL ** -0.5),
        'moe_w_gate': nrm(23, (N_MOE, N_EXPERTS, D_MODEL, FF_EXPERT), D_MODEL ** -0.5),
        'moe_w_up': nrm(24, (N_MOE, N_EXPERTS, D_MODEL, FF_EXPERT), D_MODEL ** -0.5),
        'moe_w_down': nrm(25, (N_MOE, N_EXPERTS, FF_EXPERT, D_MODEL), beta * FF_EXPERT ** -0.5),
    }


def reference(x, w_in, w_out, rwkv_mu, rwkv_w0, rwkv_w_up, rwkv_a0, rwkv_a_up, rwkv_g_up, rwkv_k_k,
              rwkv_k_a, rwkv_r_k, rwkv_ln_g, rwkv_ln_b, ret_gn_g, ret_gn_b, rel_bias, ln_g, ln_b,
              ffn_w_gate, ffn_w_up, ffn_w_down, moe_router, moe_w_gate, moe_w_up, moe_w_down):
    for layer in range(DEPTH):
        h = hybrid_mixer(x, w_in[layer], w_out[layer], rwkv_mu[layer], rwkv_w0[layer], rwkv_w_up[layer],
                         rwkv_a0[layer], rwkv_a_up[layer], rwkv_g_up[layer], rwkv_k_k[layer], rwkv_k_a[layer],
                         rwkv_r_k[layer], rwkv_ln_g[layer], rwkv_ln_b[layer], ret_gn_g[layer], ret_gn_b[layer],
                         rel_bias)
        x = layer_norm(DEEPNORM_ALPHA * x + h, ln_g[layer, 0], ln_b[layer, 0])
        j = layer // 2
        if layer % 2 == 0:
            f = swiglu(x, ffn_w_gate[j], ffn_w_up[j], ffn_w_down[j])
        else:
            f = moe_ffn(x, moe_router[j], moe_w_gate[j], moe_w_up[j], moe_w_down[j])
        x = layer_norm(DEEPNORM_ALPHA * x + f, ln_g[layer, 1], ln_b[layer, 1])
    return x
```

```python
import math
from contextlib import ExitStack
import numpy as np
import concourse.bass as bass
import concourse.mybir as mybir
from concourse.bass_utils import run_bass_kernel_spmd

F32 = mybir.dt.float32
BF16 = mybir.dt.bfloat16
ALU = mybir.AluOpType
AF = mybir.ActivationFunctionType
AX = mybir.AxisListType

D = 1024
S = 4096
DEPTH = 4
NT = S // 128
RWKV_IN = 1024
ATTN_IN = 1536
RET_IN = 1024
N_IN = 3584
FF_DENSE = 2816
FF_EXPERT = 3584
N_EXPERTS = 8
ALPHA = (2 * DEPTH) ** 0.25
LN_EPS = 1e-5
RWKV_GN_EPS = 64e-5
C0 = math.exp(-0.5)
PATTERNS = ((128, 1), (512, 4), (2048, 16))
NEG = -240000.0


class Dep:
    __slots__ = ("w", "r", "excl")

    def __init__(self, excl=False):
        self.w = None
        self.r = {}
        self.excl = excl


class Engine:
    def __init__(self, e, sem, idx):
        self.e = e
        self.sem = sem
        self.idx = idx
        self.cnt = 0
        self.seen = {}
        self.slots = []
        self.si = 0


class KB:
    NSLOT = 12

    def __init__(self, nc, es):
        self.nc = nc
        self.sems = []
        self.eng = {}
        for name, e in (("pe", nc.tensor), ("dve", nc.vector), ("act", nc.scalar),
                        ("pool", nc.gpsimd), ("sp", nc.sync)):
            sem = es.enter_context(nc.semaphore("s_" + name))
            self.sems.append(sem)
            self.eng[name] = Engine(e, sem, len(self.sems) - 1)
        for q in ("sp", "pool"):
            for i in range(self.NSLOT):
                sem = es.enter_context(nc.semaphore("d_%s%d" % (q, i)))
                self.sems.append(sem)
                self.eng[q].slots.append([len(self.sems) - 1, 0])
        self.nins = 0

    def _need(self, E, ev, raw=True):
        si, val = ev
        if val <= 0:
            return
        if si == E.idx and val > E.cnt:
            return
        if E.seen.get(si, 0) < val:
            E.e.wait_ge(self.sems[si], val)
            E.seen[si] = val

    def _waits(self, E, r, w):
        for d in r:
            if d.excl:
                if d.w is not None:
                    self._need(E, d.w)
                for si, v in d.r.items():
                    self._need(E, (si, v), raw=False)
            elif d.w is not None:
                self._need(E, d.w)
        for d in w:
            if d.w is not None:
                self._need(E, d.w, raw=False)
            for si, v in d.r.items():
                self._need(E, (si, v), raw=False)

    def _post(self, ev, r, w):
        for d in w:
            d.w = ev
            d.r = {}
        for d in r:
            if d.excl:
                d.w = ev
                d.r = {}
            elif d.r.get(ev[0], 0) < ev[1]:
                d.r[ev[0]] = ev[1]

    def op(self, eng, fn, r=(), w=(), mark=True):
        E = self.eng[eng]
        self._waits(E, r, w)
        ins = fn(E.e)
        self.nins += 1
        if mark:
            E.cnt += 1
            ins.then_inc(E.sem, 1)
            ev = (E.idx, E.cnt)
        else:
            ev = (E.idx, E.cnt + 1)
        self._post(ev, r, w)
        return ins

    def dma(self, q, out, in_, r=(), w=(), slow=False):
        E = self.eng[q]
        slot = E.slots[E.si]
        E.si = (E.si + 1) % len(E.slots)
        self._need(E, (slot[0], slot[1]))
        self._waits(E, r, w)
        if slow:
            ins = E.e.dma_start(out=out, in_=in_, allow_slow_non_contiguous=True)
        else:
            ins = E.e.dma_start(out=out, in_=in_)
        self.nins += 1
        slot[1] += 16
        ins.then_inc(self.sems[slot[0]], 16)
        ev = (slot[0], slot[1])
        self._post(ev, r, w)
        return ins

    def barrier(self):
        evs = []
        for E in self.eng.values():
            if E.cnt > 0:
                evs.append((E.idx, E.cnt))
            for s in E.slots:
                if s[1] > 0:
                    evs.append((s[0], s[1]))
        for E in self.eng.values():
            for ev in evs:
                if ev[0] != E.idx:
                    self._need(E, ev)


class Ring:
    def __init__(self, tile, n):
        self.t = tile
        self.n = n
        self.d = [Dep() for _ in range(n)]
        self.i = 0

    def next(self):
        i = self.i
        self.i = (i + 1) % self.n
        return self.t[:, i], self.d[i]


class BRing:
    def __init__(self, items):
        self.items = list(items)
        self.i = 0

    def next(self):
        it = self.items[self.i]
        self.i = (self.i + 1) % len(self.items)
        return it


def run_interleaved(gens):
    gens = list(gens)
    while gens:
        nxt = []
        for g in gens:
            try:
                next(g)
                nxt.append(g)
            except StopIteration:
                pass
        gens = nxt


def t5_bucket(dist):
    nb = 32
    max_exact = nb // 2
    large = max_exact + (np.log(np.maximum(dist, max_exact) / max_exact)
                         / math.log(2048 / max_exact) * (nb - max_exact)).astype(np.int32)
    return np.where(dist < max_exact, dist, np.minimum(large, nb - 1)).astype(np.int32)


CST = {}


def _cst_layout():
    off = 0
    for name, n in (("ident", 128), ("onesblk", 128), ("nSU", 128), ("nSL", 128), ("SU", 128),
                    ("nIU", 128), ("IU", 128), ("sel", 64), ("sel65", 64), ("dmaskT", 512),
                    ("zeta8", 4), ("xi", 256), ("scanm", 1024)):
        CST[name] = (off, n)
        off += n
    return off


CST_N = _cst_layout()


def make_consts():
    c = np.zeros((128, CST_N), np.float32)

    def put(name, arr):
        o, n = CST[name]
        c[:arr.shape[0], o:o + n] = arr.reshape(arr.shape[0], n)

    i = np.arange(128)
    put("ident", np.eye(128, dtype=np.float32))
    put("onesblk", (i[:, None] // 64 == i[None, :] // 64).astype(np.float32))
    su = (i[None, :] > i[:, None]).astype(np.float32)
    sl = (i[None, :] < i[:, None]).astype(np.float32)
    iu = (i[None, :] >= i[:, None]).astype(np.float32)
    put("nSU", -su)
    put("nSL", -sl)
    put("SU", su)
    put("nIU", -iu)
    put("IU", iu)
    put("sel", np.concatenate([np.eye(64), np.eye(64)], 0).astype(np.float32))
    s65 = np.zeros((128, 64), np.float32)
    s65[64, :] = 1.0
    put("sel65", s65)
    lg = np.log1p(-np.exp2(-5.0 - np.arange(4, dtype=np.float64)))
    m = np.arange(128)[:, None, None]
    n = np.arange(128)[None, None, :]
    h = lg[None, :, None]
    dm = np.where(n >= m, np.exp(h * np.maximum(n - m, 0)), 0.0) / 8.0
    put("dmaskT", dm.astype(np.float32))
    put("zeta8", (np.exp(lg[None, :] * (127 - np.arange(128)[:, None])) / 8.0).astype(np.float32))
    xi = np.zeros((128, 2, 128))
    for hp in range(2):
        for hh in range(2):
            xi[hh * 64:(hh + 1) * 64, hp, :] = np.exp(lg[2 * hp + hh] * (np.arange(128) + 1))[None, :]
    put("xi", xi.astype(np.float32))
    sm = np.ones((128, 1024), np.float32)
    sm[:, 0::64] = 0.0
    put("scanm", sm)
    half = 32
    inv = (10000.0 ** (-np.arange(half, dtype=np.float32) / half)).astype(np.float32)
    ang = (np.arange(S, dtype=np.float32)[None, :] * inv[:, None]).astype(np.float32)
    cos = np.cos(ang).astype(np.float32)
    sin = np.sin(ang).astype(np.float32)
    cos2 = np.concatenate([cos, cos, cos, cos], 0)
    sin2 = np.concatenate([-sin, sin, -sin, sin], 0)
    jj = np.arange(128)[:, None]
    ii = np.arange(128)[None, :]
    mk = np.zeros((2, 128, 128), np.float32)
    mk[0] = np.where(ii <= jj, 0.0, NEG)
    mk[1] = np.where(ii >= jj, 0.0, NEG)
    gamma_c = np.exp(lg * 128.0)
    return dict(cst=c, cos2=np.ascontiguousarray(cos2), sin2=np.ascontiguousarray(sin2), amask=mk,
                gamma_c=[float(g) for g in gamma_c])


def bias_bucket_index():
    jj = np.arange(128)[:, None]
    ii = np.arange(128)[None, :]
    out = np.zeros((3, 2, 128, 128), np.int64)
    for p, (win, dil) in enumerate(PATTERNS):
        rel_prev = np.clip(ii + 128 - jj, 0, None)
        rel_cur = np.clip(ii - jj, 0, None)
        out[p, 0] = t5_bucket(rel_prev * dil)
        out[p, 1] = t5_bucket(rel_cur * dil)
    return out


class Prog:
    def __init__(self, n_layers=DEPTH, dbg=False, stop=None):
        self.n_layers = n_layers
        self.dbg = dbg
        self.stop = stop
        self.consts = make_consts()
        self.nc = nc = bass.Bass("TRN2", target_bir_lowering=False)

        def din(name, shape, dt=F32):
            return nc.dram_tensor(name, list(shape), dt, kind="ExternalInput").ap()

        self.I = I = {}
        I["x"] = din("x", [S, D])
        I["w_in"] = din("w_in", [DEPTH, D, N_IN])
        I["w_out"] = din("w_out", [DEPTH, D, D])
        I["rwkv_mu"] = din("rwkv_mu", [DEPTH, RWKV_IN])
        for nm in ("rwkv_w0", "rwkv_a0", "rwkv_k_k", "rwkv_k_a", "rwkv_r_k", "rwkv_ln_g", "rwkv_ln_b",
                   "ret_gn_g", "ret_gn_b"):
            I[nm] = din(nm, [DEPTH, 256])
        I["rwkv_w_up"] = din("rwkv_w_up", [DEPTH, 64, 256])
        I["rwkv_a_up"] = din("rwkv_a_up", [DEPTH, 64, 256])
        I["rwkv_g_up"] = din("rwkv_g_up", [DEPTH, 128, 256])
        I["ln_g"] = din("ln_g", [DEPTH, 2, D])
        I["ln_b"] = din("ln_b", [DEPTH, 2, D])
        I["ffn_w_gate"] = din("ffn_w_gate", [2, D, FF_DENSE])
        I["ffn_w_up"] = din("ffn_w_up", [2, D, FF_DENSE])
        I["ffn_w_down"] = din("ffn_w_down", [2, FF_DENSE, D])
        I["moe_router"] = din("moe_router", [2, D, N_EXPERTS])
        I["moe_w_gate"] = din("moe_w_gate", [2, N_EXPERTS, D, FF_EXPERT])
        I["moe_w_up"] = din("moe_w_up", [2, N_EXPERTS, D, FF_EXPERT])
        I["moe_w_down"] = din("moe_w_down", [2, N_EXPERTS, FF_EXPERT, D])
        I["cst"] = din("cst", [128, CST_N])
        I["cos2"] = din("cos2", [128, S])
        I["sin2"] = din("sin2", [128, S])
        I["amask"] = din("amask", [2, 128, 128])
        I["biasg"] = din("biasg", [3, 8, 2, 128, 128])
        self.out = nc.dram_tensor("out", [S, D], F32, kind="ExternalOutput").ap()

        def scr(name, shape, dt):
            kind = "ExternalOutput" if dbg else "Internal"
            return nc.dram_tensor(name, list(shape), dt, kind=kind).ap()

        self.xres = scr("xres", [S, D], F32)
        self.pT = scr("pT", [N_IN, S], F32)
        self.vtok = scr("vtok", [S, 768], BF16)
        self.yT = scr("yT", [D, S], BF16)

    _uid = 0

    def un(self, n):
        self._uid += 1
        return "%s_%d" % (n, self._uid)

    def cst(self, name, rows=128):
        o, n = CST[name]
        return self.cstt[0:rows, o:o + n]

    def build(self):
        nc = self.nc
        with ExitStack() as es:
            self.kb = kb = KB(nc, es)
            self.arena = es.enter_context(nc.sbuf_tensor("arena", [128, 16384], F32))
            self.XT = self.arena[:, :].bitcast(BF16).rearrange("p (k s) -> p k s", k=8)
            self.XTd = [Dep() for _ in range(8)]
            self.pbank_t = es.enter_context(nc.psum_tensor("psum", [128, 8, 512], F32))
            self.bank = [(self.pbank_t[:, i], Dep(excl=True)) for i in range(8)]
            self.cstt = es.enter_context(nc.sbuf_tensor("cstt", [128, CST_N], F32))
            self.cstd = Dep()
            self.identb = es.enter_context(nc.sbuf_tensor("identb", [128, 128], BF16))
            self.bias8 = es.enter_context(nc.sbuf_tensor("bias8", [128, 3, 8, 2, 128], BF16))
            self.gates = es.enter_context(nc.sbuf_tensor("gates", [128, NT, N_EXPERTS], F32))
            self.gatesd = Dep()
            kb.dma("sp", self.cstt[:], self.I["cst"], w=[self.cstd])
            kb.op("dve", lambda e: e.tensor_copy(out=self.identb[:], in_=self.cst("ident")),
                  r=[self.cstd], w=[self.cstd])
            self.setup_bias()
            self.load_x0()
            kb.barrier()
            for l in range(self.n_layers):
                self.layer(l)
                if self.stop is not None and self.stop[0] == l and self.stop[1] == "layer":
                    break
            kb.barrier()
        return nc

    def setup_bias(self):
        nc, kb = self.nc, self.kb
        with ExitStack() as es:
            tg = es.enter_context(nc.sbuf_tensor(self.un("tg"), [128, 2, 16, 128], F32))
            mk = es.enter_context(nc.sbuf_tensor(self.un("mk"), [128, 2, 128], F32))
            mkd = Dep()
            kb.dma("sp", mk[:], self.I["amask"].rearrange("a j i -> j a i"), w=[mkd])
            ring = Ring(tg, 2)
            for p in range(3):
                t, d = ring.next()
                kb.dma("sp", t.rearrange("j (h a) i -> j h a i", a=2),
                       self.I["biasg"][p].rearrange("h a j i -> j h a i"), w=[d])
                for h in range(8):
                    kb.op("dve", lambda e, t=t, h=h, p=p: e.scalar_tensor_tensor(
                        out=self.bias8[:, p, h], in0=t[:, 2 * h:2 * h + 2, :], scalar=8.0, in1=mk[:],
                        op0=ALU.mult, op1=ALU.add), r=[d, mkd], w=[self.cstd])
            kb.barrier()

    def load_x0(self):
        nc, kb = self.nc, self.kb
        with ExitStack() as es:
            xt = es.enter_context(nc.sbuf_tensor(self.un("x0t"), [128, 2, D], F32))
            xr = Ring(xt, 2)
            pr = self.tr_ring()
            for t in range(NT):
                x, xd = xr.next()
                kb.dma("sp", x, self.I["x"][t * 128:(t + 1) * 128, :], w=[xd])
                kb.dma("sp", self.xres[t * 128:(t + 1) * 128, :], x, r=[xd])
                self.transpose_to_XT(x, xd, t, pr)
            kb.barrier()

    def tr_ring(self, pairs=((0, 1), (2, 3))):
        items = []
        for a, b in pairs:
            ap = self.pbank_t[:, a:b + 1].rearrange("p a (b c) -> p (a b) c", c=128)
            items.append((ap, [self.bank[a][1], self.bank[b][1]]))
        return BRing(items)

    def transpose_to_XT(self, x, xd, t, pr, f32_out=None):
        kb = self.kb
        p, pds = pr.next()
        for k in range(8):
            kb.op("pe", lambda e, k=k: e.transpose(p[:, k, :], x[:, k * 128:(k + 1) * 128], self.cst("ident")),
                  r=[xd, self.cstd], w=pds, mark=(k == 7))
        xd_blk = self.XTd[t // 4]
        kb.op("act", lambda e: e.copy(out=self.XT[:, :, t * 128:(t + 1) * 128], in_=p), r=pds, w=[xd_blk])
        if f32_out is not None:
            o, od = f32_out
            kb.op("dve", lambda e: e.tensor_copy(out=o, in_=p), r=pds, w=[od])

    def layer(self, l):
        kb = self.kb
        self.inproj(l)
        kb.barrier()
        if self.stop == (l, "inproj"):
            return
        self.retention(l)
        kb.barrier()
        if self.stop == (l, "ret"):
            return
        self.attention(l)
        kb.barrier()
        if self.stop == (l, "attn"):
            return
        self.rwkv(l)
        kb.barrier()
        if self.stop == (l, "rwkv"):
            return
        self.outproj_ln1(l)
        kb.barrier()
        if self.stop == (l, "ln1"):
            return
        self.ffn(l)
        kb.barrier()

    def inproj(self, l):
        nc, kb = self.nc, self.kb
        w_in = self.I["w_in"][l]
        with ExitStack() as es:
            wt = es.enter_context(nc.sbuf_tensor(self.un("ip_w"), [128, 2, 8, 512], BF16))
            stg = es.enter_context(nc.sbuf_tensor(self.un("ip_s"), [128, 2, S], F32))
            wv = es.enter_context(nc.sbuf_tensor(self.un("ip_wv"), [128, 8, 768], BF16))
            vt = es.enter_context(nc.sbuf_tensor(self.un("ip_vt"), [128, 2, 768], BF16))
            wr, pr, vr = Ring(wt, 2), BRing(self.bank), Ring(vt, 2)
            sdeps = [[Dep() for _ in range(8)] for _ in range(2)]
            wvd = Dep()
            kb.dma("pool", wv[:, :, 0:512], w_in[:, 2048:2560].rearrange("(k p) n -> p k n", p=128), w=[wvd])
            kb.dma("pool", wv[:, :, 512:768], w_in[:, 3072:3328].rearrange("(k p) n -> p k n", p=128), w=[wvd])
            n_ev = 0
            si = 0
            for cb in range(7):
                w, wd = wr.next()
                kb.dma("pool", w, w_in[:, cb * 512:(cb + 1) * 512].rearrange("(k p) n -> p k n", p=128), w=[wd])
                for fc in range(4):
                    st = stg[:, si]
                    sd = sdeps[si]
                    si ^= 1
                    for tb in range(8):
                        p, pd = pr.next()
                        for k in range(8):
                            kb.op("pe", lambda e, k=k: e.matmul(
                                p, lhsT=w[:, k, fc * 128:(fc + 1) * 128], rhs=self.XT[:, k, tb * 512:(tb + 1) * 512],
                                start=(k == 0), stop=(k == 7)), r=[wd, self.XTd[tb]], w=[pd], mark=(k == 7))
                        o = st[:, tb * 512:(tb + 1) * 512]
                        if n_ev % 2 == 0:
                            kb.op("act", lambda e: e.copy(out=o, in_=p), r=[pd], w=[sd[tb]])
                        else:
                            kb.op("dve", lambda e: e.tensor_copy(out=o, in_=p), r=[pd], w=[sd[tb]])
                        n_ev += 1
                    row = (cb * 4 + fc) * 128
                    kb.dma("sp", self.pT[row:row + 128, :], st, r=sd)
            for t in range(NT):
                v, vd = vr.next()
                p1, pd1 = pr.next()
                p2, pd2 = pr.next()
                for k in range(8):
                    kb.op("pe", lambda e, k=k: e.matmul(p1, lhsT=self.XT[:, k, t * 128:(t + 1) * 128], rhs=wv[:, k, 0:512],
                                                        start=(k == 0), stop=(k == 7)),
                          r=[wvd, self.XTd[t // 4]], w=[pd1], mark=(k == 7))
                for k in range(8):
                    kb.op("pe", lambda e, k=k: e.matmul(p2[:, 0:256], lhsT=self.XT[:, k, t * 128:(t + 1) * 128],
                                                        rhs=wv[:, k, 512:768], start=(k == 0), stop=(k == 7)),
                          r=[wvd, self.XTd[t // 4]], w=[pd2], mark=(k == 7))
                kb.op("act", lambda e: e.copy(out=v[:, 0:512], in_=p1), r=[pd1], w=[vd])
                kb.op("dve", lambda e: e.tensor_copy(out=v[:, 512:768], in_=p2[:, 0:256]), r=[pd2], w=[vd])
                kb.dma("sp", self.vtok[t * 128:(t + 1) * 128, :], v, r=[vd])


    def abf(self, lo, hi, **kw):
        return self.arena[:, lo:hi].bitcast(BF16)

    def retention(self, l):
        nc, kb = self.nc, self.kb
        base = RWKV_IN + ATTN_IN
        pT = self.pT
        gam = self.consts["gamma_c"]
        H0, H1 = slice(0, 64), slice(64, 128)
        with ExitStack() as es:
            sb = lambda n, sh, dt: es.enter_context(nc.sbuf_tensor(self.un(n), sh, dt))
            pp = lambda n, sh, dt: es.enter_context(nc.psum_tensor(n, sh, dt))
            kr = self.abf(0, 2048)
            qm = [self.abf(2048, 4096), self.abf(4096, 6144)]
            qx = [self.abf(6144, 8192), self.abf(8192, 10240)]
            sgb = self.abf(10240, 12288)
            krd, qmd, qxd, sgd = Dep(), Dep(), Dep(), Dep()
            cs_t = sb("rt_cs", [128, 2, 2, 1024], F32)
            tq_t = sb("rt_tq", [128, 2, 1024], F32)
            tw_t = sb("rt_tw", [128, 2, 1024], F32)
            v = sb("rt_v", [128, NT, 256], BF16)
            yo = sb("rt_yo", [128, S], BF16)
            gnp = sb("rt_gnp", [128, 4], F32)
            rst = sb("rt_rst", [128, 64], F32)
            rstb = sb("rt_rstb", [128, 64], BF16)
            vd, yod, gnd, rsd, rsbd = Dep(), Dep(), Dep(), Dep(), Dep()
            kb.dma("sp", v[:], self.vtok[:, 512:768].rearrange("(t p) c -> p t c", p=128), w=[vd])
            kb.dma("sp", gnp[:, 0:2], self.I["ret_gn_g"][l].rearrange("(a p) -> p a", p=128), w=[gnd], slow=True)
            kb.dma("sp", gnp[:, 2:4], self.I["ret_gn_b"][l].rearrange("(a p) -> p a", p=128), w=[gnd], slow=True)
            csr, tqr, twr = Ring(cs_t, 2), Ring(tq_t, 2), Ring(tw_t, 2)
            B = self.bank
            bfv = lambda i: (B[i][0].bitcast(BF16)[:, 0:128], B[i][1])
            ps_s = BRing([(B[0][0][:, 0:256], B[0][1]), (B[1][0][:, 0:256], B[1][1])])
            ps_k = BRing([bfv(2)])
            ps_kv = BRing([(B[3][0][:, 0:128], B[3][1])])
            ps_o = BRing([(B[4][0][:, 0:128], B[4][1]), (B[5][0][:, 0:128], B[5][1])])
            ps_t = BRing([bfv(6)])
            sTm_r = Ring(sb("rt_sTm", [128, 2, 256], BF16), 2)
            kz_r = Ring(sb("rt_kz", [128, 2, 128], BF16), 2)
            osq_r = Ring(sb("rt_osq", [128, 2, 128], F32), 2)
            yn_r = Ring(sb("rt_yn", [128, 2, 128], BF16), 2)
            st_r = Ring(sb("rt_st", [128, 2, 16], F32), 2)
            tf_r = Ring(sb("rt_tf", [128, 2, 128], F32), 2)
            dmask = self.cst("dmaskT")
            zeta = self.cst("zeta8")
            xi = self.cst("xi").rearrange("p (a n) -> p a n", a=2)
            for hp in range(2):
                kb.op("pool", lambda e: e.memset(qm[0][H1, :], 0.0), w=[qmd])
                kb.op("pool", lambda e: e.memset(qm[1][H0, :], 0.0), w=[qmd])
                kb.op("pool", lambda e: e.memset(qx[0][H1, :], 0.0), w=[qxd])
                kb.op("pool", lambda e: e.memset(qx[1][H0, :], 0.0), w=[qxd])
                kb.op("pool", lambda e: e.memset(rst[:], 0.0), w=[rsd])
                for pc in range(4):
                    sl = slice(pc * 1024, (pc + 1) * 1024)
                    cs, csd = csr.next()
                    kb.dma("sp", cs[:, 0], self.I["cos2"][:, sl], w=[csd])
                    kb.dma("sp", cs[:, 1], self.I["sin2"][:, sl], w=[csd])
                    for which in range(2):
                        r0 = base + which * 256 + hp * 128
                        tq, tqd = tqr.next()
                        tw, twd = twr.next()
                        kb.dma("sp", tq, pT[r0:r0 + 128, sl], w=[tqd])
                        for blk in range(4):
                            src = r0 + (blk ^ 1) * 32
                            kb.dma("sp", tw[blk * 32:(blk + 1) * 32], pT[src:src + 32, sl], w=[twd])
                        kb.op("dve", lambda e: e.tensor_tensor(out=tq, in0=tq, in1=cs[:, 0], op=ALU.mult),
                              r=[csd], w=[tqd])
                        kb.op("pool", lambda e: e.tensor_tensor(out=tw, in0=tw, in1=cs[:, 1], op=ALU.mult),
                              r=[csd], w=[twd])
                        if which == 1:
                            kb.op("dve", lambda e: e.tensor_tensor(out=kr[:, sl], in0=tq, in1=tw, op=ALU.add),
                                  r=[tqd, twd], w=[krd])
                        else:
                            for hh, rows in enumerate((H0, H1)):
                                kb.op("dve", lambda e: e.tensor_tensor(out=qm[hh][rows, sl], in0=tq[rows], in1=tw[rows],
                                                                       op=ALU.add), r=[tqd, twd], w=[qmd])
                    r0 = base + 768 + hp * 128
                    tq, tqd = tqr.next()
                    kb.dma("sp", tq, pT[r0:r0 + 128, sl], w=[tqd])
                    kb.op("act", lambda e: e.activation(out=sgb[:, sl], in_=tq, func=AF.Silu), r=[tqd], w=[sgd])
                for hh, rows in enumerate((H0, H1)):
                    kb.op("pool", lambda e: e.tensor_tensor(
                        out=qx[hh][rows, :].rearrange("p (c n) -> p c n", n=128),
                        in0=qm[hh][rows, :].rearrange("p (c n) -> p c n", n=128),
                        in1=xi[rows, hp:hp + 1, :].broadcast_to([64, NT, 128]), op=ALU.mult),
                        r=[qmd, self.cstd], w=[qxd])
                import os
                CUT = int(os.environ.get("RET_CUT", "99"))
                for c in range((int(os.environ.get("RET_NC", "32")) if CUT > 2 else 0)):
                    cs = slice(c * 128, (c + 1) * 128)
                    p_s, p_sd = ps_s.next()
                    for hh in range(2):
                        kb.op("pe", lambda e: e.matmul(p_s[:, hh * 128:(hh + 1) * 128], lhsT=kr[:, cs], rhs=qm[hh][:, cs],
                                                       start=True, stop=True), r=[krd, qmd], w=[p_sd], mark=(hh == 1))
                    sTm, sTmd = sTm_r.next()
                    kb.op("dve", lambda e: e.tensor_tensor(out=sTm, in0=p_s, in1=dmask[:, hp * 256:(hp + 1) * 256],
                                                           op=ALU.mult), r=[p_sd, self.cstd], w=[sTmd])
                    if CUT <= 3:
                        continue
                    p_k, p_kd = ps_k.next()
                    kb.op("pe", lambda e: e.transpose(p_k, kr[:, cs], self.identb[:]), r=[krd, self.cstd], w=[p_kd])
                    kz, kzd = kz_r.next()
                    for hh in range(2):
                        h = 2 * hp + hh
                        kb.op("act", lambda e: e.mul(out=kz[:, hh * 64:(hh + 1) * 64], in_=p_k[:, hh * 64:(hh + 1) * 64],
                                                     mul=zeta[:, h:h + 1]), r=[p_kd, self.cstd], w=[kzd])
                    if CUT <= 4:
                        continue
                    p_o, p_od = ps_o.next()
                    for hh in range(2):
                        h = 2 * hp + hh
                        kb.op("pe", lambda e: e.matmul(p_o[:, hh * 64:(hh + 1) * 64], lhsT=sTm[:, hh * 128:(hh + 1) * 128],
                                                       rhs=v[:, c, h * 64:(h + 1) * 64], start=True, stop=(c == 0)),
                              r=[sTmd, vd], w=[p_od], mark=(c == 0 and hh == 1))
                        if c > 0:
                            kb.op("pe", lambda e: e.matmul(p_o[:, hh * 64:(hh + 1) * 64], lhsT=qx[hh][:, cs], rhs=rstb[:],
                                                           start=False, stop=True),
                                  r=[qxd, rsbd], w=[p_od], mark=(hh == 1))
                    if CUT <= 5:
                        continue
                    if c < NT - 1:
                        p_kv, p_kvd = ps_kv.next()
                        kb.op("pe", lambda e: e.matmul(p_kv, lhsT=kz, rhs=v[:, c, hp * 128:(hp + 1) * 128],
                                                       start=True, stop=True), r=[kzd, vd], w=[p_kvd])
                        for hh, rows in enumerate((H0, H1)):
                            kb.op("dve", lambda e: e.scalar_tensor_tensor(
                                out=rst[rows, :], in0=rst[rows, :], scalar=gam[2 * hp + hh],
                                in1=p_kv[rows, hh * 64:(hh + 1) * 64], op0=ALU.mult, op1=ALU.add),
                                r=[p_kvd, rsbd], w=[rsd])
                        kb.op("dve", lambda e: e.tensor_copy(out=rstb[:], in_=rst[:]), r=[rsd], w=[rsbd])
                    if CUT <= 6:
                        continue
                    st, std = st_r.next()
                    osq, osqd = osq_r.next()
                    kb.op("act", lambda e: e.activation(out=osq, in_=p_o, func=AF.Square), r=[p_od], w=[osqd])
                    kb.op("dve", lambda e: e.reduce_sum(out=st[:, 0:2], in_=p_o.rearrange("p (h e) -> p h e", e=64),
                                                        axis=AX.X), r=[p_od], w=[std])
                    kb.op("dve", lambda e: e.reduce_sum(out=st[:, 2:4], in_=osq.rearrange("p (h e) -> p h e", e=64),
                                                        axis=AX.X), r=[osqd], w=[std])
                    self.norm_stats(st, std, 2, 64, LN_EPS)
                    if CUT <= 7:
                        continue
                    yn, ynd = yn_r.next()
                    for hh in range(2):
                        kb.op("dve", lambda e: e.tensor_scalar(out=yn[:, hh * 64:(hh + 1) * 64], in0=p_o[:, hh * 64:(hh + 1) * 64],
                                                               scalar1=st[:, 12 + hh:13 + hh], scalar2=st[:, 14 + hh:15 + hh],
                                                               op0=ALU.mult, op1=ALU.add), r=[p_od, std], w=[ynd])
                    if CUT <= 8:
                        continue
                    p_t, p_td = ps_t.next()
                    kb.op("pe", lambda e: e.transpose(p_t, yn, self.identb[:]), r=[ynd, self.cstd], w=[p_td])
                    tf, tfd = tf_r.next()
                    kb.op("dve", lambda e: e.tensor_scalar(out=tf, in0=p_t, scalar1=gnp[:, hp:hp + 1],
                                                           scalar2=gnp[:, 2 + hp:3 + hp], op0=ALU.mult, op1=ALU.add),
                          r=[p_td, gnd], w=[tfd])
                    kb.op("pool", lambda e: e.tensor_tensor(out=yo[:, cs], in0=tf, in1=sgb[:, cs], op=ALU.mult),
                          r=[tfd, sgd], w=[yod])
                kb.dma("sp", self.yT[768 + hp * 128:768 + (hp + 1) * 128, :], yo[:], r=[yod])

    def norm_stats(self, st, std, n, cnt, eps):
        kb = self.kb
        c = lambda i: st[:, i * n:(i + 1) * n]
        kb.op("dve", lambda e: e.tensor_scalar(out=c(2), in0=c(0), scalar1=1.0 / cnt, scalar2=None, op0=ALU.mult),
              r=[std], w=[std])
        kb.op("dve", lambda e: e.tensor_tensor(out=c(3), in0=c(2), in1=c(2), op=ALU.mult), r=[std], w=[std])
        kb.op("dve", lambda e: e.scalar_tensor_tensor(out=c(4), in0=c(1), scalar=1.0 / cnt, in1=c(3),
                                                      op0=ALU.mult, op1=ALU.subtract), r=[std], w=[std])
        kb.op("dve", lambda e: e.tensor_scalar(out=c(4), in0=c(4), scalar1=float(eps), scalar2=None, op0=ALU.add),
              r=[std], w=[std])
        kb.op("act", lambda e: e.activation(out=c(5), in_=c(4), func=AF.Sqrt), r=[std], w=[std])
        kb.op("dve", lambda e: e.reciprocal(out=c(6), in_=c(5)), r=[std], w=[std])
        kb.op("dve", lambda e: e.scalar_tensor_tensor(out=c(7), in0=c(2), scalar=-1.0, in1=c(6),
                                                      op0=ALU.mult, op1=ALU.mult), r=[std], w=[std])

    def attention(self, l):
        nc, kb = self.nc, self.kb
        base = RWKV_IN
        pT = self.pT
        H0, H1 = slice(0, 64), slice(64, 128)
        with ExitStack() as es:
            sb = lambda n, sh, dt: es.enter_context(nc.sbuf_tensor(self.un(n), sh, dt))
            acc = self.arena[0:64, :].rearrange("p (a h s) -> p a h s", a=2, h=2)
            accd = Dep()
            kt = sb("at_k", [128, S], BF16)
            qm = [sb("at_q0", [128, S], BF16), sb("at_q1", [128, S], BF16)]
            vp = [sb("at_v%d" % i, [128, NT, 128], BF16) for i in range(3)]
            ones = sb("at_ones", [128, 64], BF16)
            PT_r = Ring(sb("at_PT", [128, 2, 512], BF16), 2)
            rc = sb("at_rc", [64, 2, S], F32)
            yo = sb("at_yo", [64, 2, S], BF16)
            ktd, qmd, vd, od, rcd, yod = Dep(), Dep(), Dep(), Dep(), Dep(), Dep()
            kb.op("pool", lambda e: e.memset(ones[:], 1.0), w=[od])
            B = self.bank
            ps_s = BRing([B[0], B[1], B[2], B[3]])
            ps_o = BRing([B[4], B[5], B[6], B[7]])
            for pair in range(4):
                kb.op("pool", lambda e: e.memset(qm[0][H1, :], 0.0), w=[qmd])
                kb.op("pool", lambda e: e.memset(qm[1][H0, :], 0.0), w=[qmd])
                kb.op("dve", lambda e: e.memset(self.arena[0:64, :], 0.0), w=[accd])
                q0 = base + pair * 128
                k0 = base + 512 + pair * 128
                kb.dma("pool", qm[0][H0, :], pT[q0:q0 + 64, :], w=[qmd])
                kb.dma("pool", qm[1][H1, :], pT[q0 + 64:q0 + 128, :], w=[qmd])
                kb.dma("pool", kt[:], pT[k0:k0 + 128, :], w=[ktd])
                for p, (win, dil) in enumerate(PATTERNS):
                    nb = NT // dil
                    for r in range(dil):
                        src = self.vtok[:, pair * 128:(pair + 1) * 128].rearrange("(b j r) c -> r j b c", r=dil, j=128)[r]
                        kb.dma("sp", vp[p][:, r * nb:(r + 1) * nb, :], src, w=[vd])
                for p, (win, dil) in enumerate(PATTERNS):
                    nb = NT // dil
                    for r in range(dil):
                        for b in range(nb):
                            def tsl(bb):
                                st = r + dil * 128 * bb
                                return slice(st, st + dil * 127 + 1, dil)
                            qs = tsl(b)
                            halves = (0, 1) if b > 0 else (1,)
                            p_s, p_sd = ps_s.next()
                            for hh in range(2):
                                h = 2 * pair + hh
                                for half in halves:
                                    ks = tsl(b - 1) if half == 0 else qs
                                    o = p_s[:, (hh * 2 + half) * 128:(hh * 2 + half + 1) * 128]
                                    kb.op("pe", lambda e: e.matmul(o, lhsT=kt[:, ks], rhs=qm[hh][:, qs], start=True, stop=False),
                                          r=[ktd, qmd], w=[p_sd], mark=False)
                                    kb.op("pe", lambda e: e.matmul(o, lhsT=self.identb[:], rhs=self.bias8[:, p, h, half, :],
                                                                   start=False, stop=True),
                                          r=[self.cstd], w=[p_sd], mark=(hh == 1 and half == 1))
                            PT, PTd = PT_r.next()
                            if b > 0:
                                kb.op("act", lambda e: e.activation(out=PT, in_=p_s, func=AF.Exp, scale=0.125),
                                      r=[p_sd], w=[PTd])
                            else:
                                v4 = lambda t: t.rearrange("p (h a i) -> p h a i", h=2, a=2)[:, :, 1, :]
                                kb.op("act", lambda e: e.activation(out=v4(PT), in_=v4(p_s), func=AF.Exp, scale=0.125),
                                      r=[p_sd], w=[PTd])
                            p_o, p_od = ps_o.next()
                            for nd in range(2):
                                for hh in range(2):
                                    o = p_o[0:64, (nd * 2 + hh) * 128:(nd * 2 + hh + 1) * 128]
                                    for hi, half in enumerate(halves):
                                        ti = r * nb + (b - 1 if half == 0 else b)
                                        lw = vp[p][:, ti, hh * 64:(hh + 1) * 64] if nd == 0 else ones[:]
                                        kb.op("pe", lambda e: e.matmul(o, lhsT=lw, rhs=PT[:, (hh * 2 + half) * 128:(hh * 2 + half + 1) * 128],
                                                                       start=(hi == 0), stop=(hi == len(halves) - 1)),
                                              r=[PTd, vd, od], w=[p_od], mark=(nd == 1 and hh == 1 and hi == len(halves) - 1))
                            kb.op("dve", lambda e: e.tensor_tensor(
                                out=acc[:, :, :, qs], in0=acc[:, :, :, qs],
                                in1=p_o[0:64, :].rearrange("p (a h i) -> p a h i", a=2, h=2), op=ALU.add),
                                r=[p_od, accd], w=[accd])
                kb.op("dve", lambda e: e.reciprocal(out=rc[:], in_=acc[:, 1]), r=[accd], w=[rcd])
                kb.op("dve", lambda e: e.tensor_tensor(out=yo[:], in0=acc[:, 0], in1=rc[:], op=ALU.mult),
                      r=[accd, rcd], w=[yod])
                for hh in range(2):
                    r0 = 256 + (2 * pair + hh) * 64
                    kb.dma("sp", self.yT[r0:r0 + 64, :], yo[:, hh, :], r=[yod])

    def rwkv(self, l):
        nc, kb = self.nc, self.kb
        pT = self.pT
        I = self.I
        H0, H1 = slice(0, 64), slice(64, 128)
        SEG = 1024
        NCH = SEG // 64
        with ExitStack() as es:
            sb = lambda n, sh, dt: es.enter_context(nc.sbuf_tensor(self.un(n), sh, dt))
            U = [self.arena[:, i * SEG:(i + 1) * SEG] for i in range(16)]
            Ud = [Dep() for _ in range(16)]
            prm = sb("rw_prm", [128, 32], F32)
            prd = Dep()
            pcol = {}
            kb.dma("sp", prm[:, 0:8], I["rwkv_mu"][l].rearrange("(c p) -> p c", p=128), w=[prd], slow=True)
            for i, nm in enumerate(("rwkv_w0", "rwkv_a0", "rwkv_k_k", "rwkv_k_a", "rwkv_r_k", "rwkv_ln_g", "rwkv_ln_b")):
                pcol[nm] = 8 + 2 * i
                kb.dma("sp", prm[:, 8 + 2 * i:10 + 2 * i], I[nm][l].rearrange("(a p) -> p a", p=128), w=[prd], slow=True)
            pcol["omka"] = 22
            kb.op("dve", lambda e: e.tensor_scalar(out=prm[:, 22:24], in0=prm[:, pcol["rwkv_k_a"]:pcol["rwkv_k_a"] + 2],
                                                   scalar1=-1.0, scalar2=1.0, op0=ALU.mult, op1=ALU.add), r=[prd], w=[prd])
            P = lambda nm, hp: prm[:, pcol[nm] + hp:pcol[nm] + hp + 1]
            wlw = sb("rw_wlw", [128, 256], BF16)
            wla = sb("rw_wla", [128, 256], BF16)
            gup = sb("rw_gup", [128, 256], BF16)
            wd = Dep()
            kb.op("pool", lambda e: e.memset(wlw[H1, :], 0.0), w=[wd])
            kb.op("pool", lambda e: e.memset(wla[H0, :], 0.0), w=[wd])
            kb.dma("pool", wlw[H0, :], I["rwkv_w_up"][l], w=[wd])
            kb.dma("pool", wla[H1, :], I["rwkv_a_up"][l], w=[wd])
            kb.dma("pool", gup[:], I["rwkv_g_up"][l], w=[wd])
            pad = {}
            padd = {}
            for nm in ("Rt", "Kk", "Bt", "Kt", "V"):
                pad[nm] = sb("rw_p" + nm, [128, NCH, 128], F32)
                padd[nm] = Dep()
                kb.op("pool", lambda e, nm=nm: e.memset(pad[nm][:], 0.0), w=[padd[nm]])
            T6 = sb("rw_T6", [128, SEG], BF16)
            SG = sb("rw_SG", [128, SEG], BF16)
            T6d, SGd = Dep(), Dep()
            buf_r = Ring(sb("rw_buf", [128, 2, SEG + 1], F32), 2)
            tmp_r = Ring(sb("rw_tmp", [128, 2, SEG], F32), 2)
            yo_r = Ring(sb("rw_yo", [128, 2, SEG], BF16), 2)
            Hs = [sb("rw_H%d" % i, [128, 128], F32) for i in range(2)]
            Hd = [Dep(), Dep()]
            NW = 2
            wk = []
            for g in range(NW):
                t = {}
                for nm in ("nBtPT", "KtPT", "VPT", "N", "NT", "MakT", "nPrbT", "PrkT", "Pa", "Pb", "PT1", "PT2", "PT4",
                           "PT8", "PT16", "PT32", "QT", "GT0", "E", "YnP", "sq"):
                    t[nm] = (sb("rw_%s%d" % (nm, g), [128, 128], F32), Dep())
                for nm in ("Xa", "Xb"):
                    t[nm] = (sb("rw_%s%d" % (nm, g), [128, 256], F32), Dep())
                t["st"] = (sb("rw_st%d" % g, [128, 8], F32), Dep())
                kb.op("pool", lambda e, t=t: e.memset(t["YnP"][0][:], 0.0), w=[t["YnP"][1]])
                wk.append(t)
            banks = BRing(self.bank)
            ident = self.cst("ident")
            cd = self.cstd
            evc = [0]

            def copy_ev(out, in_, r, w, scale=None):
                evc[0] += 1
                if evc[0] % 2 == 0:
                    if scale is None:
                        kb.op("act", lambda e: e.copy(out=out, in_=in_), r=r, w=w)
                    else:
                        kb.op("act", lambda e: e.mul(out=out, in_=in_, mul=float(scale)), r=r, w=w)
                else:
                    if scale is None:
                        kb.op("dve", lambda e: e.tensor_copy(out=out, in_=in_), r=r, w=w)
                    else:
                        kb.op("dve", lambda e: e.tensor_scalar(out=out, in0=in_, scalar1=float(scale), scalar2=None,
                                                               op0=ALU.mult), r=r, w=w)

            def load_shift(row0, seg, c8, dst, dstd):
                buf, bd = buf_r.next()
                tmp, td = tmp_r.next()
                t0 = seg * SEG
                if seg == 0:
                    kb.op("pool", lambda e: e.memset(buf[:, 0:1], 0.0), w=[bd])
                    kb.dma("sp", buf[:, 1:SEG + 1], pT[row0:row0 + 128, 0:SEG], w=[bd])
                else:
                    kb.dma("sp", buf[:, 0:SEG + 1], pT[row0:row0 + 128, t0 - 1:t0 + SEG], w=[bd])
                kb.op("pool", lambda e: e.tensor_tensor(out=tmp, in0=buf[:, 0:SEG], in1=buf[:, 1:SEG + 1], op=ALU.subtract),
                      r=[bd], w=[td])
                kb.op("dve", lambda e: e.scalar_tensor_tensor(out=dst, in0=tmp, scalar=prm[:, c8:c8 + 1], in1=buf[:, 1:SEG + 1],
                                                              op0=ALU.mult, op1=ALU.add), r=[td, bd, prd], w=[dstd])

            def mm_to(lhsT, rhs, rd, n=128):
                bk, bd = banks.next()
                o = bk[:, 0:n]
                kb.op("pe", lambda e: e.matmul(o, lhsT=lhsT, rhs=rhs, start=True, stop=True), r=rd, w=[bd])
                return o, bd

            def chunk_gen(hp, seg, c, g):
                t = wk[g]
                gc = seg * NCH + c
                A = lambda nm: t[nm][0][:]
                Dp = lambda nm: t[nm][1]
                Rt, Kk, Bt, Kt, V = (pad[nm][:, c, :] for nm in ("Rt", "Kk", "Bt", "Kt", "V"))
                dR, dKk, dB, dK, dV = (padd[nm] for nm in ("Rt", "Kk", "Bt", "Kt", "V"))
                X = [t["Xa"], t["Xb"]]
                for src, sd, dst, dd, sc in ((Bt, dB, A("nBtPT"), Dp("nBtPT"), -1.0), (Kt, dK, A("KtPT"), Dp("KtPT"), None),
                                             (V, dV, A("VPT"), Dp("VPT"), None), (Kk, dKk, X[0][0][:, 0:128], X[0][1], None)):
                    bk, bd = banks.next()
                    kb.op("pe", lambda e: e.transpose(bk[:, 0:128], src, ident), r=[sd, cd], w=[bd])
                    copy_ev(dst, bk[:, 0:128], [bd], [dd], sc)
                yield
                for lh, ld, rh, rdd, dst, msk in ((Bt, dB, Kk, dKk, "NT", "nSU"), (Kk, dKk, Bt, dB, "N", "nSL"),
                                                 (Kt, dK, Kk, dKk, "MakT", "SU"), (Bt, dB, Rt, dR, "nPrbT", "nIU"),
                                                 (Kt, dK, Rt, dR, "PrkT", "IU")):
                    o, bd = mm_to(lh, rh, [ld, rdd])
                    kb.op("dve", lambda e: e.tensor_tensor(out=A(dst), in0=o, in1=self.cst(msk), op=ALU.mult),
                          r=[bd, cd], w=[Dp(dst)])
                yield
                o, bd = mm_to(A("MakT"), A("VPT"), [Dp("MakT"), Dp("VPT")])
                copy_ev(X[0][0][:, 128:256], o, [bd], [X[0][1]])
                yield
                Pc, PTc = ("N", "NT")
                lv_names = {2: "PT2", 4: "PT4", 8: "PT8", 16: "PT16", 32: "PT32"}
                pt_list = ["NT"]
                pbuf = ["Pa", "Pb"]
                pi = 0
                for lvl in (2, 4, 8, 16, 32):
                    o2, bd2 = mm_to(A(Pc), A(PTc), [Dp(Pc), Dp(PTc)])
                    if lvl < 32:
                        o1, bd1 = mm_to(A(PTc), A(Pc), [Dp(Pc), Dp(PTc)])
                    copy_ev(A(lv_names[lvl]), o2, [bd2], [Dp(lv_names[lvl])])
                    if lvl < 32:
                        copy_ev(A(pbuf[pi]), o1, [bd1], [Dp(pbuf[pi])])
                        Pc = pbuf[pi]
                        pi ^= 1
                    PTc = lv_names[lvl]
                    pt_list.append(PTc)
                    yield
                xi_ = 0
                for nm in reversed(pt_list):
                    Xc, Xn = X[xi_], X[xi_ ^ 1]
                    o, bd = mm_to(A(nm), Xc[0][:], [Dp(nm), Xc[1]], n=256)
                    kb.op("dve", lambda e: e.tensor_tensor(out=Xn[0][:], in0=o, in1=Xc[0][:], op=ALU.add),
                          r=[bd, Xc[1]], w=[Xn[1]])
                    xi_ ^= 1
                    yield
                W, Wd = X[xi_]
                W1, W2 = W[:, 0:128], W[:, 128:256]
                o, bd = mm_to(W1, A("nPrbT"), [Wd, Dp("nPrbT")])
                kb.op("dve", lambda e: e.tensor_tensor(out=A("QT"), in0=o, in1=Rt, op=ALU.add), r=[bd, dR], w=[Dp("QT")])
                o, bd = mm_to(W1, A("nBtPT"), [Wd, Dp("nBtPT")])
                kb.op("dve", lambda e: e.tensor_tensor(out=A("GT0"), in0=o, in1=ident, op=ALU.add), r=[bd, cd], w=[Dp("GT0")])
                bk, bd = banks.next()
                o = bk[:, 0:128]
                kb.op("pe", lambda e: e.matmul(o, lhsT=A("KtPT"), rhs=A("VPT"), start=True, stop=False),
                      r=[Dp("KtPT"), Dp("VPT")], w=[bd], mark=False)
                kb.op("pe", lambda e: e.matmul(o, lhsT=A("nBtPT"), rhs=W2, start=False, stop=True),
                      r=[Dp("nBtPT"), Wd], w=[bd])
                dc = U[12][:, c * 64 + 63:c * 64 + 64]
                kb.op("dve", lambda e: e.tensor_scalar(out=A("E"), in0=o, scalar1=dc, scalar2=None, op0=ALU.mult),
                      r=[bd, Ud[12]], w=[Dp("E")])
                yield
                Hc, Hcd = Hs[gc % 2], Hd[gc % 2]
                Hn, Hnd = Hs[(gc + 1) % 2], Hd[(gc + 1) % 2]
                bk, ybd = banks.next()
                yps = bk[:, 0:128]
                kb.op("pe", lambda e: e.matmul(yps, lhsT=A("QT"), rhs=Hc[:], start=True, stop=False),
                      r=[Dp("QT"), Hcd], w=[ybd], mark=False)
                kb.op("pe", lambda e: e.matmul(yps, lhsT=A("PrkT"), rhs=A("VPT"), start=False, stop=False),
                      r=[Dp("PrkT"), Dp("VPT")], w=[ybd], mark=False)
                kb.op("pe", lambda e: e.matmul(yps, lhsT=A("nPrbT"), rhs=W2, start=False, stop=True),
                      r=[Dp("nPrbT"), Wd], w=[ybd])
                o, bd = mm_to(A("GT0"), Hc[:], [Dp("GT0"), Hcd])
                kb.op("dve", lambda e: e.scalar_tensor_tensor(out=Hn[:], in0=o, scalar=dc, in1=A("E"), op0=ALU.mult, op1=ALU.add),
                      r=[bd, Dp("E"), Ud[12]], w=[Hnd])
                yield
                st, std = t["st"]
                kb.op("act", lambda e: e.activation(out=A("sq"), in_=yps, func=AF.Square), r=[ybd], w=[Dp("sq")])
                kb.op("dve", lambda e: e.reduce_sum(out=st[:, 0:1], in_=yps, axis=AX.X), r=[ybd], w=[std])
                kb.op("dve", lambda e: e.reduce_sum(out=st[:, 1:2], in_=A("sq"), axis=AX.X), r=[Dp("sq")], w=[std])
                self.norm_stats(st, std, 1, 64, RWKV_GN_EPS)
                for rows in (H0, H1):
                    kb.op("dve", lambda e: e.tensor_scalar(out=t["YnP"][0][rows, rows], in0=yps[rows, rows],
                                                           scalar1=st[rows, 6:7], scalar2=st[rows, 7:8],
                                                           op0=ALU.mult, op1=ALU.add), r=[ybd, std], w=[Dp("YnP")])
                o, bd = mm_to(A("YnP"), self.cst("sel"), [Dp("YnP"), cd], n=64)
                kb.op("dve", lambda e: e.tensor_scalar(out=U[10][:, c * 64:(c + 1) * 64], in0=o,
                                                       scalar1=P("rwkv_ln_g", hp), scalar2=P("rwkv_ln_b", hp),
                                                       op0=ALU.mult, op1=ALU.add), r=[bd, prd], w=[Ud[10]])
                yield

            for hp in range(2):
                for i in range(2):
                    kb.op("pool", lambda e, i=i: e.memset(Hs[i][:], 0.0), w=[Hd[i]])
                for seg in range(S // SEG):
                    blk2 = [slice(0, 512), slice(512, 1024)]
                    load_shift(768, seg, 6, U[8], Ud[8])
                    kb.op("act", lambda e: e.activation(out=T6[H0, :], in_=U[8][H0, :], func=AF.Tanh), r=[Ud[8]], w=[T6d])
                    kb.op("act", lambda e: e.copy(out=T6[H1, :], in_=U[8][H1, :]), r=[Ud[8]], w=[T6d])
                    load_shift(896, seg, 7, U[9], Ud[9])
                    kb.op("act", lambda e: e.activation(out=SG[:], in_=U[9], func=AF.Sigmoid), r=[Ud[9]], w=[SGd])
                    load_shift(hp * 128, seg, hp, U[0], Ud[0])
                    load_shift(256 + hp * 128, seg, 2 + hp, U[1], Ud[1])
                    load_shift(512 + hp * 128, seg, 4 + hp, U[2], Ud[2])
                    hs = slice(hp * 128, (hp + 1) * 128)
                    for b2 in blk2:
                        o, bd = mm_to(wlw[:, hs], T6[:, b2], [wd, T6d], n=512)
                        kb.op("act", lambda e: e.activation(out=U[4][:, b2], in_=o, func=AF.Sigmoid, bias=P("rwkv_w0", hp)),
                              r=[bd, prd], w=[Ud[4]])
                        o, bd = mm_to(wla[:, hs], T6[:, b2], [wd, T6d], n=512)
                        kb.op("act", lambda e: e.activation(out=U[3][:, b2], in_=o, func=AF.Sigmoid, bias=P("rwkv_a0", hp)),
                              r=[bd, prd], w=[Ud[3]])
                        o, bd = mm_to(gup[:, hs], SG[:, b2], [wd, SGd], n=512)
                        kb.op("act", lambda e: e.copy(out=U[5][:, b2], in_=o), r=[bd], w=[Ud[5]])
                    kb.op("dve", lambda e: e.tensor_scalar(out=U[6], in0=U[1], scalar1=P("rwkv_k_k", hp), scalar2=None,
                                                           op0=ALU.mult), r=[Ud[1], prd], w=[Ud[6]])
                    kb.op("pool", lambda e: e.tensor_tensor(out=U[8], in0=U[6], in1=U[6], op=ALU.mult), r=[Ud[6]], w=[Ud[8]])
                    for b2 in blk2:
                        o, bd = mm_to(self.cst("onesblk"), U[8][:, b2], [cd, Ud[8]], n=512)
                        kb.op("dve", lambda e: e.tensor_scalar(out=U[11][:, b2], in0=o, scalar1=1e-24, scalar2=None,
                                                               op0=ALU.max), r=[bd], w=[Ud[11]])
                    kb.op("act", lambda e: e.activation(out=U[11], in_=U[11], func=AF.Sqrt), r=[Ud[11]], w=[Ud[11]])
                    kb.op("dve", lambda e: e.reciprocal(out=U[11], in_=U[11]), r=[Ud[11]], w=[Ud[11]])
                    kb.op("pool", lambda e: e.tensor_tensor(out=U[6], in0=U[6], in1=U[11], op=ALU.mult),
                          r=[Ud[6], Ud[11]], w=[Ud[6]])
                    kb.op("dve", lambda e: e.tensor_scalar(out=U[7], in0=U[3], scalar1=P("rwkv_k_a", hp), scalar2=P("omka", hp),
                                                           op0=ALU.mult, op1=ALU.add), r=[Ud[3], prd], w=[Ud[7]])
                    kb.op("pool", lambda e: e.tensor_tensor(out=U[7], in0=U[7], in1=U[1], op=ALU.mult),
                          r=[Ud[7], Ud[1]], w=[Ud[7]])
                    kb.op("dve", lambda e: e.scalar_tensor_tensor(out=U[8], in0=U[0], scalar=P("rwkv_r_k", hp), in1=U[7],
                                                                  op0=ALU.mult, op1=ALU.mult), r=[Ud[0], Ud[7], prd], w=[Ud[8]])
                    for b2 in blk2:
                        o, bd = mm_to(self.cst("onesblk"), U[8][:, b2], [cd, Ud[8]], n=512)
                        kb.op("dve", lambda e: e.tensor_tensor(out=U[9][:, b2], in0=o, in1=U[2][:, b2], op=ALU.mult),
                              r=[bd, Ud[2]], w=[Ud[9]])
                    kb.op("pool", lambda e: e.tensor_tensor(out=U[3], in0=U[3], in1=U[6], op=ALU.mult),
                          r=[Ud[3], Ud[6]], w=[Ud[3]])
                    kb.op("dve", lambda e: e.tensor_tensor_scan(out=U[11], data0=self.cst("scanm"), data1=U[4], initial=0.0,
                                                                op0=ALU.mult, op1=ALU.add), r=[Ud[4], cd], w=[Ud[11]])
                    kb.op("act", lambda e: e.activation(out=U[12], in_=U[11], func=AF.Exp, scale=-C0), r=[Ud[11]], w=[Ud[12]])
                    kb.op("act", lambda e: e.activation(out=U[13], in_=U[11], func=AF.Exp, scale=C0), r=[Ud[11]], w=[Ud[13]])
                    kb.op("pool", lambda e: e.tensor_tensor(out=U[4], in0=U[11], in1=U[4], op=ALU.subtract),
                          r=[Ud[11], Ud[4]], w=[Ud[4]])
                    kb.op("act", lambda e: e.activation(out=U[4], in_=U[4], func=AF.Exp, scale=-C0), r=[Ud[4]], w=[Ud[4]])
                    kb.op("pool", lambda e: e.tensor_tensor(out=U[0], in0=U[0], in1=U[12], op=ALU.mult), r=[Ud[0], Ud[12]], w=[Ud[0]])
                    kb.op("dve", lambda e: e.tensor_tensor(out=U[6], in0=U[6], in1=U[4], op=ALU.mult), r=[Ud[6], Ud[4]], w=[Ud[6]])
                    kb.op("pool", lambda e: e.tensor_tensor(out=U[3], in0=U[3], in1=U[13], op=ALU.mult), r=[Ud[3], Ud[13]], w=[Ud[3]])
                    kb.op("dve", lambda e: e.tensor_tensor(out=U[7], in0=U[7], in1=U[13], op=ALU.mult), r=[Ud[7], Ud[13]], w=[Ud[7]])
                    ci = 0
                    for nm, ui in (("Rt", 0), ("Kk", 6), ("Bt", 3), ("Kt", 7), ("V", 2)):
                        for rows in (H0, H1):
                            dst = pad[nm][rows, :, rows]
                            src = U[ui][rows, :].rearrange("p (c t) -> p c t", t=64)
                            eng = ("pool", "dve", "act")[ci % 3]
                            ci += 1
                            if eng == "act":
                                kb.op("act", lambda e: e.copy(out=dst, in_=src), r=[Ud[ui]], w=[padd[nm]])
                            else:
                                kb.op(eng, lambda e: e.tensor_copy(out=dst, in_=src), r=[Ud[ui]], w=[padd[nm]])
                    for c0 in range(0, NCH, NW):
                        run_interleaved([chunk_gen(hp, seg, c0 + g, g) for g in range(NW)])
                    yo, yod = yo_r.next()
                    kb.op("pool", lambda e: e.tensor_tensor(out=U[10], in0=U[10], in1=U[9], op=ALU.add),
                          r=[Ud[10], Ud[9]], w=[Ud[10]])
                    kb.op("dve", lambda e: e.tensor_tensor(out=yo, in0=U[10], in1=U[5], op=ALU.mult),
                          r=[Ud[10], Ud[5]], w=[yod])
                    kb.dma("sp", self.yT[hp * 128:(hp + 1) * 128, seg * SEG:(seg + 1) * SEG], yo, r=[yod])

    def layer_norm_tile(self, z, zd, junk, jd, st, std, gb, gbd):
        kb = self.kb
        kb.op("act", lambda e: e.activation(out=junk, in_=z, func=AF.Square), r=[zd], w=[jd])
        kb.op("dve", lambda e: e.reduce_sum(out=st[:, 0:1], in_=z, axis=AX.X), r=[zd], w=[std])
        kb.op("dve", lambda e: e.reduce_sum(out=st[:, 1:2], in_=junk, axis=AX.X), r=[jd], w=[std])
        self.norm_stats(st, std, 1, D, LN_EPS)
        kb.op("dve", lambda e: e.tensor_scalar(out=z, in0=z, scalar1=st[:, 6:7], scalar2=st[:, 7:8],
                                               op0=ALU.mult, op1=ALU.add), r=[zd, std], w=[zd])
        kb.op("pool", lambda e: e.tensor_tensor(out=z, in0=z, in1=gb[:, 0, :], op=ALU.mult), r=[zd, gbd], w=[zd])
        kb.op("pool", lambda e: e.tensor_tensor(out=z, in0=z, in1=gb[:, 1, :], op=ALU.add), r=[zd, gbd], w=[zd])

    def load_ln_params(self, gb, gbd, l, i):
        kb = self.kb
        kb.dma("sp", gb[:, 0, :], self.I["ln_g"][l, i:i + 1, :].broadcast_to([128, D]), w=[gbd])
        kb.dma("sp", gb[:, 1, :], self.I["ln_b"][l, i:i + 1, :].broadcast_to([128, D]), w=[gbd])

    def outproj_ln1(self, l):
        nc, kb = self.nc, self.kb
        moe = (l % 2 == 1)
        j = l // 2
        with ExitStack() as es:
            sb = lambda n, sh, dt: es.enter_context(nc.sbuf_tensor(self.un(n), sh, dt))
            wo = sb("o_w", [128, 8, D], BF16)
            wod = Dep()
            kb.dma("pool", wo[:], self.I["w_out"][l].rearrange("(k p) n -> p k n", p=128), w=[wod])
            yt_r = Ring(sb("o_yt", [128, 2, 8, 512], BF16), 2)
            x_r = Ring(sb("o_x", [128, 2, D], F32), 2)
            z_r = Ring(sb("o_z", [128, 2, D], F32), 2)
            junk = sb("o_junk", [128, D], BF16)
            jd = Dep()
            st_r = Ring(sb("o_st", [128, 2, 8], F32), 2)
            gb = sb("o_gb", [128, 2, D], F32)
            gbd = Dep()
            self.load_ln_params(gb, gbd, l, 0)
            B = self.bank
            hb = BRing([(0, 1), (2, 3)])
            tr = self.tr_ring(((4, 5), (6, 7)))
            if moe:
                rt = sb("o_rt", [128, 8, N_EXPERTS], F32)
                rtd = Dep()
                kb.dma("sp", rt[:], self.I["moe_router"][j].rearrange("(k p) e -> p k e", p=128), w=[rtd], slow=True)
                xTf_r = Ring(sb("o_xTf", [128, 1, 8, 128], F32), 1)
                g_r = Ring(sb("o_g", [128, 2, 64], F32), 2)
            for tb in range(8):
                yt, ytd = yt_r.next()
                kb.dma("sp", yt, self.yT[:, tb * 512:(tb + 1) * 512].rearrange("(k p) t -> p k t", p=128), w=[ytd])
                for tt in range(4):
                    t = tb * 4 + tt
                    x, xd = x_r.next()
                    z, zd = z_r.next()
                    kb.dma("sp", x, self.xres[t * 128:(t + 1) * 128, :], w=[xd])
                    ba, bb_ = hb.next()
                    for half, bi in enumerate((ba, bb_)):
                        bk, bd = B[bi]
                        for k in range(8):
                            kb.op("pe", lambda e, k=k: e.matmul(bk[:, 0:512], lhsT=yt[:, k, tt * 128:(tt + 1) * 128],
                                                                rhs=wo[:, k, half * 512:(half + 1) * 512],
                                                                start=(k == 0), stop=(k == 7)),
                                  r=[ytd, wod], w=[bd], mark=(k == 7))
                        hs = slice(half * 512, (half + 1) * 512)
                        kb.op("dve", lambda e: e.scalar_tensor_tensor(out=z[:, hs], in0=x[:, hs], scalar=float(ALPHA),
                                                                      in1=bk[:, 0:512], op0=ALU.mult, op1=ALU.add),
                              r=[xd, bd], w=[zd])
                    st, std = st_r.next()
                    self.layer_norm_tile(z, zd, junk[:], jd, st, std, gb, gbd)
                    kb.dma("sp", self.xres[t * 128:(t + 1) * 128, :], z, r=[zd, xd])
                    if moe:
                        xTf, xTfd = xTf_r.next()
                        self.transpose_to_XT(z, zd, t, tr, f32_out=(xTf, xTfd))
                        self.router_gates(xTf, xTfd, rt, rtd, t, g_r)
                    else:
                        self.transpose_to_XT(z, zd, t, tr)

    def router_gates(self, xTf, xTfd, rt, rtd, t, g_r):
        kb = self.kb
        bk, bd = self.bank[0]
        lp = bk[:, 0:N_EXPERTS]
        for k in range(8):
            kb.op("pe", lambda e, k=k: e.matmul(lp, lhsT=xTf[:, k, :], rhs=rt[:, k, :], start=(k == 0), stop=(k == 7)),
                  r=[xTfd, rtd], w=[bd], mark=(k == 7))
        g, gd = g_r.next()
        c = lambda i: g[:, i * 8:(i + 1) * 8]
        sc = lambda i: g[:, 56 + i:57 + i]
        op = lambda fn: kb.op("dve", fn, r=[gd], w=[gd])
        kb.op("dve", lambda e: e.tensor_copy(out=c(0), in_=lp), r=[bd], w=[gd])
        op(lambda e: e.reduce_max(out=sc(0), in_=c(0), axis=AX.X))
        op(lambda e: e.tensor_scalar(out=c(1), in0=c(0), scalar1=sc(0), scalar2=None, op0=ALU.is_equal))
        op(lambda e: e.scalar_tensor_tensor(out=c(2), in0=c(1), scalar=-1e30, in1=c(0), op0=ALU.mult, op1=ALU.add))
        op(lambda e: e.reduce_max(out=sc(1), in_=c(2), axis=AX.X))
        op(lambda e: e.tensor_scalar(out=c(3), in0=c(0), scalar1=sc(1), scalar2=None, op0=ALU.is_ge))
        op(lambda e: e.tensor_scalar(out=sc(2), in0=sc(0), scalar1=-1.0, scalar2=None, op0=ALU.mult))
        kb.op("act", lambda e: e.activation(out=c(4), in_=c(0), func=AF.Exp, bias=sc(2), scale=1.0), r=[gd], w=[gd])
        op(lambda e: e.tensor_tensor(out=c(4), in0=c(4), in1=c(3), op=ALU.mult))
        op(lambda e: e.reduce_sum(out=sc(3), in_=c(4), axis=AX.X))
        op(lambda e: e.reciprocal(out=sc(4), in_=sc(3)))
        kb.op("dve", lambda e: e.tensor_scalar(out=self.gates[:, t, :], in0=c(4), scalar1=sc(4), scalar2=None, op0=ALU.mult),
              r=[gd], w=[self.gatesd])

    def ffn(self, l):
        nc, kb = self.nc, self.kb
        moe = (l % 2 == 1)
        j = l // 2
        last = (l == self.n_layers - 1)
        if moe:
            E, FF = N_EXPERTS, FF_EXPERT
            wg_a, wu_a, wd_a = self.I["moe_w_gate"][j], self.I["moe_w_up"][j], self.I["moe_w_down"][j]
        else:
            E, FF = 1, FF_DENSE
            wg_a, wu_a, wd_a = [self.I[k][j:j + 1] for k in ("ffn_w_gate", "ffn_w_up", "ffn_w_down")]
        G = 2
        ngrp = FF // (128 * G)
        HT = 2048
        with ExitStack() as es:
            sb = lambda n, sh, dt: es.enter_context(nc.sbuf_tensor(self.un(n), sh, dt))
            acc = sb("f_acc", [128, 16, D], F32)
            accd = [Dep() for _ in range(16)]
            wg_r = Ring(sb("f_wg", [128, 2, 8, G * 128], BF16), 2)
            wu_r = Ring(sb("f_wu", [128, 2, 8, G * 128], BF16), 2)
            wd_r = Ring(sb("f_wd", [128, 2, G, D], BF16), 2)
            hT_r = Ring(sb("f_hT", [128, 1, G, HT], BF16), 1)
            sg_r = Ring(sb("f_sg", [128, 2, 512], F32), 2)
            x_r = Ring(sb("f_x", [128, 1, D], F32), 1)
            junk = sb("f_junk", [128, D], BF16)
            jd = Dep()
            st_r = Ring(sb("f_st", [128, 2, 8], F32), 2)
            gb = sb("f_gb", [128, 2, D], F32)
            gbd = Dep()
            self.load_ln_params(gb, gbd, l, 1)
            B = self.bank
            pg_r = BRing([B[0], B[1]])
            pu_r = BRing([B[2], B[3]])
            po_r = BRing([B[4], B[5]])
            tr = self.tr_ring(((6, 7),))
            for half in range(S // HT):
                first = True
                for e_ in range(E):
                    for grp in range(ngrp):
                        ff0 = grp * G * 128
                        wg, wgd = wg_r.next()
                        wu, wud = wu_r.next()
                        wdn, wdd = wd_r.next()
                        kb.dma("pool", wg, wg_a[e_][:, ff0:ff0 + G * 128].rearrange("(k p) n -> p k n", p=128), w=[wgd])
                        kb.dma("pool", wu, wu_a[e_][:, ff0:ff0 + G * 128].rearrange("(k p) n -> p k n", p=128), w=[wud])
                        kb.dma("pool", wdn, wd_a[e_][ff0:ff0 + G * 128, :].rearrange("(g p) n -> p g n", p=128), w=[wdd])
                        hT, hTd = hT_r.next()
                        for gi in range(G):
                            for tb in range(HT // 512):
                                tok = slice(half * HT + tb * 512, half * HT + (tb + 1) * 512)
                                xd_blk = self.XTd[(half * HT) // 512 + tb]
                                pg, pgd = pg_r.next()
                                pu, pud = pu_r.next()
                                for k in range(8):
                                    kb.op("pe", lambda e, k=k: e.matmul(pg, lhsT=wg[:, k, gi * 128:(gi + 1) * 128],
                                                                        rhs=self.XT[:, k, tok], start=(k == 0), stop=(k == 7)),
                                          r=[wgd, xd_blk], w=[pgd], mark=(k == 7))
                                for k in range(8):
                                    kb.op("pe", lambda e, k=k: e.matmul(pu, lhsT=wu[:, k, gi * 128:(gi + 1) * 128],
                                                                        rhs=self.XT[:, k, tok], start=(k == 0), stop=(k == 7)),
                                          r=[wud, xd_blk], w=[pud], mark=(k == 7))
                                sg, sgd = sg_r.next()
                                kb.op("act", lambda e: e.activation(out=sg, in_=pg, func=AF.Silu), r=[pgd], w=[sgd])
                                kb.op("dve", lambda e: e.tensor_tensor(out=hT[:, gi, tb * 512:(tb + 1) * 512], in0=pu, in1=sg,
                                                                       op=ALU.mult), r=[pud, sgd], w=[hTd])
                        for tt in range(HT // 128):
                            t = half * (HT // 128) + tt
                            for hf in range(2):
                                po, pod = po_r.next()
                                for gi in range(G):
                                    kb.op("pe", lambda e, gi=gi: e.matmul(po, lhsT=hT[:, gi, tt * 128:(tt + 1) * 128],
                                                                          rhs=wdn[:, gi, hf * 512:(hf + 1) * 512],
                                                                          start=(gi == 0), stop=(gi == G - 1)),
                                          r=[hTd, wdd], w=[pod], mark=(gi == G - 1))
                                a = acc[:, tt, hf * 512:(hf + 1) * 512]
                                if moe:
                                    gsc = self.gates[:, t, e_:e_ + 1]
                                    if first:
                                        kb.op("dve", lambda e: e.tensor_scalar(out=a, in0=po, scalar1=gsc, scalar2=None,
                                                                               op0=ALU.mult), r=[pod, self.gatesd], w=[accd[tt]])
                                    else:
                                        kb.op("dve", lambda e: e.scalar_tensor_tensor(out=a, in0=po, scalar=gsc, in1=a,
                                                                                      op0=ALU.mult, op1=ALU.add),
                                              r=[pod, self.gatesd, accd[tt]], w=[accd[tt]])
                                else:
                                    if first:
                                        kb.op("act", lambda e: e.copy(out=a, in_=po), r=[pod], w=[accd[tt]])
                                    else:
                                        kb.op("dve", lambda e: e.tensor_tensor(out=a, in0=po, in1=a, op=ALU.add),
                                              r=[pod, accd[tt]], w=[accd[tt]])
                        first = False
                for tt in range(HT // 128):
                    t = half * (HT // 128) + tt
                    x, xd = x_r.next()
                    kb.dma("sp", x, self.xres[t * 128:(t + 1) * 128, :], w=[xd])
                    z = acc[:, tt, :]
                    zd = accd[tt]
                    kb.op("dve", lambda e: e.scalar_tensor_tensor(out=z, in0=x, scalar=float(ALPHA), in1=z,
                                                                  op0=ALU.mult, op1=ALU.add), r=[xd, zd], w=[zd])
                    st, std = st_r.next()
                    self.layer_norm_tile(z, zd, junk[:], jd, st, std, gb, gbd)
                    if last:
                        kb.dma("sp", self.out[t * 128:(t + 1) * 128, :], z, r=[zd])
                    else:
                        kb.dma("sp", self.xres[t * 128:(t + 1) * 128, :], z, r=[zd, xd])
                        self.transpose_to_XT(z, zd, t, tr)


WEIGHT_KEYS = ("w_in", "w_out", "rwkv_mu", "rwkv_w0", "rwkv_w_up", "rwkv_a0", "rwkv_a_up", "rwkv_g_up",
               "rwkv_k_k", "rwkv_k_a", "rwkv_r_k", "rwkv_ln_g", "rwkv_ln_b", "ret_gn_g", "ret_gn_b",
               "ln_g", "ln_b", "ffn_w_gate", "ffn_w_up", "ffn_w_down", "moe_router", "moe_w_gate",
               "moe_w_up", "moe_w_down")


def make_in_map(inputs, b, consts, shared=None):
    m = {}
    m["x"] = np.ascontiguousarray(np.asarray(inputs["x"])[b], dtype=np.float32)
    if shared is None:
        shared = make_shared(inputs, consts)
    m.update(shared)
    return m


def make_shared(inputs, consts):
    sh = {}
    for k in WEIGHT_KEYS:
        sh[k] = np.ascontiguousarray(np.asarray(inputs[k]), dtype=np.float32)
    rb = np.asarray(inputs["rel_bias"], dtype=np.float32)
    idx = bias_bucket_index()
    g = rb[idx]
    sh["biasg"] = np.ascontiguousarray(np.transpose(g, (0, 4, 1, 2, 3)))
    sh["cst"] = consts["cst"]
    sh["cos2"] = consts["cos2"]
    sh["sin2"] = consts["sin2"]
    sh["amask"] = consts["amask"]
    return sh


_PROG = None


def kernel(**inputs):
    global _PROG
    if _PROG is None:
        p = Prog()
        p.build()
        _PROG = p
    p = _PROG
    shared = make_shared(inputs, p.consts)
    in_maps = [make_in_map(inputs, b, p.consts, shared) for b in range(8)]
    res = run_bass_kernel_spmd(p.nc, in_maps, core_ids=list(range(8)))
    out = np.stack([np.asarray(r["out"], dtype=np.float32) for r in res.results], 0)
    return out
```

```python
import math
from contextlib import ExitStack
import numpy as np
import concourse.bass as bass
import concourse.mybir as mybir
from concourse.bass_utils import run_bass_kernel_spmd

F32 = mybir.dt.float32
BF16 = mybir.dt.bfloat16
ALU = mybir.AluOpType
AF = mybir.ActivationFunctionType
AX = mybir.AxisListType

D = 1024
S = 4096
DEPTH = 4
NT = S // 128
RWKV_IN = 1024
ATTN_IN = 1536
RET_IN = 1024
N_IN = 3584
FF_DENSE = 2816
FF_EXPERT = 3584
N_EXPERTS = 8
ALPHA = (2 * DEPTH) ** 0.25
LN_EPS = 1e-5
RWKV_GN_EPS = 64e-5
C0 = math.exp(-0.5)
PATTERNS = ((128, 1), (512, 4), (2048, 16))
NEG = -240000.0


class Dep:
    __slots__ = ("w", "r", "excl")

    def __init__(self, excl=False):
        self.w = None
        self.r = {}
        self.excl = excl


class Engine:
    def __init__(self, e, sem, idx):
        self.e = e
        self.sem = sem
        self.idx = idx
        self.cnt = 0
        self.seen = {}
        self.slots = []
        self.si = 0


class KB:
    NSLOT = 12

    def __init__(self, nc, es):
        self.nc = nc
        self.sems = []
        self.eng = {}
        for name, e in (("pe", nc.tensor), ("dve", nc.vector), ("act", nc.scalar),
                        ("pool", nc.gpsimd), ("sp", nc.sync)):
            sem = es.enter_context(nc.semaphore("s_" + name))
            self.sems.append(sem)
            self.eng[name] = Engine(e, sem, len(self.sems) - 1)
        for q in ("sp", "pool"):
            for i in range(self.NSLOT):
                sem = es.enter_context(nc.semaphore("d_%s%d" % (q, i)))
                self.sems.append(sem)
                self.eng[q].slots.append([len(self.sems) - 1, 0])
        self.nins = 0

    def _need(self, E, ev, raw=True):
        si, val = ev
        if val <= 0:
            return
        if si == E.idx and val > E.cnt:
            return
        if E.seen.get(si, 0) < val:
            E.e.wait_ge(self.sems[si], val)
            E.seen[si] = val

    def _waits(self, E, r, w):
        for d in r:
            if d.excl:
                if d.w is not None:
                    self._need(E, d.w)
                for si, v in d.r.items():
                    self._need(E, (si, v), raw=False)
            elif d.w is not None:
                self._need(E, d.w)
        for d in w:
            if d.w is not None:
                self._need(E, d.w, raw=False)
            for si, v in d.r.items():
                self._need(E, (si, v), raw=False)

    def _post(self, ev, r, w):
        for d in w:
            d.w = ev
            d.r = {}
        for d in r:
            if d.excl:
                d.w = ev
                d.r = {}
            elif d.r.get(ev[0], 0) < ev[1]:
                d.r[ev[0]] = ev[1]

    def op(self, eng, fn, r=(), w=(), mark=True):
        E = self.eng[eng]
        self._waits(E, r, w)
        ins = fn(E.e)
        self.nins += 1
        if mark:
            E.cnt += 1
            ins.then_inc(E.sem, 1)
            ev = (E.idx, E.cnt)
        else:
            ev = (E.idx, E.cnt + 1)
        self._post(ev, r, w)
        return ins

    def dma(self, q, out, in_, r=(), w=(), slow=False):
        E = self.eng[q]
        slot = E.slots[E.si]
        E.si = (E.si + 1) % len(E.slots)
        self._need(E, (slot[0], slot[1]))
        self._waits(E, r, w)
        if slow:
            ins = E.e.dma_start(out=out, in_=in_, allow_slow_non_contiguous=True)
        else:
            ins = E.e.dma_start(out=out, in_=in_)
        self.nins += 1
        slot[1] += 16
        ins.then_inc(self.sems[slot[0]], 16)
        ev = (slot[0], slot[1])
        self._post(ev, r, w)
        return ins

    def barrier(self):
        evs = []
        for E in self.eng.values():
            if E.cnt > 0:
                evs.append((E.idx, E.cnt))
            for s in E.slots:
                if s[1] > 0:
                    evs.append((s[0], s[1]))
        for E in self.eng.values():
            for ev in evs:
                if ev[0] != E.idx:
                    self._need(E, ev)


class Ring:
    def __init__(self, tile, n):
        self.t = tile
        self.n = n
        self.d = [Dep() for _ in range(n)]
        self.i = 0

    def next(self):
        i = self.i
        self.i = (i + 1) % self.n
        return self.t[:, i], self.d[i]


class BRing:
    def __init__(self, items):
        self.items = list(items)
        self.i = 0

    def next(self):
        it = self.items[self.i]
        self.i = (self.i + 1) % len(self.items)
        return it


def run_interleaved(gens):
    gens = list(gens)
    while gens:
        nxt = []
        for g in gens:
            try:
                next(g)
                nxt.append(g)
            except StopIteration:
                pass
        gens = nxt


def t5_bucket(dist):
    nb = 32
    max_exact = nb // 2
    large = max_exact + (np.log(np.maximum(dist, max_exact) / max_exact)
                         / math.log(2048 / max_exact) * (nb - max_exact)).astype(np.int32)
    return np.where(dist < max_exact, dist, np.minimum(large, nb - 1)).astype(np.int32)


CST = {}


def _cst_layout():
    off = 0
    for name, n in (("ident", 128), ("onesblk", 128), ("nSU", 128), ("nSL", 128), ("SU", 128),
                    ("nIU", 128), ("IU", 128), ("sel", 64), ("sel65", 64), ("dmaskT", 512),
                    ("zeta8", 4), ("xi", 256), ("scanm", 1024)):
        CST[name] = (off, n)
        off += n
    return off


CST_N = _cst_layout()


def make_consts():
    c = np.zeros((128, CST_N), np.float32)

    def put(name, arr):
        o, n = CST[name]
        c[:arr.shape[0], o:o + n] = arr.reshape(arr.shape[0], n)

    i = np.arange(128)
    put("ident", np.eye(128, dtype=np.float32))
    put("onesblk", (i[:, None] // 64 == i[None, :] // 64).astype(np.float32))
    su = (i[None, :] > i[:, None]).astype(np.float32)
    sl = (i[None, :] < i[:, None]).astype(np.float32)
    iu = (i[None, :] >= i[:, None]).astype(np.float32)
    put("nSU", -su)
    put("nSL", -sl)
    put("SU", su)
    put("nIU", -iu)
    put("IU", iu)
    put("sel", np.concatenate([np.eye(64), np.eye(64)], 0).astype(np.float32))
    s65 = np.zeros((128, 64), np.float32)
    s65[64, :] = 1.0
    put("sel65", s65)
    lg = np.log1p(-np.exp2(-5.0 - np.arange(4, dtype=np.float64)))
    m = np.arange(128)[:, None, None]
    n = np.arange(128)[None, None, :]
    h = lg[None, :, None]
    dm = np.where(n >= m, np.exp(h * np.maximum(n - m, 0)), 0.0) / 8.0
    put("dmaskT", dm.astype(np.float32))
    put("zeta8", (np.exp(lg[None, :] * (127 - np.arange(128)[:, None])) / 8.0).astype(np.float32))
    xi = np.zeros((128, 2, 128))
    for hp in range(2):
        for hh in range(2):
            xi[hh * 64:(hh + 1) * 64, hp, :] = np.exp(lg[2 * hp + hh] * (np.arange(128) + 1))[None, :]
    put("xi", xi.astype(np.float32))
    sm = np.ones((128, 1024), np.float32)
    sm[:, 0::64] = 0.0
    put("scanm", sm)
    half = 32
    inv = (10000.0 ** (-np.arange(half, dtype=np.float32) / half)).astype(np.float32)
    ang = (np.arange(S, dtype=np.float32)[None, :] * inv[:, None]).astype(np.float32)
    cos = np.cos(ang).astype(np.float32)
    sin = np.sin(ang).astype(np.float32)
    cos2 = np.concatenate([cos, cos, cos, cos], 0)
    sin2 = np.concatenate([-sin, sin, -sin, sin], 0)
    jj = np.arange(128)[:, None]
    ii = np.arange(128)[None, :]
    mk = np.zeros((2, 128, 128), np.float32)
    mk[0] = np.where(ii <= jj, 0.0, NEG)
    mk[1] = np.where(ii >= jj, 0.0, NEG)
    gamma_c = np.exp(lg * 128.0)
    return dict(cst=c, cos2=np.ascontiguousarray(cos2), sin2=np.ascontiguousarray(sin2), amask=mk,
                gamma_c=[float(g) for g in gamma_c])


def bias_bucket_index():
    jj = np.arange(128)[:, None]
    ii = np.arange(128)[None, :]
    out = np.zeros((3, 2, 128, 128), np.int64)
    for p, (win, dil) in enumerate(PATTERNS):
        rel_prev = np.clip(ii + 128 - jj, 0, None)
        rel_cur = np.clip(ii - jj, 0, None)
        out[p, 0] = t5_bucket(rel_prev * dil)
        out[p, 1] = t5_bucket(rel_cur * dil)
    return out


class Prog:
    def __init__(self, n_layers=DEPTH, dbg=False, stop=None):
        self.n_layers = n_layers
        self.dbg = dbg
        self.stop = stop
        self.consts = make_consts()
        self.nc = nc = bass.Bass("TRN2", target_bir_lowering=False)

        def din(name, shape, dt=F32):
            return nc.dram_tensor(name, list(shape), dt, kind="ExternalInput").ap()

        self.I = I = {}
        I["x"] = din("x", [S, D])
        I["w_in"] = din("w_in", [DEPTH, D, N_IN])
        I["w_out"] = din("w_out", [DEPTH, D, D])
        I["rwkv_mu"] = din("rwkv_mu", [DEPTH, RWKV_IN])
        for nm in ("rwkv_w0", "rwkv_a0", "rwkv_k_k", "rwkv_k_a", "rwkv_r_k", "rwkv_ln_g", "rwkv_ln_b",
                   "ret_gn_g", "ret_gn_b"):
            I[nm] = din(nm, [DEPTH, 256])
        I["rwkv_w_up"] = din("rwkv_w_up", [DEPTH, 64, 256])
        I["rwkv_a_up"] = din("rwkv_a_up", [DEPTH, 64, 256])
        I["rwkv_g_up"] = din("rwkv_g_up", [DEPTH, 128, 256])
        I["ln_g"] = din("ln_g", [DEPTH, 2, D])
        I["ln_b"] = din("ln_b", [DEPTH, 2, D])
        I["ffn_w_gate"] = din("ffn_w_gate", [2, D, FF_DENSE])
        I["ffn_w_up"] = din("ffn_w_up", [2, D, FF_DENSE])
        I["ffn_w_down"] = din("ffn_w_down", [2, FF_DENSE, D])
        I["moe_router"] = din("moe_router", [2, D, N_EXPERTS])
        I["moe_w_gate"] = din("moe_w_gate", [2, N_EXPERTS, D, FF_EXPERT])
        I["moe_w_up"] = din("moe_w_up", [2, N_EXPERTS, D, FF_EXPERT])
        I["moe_w_down"] = din("moe_w_down", [2, N_EXPERTS, FF_EXPERT, D])
        I["cst"] = din("cst", [128, CST_N])
        I["cos2"] = din("cos2", [128, S])
        I["sin2"] = din("sin2", [128, S])
        I["amask"] = din("amask", [2, 128, 128])
        I["biasg"] = din("biasg", [3, 8, 2, 128, 128])
        self.out = nc.dram_tensor("out", [S, D], F32, kind="ExternalOutput").ap()

        def scr(name, shape, dt):
            kind = "ExternalOutput" if dbg else "Internal"
            return nc.dram_tensor(name, list(shape), dt, kind=kind).ap()

        self.xres = scr("xres", [S, D], F32)
        self.pT = scr("pT", [N_IN, S], F32)
        self.vtok = scr("vtok", [S, 768], BF16)
        self.yT = scr("yT", [D, S], BF16)

    _uid = 0

    def un(self, n):
        self._uid += 1
        return "%s_%d" % (n, self._uid)

    def cst(self, name, rows=128):
        o, n = CST[name]
        return self.cstt[0:rows, o:o + n]

    def build(self):
        nc = self.nc
        with ExitStack() as es:
            self.kb = kb = KB(nc, es)
            self.arena = es.enter_context(nc.sbuf_tensor("arena", [128, 16384], F32))
            self.XT = self.arena[:, :].bitcast(BF16).rearrange("p (k s) -> p k s", k=8)
            self.XTd = [Dep() for _ in range(8)]
            self.pbank_t = es.enter_context(nc.psum_tensor("psum", [128, 8, 512], F32))
            self.bank = [(self.pbank_t[:, i], Dep(excl=True)) for i in range(8)]
            self.cstt = es.enter_context(nc.sbuf_tensor("cstt", [128, CST_N], F32))
            self.cstd = Dep()
            self.identb = es.enter_context(nc.sbuf_tensor("identb", [128, 128], BF16))
            self.gates = es.enter_context(nc.sbuf_tensor("gates", [128, NT, N_EXPERTS], F32))
            self.gatesd = Dep()
            kb.dma("sp", self.cstt[:], self.I["cst"], w=[self.cstd])
            kb.op("dve", lambda e: e.tensor_copy(out=self.identb[:], in_=self.cst("ident")),
                  r=[self.cstd], w=[self.cstd])
            self.load_x0()
            kb.barrier()
            for l in range(self.n_layers):
                self.layer(l)
                if self.stop is not None and self.stop[0] == l and self.stop[1] == "layer":
                    break
            kb.barrier()
        return nc

    def setup_bias(self):
        nc, kb = self.nc, self.kb
        with ExitStack() as es:
            self.bias8d = Dep()
            tg = es.enter_context(nc.sbuf_tensor(self.un("tg"), [128, 2, 16, 128], F32))
            mk = es.enter_context(nc.sbuf_tensor(self.un("mk"), [128, 2, 128], F32))
            mkd = Dep()
            kb.dma("sp", mk[:], self.I["amask"].rearrange("a j i -> j a i"), w=[mkd])
            ring = Ring(tg, 2)
            for p in range(3):
                t, d = ring.next()
                kb.dma("sp", t.rearrange("j (h a) i -> j h a i", a=2),
                       self.I["biasg"][p].rearrange("h a j i -> j h a i"), w=[d])
                for h in range(8):
                    kb.op("dve", lambda e, t=t, h=h, p=p: e.scalar_tensor_tensor(
                        out=self.bias8[:, p, h], in0=t[:, 2 * h:2 * h + 2, :], scalar=8.0, in1=mk[:],
                        op0=ALU.mult, op1=ALU.add), r=[d, mkd], w=[self.bias8d])
            kb.barrier()

    def load_x0(self):
        nc, kb = self.nc, self.kb
        with ExitStack() as es:
            xt = es.enter_context(nc.sbuf_tensor(self.un("x0t"), [128, 2, D], F32))
            xr = Ring(xt, 2)
            pr = self.tr_ring()
            for t in range(NT):
                x, xd = xr.next()
                kb.dma("sp", x, self.I["x"][t * 128:(t + 1) * 128, :], w=[xd])
                kb.dma("sp", self.xres[t * 128:(t + 1) * 128, :], x, r=[xd])
                self.transpose_to_XT(x, xd, t, pr)
            kb.barrier()

    def tr_ring(self, pairs=((0, 1), (2, 3))):
        items = []
        for a, b in pairs:
            ap = self.pbank_t[:, a:b + 1].rearrange("p a (b c) -> p (a b) c", c=128)
            items.append((ap, [self.bank[a][1], self.bank[b][1]]))
        return BRing(items)

    def transpose_to_XT(self, x, xd, t, pr, f32_out=None):
        kb = self.kb
        p, pds = pr.next()
        for k in range(8):
            kb.op("pe", lambda e, k=k: e.transpose(p[:, k, :], x[:, k * 128:(k + 1) * 128], self.cst("ident")),
                  r=[xd, self.cstd], w=pds, mark=(k == 7))
        xd_blk = self.XTd[t // 4]
        kb.op("act", lambda e: e.copy(out=self.XT[:, :, t * 128:(t + 1) * 128], in_=p), r=pds, w=[xd_blk])
        if f32_out is not None:
            o, od = f32_out
            kb.op("dve", lambda e: e.tensor_copy(out=o, in_=p), r=pds, w=[od])

    def layer(self, l):
        kb = self.kb
        self.inproj(l)
        kb.barrier()
        if self.stop == (l, "inproj"):
            return
        self.retention(l)
        kb.barrier()
        if self.stop == (l, "ret"):
            return
        self.attention(l)
        kb.barrier()
        if self.stop == (l, "attn"):
            return
        self.rwkv(l)
        kb.barrier()
        if self.stop == (l, "rwkv"):
            return
        self.outproj_ln1(l)
        kb.barrier()
        if self.stop == (l, "ln1"):
            return
        self.ffn(l)
        kb.barrier()

    def inproj(self, l):
        nc, kb = self.nc, self.kb
        w_in = self.I["w_in"][l]
        with ExitStack() as es:
            wt = es.enter_context(nc.sbuf_tensor(self.un("ip_w"), [128, 2, 8, 512], BF16))
            stg = es.enter_context(nc.sbuf_tensor(self.un("ip_s"), [128, 2, S], F32))
            wv = es.enter_context(nc.sbuf_tensor(self.un("ip_wv"), [128, 8, 768], BF16))
            vt = es.enter_context(nc.sbuf_tensor(self.un("ip_vt"), [128, 2, 768], BF16))
            wr, pr, vr = Ring(wt, 2), BRing(self.bank), Ring(vt, 2)
            sdeps = [[Dep() for _ in range(8)] for _ in range(2)]
            wvd = Dep()
            kb.dma("pool", wv[:, :, 0:512], w_in[:, 2048:2560].rearrange("(k p) n -> p k n", p=128), w=[wvd])
            kb.dma("pool", wv[:, :, 512:768], w_in[:, 3072:3328].rearrange("(k p) n -> p k n", p=128), w=[wvd])
            n_ev = 0
            si = 0
            for cb in range(7):
                w, wd = wr.next()
                kb.dma("pool", w, w_in[:, cb * 512:(cb + 1) * 512].rearrange("(k p) n -> p k n", p=128), w=[wd])
                for fc in range(4):
                    st = stg[:, si]
                    sd = sdeps[si]
                    si ^= 1
                    for tb in range(8):
                        p, pd = pr.next()
                        for k in range(8):
                            kb.op("pe", lambda e, k=k: e.matmul(
                                p, lhsT=w[:, k, fc * 128:(fc + 1) * 128], rhs=self.XT[:, k, tb * 512:(tb + 1) * 512],
                                start=(k == 0), stop=(k == 7)), r=[wd, self.XTd[tb]], w=[pd], mark=(k == 7))
                        o = st[:, tb * 512:(tb + 1) * 512]
                        if n_ev % 2 == 0:
                            kb.op("act", lambda e: e.copy(out=o, in_=p), r=[pd], w=[sd[tb]])
                        else:
                            kb.op("dve", lambda e: e.tensor_copy(out=o, in_=p), r=[pd], w=[sd[tb]])
                        n_ev += 1
                    row = (cb * 4 + fc) * 128
                    kb.dma("sp", self.pT[row:row + 128, :], st, r=sd)
            for t in range(NT):
                v, vd = vr.next()
                p1, pd1 = pr.next()
                p2, pd2 = pr.next()
                for k in range(8):
                    kb.op("pe", lambda e, k=k: e.matmul(p1, lhsT=self.XT[:, k, t * 128:(t + 1) * 128], rhs=wv[:, k, 0:512],
                                                        start=(k == 0), stop=(k == 7)),
                          r=[wvd, self.XTd[t // 4]], w=[pd1], mark=(k == 7))
                for k in range(8):
                    kb.op("pe", lambda e, k=k: e.matmul(p2[:, 0:256], lhsT=self.XT[:, k, t * 128:(t + 1) * 128],
                                                        rhs=wv[:, k, 512:768], start=(k == 0), stop=(k == 7)),
                          r=[wvd, self.XTd[t // 4]], w=[pd2], mark=(k == 7))
                kb.op("act", lambda e: e.copy(out=v[:, 0:512], in_=p1), r=[pd1], w=[vd])
                kb.op("dve", lambda e: e.tensor_copy(out=v[:, 512:768], in_=p2[:, 0:256]), r=[pd2], w=[vd])
                kb.dma("sp", self.vtok[t * 128:(t + 1) * 128, :], v, r=[vd])


    def abf(self, lo, hi, **kw):
        return self.arena[:, lo:hi].bitcast(BF16)

    def retention(self, l):
        nc, kb = self.nc, self.kb
        base = RWKV_IN + ATTN_IN
        pT = self.pT
        gam = self.consts["gamma_c"]
        H0, H1 = slice(0, 64), slice(64, 128)
        with ExitStack() as es:
            sb = lambda n, sh, dt: es.enter_context(nc.sbuf_tensor(self.un(n), sh, dt))
            pp = lambda n, sh, dt: es.enter_context(nc.psum_tensor(n, sh, dt))
            kr = self.abf(0, 2048)
            qm = [self.abf(2048, 4096), self.abf(4096, 6144)]
            qx = [self.abf(6144, 8192), self.abf(8192, 10240)]
            sgb = self.abf(10240, 12288)
            krd, qmd, qxd, sgd = Dep(), Dep(), Dep(), Dep()
            cs_t = sb("rt_cs", [128, 2, 2, 1024], F32)
            tq_t = sb("rt_tq", [128, 2, 1024], F32)
            tw_t = sb("rt_tw", [128, 2, 1024], F32)
            v = sb("rt_v", [128, NT, 256], BF16)
            yo = sb("rt_yo", [128, S], BF16)
            gnp = sb("rt_gnp", [128, 4], F32)
            rst = sb("rt_rst", [128, 64], F32)
            rstb = sb("rt_rstb", [128, 64], BF16)
            vd, yod, gnd, rsd, rsbd = Dep(), Dep(), Dep(), Dep(), Dep()
            kb.dma("sp", v[:], self.vtok[:, 512:768].rearrange("(t p) c -> p t c", p=128), w=[vd])
            kb.dma("sp", gnp[:, 0:2], self.I["ret_gn_g"][l].rearrange("(a p) -> p a", p=128), w=[gnd], slow=True)
            kb.dma("sp", gnp[:, 2:4], self.I["ret_gn_b"][l].rearrange("(a p) -> p a", p=128), w=[gnd], slow=True)
            csr, tqr, twr = Ring(cs_t, 2), Ring(tq_t, 2), Ring(tw_t, 2)
            B = self.bank
            bfv = lambda i: (B[i][0].bitcast(BF16)[:, 0:128], B[i][1])
            ps_s = BRing([(B[0][0][:, 0:256], B[0][1]), (B[1][0][:, 0:256], B[1][1])])
            ps_k = BRing([bfv(2)])
            ps_kv = BRing([(B[3][0][:, 0:128], B[3][1])])
            ps_o = BRing([(B[4][0][:, 0:128], B[4][1]), (B[5][0][:, 0:128], B[5][1])])
            ps_t = BRing([bfv(6)])
            sTm_r = Ring(sb("rt_sTm", [128, 2, 256], BF16), 2)
            kz_r = Ring(sb("rt_kz", [128, 2, 128], BF16), 2)
            osq_r = Ring(sb("rt_osq", [128, 2, 128], F32), 2)
            yn_r = Ring(sb("rt_yn", [128, 2, 128], BF16), 2)
            st_r = Ring(sb("rt_st", [128, 2, 16], F32), 2)
            tf_r = Ring(sb("rt_tf", [128, 2, 128], F32), 2)
            dmask = self.cst("dmaskT")
            zeta = self.cst("zeta8")
            xi = self.cst("xi").rearrange("p (a n) -> p a n", a=2)
            for hp in range(2):
                kb.op("pool", lambda e: e.memset(qm[0][H1, :], 0.0), w=[qmd])
                kb.op("pool", lambda e: e.memset(qm[1][H0, :], 0.0), w=[qmd])
                kb.op("pool", lambda e: e.memset(qx[0][H1, :], 0.0), w=[qxd])
                kb.op("pool", lambda e: e.memset(qx[1][H0, :], 0.0), w=[qxd])
                kb.op("pool", lambda e: e.memset(rst[:], 0.0), w=[rsd])
                for pc in range(4):
                    sl = slice(pc * 1024, (pc + 1) * 1024)
                    cs, csd = csr.next()
                    kb.dma("sp", cs[:, 0], self.I["cos2"][:, sl], w=[csd])
                    kb.dma("sp", cs[:, 1], self.I["sin2"][:, sl], w=[csd])
                    for which in range(2):
                        r0 = base + which * 256 + hp * 128
                        tq, tqd = tqr.next()
                        tw, twd = twr.next()
                        kb.dma("sp", tq, pT[r0:r0 + 128, sl], w=[tqd])
                        for blk in range(4):
                            src = r0 + (blk ^ 1) * 32
                            kb.dma("sp", tw[blk * 32:(blk + 1) * 32], pT[src:src + 32, sl], w=[twd])
                        kb.op("dve", lambda e: e.tensor_tensor(out=tq, in0=tq, in1=cs[:, 0], op=ALU.mult),
                              r=[csd], w=[tqd])
                        kb.op("pool", lambda e: e.tensor_tensor(out=tw, in0=tw, in1=cs[:, 1], op=ALU.mult),
                              r=[csd], w=[twd])
                        if which == 1:
                            kb.op("dve", lambda e: e.tensor_tensor(out=kr[:, sl], in0=tq, in1=tw, op=ALU.add),
                                  r=[tqd, twd], w=[krd])
                        else:
                            for hh, rows in enumerate((H0, H1)):
                                kb.op("dve", lambda e: e.tensor_tensor(out=qm[hh][rows, sl], in0=tq[rows], in1=tw[rows],
                                                                       op=ALU.add), r=[tqd, twd], w=[qmd])
                    r0 = base + 768 + hp * 128
                    tq, tqd = tqr.next()
                    kb.dma("sp", tq, pT[r0:r0 + 128, sl], w=[tqd])
                    kb.op("act", lambda e: e.activation(out=sgb[:, sl], in_=tq, func=AF.Silu), r=[tqd], w=[sgd])
                for hh, rows in enumerate((H0, H1)):
                    kb.op("pool", lambda e: e.tensor_tensor(
                        out=qx[hh][rows, :].rearrange("p (c n) -> p c n", n=128),
                        in0=qm[hh][rows, :].rearrange("p (c n) -> p c n", n=128),
                        in1=xi[rows, hp:hp + 1, :].broadcast_to([64, NT, 128]), op=ALU.mult),
                        r=[qmd, self.cstd], w=[qxd])
                import os
                CUT = int(os.environ.get("RET_CUT", "99"))
                for c in range((int(os.environ.get("RET_NC", "32")) if CUT > 2 else 0)):
                    cs = slice(c * 128, (c + 1) * 128)
                    p_s, p_sd = ps_s.next()
                    for hh in range(2):
                        kb.op("pe", lambda e: e.matmul(p_s[:, hh * 128:(hh + 1) * 128], lhsT=kr[:, cs], rhs=qm[hh][:, cs],
                                                       start=True, stop=True), r=[krd, qmd], w=[p_sd], mark=(hh == 1))
                    sTm, sTmd = sTm_r.next()
                    kb.op("dve", lambda e: e.tensor_tensor(out=sTm, in0=p_s, in1=dmask[:, hp * 256:(hp + 1) * 256],
                                                           op=ALU.mult), r=[p_sd, self.cstd], w=[sTmd])
                    if CUT <= 3:
                        continue
                    p_k, p_kd = ps_k.next()
                    kb.op("pe", lambda e: e.transpose(p_k, kr[:, cs], self.identb[:]), r=[krd, self.cstd], w=[p_kd])
                    kz, kzd = kz_r.next()
                    for hh in range(2):
                        h = 2 * hp + hh
                        kb.op("act", lambda e: e.mul(out=kz[:, hh * 64:(hh + 1) * 64], in_=p_k[:, hh * 64:(hh + 1) * 64],
                                                     mul=zeta[:, h:h + 1]), r=[p_kd, self.cstd], w=[kzd])
                    if CUT <= 4:
                        continue
                    p_o, p_od = ps_o.next()
                    for hh in range(2):
                        h = 2 * hp + hh
                        kb.op("pe", lambda e: e.matmul(p_o[:, hh * 64:(hh + 1) * 64], lhsT=sTm[:, hh * 128:(hh + 1) * 128],
                                                       rhs=v[:, c, h * 64:(h + 1) * 64], start=True, stop=(c == 0)),
                              r=[sTmd, vd], w=[p_od], mark=(c == 0 and hh == 1))
                        if c > 0:
                            kb.op("pe", lambda e: e.matmul(p_o[:, hh * 64:(hh + 1) * 64], lhsT=qx[hh][:, cs], rhs=rstb[:],
                                                           start=False, stop=True),
                                  r=[qxd, rsbd], w=[p_od], mark=(hh == 1))
                    if CUT <= 5:
                        continue
                    if c < NT - 1:
                        p_kv, p_kvd = ps_kv.next()
                        kb.op("pe", lambda e: e.matmul(p_kv, lhsT=kz, rhs=v[:, c, hp * 128:(hp + 1) * 128],
                                                       start=True, stop=True), r=[kzd, vd], w=[p_kvd])
                        for hh, rows in enumerate((H0, H1)):
                            kb.op("dve", lambda e: e.scalar_tensor_tensor(
                                out=rst[rows, :], in0=rst[rows, :], scalar=gam[2 * hp + hh],
                                in1=p_kv[rows, hh * 64:(hh + 1) * 64], op0=ALU.mult, op1=ALU.add),
                                r=[p_kvd, rsbd], w=[rsd])
                        kb.op("dve", lambda e: e.tensor_copy(out=rstb[:], in_=rst[:]), r=[rsd], w=[rsbd])
                    if CUT <= 6:
                        continue
                    st, std = st_r.next()
                    osq, osqd = osq_r.next()
                    kb.op("act", lambda e: e.activation(out=osq, in_=p_o, func=AF.Square), r=[p_od], w=[osqd])
                    kb.op("dve", lambda e: e.reduce_sum(out=st[:, 0:2], in_=p_o.rearrange("p (h e) -> p h e", e=64),
                                                        axis=AX.X), r=[p_od], w=[std])
                    kb.op("dve", lambda e: e.reduce_sum(out=st[:, 2:4], in_=osq.rearrange("p (h e) -> p h e", e=64),
                                                        axis=AX.X), r=[osqd], w=[std])
                    self.norm_stats(st, std, 2, 64, LN_EPS)
                    if CUT <= 7:
                        continue
                    yn, ynd = yn_r.next()
                    for hh in range(2):
                        kb.op("dve", lambda e: e.tensor_scalar(out=yn[:, hh * 64:(hh + 1) * 64], in0=p_o[:, hh * 64:(hh + 1) * 64],
                                                               scalar1=st[:, 12 + hh:13 + hh], scalar2=st[:, 14 + hh:15 + hh],
                                                               op0=ALU.mult, op1=ALU.add), r=[p_od, std], w=[ynd])
                    if CUT <= 8:
                        continue
                    p_t, p_td = ps_t.next()
                    kb.op("pe", lambda e: e.transpose(p_t, yn, self.identb[:]), r=[ynd, self.cstd], w=[p_td])
                    tf, tfd = tf_r.next()
                    kb.op("dve", lambda e: e.tensor_scalar(out=tf, in0=p_t, scalar1=gnp[:, hp:hp + 1],
                                                           scalar2=gnp[:, 2 + hp:3 + hp], op0=ALU.mult, op1=ALU.add),
                          r=[p_td, gnd], w=[tfd])
                    kb.op("pool", lambda e: e.tensor_tensor(out=yo[:, cs], in0=tf, in1=sgb[:, cs], op=ALU.mult),
                          r=[tfd, sgd], w=[yod])
                kb.dma("sp", self.yT[768 + hp * 128:768 + (hp + 1) * 128, :], yo[:], r=[yod])

    def norm_stats(self, st, std, n, cnt, eps):
        kb = self.kb
        c = lambda i: st[:, i * n:(i + 1) * n]
        kb.op("dve", lambda e: e.tensor_scalar(out=c(2), in0=c(0), scalar1=1.0 / cnt, scalar2=None, op0=ALU.mult),
              r=[std], w=[std])
        kb.op("dve", lambda e: e.tensor_tensor(out=c(3), in0=c(2), in1=c(2), op=ALU.mult), r=[std], w=[std])
        kb.op("dve", lambda e: e.scalar_tensor_tensor(out=c(4), in0=c(1), scalar=1.0 / cnt, in1=c(3),
                                                      op0=ALU.mult, op1=ALU.subtract), r=[std], w=[std])
        kb.op("dve", lambda e: e.tensor_scalar(out=c(4), in0=c(4), scalar1=float(eps), scalar2=None, op0=ALU.add),
              r=[std], w=[std])
        kb.op("act", lambda e: e.activation(out=c(5), in_=c(4), func=AF.Sqrt), r=[std], w=[std])
        kb.op("dve", lambda e: e.reciprocal(out=c(6), in_=c(5)), r=[std], w=[std])
        kb.op("dve", lambda e: e.scalar_tensor_tensor(out=c(7), in0=c(2), scalar=-1.0, in1=c(6),
                                                      op0=ALU.mult, op1=ALU.mult), r=[std], w=[std])

    def attention(self, l):
        nc, kb = self.nc, self.kb
        base = RWKV_IN
        pT = self.pT
        H0, H1 = slice(0, 64), slice(64, 128)
        with ExitStack() as es:
            sb = lambda n, sh, dt: es.enter_context(nc.sbuf_tensor(self.un(n), sh, dt))
            self.bias8 = sb("bias8", [128, 3, 8, 2, 128], BF16)
            self.setup_bias()
            acc = self.arena[0:64, :].rearrange("p (a h s) -> p a h s", a=2, h=2)
            accd = Dep()
            kt = sb("at_k", [128, S], BF16)
            qm = [sb("at_q0", [128, S], BF16), sb("at_q1", [128, S], BF16)]
            vp = [sb("at_v%d" % i, [128, NT, 128], BF16) for i in range(3)]
            ones = sb("at_ones", [128, 64], BF16)
            PT_r = Ring(sb("at_PT", [128, 2, 512], BF16), 2)
            rc = sb("at_rc", [64, 2, S], F32)
            yo = sb("at_yo", [64, 2, S], BF16)
            ktd, qmd, vd, od, rcd, yod = Dep(), Dep(), Dep(), Dep(), Dep(), Dep()
            kb.op("pool", lambda e: e.memset(ones[:], 1.0), w=[od])
            B = self.bank
            ps_s = BRing([B[0], B[1], B[2], B[3]])
            ps_o = BRing([B[4], B[5], B[6], B[7]])
            for pair in range(4):
                kb.op("pool", lambda e: e.memset(qm[0][H1, :], 0.0), w=[qmd])
                kb.op("pool", lambda e: e.memset(qm[1][H0, :], 0.0), w=[qmd])
                kb.op("dve", lambda e: e.memset(self.arena[0:64, :], 0.0), w=[accd])
                q0 = base + pair * 128
                k0 = base + 512 + pair * 128
                kb.dma("pool", qm[0][H0, :], pT[q0:q0 + 64, :], w=[qmd])
                kb.dma("pool", qm[1][H1, :], pT[q0 + 64:q0 + 128, :], w=[qmd])
                kb.dma("pool", kt[:], pT[k0:k0 + 128, :], w=[ktd])
                for p, (win, dil) in enumerate(PATTERNS):
                    nb = NT // dil
                    for r in range(dil):
                        src = self.vtok[:, pair * 128:(pair + 1) * 128].rearrange("(b j r) c -> r j b c", r=dil, j=128)[r]
                        kb.dma("sp", vp[p][:, r * nb:(r + 1) * nb, :], src, w=[vd])
                for p, (win, dil) in enumerate(PATTERNS):
                    nb = NT // dil
                    for r in range(dil):
                        for b in range(nb):
                            def tsl(bb):
                                st = r + dil * 128 * bb
                                return slice(st, st + dil * 127 + 1, dil)
                            qs = tsl(b)
                            halves = (0, 1) if b > 0 else (1,)
                            p_s, p_sd = ps_s.next()
                            for hh in range(2):
                                h = 2 * pair + hh
                                for half in halves:
                                    ks = tsl(b - 1) if half == 0 else qs
                                    o = p_s[:, (hh * 2 + half) * 128:(hh * 2 + half + 1) * 128]
                                    kb.op("pe", lambda e: e.matmul(o, lhsT=kt[:, ks], rhs=qm[hh][:, qs], start=True, stop=False),
                                          r=[ktd, qmd], w=[p_sd], mark=False)
                                    kb.op("pe", lambda e: e.matmul(o, lhsT=self.identb[:], rhs=self.bias8[:, p, h, half, :],
                                                                   start=False, stop=True),
                                          r=[self.cstd, self.bias8d], w=[p_sd], mark=(hh == 1 and half == 1))
                            PT, PTd = PT_r.next()
                            if b > 0:
                                kb.op("act", lambda e: e.activation(out=PT, in_=p_s, func=AF.Exp, scale=0.125),
                                      r=[p_sd], w=[PTd])
                            else:
                                v4 = lambda t: t.rearrange("p (h a i) -> p h a i", h=2, a=2)[:, :, 1, :]
                                kb.op("act", lambda e: e.activation(out=v4(PT), in_=v4(p_s), func=AF.Exp, scale=0.125),
                                      r=[p_sd], w=[PTd])
                            p_o, p_od = ps_o.next()
                            for nd in range(2):
                                for hh in range(2):
                                    o = p_o[0:64, (nd * 2 + hh) * 128:(nd * 2 + hh + 1) * 128]
                                    for hi, half in enumerate(halves):
                                        ti = r * nb + (b - 1 if half == 0 else b)
                                        lw = vp[p][:, ti, hh * 64:(hh + 1) * 64] if nd == 0 else ones[:]
                                        kb.op("pe", lambda e: e.matmul(o, lhsT=lw, rhs=PT[:, (hh * 2 + half) * 128:(hh * 2 + half + 1) * 128],
                                                                       start=(hi == 0), stop=(hi == len(halves) - 1)),
                                              r=[PTd, vd, od], w=[p_od], mark=(nd == 1 and hh == 1 and hi == len(halves) - 1))
                            kb.op("dve", lambda e: e.tensor_tensor(
                                out=acc[:, :, :, qs], in0=acc[:, :, :, qs],
                                in1=p_o[0:64, :].rearrange("p (a h i) -> p a h i", a=2, h=2), op=ALU.add),
                                r=[p_od, accd], w=[accd])
                kb.op("dve", lambda e: e.reciprocal(out=rc[:], in_=acc[:, 1]), r=[accd], w=[rcd])
                kb.op("dve", lambda e: e.tensor_tensor(out=yo[:], in0=acc[:, 0], in1=rc[:], op=ALU.mult),
                      r=[accd, rcd], w=[yod])
                for hh in range(2):
                    r0 = 256 + (2 * pair + hh) * 64
                    kb.dma("sp", self.yT[r0:r0 + 64, :], yo[:, hh, :], r=[yod])

    def rwkv(self, l):
        nc, kb = self.nc, self.kb
        pT = self.pT
        I = self.I
        H0, H1 = slice(0, 64), slice(64, 128)
        SEG = 1024
        NCH = SEG // 64
        with ExitStack() as es:
            sb = lambda n, sh, dt: es.enter_context(nc.sbuf_tensor(self.un(n), sh, dt))
            U = [self.arena[:, i * SEG:(i + 1) * SEG] for i in range(16)]
            Ud = [Dep() for _ in range(16)]
            prm = sb("rw_prm", [128, 32], F32)
            prd = Dep()
            pcol = {}
            kb.dma("sp", prm[:, 0:8], I["rwkv_mu"][l].rearrange("(c p) -> p c", p=128), w=[prd], slow=True)
            for i, nm in enumerate(("rwkv_w0", "rwkv_a0", "rwkv_k_k", "rwkv_k_a", "rwkv_r_k", "rwkv_ln_g", "rwkv_ln_b")):
                pcol[nm] = 8 + 2 * i
                kb.dma("sp", prm[:, 8 + 2 * i:10 + 2 * i], I[nm][l].rearrange("(a p) -> p a", p=128), w=[prd], slow=True)
            pcol["omka"] = 22
            kb.op("dve", lambda e: e.tensor_scalar(out=prm[:, 22:24], in0=prm[:, pcol["rwkv_k_a"]:pcol["rwkv_k_a"] + 2],
                                                   scalar1=-1.0, scalar2=1.0, op0=ALU.mult, op1=ALU.add), r=[prd], w=[prd])
            P = lambda nm, hp: prm[:, pcol[nm] + hp:pcol[nm] + hp + 1]
            wlw = sb("rw_wlw", [128, 256], BF16)
            wla = sb("rw_wla", [128, 256], BF16)
            gup = sb("rw_gup", [128, 256], BF16)
            wd = Dep()
            kb.op("pool", lambda e: e.memset(wlw[H1, :], 0.0), w=[wd])
            kb.op("pool", lambda e: e.memset(wla[H0, :], 0.0), w=[wd])
            kb.dma("pool", wlw[H0, :], I["rwkv_w_up"][l], w=[wd])
            kb.dma("pool", wla[H1, :], I["rwkv_a_up"][l], w=[wd])
            kb.dma("pool", gup[:], I["rwkv_g_up"][l], w=[wd])
            pad = {}
            padd = {}
            for nm in ("Rt", "Kk", "Bt", "Kt", "V"):
                pad[nm] = sb("rw_p" + nm, [128, NCH, 128], F32)
                padd[nm] = Dep()
                kb.op("pool", lambda e, nm=nm: e.memset(pad[nm][:], 0.0), w=[padd[nm]])
            T6 = sb("rw_T6", [128, SEG], BF16)
            SG = sb("rw_SG", [128, SEG], BF16)
            T6d, SGd = Dep(), Dep()
            buf_r = Ring(sb("rw_buf", [128, 2, SEG + 1], F32), 2)
            tmp_r = Ring(sb("rw_tmp", [128, 2, SEG], F32), 2)
            yo_r = Ring(sb("rw_yo", [128, 2, SEG], BF16), 2)
            Hs = [sb("rw_H%d" % i, [128, 128], F32) for i in range(2)]
            Hd = [Dep(), Dep()]
            import os
            NW = int(os.environ.get('RW_NW', '4'))
            wk = []
            for g in range(NW):
                t = {}
                for nm in ("nBtPT", "KtPT", "VPT", "N", "NT", "MakT", "nPrbT", "PrkT", "Pa", "Pb", "PT1", "PT2", "PT4",
                           "PT8", "PT16", "PT32", "QT", "GT0", "E", "YnP", "sq"):
                    t[nm] = (sb("rw_%s%d" % (nm, g), [128, 128], F32), Dep())
                for nm in ("Xa", "Xb"):
                    t[nm] = (sb("rw_%s%d" % (nm, g), [128, 256], F32), Dep())
                t["st"] = (sb("rw_st%d" % g, [128, 8], F32), Dep())
                kb.op("pool", lambda e, t=t: e.memset(t["YnP"][0][:], 0.0), w=[t["YnP"][1]])
                wk.append(t)
            banks = BRing(self.bank)
            ident = self.cst("ident")
            cd = self.cstd
            evc = [0]

            def copy_ev(out, in_, r, w, scale=None):
                evc[0] += 1
                if evc[0] % 2 == 0:
                    if scale is None:
                        kb.op("act", lambda e: e.copy(out=out, in_=in_), r=r, w=w)
                    else:
                        kb.op("act", lambda e: e.mul(out=out, in_=in_, mul=float(scale)), r=r, w=w)
                else:
                    if scale is None:
                        kb.op("dve", lambda e: e.tensor_copy(out=out, in_=in_), r=r, w=w)
                    else:
                        kb.op("dve", lambda e: e.tensor_scalar(out=out, in0=in_, scalar1=float(scale), scalar2=None,
                                                               op0=ALU.mult), r=r, w=w)

            def load_shift(row0, seg, c8, dst, dstd):
                buf, bd = buf_r.next()
                tmp, td = tmp_r.next()
                t0 = seg * SEG
                if seg == 0:
                    kb.op("pool", lambda e: e.memset(buf[:, 0:1], 0.0), w=[bd])
                    kb.dma("sp", buf[:, 1:SEG + 1], pT[row0:row0 + 128, 0:SEG], w=[bd])
                else:
                    kb.dma("sp", buf[:, 0:SEG + 1], pT[row0:row0 + 128, t0 - 1:t0 + SEG], w=[bd])
                kb.op("pool", lambda e: e.tensor_tensor(out=tmp, in0=buf[:, 0:SEG], in1=buf[:, 1:SEG + 1], op=ALU.subtract),
                      r=[bd], w=[td])
                kb.op("dve", lambda e: e.scalar_tensor_tensor(out=dst, in0=tmp, scalar=prm[:, c8:c8 + 1], in1=buf[:, 1:SEG + 1],
                                                              op0=ALU.mult, op1=ALU.add), r=[td, bd, prd], w=[dstd])

            def mm_to(lhsT, rhs, rd, n=128):
                bk, bd = banks.next()
                o = bk[:, 0:n]
                kb.op("pe", lambda e: e.matmul(o, lhsT=lhsT, rhs=rhs, start=True, stop=True), r=rd, w=[bd])
                return o, bd

            def chunk_gen(hp, seg, c, g):
                t = wk[g]
                gc = seg * NCH + c
                A = lambda nm: t[nm][0][:]
                Dp = lambda nm: t[nm][1]
                Rt, Kk, Bt, Kt, V = (pad[nm][:, c, :] for nm in ("Rt", "Kk", "Bt", "Kt", "V"))
                dR, dKk, dB, dK, dV = (padd[nm] for nm in ("Rt", "Kk", "Bt", "Kt", "V"))
                X = [t["Xa"], t["Xb"]]
                for src, sd, dst, dd, sc in ((Bt, dB, A("nBtPT"), Dp("nBtPT"), -1.0), (Kt, dK, A("KtPT"), Dp("KtPT"), None),
                                             (V, dV, A("VPT"), Dp("VPT"), None), (Kk, dKk, X[0][0][:, 0:128], X[0][1], None)):
                    bk, bd = banks.next()
                    kb.op("pe", lambda e: e.transpose(bk[:, 0:128], src, ident), r=[sd, cd], w=[bd])
                    copy_ev(dst, bk[:, 0:128], [bd], [dd], sc)
                yield
                for lh, ld, rh, rdd, dst, msk in ((Bt, dB, Kk, dKk, "NT", "nSU"), (Kk, dKk, Bt, dB, "N", "nSL"),
                                                 (Kt, dK, Kk, dKk, "MakT", "SU"), (Bt, dB, Rt, dR, "nPrbT", "nIU"),
                                                 (Kt, dK, Rt, dR, "PrkT", "IU")):
                    o, bd = mm_to(lh, rh, [ld, rdd])
                    kb.op("dve", lambda e: e.tensor_tensor(out=A(dst), in0=o, in1=self.cst(msk), op=ALU.mult),
                          r=[bd, cd], w=[Dp(dst)])
                yield
                o, bd = mm_to(A("MakT"), A("VPT"), [Dp("MakT"), Dp("VPT")])
                copy_ev(X[0][0][:, 128:256], o, [bd], [X[0][1]])
                yield
                Pc, PTc = ("N", "NT")
                lv_names = {2: "PT2", 4: "PT4", 8: "PT8", 16: "PT16", 32: "PT32"}
                pt_list = ["NT"]
                pbuf = ["Pa", "Pb"]
                pi = 0
                for lvl in (2, 4, 8, 16, 32):
                    o2, bd2 = mm_to(A(Pc), A(PTc), [Dp(Pc), Dp(PTc)])
                    if lvl < 32:
                        o1, bd1 = mm_to(A(PTc), A(Pc), [Dp(Pc), Dp(PTc)])
                    copy_ev(A(lv_names[lvl]), o2, [bd2], [Dp(lv_names[lvl])])
                    if lvl < 32:
                        copy_ev(A(pbuf[pi]), o1, [bd1], [Dp(pbuf[pi])])
                        Pc = pbuf[pi]
                        pi ^= 1
                    PTc = lv_names[lvl]
                    pt_list.append(PTc)
                    yield
                xi_ = 0
                for nm in reversed(pt_list):
                    Xc, Xn = X[xi_], X[xi_ ^ 1]
                    o, bd = mm_to(A(nm), Xc[0][:], [Dp(nm), Xc[1]], n=256)
                    kb.op("dve", lambda e: e.tensor_tensor(out=Xn[0][:], in0=o, in1=Xc[0][:], op=ALU.add),
                          r=[bd, Xc[1]], w=[Xn[1]])
                    xi_ ^= 1
                    yield
                W, Wd = X[xi_]
                W1, W2 = W[:, 0:128], W[:, 128:256]
                o, bd = mm_to(W1, A("nPrbT"), [Wd, Dp("nPrbT")])
                kb.op("dve", lambda e: e.tensor_tensor(out=A("QT"), in0=o, in1=Rt, op=ALU.add), r=[bd, dR], w=[Dp("QT")])
                o, bd = mm_to(W1, A("nBtPT"), [Wd, Dp("nBtPT")])
                kb.op("dve", lambda e: e.tensor_tensor(out=A("GT0"), in0=o, in1=ident, op=ALU.add), r=[bd, cd], w=[Dp("GT0")])
                bk, bd = banks.next()
                o = bk[:, 0:128]
                kb.op("pe", lambda e: e.matmul(o, lhsT=A("KtPT"), rhs=A("VPT"), start=True, stop=False),
                      r=[Dp("KtPT"), Dp("VPT")], w=[bd], mark=False)
                kb.op("pe", lambda e: e.matmul(o, lhsT=A("nBtPT"), rhs=W2, start=False, stop=True),
                      r=[Dp("nBtPT"), Wd], w=[bd])
                dc = U[12][:, c * 64 + 63:c * 64 + 64]
                kb.op("dve", lambda e: e.tensor_scalar(out=A("E"), in0=o, scalar1=dc, scalar2=None, op0=ALU.mult),
                      r=[bd, Ud[12]], w=[Dp("E")])
                yield
                Hc, Hcd = Hs[gc % 2], Hd[gc % 2]
                Hn, Hnd = Hs[(gc + 1) % 2], Hd[(gc + 1) % 2]
                bk, ybd = banks.next()
                yps = bk[:, 0:128]
                kb.op("pe", lambda e: e.matmul(yps, lhsT=A("QT"), rhs=Hc[:], start=True, stop=False),
                      r=[Dp("QT"), Hcd], w=[ybd], mark=False)
                kb.op("pe", lambda e: e.matmul(yps, lhsT=A("PrkT"), rhs=A("VPT"), start=False, stop=False),
                      r=[Dp("PrkT"), Dp("VPT")], w=[ybd], mark=False)
                kb.op("pe", lambda e: e.matmul(yps, lhsT=A("nPrbT"), rhs=W2, start=False, stop=True),
                      r=[Dp("nPrbT"), Wd], w=[ybd])
                o, bd = mm_to(A("GT0"), Hc[:], [Dp("GT0"), Hcd])
                kb.op("dve", lambda e: e.scalar_tensor_tensor(out=Hn[:], in0=o, scalar=dc, in1=A("E"), op0=ALU.mult, op1=ALU.add),
                      r=[bd, Dp("E"), Ud[12]], w=[Hnd])
                yield
                st, std = t["st"]
                kb.op("act", lambda e: e.activation(out=A("sq"), in_=yps, func=AF.Square), r=[ybd], w=[Dp("sq")])
                kb.op("dve", lambda e: e.reduce_sum(out=st[:, 0:1], in_=yps, axis=AX.X), r=[ybd], w=[std])
                kb.op("dve", lambda e: e.reduce_sum(out=st[:, 1:2], in_=A("sq"), axis=AX.X), r=[Dp("sq")], w=[std])
                self.norm_stats(st, std, 1, 64, RWKV_GN_EPS)
                for rows in (H0, H1):
                    kb.op("dve", lambda e: e.tensor_scalar(out=t["YnP"][0][rows, rows], in0=yps[rows, rows],
                                                           scalar1=st[rows, 6:7], scalar2=st[rows, 7:8],
                                                           op0=ALU.mult, op1=ALU.add), r=[ybd, std], w=[Dp("YnP")])
                o, bd = mm_to(A("YnP"), self.cst("sel"), [Dp("YnP"), cd], n=64)
                kb.op("dve", lambda e: e.tensor_scalar(out=U[10][:, c * 64:(c + 1) * 64], in0=o,
                                                       scalar1=P("rwkv_ln_g", hp), scalar2=P("rwkv_ln_b", hp),
                                                       op0=ALU.mult, op1=ALU.add), r=[bd, prd], w=[Ud[10]])
                yield

            for hp in range(2):
                for i in range(2):
                    kb.op("pool", lambda e, i=i: e.memset(Hs[i][:], 0.0), w=[Hd[i]])
                for seg in range(S // SEG):
                    blk2 = [slice(0, 512), slice(512, 1024)]
                    load_shift(768, seg, 6, U[8], Ud[8])
                    kb.op("act", lambda e: e.activation(out=T6[H0, :], in_=U[8][H0, :], func=AF.Tanh), r=[Ud[8]], w=[T6d])
                    kb.op("act", lambda e: e.copy(out=T6[H1, :], in_=U[8][H1, :]), r=[Ud[8]], w=[T6d])
                    load_shift(896, seg, 7, U[9], Ud[9])
                    kb.op("act", lambda e: e.activation(out=SG[:], in_=U[9], func=AF.Sigmoid), r=[Ud[9]], w=[SGd])
                    load_shift(hp * 128, seg, hp, U[0], Ud[0])
                    load_shift(256 + hp * 128, seg, 2 + hp, U[1], Ud[1])
                    load_shift(512 + hp * 128, seg, 4 + hp, U[2], Ud[2])
                    hs = slice(hp * 128, (hp + 1) * 128)
                    for b2 in blk2:
                        o, bd = mm_to(wlw[:, hs], T6[:, b2], [wd, T6d], n=512)
                        kb.op("act", lambda e: e.activation(out=U[4][:, b2], in_=o, func=AF.Sigmoid, bias=P("rwkv_w0", hp)),
                              r=[bd, prd], w=[Ud[4]])
                        o, bd = mm_to(wla[:, hs], T6[:, b2], [wd, T6d], n=512)
                        kb.op("act", lambda e: e.activation(out=U[3][:, b2], in_=o, func=AF.Sigmoid, bias=P("rwkv_a0", hp)),
                              r=[bd, prd], w=[Ud[3]])
                        o, bd = mm_to(gup[:, hs], SG[:, b2], [wd, SGd], n=512)
                        kb.op("act", lambda e: e.copy(out=U[5][:, b2], in_=o), r=[bd], w=[Ud[5]])
                    kb.op("dve", lambda e: e.tensor_scalar(out=U[6], in0=U[1], scalar1=P("rwkv_k_k", hp), scalar2=None,
                                                           op0=ALU.mult), r=[Ud[1], prd], w=[Ud[6]])
                    kb.op("pool", lambda e: e.tensor_tensor(out=U[8], in0=U[6], in1=U[6], op=ALU.mult), r=[Ud[6]], w=[Ud[8]])
                    for b2 in blk2:
                        o, bd = mm_to(self.cst("onesblk"), U[8][:, b2], [cd, Ud[8]], n=512)
                        kb.op("dve", lambda e: e.tensor_scalar(out=U[11][:, b2], in0=o, scalar1=1e-24, scalar2=None,
                                                               op0=ALU.max), r=[bd], w=[Ud[11]])
                    kb.op("act", lambda e: e.activation(out=U[11], in_=U[11], func=AF.Sqrt), r=[Ud[11]], w=[Ud[11]])
                    kb.op("dve", lambda e: e.reciprocal(out=U[11], in_=U[11]), r=[Ud[11]], w=[Ud[11]])
                    kb.op("pool", lambda e: e.tensor_tensor(out=U[6], in0=U[6], in1=U[11], op=ALU.mult),
                          r=[Ud[6], Ud[11]], w=[Ud[6]])
                    kb.op("dve", lambda e: e.tensor_scalar(out=U[7], in0=U[3], scalar1=P("rwkv_k_a", hp), scalar2=P("omka", hp),
                                                           op0=ALU.mult, op1=ALU.add), r=[Ud[3], prd], w=[Ud[7]])
                    kb.op("pool", lambda e: e.tensor_tensor(out=U[7], in0=U[7], in1=U[1], op=ALU.mult),
                          r=[Ud[7], Ud[1]], w=[Ud[7]])
                    kb.op("dve", lambda e: e.scalar_tensor_tensor(out=U[8], in0=U[0], scalar=P("rwkv_r_k", hp), in1=U[7],
                                                                  op0=ALU.mult, op1=ALU.mult), r=[Ud[0], Ud[7], prd], w=[Ud[8]])
                    for b2 in blk2:
                        o, bd = mm_to(self.cst("onesblk"), U[8][:, b2], [cd, Ud[8]], n=512)
                        kb.op("dve", lambda e: e.tensor_tensor(out=U[9][:, b2], in0=o, in1=U[2][:, b2], op=ALU.mult),
                              r=[bd, Ud[2]], w=[Ud[9]])
                    kb.op("pool", lambda e: e.tensor_tensor(out=U[3], in0=U[3], in1=U[6], op=ALU.mult),
                          r=[Ud[3], Ud[6]], w=[Ud[3]])
                    kb.op("dve", lambda e: e.tensor_tensor_scan(out=U[11], data0=self.cst("scanm"), data1=U[4], initial=0.0,
                                                                op0=ALU.mult, op1=ALU.add), r=[Ud[4], cd], w=[Ud[11]])
                    kb.op("act", lambda e: e.activation(out=U[12], in_=U[11], func=AF.Exp, scale=-C0), r=[Ud[11]], w=[Ud[12]])
                    kb.op("act", lambda e: e.activation(out=U[13], in_=U[11], func=AF.Exp, scale=C0), r=[Ud[11]], w=[Ud[13]])
                    kb.op("pool", lambda e: e.tensor_tensor(out=U[4], in0=U[11], in1=U[4], op=ALU.subtract),
                          r=[Ud[11], Ud[4]], w=[Ud[4]])
                    kb.op("act", lambda e: e.activation(out=U[4], in_=U[4], func=AF.Exp, scale=-C0), r=[Ud[4]], w=[Ud[4]])
                    kb.op("pool", lambda e: e.tensor_tensor(out=U[0], in0=U[0], in1=U[12], op=ALU.mult), r=[Ud[0], Ud[12]], w=[Ud[0]])
                    kb.op("dve", lambda e: e.tensor_tensor(out=U[6], in0=U[6], in1=U[4], op=ALU.mult), r=[Ud[6], Ud[4]], w=[Ud[6]])
                    kb.op("pool", lambda e: e.tensor_tensor(out=U[3], in0=U[3], in1=U[13], op=ALU.mult), r=[Ud[3], Ud[13]], w=[Ud[3]])
                    kb.op("dve", lambda e: e.tensor_tensor(out=U[7], in0=U[7], in1=U[13], op=ALU.mult), r=[Ud[7], Ud[13]], w=[Ud[7]])
                    ci = 0
                    for nm, ui in (("Rt", 0), ("Kk", 6), ("Bt", 3), ("Kt", 7), ("V", 2)):
                        for rows in (H0, H1):
                            dst = pad[nm][rows, :, rows]
                            src = U[ui][rows, :].rearrange("p (c t) -> p c t", t=64)
                            eng = ("pool", "dve", "act")[ci % 3]
                            ci += 1
                            if eng == "act":
                                kb.op("act", lambda e: e.copy(out=dst, in_=src), r=[Ud[ui]], w=[padd[nm]])
                            else:
                                kb.op(eng, lambda e: e.tensor_copy(out=dst, in_=src), r=[Ud[ui]], w=[padd[nm]])
                    for c0 in range(0, NCH, NW):
                        run_interleaved([chunk_gen(hp, seg, c0 + g, g) for g in range(NW)])
                    yo, yod = yo_r.next()
                    kb.op("pool", lambda e: e.tensor_tensor(out=U[10], in0=U[10], in1=U[9], op=ALU.add),
                          r=[Ud[10], Ud[9]], w=[Ud[10]])
                    kb.op("dve", lambda e: e.tensor_tensor(out=yo, in0=U[10], in1=U[5], op=ALU.mult),
                          r=[Ud[10], Ud[5]], w=[yod])
                    kb.dma("sp", self.yT[hp * 128:(hp + 1) * 128, seg * SEG:(seg + 1) * SEG], yo, r=[yod])

    def layer_norm_tile(self, z, zd, junk, jd, st, std, gb, gbd):
        kb = self.kb
        kb.op("act", lambda e: e.activation(out=junk, in_=z, func=AF.Square), r=[zd], w=[jd])
        kb.op("dve", lambda e: e.reduce_sum(out=st[:, 0:1], in_=z, axis=AX.X), r=[zd], w=[std])
        kb.op("dve", lambda e: e.reduce_sum(out=st[:, 1:2], in_=junk, axis=AX.X), r=[jd], w=[std])
        self.norm_stats(st, std, 1, D, LN_EPS)
        kb.op("dve", lambda e: e.tensor_scalar(out=z, in0=z, scalar1=st[:, 6:7], scalar2=st[:, 7:8],
                                               op0=ALU.mult, op1=ALU.add), r=[zd, std], w=[zd])
        kb.op("pool", lambda e: e.tensor_tensor(out=z, in0=z, in1=gb[:, 0, :], op=ALU.mult), r=[zd, gbd], w=[zd])
        kb.op("pool", lambda e: e.tensor_tensor(out=z, in0=z, in1=gb[:, 1, :], op=ALU.add), r=[zd, gbd], w=[zd])

    def load_ln_params(self, gb, gbd, l, i):
        kb = self.kb
        kb.dma("sp", gb[:, 0, :], self.I["ln_g"][l, i:i + 1, :].broadcast_to([128, D]), w=[gbd])
        kb.dma("sp", gb[:, 1, :], self.I["ln_b"][l, i:i + 1, :].broadcast_to([128, D]), w=[gbd])

    def outproj_ln1(self, l):
        nc, kb = self.nc, self.kb
        moe = (l % 2 == 1)
        j = l // 2
        with ExitStack() as es:
            sb = lambda n, sh, dt: es.enter_context(nc.sbuf_tensor(self.un(n), sh, dt))
            wo = sb("o_w", [128, 8, D], BF16)
            wod = Dep()
            kb.dma("pool", wo[:], self.I["w_out"][l].rearrange("(k p) n -> p k n", p=128), w=[wod])
            yt_r = Ring(sb("o_yt", [128, 2, 8, 512], BF16), 2)
            x_r = Ring(sb("o_x", [128, 2, D], F32), 2)
            z_r = Ring(sb("o_z", [128, 2, D], F32), 2)
            junk = sb("o_junk", [128, D], BF16)
            jd = Dep()
            st_r = Ring(sb("o_st", [128, 2, 8], F32), 2)
            gb = sb("o_gb", [128, 2, D], F32)
            gbd = Dep()
            self.load_ln_params(gb, gbd, l, 0)
            B = self.bank
            hb = BRing([(0, 1), (2, 3)])
            tr = self.tr_ring(((4, 5), (6, 7)))
            if moe:
                rt = sb("o_rt", [128, 8, N_EXPERTS], F32)
                rtd = Dep()
                kb.dma("sp", rt[:], self.I["moe_router"][j].rearrange("(k p) e -> p k e", p=128), w=[rtd], slow=True)
                xTf_r = Ring(sb("o_xTf", [128, 1, 8, 128], F32), 1)
                g_r = Ring(sb("o_g", [128, 2, 64], F32), 2)
            for tb in range(8):
                yt, ytd = yt_r.next()
                kb.dma("sp", yt, self.yT[:, tb * 512:(tb + 1) * 512].rearrange("(k p) t -> p k t", p=128), w=[ytd])
                for tt in range(4):
                    t = tb * 4 + tt
                    x, xd = x_r.next()
                    z, zd = z_r.next()
                    kb.dma("sp", x, self.xres[t * 128:(t + 1) * 128, :], w=[xd])
                    ba, bb_ = hb.next()
                    for half, bi in enumerate((ba, bb_)):
                        bk, bd = B[bi]
                        for k in range(8):
                            kb.op("pe", lambda e, k=k: e.matmul(bk[:, 0:512], lhsT=yt[:, k, tt * 128:(tt + 1) * 128],
                                                                rhs=wo[:, k, half * 512:(half + 1) * 512],
                                                                start=(k == 0), stop=(k == 7)),
                                  r=[ytd, wod], w=[bd], mark=(k == 7))
                        hs = slice(half * 512, (half + 1) * 512)
                        kb.op("dve", lambda e: e.scalar_tensor_tensor(out=z[:, hs], in0=x[:, hs], scalar=float(ALPHA),
                                                                      in1=bk[:, 0:512], op0=ALU.mult, op1=ALU.add),
                              r=[xd, bd], w=[zd])
                    st, std = st_r.next()
                    self.layer_norm_tile(z, zd, junk[:], jd, st, std, gb, gbd)
                    kb.dma("sp", self.xres[t * 128:(t + 1) * 128, :], z, r=[zd, xd])
                    if moe:
                        xTf, xTfd = xTf_r.next()
                        self.transpose_to_XT(z, zd, t, tr, f32_out=(xTf, xTfd))
                        self.router_gates(xTf, xTfd, rt, rtd, t, g_r)
                    else:
                        self.transpose_to_XT(z, zd, t, tr)

    def router_gates(self, xTf, xTfd, rt, rtd, t, g_r):
        kb = self.kb
        bk, bd = self.bank[0]
        lp = bk[:, 0:N_EXPERTS]
        for k in range(8):
            kb.op("pe", lambda e, k=k: e.matmul(lp, lhsT=xTf[:, k, :], rhs=rt[:, k, :], start=(k == 0), stop=(k == 7)),
                  r=[xTfd, rtd], w=[bd], mark=(k == 7))
        g, gd = g_r.next()
        c = lambda i: g[:, i * 8:(i + 1) * 8]
        sc = lambda i: g[:, 56 + i:57 + i]
        op = lambda fn: kb.op("dve", fn, r=[gd], w=[gd])
        kb.op("dve", lambda e: e.tensor_copy(out=c(0), in_=lp), r=[bd], w=[gd])
        op(lambda e: e.reduce_max(out=sc(0), in_=c(0), axis=AX.X))
        op(lambda e: e.tensor_scalar(out=c(1), in0=c(0), scalar1=sc(0), scalar2=None, op0=ALU.is_equal))
        op(lambda e: e.scalar_tensor_tensor(out=c(2), in0=c(1), scalar=-1e30, in1=c(0), op0=ALU.mult, op1=ALU.add))
        op(lambda e: e.reduce_max(out=sc(1), in_=c(2), axis=AX.X))
        op(lambda e: e.tensor_scalar(out=c(3), in0=c(0), scalar1=sc(1), scalar2=None, op0=ALU.is_ge))
        op(lambda e: e.tensor_scalar(out=sc(2), in0=sc(0), scalar1=-1.0, scalar2=None, op0=ALU.mult))
        kb.op("act", lambda e: e.activation(out=c(4), in_=c(0), func=AF.Exp, bias=sc(2), scale=1.0), r=[gd], w=[gd])
        op(lambda e: e.tensor_tensor(out=c(4), in0=c(4), in1=c(3), op=ALU.mult))
        op(lambda e: e.reduce_sum(out=sc(3), in_=c(4), axis=AX.X))
        op(lambda e: e.reciprocal(out=sc(4), in_=sc(3)))
        kb.op("dve", lambda e: e.tensor_scalar(out=self.gates[:, t, :], in0=c(4), scalar1=sc(4), scalar2=None, op0=ALU.mult),
              r=[gd], w=[self.gatesd])

    def ffn(self, l):
        nc, kb = self.nc, self.kb
        moe = (l % 2 == 1)
        j = l // 2
        last = (l == self.n_layers - 1)
        if moe:
            E, FF = N_EXPERTS, FF_EXPERT
            wg_a, wu_a, wd_a = self.I["moe_w_gate"][j], self.I["moe_w_up"][j], self.I["moe_w_down"][j]
        else:
            E, FF = 1, FF_DENSE
            wg_a, wu_a, wd_a = [self.I[k][j:j + 1] for k in ("ffn_w_gate", "ffn_w_up", "ffn_w_down")]
        G = 2
        ngrp = FF // (128 * G)
        HT = 2048
        with ExitStack() as es:
            sb = lambda n, sh, dt: es.enter_context(nc.sbuf_tensor(self.un(n), sh, dt))
            acc = sb("f_acc", [128, 16, D], F32)
            accd = [Dep() for _ in range(16)]
            wg_r = Ring(sb("f_wg", [128, 2, 8, G * 128], BF16), 2)
            wu_r = Ring(sb("f_wu", [128, 2, 8, G * 128], BF16), 2)
            wd_r = Ring(sb("f_wd", [128, 2, G, D], BF16), 2)
            sg_r = Ring(sb("f_sg", [128, 2, 512], F32), 2)
            x_r = Ring(sb("f_x", [128, 1, D], F32), 1)
            junk = sb("f_junk", [128, D], BF16)
            jd = Dep()
            st_r = Ring(sb("f_st", [128, 2, 8], F32), 2)
            gb = sb("f_gb", [128, 2, D], F32)
            gbd = Dep()
            self.load_ln_params(gb, gbd, l, 1)
            B = self.bank
            pg_r = BRing([B[0], B[1]])
            pu_r = BRing([B[2], B[3]])
            po_r = BRing([B[4], B[5]])
            tr = self.tr_ring(((6, 7),))
            hT_r = Ring(sb("f_hT2", [128, 2, G, HT], BF16), 2)
            for half in range(S // HT):
                groups = [(e_, grp) for e_ in range(E) for grp in range(ngrp)]
                N = len(groups)
                wgu = {}
                wdw = {}
                hTs = {}

                def load_gu(m):
                    e_, grp = groups[m]
                    ff0 = grp * G * 128
                    wg, wgd = wg_r.next()
                    wu, wud = wu_r.next()
                    kb.dma("pool", wg, wg_a[e_][:, ff0:ff0 + G * 128].rearrange("(k p) n -> p k n", p=128), w=[wgd])
                    kb.dma("pool", wu, wu_a[e_][:, ff0:ff0 + G * 128].rearrange("(k p) n -> p k n", p=128), w=[wud])
                    wgu[m] = (wg, wgd, wu, wud)

                def load_d(m):
                    e_, grp = groups[m]
                    ff0 = grp * G * 128
                    wdn, wdd = wd_r.next()
                    kb.dma("pool", wdn, wd_a[e_][ff0:ff0 + G * 128, :].rearrange("(g p) n -> p g n", p=128), w=[wdd])
                    wdw[m] = (wdn, wdd)

                def gu_units(m):
                    wg, wgd, wu, wud = wgu[m]
                    hT, hTd = hT_r.next()
                    hTs[m] = (hT, hTd)
                    us = []
                    for gi in range(G):
                        for tb in range(HT // 512):
                            def u(gi=gi, tb=tb):
                                tok = slice(half * HT + tb * 512, half * HT + (tb + 1) * 512)
                                xd_blk = self.XTd[(half * HT) // 512 + tb]
                                pg, pgd = pg_r.next()
                                pu, pud = pu_r.next()
                                for k in range(8):
                                    kb.op("pe", lambda e, k=k: e.matmul(pg, lhsT=wg[:, k, gi * 128:(gi + 1) * 128],
                                                                        rhs=self.XT[:, k, tok], start=(k == 0), stop=(k == 7)),
                                          r=[wgd, xd_blk], w=[pgd], mark=(k == 7))
                                for k in range(8):
                                    kb.op("pe", lambda e, k=k: e.matmul(pu, lhsT=wu[:, k, gi * 128:(gi + 1) * 128],
                                                                        rhs=self.XT[:, k, tok], start=(k == 0), stop=(k == 7)),
                                          r=[wud, xd_blk], w=[pud], mark=(k == 7))
                                sg, sgd = sg_r.next()
                                kb.op("act", lambda e: e.activation(out=sg, in_=pg, func=AF.Silu), r=[pgd], w=[sgd])
                                kb.op("dve", lambda e: e.tensor_tensor(out=hT[:, gi, tb * 512:(tb + 1) * 512], in0=pu, in1=sg,
                                                                       op=ALU.mult), r=[pud, sgd], w=[hTd])
                            us.append(u)
                    return us

                def dn_units(m):
                    e_, grp = groups[m]
                    wdn, wdd = wdw[m]
                    hT, hTd = hTs[m]
                    first = (m == 0)
                    us = []
                    for tt in range(HT // 128):
                        for hf in range(2):
                            def u(tt=tt, hf=hf):
                                t = half * (HT // 128) + tt
                                po, pod = po_r.next()
                                for gi in range(G):
                                    kb.op("pe", lambda e, gi=gi: e.matmul(po, lhsT=hT[:, gi, tt * 128:(tt + 1) * 128],
                                                                          rhs=wdn[:, gi, hf * 512:(hf + 1) * 512],
                                                                          start=(gi == 0), stop=(gi == G - 1)),
                                          r=[hTd, wdd], w=[pod], mark=(gi == G - 1))
                                a = acc[:, tt, hf * 512:(hf + 1) * 512]
                                if moe:
                                    gsc = self.gates[:, t, e_:e_ + 1]
                                    if first:
                                        kb.op("dve", lambda e: e.tensor_scalar(out=a, in0=po, scalar1=gsc, scalar2=None,
                                                                               op0=ALU.mult), r=[pod, self.gatesd], w=[accd[tt]])
                                    else:
                                        kb.op("dve", lambda e: e.scalar_tensor_tensor(out=a, in0=po, scalar=gsc, in1=a,
                                                                                      op0=ALU.mult, op1=ALU.add),
                                              r=[pod, self.gatesd, accd[tt]], w=[accd[tt]])
                                else:
                                    if first:
                                        kb.op("act", lambda e: e.copy(out=a, in_=po), r=[pod], w=[accd[tt]])
                                    else:
                                        kb.op("dve", lambda e: e.tensor_tensor(out=a, in0=po, in1=a, op=ALU.add),
                                              r=[pod, accd[tt]], w=[accd[tt]])
                            us.append(u)
                    return us

                load_gu(0)
                load_d(0)
                if N > 1:
                    load_gu(1)
                for u in gu_units(0):
                    u()
                for n in range(N):
                    if n + 1 < N:
                        load_d(n + 1)
                    if n + 2 < N:
                        load_gu(n + 2)
                    dn = dn_units(n)
                    gu = gu_units(n + 1) if n + 1 < N else []
                    per = len(dn) // max(len(gu), 1)
                    di = 0
                    for gi_, u in enumerate(gu):
                        u()
                        for _ in range(per):
                            dn[di]()
                            di += 1
                    while di < len(dn):
                        dn[di]()
                        di += 1
                for tt in range(HT // 128):
                    t = half * (HT // 128) + tt
                    x, xd = x_r.next()
                    kb.dma("sp", x, self.xres[t * 128:(t + 1) * 128, :], w=[xd])
                    z = acc[:, tt, :]
                    zd = accd[tt]
                    kb.op("dve", lambda e: e.scalar_tensor_tensor(out=z, in0=x, scalar=float(ALPHA), in1=z,
                                                                  op0=ALU.mult, op1=ALU.add), r=[xd, zd], w=[zd])
                    st, std = st_r.next()
                    self.layer_norm_tile(z, zd, junk[:], jd, st, std, gb, gbd)
                    if last:
                        kb.dma("sp", self.out[t * 128:(t + 1) * 128, :], z, r=[zd])
                    else:
                        kb.dma("sp", self.xres[t * 128:(t + 1) * 128, :], z, r=[zd, xd])
                        self.transpose_to_XT(z, zd, t, tr)


WEIGHT_KEYS = ("w_in", "w_out", "rwkv_mu", "rwkv_w0", "rwkv_w_up", "rwkv_a0", "rwkv_a_up", "rwkv_g_up",
               "rwkv_k_k", "rwkv_k_a", "rwkv_r_k", "rwkv_ln_g", "rwkv_ln_b", "ret_gn_g", "ret_gn_b",
               "ln_g", "ln_b", "ffn_w_gate", "ffn_w_up", "ffn_w_down", "moe_router", "moe_w_gate",
               "moe_w_up", "moe_w_down")


def make_in_map(inputs, b, consts, shared=None):
    m = {}
    m["x"] = np.ascontiguousarray(np.asarray(inputs["x"])[b], dtype=np.float32)
    if shared is None:
        shared = make_shared(inputs, consts)
    m.update(shared)
    return m


def make_shared(inputs, consts):
    sh = {}
    for k in WEIGHT_KEYS:
        sh[k] = np.ascontiguousarray(np.asarray(inputs[k]), dtype=np.float32)
    rb = np.asarray(inputs["rel_bias"], dtype=np.float32)
    idx = bias_bucket_index()
    g = rb[idx]
    sh["biasg"] = np.ascontiguousarray(np.transpose(g, (0, 4, 1, 2, 3)))
    sh["cst"] = consts["cst"]
    sh["cos2"] = consts["cos2"]
    sh["sin2"] = consts["sin2"]
    sh["amask"] = consts["amask"]
    return sh


_PROG = None


def kernel(**inputs):
    global _PROG
    if _PROG is None:
        p = Prog()
        p.build()
        _PROG = p
    p = _PROG
    shared = make_shared(inputs, p.consts)
    in_maps = [make_in_map(inputs, b, p.consts, shared) for b in range(8)]
    res = run_bass_kernel_spmd(p.nc, in_maps, core_ids=list(range(8)))
    out = np.stack([np.asarray(r["out"], dtype=np.float32) for r in res.results], 0)
    return out
```

```python
import math
from contextlib import ExitStack
import numpy as np
import concourse.bass as bass
import concourse.mybir as mybir
from concourse.bass_utils import run_bass_kernel_spmd

F32 = mybir.dt.float32
BF16 = mybir.dt.bfloat16
ALU = mybir.AluOpType
AF = mybir.ActivationFunctionType
AX = mybir.AxisListType

D = 1024
S = 4096
DEPTH = 4
NT = S // 128
RWKV_IN = 1024
ATTN_IN = 1536
RET_IN = 1024
N_IN = 3584
FF_DENSE = 2816
FF_EXPERT = 3584
N_EXPERTS = 8
ALPHA = (2 * DEPTH) ** 0.25
LN_EPS = 1e-5
RWKV_GN_EPS = 64e-5
C0 = math.exp(-0.5)
PATTERNS = ((128, 1), (512, 4), (2048, 16))
NEG = -240000.0


class Dep:
    __slots__ = ("w", "r", "excl")

    def __init__(self, excl=False):
        self.w = None
        self.r = {}
        self.excl = excl


class Engine:
    def __init__(self, e, sem, idx):
        self.e = e
        self.sem = sem
        self.idx = idx
        self.cnt = 0
        self.seen = {}
        self.slots = []
        self.si = 0


class KB:
    NSLOT = 12

    def __init__(self, nc, es):
        self.nc = nc
        self.sems = []
        self.eng = {}
        for name, e in (("pe", nc.tensor), ("dve", nc.vector), ("act", nc.scalar),
                        ("pool", nc.gpsimd), ("sp", nc.sync)):
            sem = es.enter_context(nc.semaphore("s_" + name))
            self.sems.append(sem)
            self.eng[name] = Engine(e, sem, len(self.sems) - 1)
        for q in ("sp", "pool"):
            for i in range(self.NSLOT):
                sem = es.enter_context(nc.semaphore("d_%s%d" % (q, i)))
                self.sems.append(sem)
                self.eng[q].slots.append([len(self.sems) - 1, 0])
        self.nins = 0

    def _need(self, E, ev, raw=True):
        si, val = ev
        if val <= 0:
            return
        if si == E.idx and val > E.cnt:
            return
        if E.seen.get(si, 0) < val:
            E.e.wait_ge(self.sems[si], val)
            E.seen[si] = val

    def _waits(self, E, r, w):
        for d in r:
            if d.excl:
                if d.w is not None:
                    self._need(E, d.w)
                for si, v in d.r.items():
                    self._need(E, (si, v), raw=False)
            elif d.w is not None:
                self._need(E, d.w)
        for d in w:
            if d.w is not None:
                self._need(E, d.w, raw=False)
            for si, v in d.r.items():
                self._need(E, (si, v), raw=False)

    def _post(self, ev, r, w):
        for d in w:
            d.w = ev
            d.r = {}
        for d in r:
            if d.excl:
                d.w = ev
                d.r = {}
            elif d.r.get(ev[0], 0) < ev[1]:
                d.r[ev[0]] = ev[1]

    def op(self, eng, fn, r=(), w=(), mark=True):
        E = self.eng[eng]
        self._waits(E, r, w)
        ins = fn(E.e)
        self.nins += 1
        if mark:
            E.cnt += 1
            ins.then_inc(E.sem, 1)
            ev = (E.idx, E.cnt)
        else:
            ev = (E.idx, E.cnt + 1)
        self._post(ev, r, w)
        return ins

    def dma(self, q, out, in_, r=(), w=(), slow=False):
        E = self.eng[q]
        slot = E.slots[E.si]
        E.si = (E.si + 1) % len(E.slots)
        self._need(E, (slot[0], slot[1]))
        self._waits(E, r, w)
        if slow:
            ins = E.e.dma_start(out=out, in_=in_, allow_slow_non_contiguous=True)
        else:
            ins = E.e.dma_start(out=out, in_=in_)
        self.nins += 1
        slot[1] += 16
        ins.then_inc(self.sems[slot[0]], 16)
        ev = (slot[0], slot[1])
        self._post(ev, r, w)
        return ins

    def barrier(self):
        evs = []
        for E in self.eng.values():
            if E.cnt > 0:
                evs.append((E.idx, E.cnt))
            for s in E.slots:
                if s[1] > 0:
                    evs.append((s[0], s[1]))
        for E in self.eng.values():
            for ev in evs:
                if ev[0] != E.idx:
                    self._need(E, ev)


class Ring:
    def __init__(self, tile, n):
        self.t = tile
        self.n = n
        self.d = [Dep() for _ in range(n)]
        self.i = 0

    def next(self):
        i = self.i
        self.i = (i + 1) % self.n
        return self.t[:, i], self.d[i]


class BRing:
    def __init__(self, items):
        self.items = list(items)
        self.i = 0

    def next(self):
        it = self.items[self.i]
        self.i = (self.i + 1) % len(self.items)
        return it


def run_interleaved(gens):
    gens = list(gens)
    while gens:
        nxt = []
        for g in gens:
            try:
                next(g)
                nxt.append(g)
            except StopIteration:
                pass
        gens = nxt


def t5_bucket(dist):
    nb = 32
    max_exact = nb // 2
    large = max_exact + (np.log(np.maximum(dist, max_exact) / max_exact)
                         / math.log(2048 / max_exact) * (nb - max_exact)).astype(np.int32)
    return np.where(dist < max_exact, dist, np.minimum(large, nb - 1)).astype(np.int32)


CST = {}


def _cst_layout():
    off = 0
    for name, n in (("ident", 128), ("onesblk", 128), ("nSU", 128), ("nSL", 128), ("SU", 128),
                    ("nIU", 128), ("IU", 128), ("sel", 64), ("sel65", 64), ("dmaskT", 512),
                    ("zeta8", 4), ("xi", 256), ("scanm", 1024)):
        CST[name] = (off, n)
        off += n
    return off


CST_N = _cst_layout()


def make_consts():
    c = np.zeros((128, CST_N), np.float32)

    def put(name, arr):
        o, n = CST[name]
        c[:arr.shape[0], o:o + n] = arr.reshape(arr.shape[0], n)

    i = np.arange(128)
    put("ident", np.eye(128, dtype=np.float32))
    put("onesblk", (i[:, None] // 64 == i[None, :] // 64).astype(np.float32))
    su = (i[None, :] > i[:, None]).astype(np.float32)
    sl = (i[None, :] < i[:, None]).astype(np.float32)
    iu = (i[None, :] >= i[:, None]).astype(np.float32)
    put("nSU", -su)
    put("nSL", -sl)
    put("SU", su)
    put("nIU", -iu)
    put("IU", iu)
    put("sel", np.concatenate([np.eye(64), np.eye(64)], 0).astype(np.float32))
    s65 = np.zeros((128, 64), np.float32)
    s65[64, :] = 1.0
    put("sel65", s65)
    lg = np.log1p(-np.exp2(-5.0 - np.arange(4, dtype=np.float64)))
    m = np.arange(128)[:, None, None]
    n = np.arange(128)[None, None, :]
    h = lg[None, :, None]
    dm = np.where(n >= m, np.exp(h * np.maximum(n - m, 0)), 0.0) / 8.0
    put("dmaskT", dm.astype(np.float32))
    put("zeta8", (np.exp(lg[None, :] * (127 - np.arange(128)[:, None])) / 8.0).astype(np.float32))
    xi = np.zeros((128, 2, 128))
    for hp in range(2):
        for hh in range(2):
            xi[hh * 64:(hh + 1) * 64, hp, :] = np.exp(lg[2 * hp + hh] * (np.arange(128) + 1))[None, :]
    put("xi", xi.astype(np.float32))
    sm = np.ones((128, 1024), np.float32)
    sm[:, 0::64] = 0.0
    put("scanm", sm)
    half = 32
    inv = (10000.0 ** (-np.arange(half, dtype=np.float32) / half)).astype(np.float32)
    ang = (np.arange(S, dtype=np.float32)[None, :] * inv[:, None]).astype(np.float32)
    cos = np.cos(ang).astype(np.float32)
    sin = np.sin(ang).astype(np.float32)
    cos2 = np.concatenate([cos, cos, cos, cos], 0)
    sin2 = np.concatenate([-sin, sin, -sin, sin], 0)
    jj = np.arange(128)[:, None]
    ii = np.arange(128)[None, :]
    mk = np.zeros((2, 128, 128), np.float32)
    mk[0] = np.where(ii <= jj, 0.0, NEG)
    mk[1] = np.where(ii >= jj, 0.0, NEG)
    gamma_c = np.exp(lg * 128.0)
    return dict(cst=c, cos2=np.ascontiguousarray(cos2), sin2=np.ascontiguousarray(sin2), amask=mk,
                gamma_c=[float(g) for g in gamma_c])


def bias_bucket_index():
    jj = np.arange(128)[:, None]
    ii = np.arange(128)[None, :]
    out = np.zeros((3, 2, 128, 128), np.int64)
    for p, (win, dil) in enumerate(PATTERNS):
        rel_prev = np.clip(ii + 128 - jj, 0, None)
        rel_cur = np.clip(ii - jj, 0, None)
        out[p, 0] = t5_bucket(rel_prev * dil)
        out[p, 1] = t5_bucket(rel_cur * dil)
    return out


class Prog:
    def __init__(self, n_layers=DEPTH, dbg=False, stop=None):
        self.n_layers = n_layers
        self.dbg = dbg
        self.stop = stop
        self.consts = make_consts()
        self.nc = nc = bass.Bass("TRN2", target_bir_lowering=False)

        def din(name, shape, dt=F32):
            return nc.dram_tensor(name, list(shape), dt, kind="ExternalInput").ap()

        self.I = I = {}
        I["x"] = din("x", [S, D])
        I["w_in"] = din("w_in", [DEPTH, D, N_IN])
        I["w_out"] = din("w_out", [DEPTH, D, D])
        I["rwkv_mu"] = din("rwkv_mu", [DEPTH, RWKV_IN])
        for nm in ("rwkv_w0", "rwkv_a0", "rwkv_k_k", "rwkv_k_a", "rwkv_r_k", "rwkv_ln_g", "rwkv_ln_b",
                   "ret_gn_g", "ret_gn_b"):
            I[nm] = din(nm, [DEPTH, 256])
        I["rwkv_w_up"] = din("rwkv_w_up", [DEPTH, 64, 256])
        I["rwkv_a_up"] = din("rwkv_a_up", [DEPTH, 64, 256])
        I["rwkv_g_up"] = din("rwkv_g_up", [DEPTH, 128, 256])
        I["ln_g"] = din("ln_g", [DEPTH, 2, D])
        I["ln_b"] = din("ln_b", [DEPTH, 2, D])
        I["ffn_w_gate"] = din("ffn_w_gate", [2, D, FF_DENSE])
        I["ffn_w_up"] = din("ffn_w_up", [2, D, FF_DENSE])
        I["ffn_w_down"] = din("ffn_w_down", [2, FF_DENSE, D])
        I["moe_router"] = din("moe_router", [2, D, N_EXPERTS])
        I["moe_w_gate"] = din("moe_w_gate", [2, N_EXPERTS, D, FF_EXPERT])
        I["moe_w_up"] = din("moe_w_up", [2, N_EXPERTS, D, FF_EXPERT])
        I["moe_w_down"] = din("moe_w_down", [2, N_EXPERTS, FF_EXPERT, D])
        I["cst"] = din("cst", [128, CST_N])
        I["cos2"] = din("cos2", [128, S])
        I["sin2"] = din("sin2", [128, S])
        I["amask"] = din("amask", [2, 128, 128])
        I["biasg"] = din("biasg", [3, 8, 2, 128, 128])
        self.out = nc.dram_tensor("out", [S, D], F32, kind="ExternalOutput").ap()

        def scr(name, shape, dt):
            kind = "ExternalOutput" if dbg else "Internal"
            return nc.dram_tensor(name, list(shape), dt, kind=kind).ap()

        self.xres = scr("xres", [S, D], F32)
        self.pT = scr("pT", [N_IN, S], F32)
        self.vtok = scr("vtok", [S, 768], BF16)
        self.yT = scr("yT", [D, S], BF16)

    _uid = 0

    def un(self, n):
        self._uid += 1
        return "%s_%d" % (n, self._uid)

    def cst(self, name, rows=128):
        o, n = CST[name]
        return self.cstt[0:rows, o:o + n]

    def build(self):
        nc = self.nc
        with ExitStack() as es:
            self.kb = kb = KB(nc, es)
            self.arena = es.enter_context(nc.sbuf_tensor("arena", [128, 16384], F32))
            self.XT = self.arena[:, :].bitcast(BF16).rearrange("p (k s) -> p k s", k=8)
            self.XTd = [Dep() for _ in range(8)]
            self.pbank_t = es.enter_context(nc.psum_tensor("psum", [128, 8, 512], F32))
            self.bank = [(self.pbank_t[:, i], Dep(excl=True)) for i in range(8)]
            self.cstt = es.enter_context(nc.sbuf_tensor("cstt", [128, CST_N], F32))
            self.cstd = Dep()
            self.identb = es.enter_context(nc.sbuf_tensor("identb", [128, 128], BF16))
            self.gates = es.enter_context(nc.sbuf_tensor("gates", [128, NT, N_EXPERTS], F32))
            self.gatesd = Dep()
            kb.dma("sp", self.cstt[:], self.I["cst"], w=[self.cstd])
            kb.op("dve", lambda e: e.tensor_copy(out=self.identb[:], in_=self.cst("ident")),
                  r=[self.cstd], w=[self.cstd])
            self.load_x0()
            kb.barrier()
            for l in range(self.n_layers):
                self.layer(l)
                if self.stop is not None and self.stop[0] == l and self.stop[1] == "layer":
                    break
            kb.barrier()
        return nc

    def setup_bias(self):
        nc, kb = self.nc, self.kb
        with ExitStack() as es:
            self.bias8d = Dep()
            tg = es.enter_context(nc.sbuf_tensor(self.un("tg"), [128, 2, 16, 128], F32))
            mk = es.enter_context(nc.sbuf_tensor(self.un("mk"), [128, 2, 128], F32))
            mkd = Dep()
            kb.dma("sp", mk[:], self.I["amask"].rearrange("a j i -> j a i"), w=[mkd])
            ring = Ring(tg, 2)
            for p in range(3):
                t, d = ring.next()
                kb.dma("sp", t.rearrange("j (h a) i -> j h a i", a=2),
                       self.I["biasg"][p].rearrange("h a j i -> j h a i"), w=[d])
                for h in range(8):
                    kb.op("dve", lambda e, t=t, h=h, p=p: e.scalar_tensor_tensor(
                        out=self.bias8[:, p, h], in0=t[:, 2 * h:2 * h + 2, :], scalar=8.0, in1=mk[:],
                        op0=ALU.mult, op1=ALU.add), r=[d, mkd], w=[self.bias8d])
            kb.barrier()

    def load_x0(self):
        nc, kb = self.nc, self.kb
        with ExitStack() as es:
            xt = es.enter_context(nc.sbuf_tensor(self.un("x0t"), [128, 2, D], F32))
            xr = Ring(xt, 2)
            pr = self.tr_ring()
            for t in range(NT):
                x, xd = xr.next()
                kb.dma("sp", x, self.I["x"][t * 128:(t + 1) * 128, :], w=[xd])
                kb.dma("sp", self.xres[t * 128:(t + 1) * 128, :], x, r=[xd])
                self.transpose_to_XT(x, xd, t, pr)
            kb.barrier()

    def tr_ring(self, pairs=((0, 1), (2, 3))):
        items = []
        for a, b in pairs:
            ap = self.pbank_t[:, a:b + 1].rearrange("p a (b c) -> p (a b) c", c=128)
            items.append((ap, [self.bank[a][1], self.bank[b][1]]))
        return BRing(items)

    def transpose_to_XT(self, x, xd, t, pr, f32_out=None):
        kb = self.kb
        p, pds = pr.next()
        for k in range(8):
            kb.op("pe", lambda e, k=k: e.transpose(p[:, k, :], x[:, k * 128:(k + 1) * 128], self.cst("ident")),
                  r=[xd, self.cstd], w=pds, mark=(k == 7))
        xd_blk = self.XTd[t // 4]
        kb.op("act", lambda e: e.copy(out=self.XT[:, :, t * 128:(t + 1) * 128], in_=p), r=pds, w=[xd_blk])
        if f32_out is not None:
            o, od = f32_out
            kb.op("dve", lambda e: e.tensor_copy(out=o, in_=p), r=pds, w=[od])

    def layer(self, l):
        kb = self.kb
        self.inproj(l)
        kb.barrier()
        if self.stop == (l, "inproj"):
            return
        self.retention(l)
        kb.barrier()
        if self.stop == (l, "ret"):
            return
        self.attention(l)
        kb.barrier()
        if self.stop == (l, "attn"):
            return
        self.rwkv(l)
        kb.barrier()
        if self.stop == (l, "rwkv"):
            return
        self.outproj_ln1(l)
        kb.barrier()
        if self.stop == (l, "ln1"):
            return
        self.ffn(l)
        kb.barrier()

    def inproj(self, l):
        nc, kb = self.nc, self.kb
        w_in = self.I["w_in"][l]
        with ExitStack() as es:
            wt = es.enter_context(nc.sbuf_tensor(self.un("ip_w"), [128, 2, 8, 512], BF16))
            stg = es.enter_context(nc.sbuf_tensor(self.un("ip_s"), [128, 2, S], F32))
            wv = es.enter_context(nc.sbuf_tensor(self.un("ip_wv"), [128, 8, 768], BF16))
            vt = es.enter_context(nc.sbuf_tensor(self.un("ip_vt"), [128, 2, 768], BF16))
            wr, pr, vr = Ring(wt, 2), BRing(self.bank), Ring(vt, 2)
            sdeps = [[Dep() for _ in range(8)] for _ in range(2)]
            wvd = Dep()
            kb.dma("pool", wv[:, :, 0:512], w_in[:, 2048:2560].rearrange("(k p) n -> p k n", p=128), w=[wvd])
            kb.dma("pool", wv[:, :, 512:768], w_in[:, 3072:3328].rearrange("(k p) n -> p k n", p=128), w=[wvd])
            n_ev = 0
            si = 0
            for cb in range(7):
                w, wd = wr.next()
                kb.dma("pool", w, w_in[:, cb * 512:(cb + 1) * 512].rearrange("(k p) n -> p k n", p=128), w=[wd])
                for fc in range(4):
                    st = stg[:, si]
                    sd = sdeps[si]
                    si ^= 1
                    for tb in range(8):
                        p, pd = pr.next()
                        for k in range(8):
                            kb.op("pe", lambda e, k=k: e.matmul(
                                p, lhsT=w[:, k, fc * 128:(fc + 1) * 128], rhs=self.XT[:, k, tb * 512:(tb + 1) * 512],
                                start=(k == 0), stop=(k == 7)), r=[wd, self.XTd[tb]], w=[pd], mark=(k == 7))
                        o = st[:, tb * 512:(tb + 1) * 512]
                        if n_ev % 2 == 0:
                            kb.op("act", lambda e: e.copy(out=o, in_=p), r=[pd], w=[sd[tb]])
                        else:
                            kb.op("dve", lambda e: e.tensor_copy(out=o, in_=p), r=[pd], w=[sd[tb]])
                        n_ev += 1
                    row = (cb * 4 + fc) * 128
                    kb.dma("sp", self.pT[row:row + 128, :], st, r=sd)
            for t in range(NT):
                v, vd = vr.next()
                p1, pd1 = pr.next()
                p2, pd2 = pr.next()
                for k in range(8):
                    kb.op("pe", lambda e, k=k: e.matmul(p1, lhsT=self.XT[:, k, t * 128:(t + 1) * 128], rhs=wv[:, k, 0:512],
                                                        start=(k == 0), stop=(k == 7)),
                          r=[wvd, self.XTd[t // 4]], w=[pd1], mark=(k == 7))
                for k in range(8):
                    kb.op("pe", lambda e, k=k: e.matmul(p2[:, 0:256], lhsT=self.XT[:, k, t * 128:(t + 1) * 128],
                                                        rhs=wv[:, k, 512:768], start=(k == 0), stop=(k == 7)),
                          r=[wvd, self.XTd[t // 4]], w=[pd2], mark=(k == 7))
                kb.op("act", lambda e: e.copy(out=v[:, 0:512], in_=p1), r=[pd1], w=[vd])
                kb.op("dve", lambda e: e.tensor_copy(out=v[:, 512:768], in_=p2[:, 0:256]), r=[pd2], w=[vd])
                kb.dma("sp", self.vtok[t * 128:(t + 1) * 128, :], v, r=[vd])


    def abf(self, lo, hi, **kw):
        return self.arena[:, lo:hi].bitcast(BF16)

    def retention(self, l):
        nc, kb = self.nc, self.kb
        base = RWKV_IN + ATTN_IN
        pT = self.pT
        gam = self.consts["gamma_c"]
        H0, H1 = slice(0, 64), slice(64, 128)
        with ExitStack() as es:
            sb = lambda n, sh, dt: es.enter_context(nc.sbuf_tensor(self.un(n), sh, dt))
            pp = lambda n, sh, dt: es.enter_context(nc.psum_tensor(n, sh, dt))
            kr = self.abf(0, 2048)
            qm = [self.abf(2048, 4096), self.abf(4096, 6144)]
            qx = [self.abf(6144, 8192), self.abf(8192, 10240)]
            sgb = self.abf(10240, 12288)
            krd, qmd, qxd, sgd = Dep(), Dep(), Dep(), Dep()
            cs_t = sb("rt_cs", [128, 2, 2, 1024], F32)
            tq_t = sb("rt_tq", [128, 2, 1024], F32)
            tw_t = sb("rt_tw", [128, 2, 1024], F32)
            v = sb("rt_v", [128, NT, 256], BF16)
            yo = sb("rt_yo", [128, S], BF16)
            gnp = sb("rt_gnp", [128, 4], F32)
            rst = sb("rt_rst", [128, 64], F32)
            rstb = sb("rt_rstb", [128, 64], BF16)
            vd, yod, gnd, rsd, rsbd = Dep(), Dep(), Dep(), Dep(), Dep()
            kb.dma("sp", v[:], self.vtok[:, 512:768].rearrange("(t p) c -> p t c", p=128), w=[vd])
            kb.dma("sp", gnp[:, 0:2], self.I["ret_gn_g"][l].rearrange("(a p) -> p a", p=128), w=[gnd], slow=True)
            kb.dma("sp", gnp[:, 2:4], self.I["ret_gn_b"][l].rearrange("(a p) -> p a", p=128), w=[gnd], slow=True)
            csr, tqr, twr = Ring(cs_t, 2), Ring(tq_t, 2), Ring(tw_t, 2)
            B = self.bank
            bfv = lambda i: (B[i][0].bitcast(BF16)[:, 0:128], B[i][1])
            ps_s = BRing([(B[0][0][:, 0:256], B[0][1]), (B[1][0][:, 0:256], B[1][1])])
            ps_k = BRing([bfv(2)])
            ps_kv = BRing([(B[3][0][:, 0:128], B[3][1])])
            ps_o = BRing([(B[4][0][:, 0:128], B[4][1]), (B[5][0][:, 0:128], B[5][1])])
            ps_t = BRing([bfv(6)])
            sTm_r = Ring(sb("rt_sTm", [128, 2, 256], BF16), 2)
            kz_r = Ring(sb("rt_kz", [128, 2, 128], BF16), 2)
            osq_r = Ring(sb("rt_osq", [128, 2, 128], F32), 2)
            yn_r = Ring(sb("rt_yn", [128, 2, 128], BF16), 2)
            st_r = Ring(sb("rt_st", [128, 2, 16], F32), 2)
            tf_r = Ring(sb("rt_tf", [128, 2, 128], F32), 2)
            dmask = self.cst("dmaskT")
            zeta = self.cst("zeta8")
            xi = self.cst("xi").rearrange("p (a n) -> p a n", a=2)
            for hp in range(2):
                kb.op("pool", lambda e: e.memset(qm[0][H1, :], 0.0), w=[qmd])
                kb.op("pool", lambda e: e.memset(qm[1][H0, :], 0.0), w=[qmd])
                kb.op("pool", lambda e: e.memset(qx[0][H1, :], 0.0), w=[qxd])
                kb.op("pool", lambda e: e.memset(qx[1][H0, :], 0.0), w=[qxd])
                kb.op("pool", lambda e: e.memset(rst[:], 0.0), w=[rsd])
                for pc in range(4):
                    sl = slice(pc * 1024, (pc + 1) * 1024)
                    cs, csd = csr.next()
                    kb.dma("sp", cs[:, 0], self.I["cos2"][:, sl], w=[csd])
                    kb.dma("sp", cs[:, 1], self.I["sin2"][:, sl], w=[csd])
                    for which in range(2):
                        r0 = base + which * 256 + hp * 128
                        tq, tqd = tqr.next()
                        tw, twd = twr.next()
                        kb.dma("sp", tq, pT[r0:r0 + 128, sl], w=[tqd])
                        for blk in range(4):
                            src = r0 + (blk ^ 1) * 32
                            kb.dma("sp", tw[blk * 32:(blk + 1) * 32], pT[src:src + 32, sl], w=[twd])
                        kb.op("dve", lambda e: e.tensor_tensor(out=tq, in0=tq, in1=cs[:, 0], op=ALU.mult),
                              r=[csd], w=[tqd])
                        kb.op("pool", lambda e: e.tensor_tensor(out=tw, in0=tw, in1=cs[:, 1], op=ALU.mult),
                              r=[csd], w=[twd])
                        if which == 1:
                            kb.op("dve", lambda e: e.tensor_tensor(out=kr[:, sl], in0=tq, in1=tw, op=ALU.add),
                                  r=[tqd, twd], w=[krd])
                        else:
                            for hh, rows in enumerate((H0, H1)):
                                kb.op("dve", lambda e: e.tensor_tensor(out=qm[hh][rows, sl], in0=tq[rows], in1=tw[rows],
                                                                       op=ALU.add), r=[tqd, twd], w=[qmd])
                    r0 = base + 768 + hp * 128
                    tq, tqd = tqr.next()
                    kb.dma("sp", tq, pT[r0:r0 + 128, sl], w=[tqd])
                    kb.op("act", lambda e: e.activation(out=sgb[:, sl], in_=tq, func=AF.Silu), r=[tqd], w=[sgd])
                for hh, rows in enumerate((H0, H1)):
                    kb.op("pool", lambda e: e.tensor_tensor(
                        out=qx[hh][rows, :].rearrange("p (c n) -> p c n", n=128),
                        in0=qm[hh][rows, :].rearrange("p (c n) -> p c n", n=128),
                        in1=xi[rows, hp:hp + 1, :].broadcast_to([64, NT, 128]), op=ALU.mult),
                        r=[qmd, self.cstd], w=[qxd])
                import os
                CUT = int(os.environ.get("RET_CUT", "99"))
                for c in range((int(os.environ.get("RET_NC", "32")) if CUT > 2 else 0)):
                    cs = slice(c * 128, (c + 1) * 128)
                    p_s, p_sd = ps_s.next()
                    for hh in range(2):
                        kb.op("pe", lambda e: e.matmul(p_s[:, hh * 128:(hh + 1) * 128], lhsT=kr[:, cs], rhs=qm[hh][:, cs],
                                                       start=True, stop=True), r=[krd, qmd], w=[p_sd], mark=(hh == 1))
                    sTm, sTmd = sTm_r.next()
                    kb.op("dve", lambda e: e.tensor_tensor(out=sTm, in0=p_s, in1=dmask[:, hp * 256:(hp + 1) * 256],
                                                           op=ALU.mult), r=[p_sd, self.cstd], w=[sTmd])
                    if CUT <= 3:
                        continue
                    p_k, p_kd = ps_k.next()
                    kb.op("pe", lambda e: e.transpose(p_k, kr[:, cs], self.identb[:]), r=[krd, self.cstd], w=[p_kd])
                    kz, kzd = kz_r.next()
                    for hh in range(2):
                        h = 2 * hp + hh
                        kb.op("act", lambda e: e.mul(out=kz[:, hh * 64:(hh + 1) * 64], in_=p_k[:, hh * 64:(hh + 1) * 64],
                                                     mul=zeta[:, h:h + 1]), r=[p_kd, self.cstd], w=[kzd])
                    if CUT <= 4:
                        continue
                    p_o, p_od = ps_o.next()
                    for hh in range(2):
                        h = 2 * hp + hh
                        kb.op("pe", lambda e: e.matmul(p_o[:, hh * 64:(hh + 1) * 64], lhsT=sTm[:, hh * 128:(hh + 1) * 128],
                                                       rhs=v[:, c, h * 64:(h + 1) * 64], start=True, stop=(c == 0)),
                              r=[sTmd, vd], w=[p_od], mark=(c == 0 and hh == 1))
                        if c > 0:
                            kb.op("pe", lambda e: e.matmul(p_o[:, hh * 64:(hh + 1) * 64], lhsT=qx[hh][:, cs], rhs=rstb[:],
                                                           start=False, stop=True),
                                  r=[qxd, rsbd], w=[p_od], mark=(hh == 1))
                    if CUT <= 5:
                        continue
                    if c < NT - 1:
                        p_kv, p_kvd = ps_kv.next()
                        kb.op("pe", lambda e: e.matmul(p_kv, lhsT=kz, rhs=v[:, c, hp * 128:(hp + 1) * 128],
                                                       start=True, stop=True), r=[kzd, vd], w=[p_kvd])
                        for hh, rows in enumerate((H0, H1)):
                            kb.op("dve", lambda e: e.scalar_tensor_tensor(
                                out=rst[rows, :], in0=rst[rows, :], scalar=gam[2 * hp + hh],
                                in1=p_kv[rows, hh * 64:(hh + 1) * 64], op0=ALU.mult, op1=ALU.add),
                                r=[p_kvd, rsbd], w=[rsd])
                        kb.op("dve", lambda e: e.tensor_copy(out=rstb[:], in_=rst[:]), r=[rsd], w=[rsbd])
                    if CUT <= 6:
                        continue
                    st, std = st_r.next()
                    osq, osqd = osq_r.next()
                    kb.op("act", lambda e: e.activation(out=osq, in_=p_o, func=AF.Square), r=[p_od], w=[osqd])
                    kb.op("dve", lambda e: e.reduce_sum(out=st[:, 0:2], in_=p_o.rearrange("p (h e) -> p h e", e=64),
                                                        axis=AX.X), r=[p_od], w=[std])
                    kb.op("dve", lambda e: e.reduce_sum(out=st[:, 2:4], in_=osq.rearrange("p (h e) -> p h e", e=64),
                                                        axis=AX.X), r=[osqd], w=[std])
                    self.norm_stats(st, std, 2, 64, LN_EPS)
                    if CUT <= 7:
                        continue
                    yn, ynd = yn_r.next()
                    for hh in range(2):
                        kb.op("dve", lambda e: e.tensor_scalar(out=yn[:, hh * 64:(hh + 1) * 64], in0=p_o[:, hh * 64:(hh + 1) * 64],
                                                               scalar1=st[:, 12 + hh:13 + hh], scalar2=st[:, 14 + hh:15 + hh],
                                                               op0=ALU.mult, op1=ALU.add), r=[p_od, std], w=[ynd])
                    if CUT <= 8:
                        continue
                    p_t, p_td = ps_t.next()
                    kb.op("pe", lambda e: e.transpose(p_t, yn, self.identb[:]), r=[ynd, self.cstd], w=[p_td])
                    tf, tfd = tf_r.next()
                    kb.op("dve", lambda e: e.tensor_scalar(out=tf, in0=p_t, scalar1=gnp[:, hp:hp + 1],
                                                           scalar2=gnp[:, 2 + hp:3 + hp], op0=ALU.mult, op1=ALU.add),
                          r=[p_td, gnd], w=[tfd])
                    kb.op("pool", lambda e: e.tensor_tensor(out=yo[:, cs], in0=tf, in1=sgb[:, cs], op=ALU.mult),
                          r=[tfd, sgd], w=[yod])
                kb.dma("sp", self.yT[768 + hp * 128:768 + (hp + 1) * 128, :], yo[:], r=[yod])

    def norm_stats(self, st, std, n, cnt, eps):
        kb = self.kb
        c = lambda i: st[:, i * n:(i + 1) * n]
        kb.op("dve", lambda e: e.tensor_scalar(out=c(2), in0=c(0), scalar1=1.0 / cnt, scalar2=None, op0=ALU.mult),
              r=[std], w=[std])
        kb.op("dve", lambda e: e.tensor_tensor(out=c(3), in0=c(2), in1=c(2), op=ALU.mult), r=[std], w=[std])
        kb.op("dve", lambda e: e.scalar_tensor_tensor(out=c(4), in0=c(1), scalar=1.0 / cnt, in1=c(3),
                                                      op0=ALU.mult, op1=ALU.subtract), r=[std], w=[std])
        kb.op("dve", lambda e: e.tensor_scalar(out=c(4), in0=c(4), scalar1=float(eps), scalar2=None, op0=ALU.add),
              r=[std], w=[std])
        kb.op("act", lambda e: e.activation(out=c(5), in_=c(4), func=AF.Sqrt), r=[std], w=[std])
        kb.op("dve", lambda e: e.reciprocal(out=c(6), in_=c(5)), r=[std], w=[std])
        kb.op("dve", lambda e: e.scalar_tensor_tensor(out=c(7), in0=c(2), scalar=-1.0, in1=c(6),
                                                      op0=ALU.mult, op1=ALU.mult), r=[std], w=[std])

    def attention(self, l):
        nc, kb = self.nc, self.kb
        base = RWKV_IN
        pT = self.pT
        H0, H1 = slice(0, 64), slice(64, 128)
        with ExitStack() as es:
            sb = lambda n, sh, dt: es.enter_context(nc.sbuf_tensor(self.un(n), sh, dt))
            self.bias8 = sb("bias8", [128, 3, 8, 2, 128], BF16)
            self.setup_bias()
            acc = self.arena[0:64, :].rearrange("p (a h s) -> p a h s", a=2, h=2)
            accd = Dep()
            kt = sb("at_k", [128, S], BF16)
            qm = [sb("at_q0", [128, S], BF16), sb("at_q1", [128, S], BF16)]
            vp = [sb("at_v%d" % i, [128, NT, 128], BF16) for i in range(3)]
            ones = sb("at_ones", [128, 64], BF16)
            PT_r = Ring(sb("at_PT", [128, 4, 512], BF16), 4)
            rc = sb("at_rc", [64, 2, S], F32)
            yo = sb("at_yo", [64, 2, S], BF16)
            ktd, qmd, vd, od, rcd, yod = Dep(), Dep(), Dep(), Dep(), Dep(), Dep()
            kb.op("pool", lambda e: e.memset(ones[:], 1.0), w=[od])
            B = self.bank
            ps_s = BRing([B[0], B[1], B[2], B[3]])
            ps_o = BRing([B[4], B[5], B[6], B[7]])
            for pair in range(4):
                kb.op("pool", lambda e: e.memset(qm[0][H1, :], 0.0), w=[qmd])
                kb.op("pool", lambda e: e.memset(qm[1][H0, :], 0.0), w=[qmd])
                kb.op("dve", lambda e: e.memset(self.arena[0:64, :], 0.0), w=[accd])
                q0 = base + pair * 128
                k0 = base + 512 + pair * 128
                kb.dma("pool", qm[0][H0, :], pT[q0:q0 + 64, :], w=[qmd])
                kb.dma("pool", qm[1][H1, :], pT[q0 + 64:q0 + 128, :], w=[qmd])
                kb.dma("pool", kt[:], pT[k0:k0 + 128, :], w=[ktd])
                for p, (win, dil) in enumerate(PATTERNS):
                    nb = NT // dil
                    for r in range(dil):
                        src = self.vtok[:, pair * 128:(pair + 1) * 128].rearrange("(b j r) c -> r j b c", r=dil, j=128)[r]
                        kb.dma("sp", vp[p][:, r * nb:(r + 1) * nb, :], src, w=[vd])
                units = [(p, dil, NT // dil, r, b) for p, (win, dil) in enumerate(PATTERNS)
                         for r in range(dil) for b in range(NT // dil)]

                def tsl(dil, r, bb):
                    st = r + dil * 128 * bb
                    return slice(st, st + dil * 127 + 1, dil)

                def emit_scores(u):
                    p, dil, nb, r, b = u
                    qs = tsl(dil, r, b)
                    halves = (0, 1) if b > 0 else (1,)
                    p_s, p_sd = ps_s.next()
                    for hh in range(2):
                        h = 2 * pair + hh
                        for half in halves:
                            ks = tsl(dil, r, b - 1) if half == 0 else qs
                            o = p_s[:, (hh * 2 + half) * 128:(hh * 2 + half + 1) * 128]
                            kb.op("pe", lambda e: e.matmul(o, lhsT=kt[:, ks], rhs=qm[hh][:, qs], start=True, stop=False),
                                  r=[ktd, qmd], w=[p_sd], mark=False)
                            kb.op("pe", lambda e: e.matmul(o, lhsT=self.identb[:], rhs=self.bias8[:, p, h, half, :],
                                                           start=False, stop=True),
                                  r=[self.cstd, self.bias8d], w=[p_sd], mark=(hh == 1 and half == 1))
                    PT, PTd = PT_r.next()
                    if b > 0:
                        kb.op("act", lambda e: e.activation(out=PT, in_=p_s, func=AF.Exp, scale=0.125),
                              r=[p_sd], w=[PTd])
                    else:
                        v4 = lambda t: t.rearrange("p (h a i) -> p h a i", h=2, a=2)[:, :, 1, :]
                        kb.op("act", lambda e: e.activation(out=v4(PT), in_=v4(p_s), func=AF.Exp, scale=0.125),
                              r=[p_sd], w=[PTd])
                    return PT, PTd

                def emit_pv(u, ctx):
                    p, dil, nb, r, b = u
                    PT, PTd = ctx
                    qs = tsl(dil, r, b)
                    halves = (0, 1) if b > 0 else (1,)
                    p_o, p_od = ps_o.next()
                    for nd in range(2):
                        for hh in range(2):
                            o = p_o[0:64, (nd * 2 + hh) * 128:(nd * 2 + hh + 1) * 128]
                            for hi, half in enumerate(halves):
                                ti = r * nb + (b - 1 if half == 0 else b)
                                lw = vp[p][:, ti, hh * 64:(hh + 1) * 64] if nd == 0 else ones[:]
                                kb.op("pe", lambda e: e.matmul(o, lhsT=lw, rhs=PT[:, (hh * 2 + half) * 128:(hh * 2 + half + 1) * 128],
                                                               start=(hi == 0), stop=(hi == len(halves) - 1)),
                                      r=[PTd, vd, od], w=[p_od], mark=(nd == 1 and hh == 1 and hi == len(halves) - 1))
                    kb.op("dve", lambda e: e.tensor_tensor(
                        out=acc[:, :, :, qs], in0=acc[:, :, :, qs],
                        in1=p_o[0:64, :].rearrange("p (a h i) -> p a h i", a=2, h=2), op=ALU.add),
                        r=[p_od, accd], w=[accd])

                LOOK = 2
                ctxs = {}
                for i in range(min(LOOK, len(units))):
                    ctxs[i] = emit_scores(units[i])
                for i, u in enumerate(units):
                    if i + LOOK < len(units):
                        ctxs[i + LOOK] = emit_scores(units[i + LOOK])
                    emit_pv(u, ctxs.pop(i))
                kb.op("dve", lambda e: e.reciprocal(out=rc[:], in_=acc[:, 1]), r=[accd], w=[rcd])
                kb.op("dve", lambda e: e.tensor_tensor(out=yo[:], in0=acc[:, 0], in1=rc[:], op=ALU.mult),
                      r=[accd, rcd], w=[yod])
                for hh in range(2):
                    r0 = 256 + (2 * pair + hh) * 64
                    kb.dma("sp", self.yT[r0:r0 + 64, :], yo[:, hh, :], r=[yod])

    def rwkv(self, l):
        nc, kb = self.nc, self.kb
        pT = self.pT
        I = self.I
        H0, H1 = slice(0, 64), slice(64, 128)
        SEG = 1024
        NCH = SEG // 64
        with ExitStack() as es:
            sb = lambda n, sh, dt: es.enter_context(nc.sbuf_tensor(self.un(n), sh, dt))
            U = [self.arena[:, i * SEG:(i + 1) * SEG] for i in range(16)]
            Ud = [Dep() for _ in range(16)]
            prm = sb("rw_prm", [128, 32], F32)
            prd = Dep()
            pcol = {}
            kb.dma("sp", prm[:, 0:8], I["rwkv_mu"][l].rearrange("(c p) -> p c", p=128), w=[prd], slow=True)
            for i, nm in enumerate(("rwkv_w0", "rwkv_a0", "rwkv_k_k", "rwkv_k_a", "rwkv_r_k", "rwkv_ln_g", "rwkv_ln_b")):
                pcol[nm] = 8 + 2 * i
                kb.dma("sp", prm[:, 8 + 2 * i:10 + 2 * i], I[nm][l].rearrange("(a p) -> p a", p=128), w=[prd], slow=True)
            pcol["omka"] = 22
            kb.op("dve", lambda e: e.tensor_scalar(out=prm[:, 22:24], in0=prm[:, pcol["rwkv_k_a"]:pcol["rwkv_k_a"] + 2],
                                                   scalar1=-1.0, scalar2=1.0, op0=ALU.mult, op1=ALU.add), r=[prd], w=[prd])
            P = lambda nm, hp: prm[:, pcol[nm] + hp:pcol[nm] + hp + 1]
            wlw = sb("rw_wlw", [128, 256], BF16)
            wla = sb("rw_wla", [128, 256], BF16)
            gup = sb("rw_gup", [128, 256], BF16)
            wd = Dep()
            kb.op("pool", lambda e: e.memset(wlw[H1, :], 0.0), w=[wd])
            kb.op("pool", lambda e: e.memset(wla[H0, :], 0.0), w=[wd])
            kb.dma("pool", wlw[H0, :], I["rwkv_w_up"][l], w=[wd])
            kb.dma("pool", wla[H1, :], I["rwkv_a_up"][l], w=[wd])
            kb.dma("pool", gup[:], I["rwkv_g_up"][l], w=[wd])
            pad = {}
            padd = {}
            for nm in ("Rt", "Kk", "Bt", "Kt", "V"):
                pad[nm] = sb("rw_p" + nm, [128, NCH, 128], F32)
                padd[nm] = Dep()
                kb.op("pool", lambda e, nm=nm: e.memset(pad[nm][:], 0.0), w=[padd[nm]])
            T6 = sb("rw_T6", [128, SEG], BF16)
            SG = sb("rw_SG", [128, SEG], BF16)
            T6d, SGd = Dep(), Dep()
            buf_r = Ring(sb("rw_buf", [128, 2, SEG + 1], F32), 2)
            tmp_r = Ring(sb("rw_tmp", [128, 2, SEG], F32), 2)
            yo_r = Ring(sb("rw_yo", [128, 2, SEG], BF16), 2)
            Hs = [sb("rw_H%d" % i, [128, 128], F32) for i in range(2)]
            Hd = [Dep(), Dep()]
            import os
            NW = int(os.environ.get('RW_NW', '4'))
            wk = []
            for g in range(NW):
                t = {}
                for nm in ("nBtPT", "KtPT", "VPT", "N", "NT", "MakT", "nPrbT", "PrkT", "Pa", "Pb", "PT1", "PT2", "PT4",
                           "PT8", "PT16", "PT32", "QT", "GT0", "E", "YnP", "sq"):
                    t[nm] = (sb("rw_%s%d" % (nm, g), [128, 128], F32), Dep())
                for nm in ("Xa", "Xb"):
                    t[nm] = (sb("rw_%s%d" % (nm, g), [128, 256], F32), Dep())
                t["st"] = (sb("rw_st%d" % g, [128, 8], F32), Dep())
                kb.op("pool", lambda e, t=t: e.memset(t["YnP"][0][:], 0.0), w=[t["YnP"][1]])
                wk.append(t)
            banks = BRing(self.bank)
            ident = self.cst("ident")
            cd = self.cstd
            evc = [0]

            def copy_ev(out, in_, r, w, scale=None):
                evc[0] += 1
                if evc[0] % 2 == 0:
                    if scale is None:
                        kb.op("act", lambda e: e.copy(out=out, in_=in_), r=r, w=w)
                    else:
                        kb.op("act", lambda e: e.mul(out=out, in_=in_, mul=float(scale)), r=r, w=w)
                else:
                    if scale is None:
                        kb.op("dve", lambda e: e.tensor_copy(out=out, in_=in_), r=r, w=w)
                    else:
                        kb.op("dve", lambda e: e.tensor_scalar(out=out, in0=in_, scalar1=float(scale), scalar2=None,
                                                               op0=ALU.mult), r=r, w=w)

            def load_shift(row0, seg, c8, dst, dstd):
                buf, bd = buf_r.next()
                tmp, td = tmp_r.next()
                t0 = seg * SEG
                if seg == 0:
                    kb.op("pool", lambda e: e.memset(buf[:, 0:1], 0.0), w=[bd])
                    kb.dma("sp", buf[:, 1:SEG + 1], pT[row0:row0 + 128, 0:SEG], w=[bd])
                else:
                    kb.dma("sp", buf[:, 0:SEG + 1], pT[row0:row0 + 128, t0 - 1:t0 + SEG], w=[bd])
                kb.op("pool", lambda e: e.tensor_tensor(out=tmp, in0=buf[:, 0:SEG], in1=buf[:, 1:SEG + 1], op=ALU.subtract),
                      r=[bd], w=[td])
                kb.op("dve", lambda e: e.scalar_tensor_tensor(out=dst, in0=tmp, scalar=prm[:, c8:c8 + 1], in1=buf[:, 1:SEG + 1],
                                                              op0=ALU.mult, op1=ALU.add), r=[td, bd, prd], w=[dstd])

            def mm_to(lhsT, rhs, rd, n=128):
                bk, bd = banks.next()
                o = bk[:, 0:n]
                kb.op("pe", lambda e: e.matmul(o, lhsT=lhsT, rhs=rhs, start=True, stop=True), r=rd, w=[bd])
                return o, bd

            def chunk_gen(hp, seg, c, g):
                t = wk[g]
                gc = seg * NCH + c
                A = lambda nm: t[nm][0][:]
                Dp = lambda nm: t[nm][1]
                Rt, Kk, Bt, Kt, V = (pad[nm][:, c, :] for nm in ("Rt", "Kk", "Bt", "Kt", "V"))
                dR, dKk, dB, dK, dV = (padd[nm] for nm in ("Rt", "Kk", "Bt", "Kt", "V"))
                X = [t["Xa"], t["Xb"]]
                for src, sd, dst, dd, sc in ((Bt, dB, A("nBtPT"), Dp("nBtPT"), -1.0), (Kt, dK, A("KtPT"), Dp("KtPT"), None),
                                             (V, dV, A("VPT"), Dp("VPT"), None), (Kk, dKk, X[0][0][:, 0:128], X[0][1], None)):
                    bk, bd = banks.next()
                    kb.op("pe", lambda e: e.transpose(bk[:, 0:128], src, ident), r=[sd, cd], w=[bd])
                    copy_ev(dst, bk[:, 0:128], [bd], [dd], sc)
                yield
                for lh, ld, rh, rdd, dst, msk in ((Bt, dB, Kk, dKk, "NT", "nSU"), (Kk, dKk, Bt, dB, "N", "nSL"),
                                                 (Kt, dK, Kk, dKk, "MakT", "SU"), (Bt, dB, Rt, dR, "nPrbT", "nIU"),
                                                 (Kt, dK, Rt, dR, "PrkT", "IU")):
                    o, bd = mm_to(lh, rh, [ld, rdd])
                    kb.op("dve", lambda e: e.tensor_tensor(out=A(dst), in0=o, in1=self.cst(msk), op=ALU.mult),
                          r=[bd, cd], w=[Dp(dst)])
                yield
                o, bd = mm_to(A("MakT"), A("VPT"), [Dp("MakT"), Dp("VPT")])
                copy_ev(X[0][0][:, 128:256], o, [bd], [X[0][1]])
                yield
                Pc, PTc = ("N", "NT")
                lv_names = {2: "PT2", 4: "PT4", 8: "PT8", 16: "PT16", 32: "PT32"}
                pt_list = ["NT"]
                pbuf = ["Pa", "Pb"]
                pi = 0
                for lvl in (2, 4, 8, 16, 32):
                    o2, bd2 = mm_to(A(Pc), A(PTc), [Dp(Pc), Dp(PTc)])
                    if lvl < 32:
                        o1, bd1 = mm_to(A(PTc), A(Pc), [Dp(Pc), Dp(PTc)])
                    copy_ev(A(lv_names[lvl]), o2, [bd2], [Dp(lv_names[lvl])])
                    if lvl < 32:
                        copy_ev(A(pbuf[pi]), o1, [bd1], [Dp(pbuf[pi])])
                        Pc = pbuf[pi]
                        pi ^= 1
                    PTc = lv_names[lvl]
                    pt_list.append(PTc)
                    yield
                xi_ = 0
                for nm in reversed(pt_list):
                    Xc, Xn = X[xi_], X[xi_ ^ 1]
                    o, bd = mm_to(A(nm), Xc[0][:], [Dp(nm), Xc[1]], n=256)
                    kb.op("dve", lambda e: e.tensor_tensor(out=Xn[0][:], in0=o, in1=Xc[0][:], op=ALU.add),
                          r=[bd, Xc[1]], w=[Xn[1]])
                    xi_ ^= 1
                    yield
                W, Wd = X[xi_]
                W1, W2 = W[:, 0:128], W[:, 128:256]
                o, bd = mm_to(W1, A("nPrbT"), [Wd, Dp("nPrbT")])
                kb.op("dve", lambda e: e.tensor_tensor(out=A("QT"), in0=o, in1=Rt, op=ALU.add), r=[bd, dR], w=[Dp("QT")])
                o, bd = mm_to(W1, A("nBtPT"), [Wd, Dp("nBtPT")])
                kb.op("dve", lambda e: e.tensor_tensor(out=A("GT0"), in0=o, in1=ident, op=ALU.add), r=[bd, cd], w=[Dp("GT0")])
                bk, bd = banks.next()
                o = bk[:, 0:128]
                kb.op("pe", lambda e: e.matmul(o, lhsT=A("KtPT"), rhs=A("VPT"), start=True, stop=False),
                      r=[Dp("KtPT"), Dp("VPT")], w=[bd], mark=False)
                kb.op("pe", lambda e: e.matmul(o, lhsT=A("nBtPT"), rhs=W2, start=False, stop=True),
                      r=[Dp("nBtPT"), Wd], w=[bd])
                dc = U[12][:, c * 64 + 63:c * 64 + 64]
                kb.op("dve", lambda e: e.tensor_scalar(out=A("E"), in0=o, scalar1=dc, scalar2=None, op0=ALU.mult),
                      r=[bd, Ud[12]], w=[Dp("E")])
                yield
                Hc, Hcd = Hs[gc % 2], Hd[gc % 2]
                Hn, Hnd = Hs[(gc + 1) % 2], Hd[(gc + 1) % 2]
                bk, ybd = banks.next()
                yps = bk[:, 0:128]
                kb.op("pe", lambda e: e.matmul(yps, lhsT=A("QT"), rhs=Hc[:], start=True, stop=False),
                      r=[Dp("QT"), Hcd], w=[ybd], mark=False)
                kb.op("pe", lambda e: e.matmul(yps, lhsT=A("PrkT"), rhs=A("VPT"), start=False, stop=False),
                      r=[Dp("PrkT"), Dp("VPT")], w=[ybd], mark=False)
                kb.op("pe", lambda e: e.matmul(yps, lhsT=A("nPrbT"), rhs=W2, start=False, stop=True),
                      r=[Dp("nPrbT"), Wd], w=[ybd])
                o, bd = mm_to(A("GT0"), Hc[:], [Dp("GT0"), Hcd])
                kb.op("dve", lambda e: e.scalar_tensor_tensor(out=Hn[:], in0=o, scalar=dc, in1=A("E"), op0=ALU.mult, op1=ALU.add),
                      r=[bd, Dp("E"), Ud[12]], w=[Hnd])
                yield
                st, std = t["st"]
                kb.op("act", lambda e: e.activation(out=A("sq"), in_=yps, func=AF.Square), r=[ybd], w=[Dp("sq")])
                kb.op("dve", lambda e: e.reduce_sum(out=st[:, 0:1], in_=yps, axis=AX.X), r=[ybd], w=[std])
                kb.op("dve", lambda e: e.reduce_sum(out=st[:, 1:2], in_=A("sq"), axis=AX.X), r=[Dp("sq")], w=[std])
                self.norm_stats(st, std, 1, 64, RWKV_GN_EPS)
                for rows in (H0, H1):
                    kb.op("dve", lambda e: e.tensor_scalar(out=t["YnP"][0][rows, rows], in0=yps[rows, rows],
                                                           scalar1=st[rows, 6:7], scalar2=st[rows, 7:8],
                                                           op0=ALU.mult, op1=ALU.add), r=[ybd, std], w=[Dp("YnP")])
                o, bd = mm_to(A("YnP"), self.cst("sel"), [Dp("YnP"), cd], n=64)
                kb.op("dve", lambda e: e.tensor_scalar(out=U[10][:, c * 64:(c + 1) * 64], in0=o,
                                                       scalar1=P("rwkv_ln_g", hp), scalar2=P("rwkv_ln_b", hp),
                                                       op0=ALU.mult, op1=ALU.add), r=[bd, prd], w=[Ud[10]])
                yield

            for hp in range(2):
                for i in range(2):
                    kb.op("pool", lambda e, i=i: e.memset(Hs[i][:], 0.0), w=[Hd[i]])
                for seg in range(S // SEG):
                    blk2 = [slice(0, 512), slice(512, 1024)]
                    load_shift(768, seg, 6, U[8], Ud[8])
                    kb.op("act", lambda e: e.activation(out=T6[H0, :], in_=U[8][H0, :], func=AF.Tanh), r=[Ud[8]], w=[T6d])
                    kb.op("act", lambda e: e.copy(out=T6[H1, :], in_=U[8][H1, :]), r=[Ud[8]], w=[T6d])
                    load_shift(896, seg, 7, U[9], Ud[9])
                    kb.op("act", lambda e: e.activation(out=SG[:], in_=U[9], func=AF.Sigmoid), r=[Ud[9]], w=[SGd])
                    load_shift(hp * 128, seg, hp, U[0], Ud[0])
                    load_shift(256 + hp * 128, seg, 2 + hp, U[1], Ud[1])
                    load_shift(512 + hp * 128, seg, 4 + hp, U[2], Ud[2])
                    hs = slice(hp * 128, (hp + 1) * 128)
                    for b2 in blk2:
                        o, bd = mm_to(wlw[:, hs], T6[:, b2], [wd, T6d], n=512)
                        kb.op("act", lambda e: e.activation(out=U[4][:, b2], in_=o, func=AF.Sigmoid, bias=P("rwkv_w0", hp)),
                              r=[bd, prd], w=[Ud[4]])
                        o, bd = mm_to(wla[:, hs], T6[:, b2], [wd, T6d], n=512)
                        kb.op("act", lambda e: e.activation(out=U[3][:, b2], in_=o, func=AF.Sigmoid, bias=P("rwkv_a0", hp)),
                              r=[bd, prd], w=[Ud[3]])
                        o, bd = mm_to(gup[:, hs], SG[:, b2], [wd, SGd], n=512)
                        kb.op("act", lambda e: e.copy(out=U[5][:, b2], in_=o), r=[bd], w=[Ud[5]])
                    kb.op("dve", lambda e: e.tensor_scalar(out=U[6], in0=U[1], scalar1=P("rwkv_k_k", hp), scalar2=None,
                                                           op0=ALU.mult), r=[Ud[1], prd], w=[Ud[6]])
                    kb.op("pool", lambda e: e.tensor_tensor(out=U[8], in0=U[6], in1=U[6], op=ALU.mult), r=[Ud[6]], w=[Ud[8]])
                    for b2 in blk2:
                        o, bd = mm_to(self.cst("onesblk"), U[8][:, b2], [cd, Ud[8]], n=512)
                        kb.op("dve", lambda e: e.tensor_scalar(out=U[11][:, b2], in0=o, scalar1=1e-24, scalar2=None,
                                                               op0=ALU.max), r=[bd], w=[Ud[11]])
                    kb.op("act", lambda e: e.activation(out=U[11], in_=U[11], func=AF.Sqrt), r=[Ud[11]], w=[Ud[11]])
                    kb.op("dve", lambda e: e.reciprocal(out=U[11], in_=U[11]), r=[Ud[11]], w=[Ud[11]])
                    kb.op("pool", lambda e: e.tensor_tensor(out=U[6], in0=U[6], in1=U[11], op=ALU.mult),
                          r=[Ud[6], Ud[11]], w=[Ud[6]])
                    kb.op("dve", lambda e: e.tensor_scalar(out=U[7], in0=U[3], scalar1=P("rwkv_k_a", hp), scalar2=P("omka", hp),
                                                           op0=ALU.mult, op1=ALU.add), r=[Ud[3], prd], w=[Ud[7]])
                    kb.op("pool", lambda e: e.tensor_tensor(out=U[7], in0=U[7], in1=U[1], op=ALU.mult),
                          r=[Ud[7], Ud[1]], w=[Ud[7]])
                    kb.op("dve", lambda e: e.scalar_tensor_tensor(out=U[8], in0=U[0], scalar=P("rwkv_r_k", hp), in1=U[7],
                                                                  op0=ALU.mult, op1=ALU.mult), r=[Ud[0], Ud[7], prd], w=[Ud[8]])
                    for b2 in blk2:
                        o, bd = mm_to(self.cst("onesblk"), U[8][:, b2], [cd, Ud[8]], n=512)
                        kb.op("dve", lambda e: e.tensor_tensor(out=U[9][:, b2], in0=o, in1=U[2][:, b2], op=ALU.mult),
                              r=[bd, Ud[2]], w=[Ud[9]])
                    kb.op("pool", lambda e: e.tensor_tensor(out=U[3], in0=U[3], in1=U[6], op=ALU.mult),
                          r=[Ud[3], Ud[6]], w=[Ud[3]])
                    kb.op("dve", lambda e: e.tensor_tensor_scan(out=U[11], data0=self.cst("scanm"), data1=U[4], initial=0.0,
                                                                op0=ALU.mult, op1=ALU.add), r=[Ud[4], cd], w=[Ud[11]])
                    kb.op("act", lambda e: e.activation(out=U[12], in_=U[11], func=AF.Exp, scale=-C0), r=[Ud[11]], w=[Ud[12]])
                    kb.op("act", lambda e: e.activation(out=U[13], in_=U[11], func=AF.Exp, scale=C0), r=[Ud[11]], w=[Ud[13]])
                    kb.op("pool", lambda e: e.tensor_tensor(out=U[4], in0=U[11], in1=U[4], op=ALU.subtract),
                          r=[Ud[11], Ud[4]], w=[Ud[4]])
                    kb.op("act", lambda e: e.activation(out=U[4], in_=U[4], func=AF.Exp, scale=-C0), r=[Ud[4]], w=[Ud[4]])
                    kb.op("pool", lambda e: e.tensor_tensor(out=U[0], in0=U[0], in1=U[12], op=ALU.mult), r=[Ud[0], Ud[12]], w=[Ud[0]])
                    kb.op("dve", lambda e: e.tensor_tensor(out=U[6], in0=U[6], in1=U[4], op=ALU.mult), r=[Ud[6], Ud[4]], w=[Ud[6]])
                    kb.op("pool", lambda e: e.tensor_tensor(out=U[3], in0=U[3], in1=U[13], op=ALU.mult), r=[Ud[3], Ud[13]], w=[Ud[3]])
                    kb.op("dve", lambda e: e.tensor_tensor(out=U[7], in0=U[7], in1=U[13], op=ALU.mult), r=[Ud[7], Ud[13]], w=[Ud[7]])
                    ci = 0
                    for nm, ui in (("Rt", 0), ("Kk", 6), ("Bt", 3), ("Kt", 7), ("V", 2)):
                        for rows in (H0, H1):
                            dst = pad[nm][rows, :, rows]
                            src = U[ui][rows, :].rearrange("p (c t) -> p c t", t=64)
                            eng = ("pool", "dve", "act")[ci % 3]
                            ci += 1
                            if eng == "act":
                                kb.op("act", lambda e: e.copy(out=dst, in_=src), r=[Ud[ui]], w=[padd[nm]])
                            else:
                                kb.op(eng, lambda e: e.tensor_copy(out=dst, in_=src), r=[Ud[ui]], w=[padd[nm]])
                    for c0 in range(0, NCH, NW):
                        run_interleaved([chunk_gen(hp, seg, c0 + g, g) for g in range(NW)])
                    yo, yod = yo_r.next()
                    kb.op("pool", lambda e: e.tensor_tensor(out=U[10], in0=U[10], in1=U[9], op=ALU.add),
                          r=[Ud[10], Ud[9]], w=[Ud[10]])
                    kb.op("dve", lambda e: e.tensor_tensor(out=yo, in0=U[10], in1=U[5], op=ALU.mult),
                          r=[Ud[10], Ud[5]], w=[yod])
                    kb.dma("sp", self.yT[hp * 128:(hp + 1) * 128, seg * SEG:(seg + 1) * SEG], yo, r=[yod])

    def layer_norm_tile(self, z, zd, junk, jd, st, std, gb, gbd):
        kb = self.kb
        kb.op("act", lambda e: e.activation(out=junk, in_=z, func=AF.Square), r=[zd], w=[jd])
        kb.op("dve", lambda e: e.reduce_sum(out=st[:, 0:1], in_=z, axis=AX.X), r=[zd], w=[std])
        kb.op("dve", lambda e: e.reduce_sum(out=st[:, 1:2], in_=junk, axis=AX.X), r=[jd], w=[std])
        self.norm_stats(st, std, 1, D, LN_EPS)
        kb.op("dve", lambda e: e.tensor_scalar(out=z, in0=z, scalar1=st[:, 6:7], scalar2=st[:, 7:8],
                                               op0=ALU.mult, op1=ALU.add), r=[zd, std], w=[zd])
        kb.op("pool", lambda e: e.tensor_tensor(out=z, in0=z, in1=gb[:, 0, :], op=ALU.mult), r=[zd, gbd], w=[zd])
        kb.op("pool", lambda e: e.tensor_tensor(out=z, in0=z, in1=gb[:, 1, :], op=ALU.add), r=[zd, gbd], w=[zd])

    def load_ln_params(self, gb, gbd, l, i):
        kb = self.kb
        kb.dma("sp", gb[:, 0, :], self.I["ln_g"][l, i:i + 1, :].broadcast_to([128, D]), w=[gbd])
        kb.dma("sp", gb[:, 1, :], self.I["ln_b"][l, i:i + 1, :].broadcast_to([128, D]), w=[gbd])

    def outproj_ln1(self, l):
        nc, kb = self.nc, self.kb
        moe = (l % 2 == 1)
        j = l // 2
        with ExitStack() as es:
            sb = lambda n, sh, dt: es.enter_context(nc.sbuf_tensor(self.un(n), sh, dt))
            wo = sb("o_w", [128, 8, D], BF16)
            wod = Dep()
            kb.dma("pool", wo[:], self.I["w_out"][l].rearrange("(k p) n -> p k n", p=128), w=[wod])
            yt_r = Ring(sb("o_yt", [128, 2, 8, 512], BF16), 2)
            x_r = Ring(sb("o_x", [128, 2, D], F32), 2)
            z_r = Ring(sb("o_z", [128, 2, D], F32), 2)
            junk = sb("o_junk", [128, D], BF16)
            jd = Dep()
            st_r = Ring(sb("o_st", [128, 2, 8], F32), 2)
            gb = sb("o_gb", [128, 2, D], F32)
            gbd = Dep()
            self.load_ln_params(gb, gbd, l, 0)
            B = self.bank
            hb = BRing([(0, 1), (2, 3)])
            tr = self.tr_ring(((4, 5), (6, 7)))
            if moe:
                rt = sb("o_rt", [128, 8, N_EXPERTS], F32)
                rtd = Dep()
                kb.dma("sp", rt[:], self.I["moe_router"][j].rearrange("(k p) e -> p k e", p=128), w=[rtd], slow=True)
                xTf_r = Ring(sb("o_xTf", [128, 1, 8, 128], F32), 1)
                g_r = Ring(sb("o_g", [128, 2, 64], F32), 2)
            for tb in range(8):
                yt, ytd = yt_r.next()
                kb.dma("sp", yt, self.yT[:, tb * 512:(tb + 1) * 512].rearrange("(k p) t -> p k t", p=128), w=[ytd])
                for tt in range(4):
                    t = tb * 4 + tt
                    x, xd = x_r.next()
                    z, zd = z_r.next()
                    kb.dma("sp", x, self.xres[t * 128:(t + 1) * 128, :], w=[xd])
                    ba, bb_ = hb.next()
                    for half, bi in enumerate((ba, bb_)):
                        bk, bd = B[bi]
                        for k in range(8):
                            kb.op("pe", lambda e, k=k: e.matmul(bk[:, 0:512], lhsT=yt[:, k, tt * 128:(tt + 1) * 128],
                                                                rhs=wo[:, k, half * 512:(half + 1) * 512],
                                                                start=(k == 0), stop=(k == 7)),
                                  r=[ytd, wod], w=[bd], mark=(k == 7))
                        hs = slice(half * 512, (half + 1) * 512)
                        kb.op("dve", lambda e: e.scalar_tensor_tensor(out=z[:, hs], in0=x[:, hs], scalar=float(ALPHA),
                                                                      in1=bk[:, 0:512], op0=ALU.mult, op1=ALU.add),
                              r=[xd, bd], w=[zd])
                    st, std = st_r.next()
                    self.layer_norm_tile(z, zd, junk[:], jd, st, std, gb, gbd)
                    kb.dma("sp", self.xres[t * 128:(t + 1) * 128, :], z, r=[zd, xd])
                    if moe:
                        xTf, xTfd = xTf_r.next()
                        self.transpose_to_XT(z, zd, t, tr, f32_out=(xTf, xTfd))
                        self.router_gates(xTf, xTfd, rt, rtd, t, g_r)
                    else:
                        self.transpose_to_XT(z, zd, t, tr)

    def router_gates(self, xTf, xTfd, rt, rtd, t, g_r):
        kb = self.kb
        bk, bd = self.bank[0]
        lp = bk[:, 0:N_EXPERTS]
        for k in range(8):
            kb.op("pe", lambda e, k=k: e.matmul(lp, lhsT=xTf[:, k, :], rhs=rt[:, k, :], start=(k == 0), stop=(k == 7)),
                  r=[xTfd, rtd], w=[bd], mark=(k == 7))
        g, gd = g_r.next()
        c = lambda i: g[:, i * 8:(i + 1) * 8]
        sc = lambda i: g[:, 56 + i:57 + i]
        op = lambda fn: kb.op("dve", fn, r=[gd], w=[gd])
        kb.op("dve", lambda e: e.tensor_copy(out=c(0), in_=lp), r=[bd], w=[gd])
        op(lambda e: e.reduce_max(out=sc(0), in_=c(0), axis=AX.X))
        op(lambda e: e.tensor_scalar(out=c(1), in0=c(0), scalar1=sc(0), scalar2=None, op0=ALU.is_equal))
        op(lambda e: e.scalar_tensor_tensor(out=c(2), in0=c(1), scalar=-1e30, in1=c(0), op0=ALU.mult, op1=ALU.add))
        op(lambda e: e.reduce_max(out=sc(1), in_=c(2), axis=AX.X))
        op(lambda e: e.tensor_scalar(out=c(3), in0=c(0), scalar1=sc(1), scalar2=None, op0=ALU.is_ge))
        op(lambda e: e.tensor_scalar(out=sc(2), in0=sc(0), scalar1=-1.0, scalar2=None, op0=ALU.mult))
        kb.op("act", lambda e: e.activation(out=c(4), in_=c(0), func=AF.Exp, bias=sc(2), scale=1.0), r=[gd], w=[gd])
        op(lambda e: e.tensor_tensor(out=c(4), in0=c(4), in1=c(3), op=ALU.mult))
        op(lambda e: e.reduce_sum(out=sc(3), in_=c(4), axis=AX.X))
        op(lambda e: e.reciprocal(out=sc(4), in_=sc(3)))
        kb.op("dve", lambda e: e.tensor_scalar(out=self.gates[:, t, :], in0=c(4), scalar1=sc(4), scalar2=None, op0=ALU.mult),
              r=[gd], w=[self.gatesd])

    def ffn(self, l):
        nc, kb = self.nc, self.kb
        moe = (l % 2 == 1)
        j = l // 2
        last = (l == self.n_layers - 1)
        if moe:
            E, FF = N_EXPERTS, FF_EXPERT
            wg_a, wu_a, wd_a = self.I["moe_w_gate"][j], self.I["moe_w_up"][j], self.I["moe_w_down"][j]
        else:
            E, FF = 1, FF_DENSE
            wg_a, wu_a, wd_a = [self.I[k][j:j + 1] for k in ("ffn_w_gate", "ffn_w_up", "ffn_w_down")]
        G = 2
        ngrp = FF // (128 * G)
        HT = 2048
        with ExitStack() as es:
            sb = lambda n, sh, dt: es.enter_context(nc.sbuf_tensor(self.un(n), sh, dt))
            acc = sb("f_acc", [128, 16, D], F32)
            accd = [Dep() for _ in range(16)]
            wg_r = Ring(sb("f_wg", [128, 2, 8, G * 128], BF16), 2)
            wu_r = Ring(sb("f_wu", [128, 2, 8, G * 128], BF16), 2)
            wd_r = Ring(sb("f_wd", [128, 2, G, D], BF16), 2)
            sg_r = Ring(sb("f_sg", [128, 2, 512], F32), 2)
            x_r = Ring(sb("f_x", [128, 1, D], F32), 1)
            junk = sb("f_junk", [128, D], BF16)
            jd = Dep()
            st_r = Ring(sb("f_st", [128, 2, 8], F32), 2)
            gb = sb("f_gb", [128, 2, D], F32)
            gbd = Dep()
            self.load_ln_params(gb, gbd, l, 1)
            B = self.bank
            pg_r = BRing([B[0], B[1]])
            pu_r = BRing([B[2], B[3]])
            po_r = BRing([B[4], B[5], B[6], B[7]])
            tr = self.tr_ring(((6, 7),))
            hT_r = Ring(sb("f_hT2", [128, 2, G, HT], BF16), 2)
            for half in range(S // HT):
                groups = [(e_, grp) for e_ in range(E) for grp in range(ngrp)]
                N = len(groups)
                wgu = {}
                wdw = {}
                hTs = {}

                def load_gu(m):
                    e_, grp = groups[m]
                    ff0 = grp * G * 128
                    wg, wgd = wg_r.next()
                    wu, wud = wu_r.next()
                    kb.dma("pool", wg, wg_a[e_][:, ff0:ff0 + G * 128].rearrange("(k p) n -> p k n", p=128), w=[wgd])
                    kb.dma("pool", wu, wu_a[e_][:, ff0:ff0 + G * 128].rearrange("(k p) n -> p k n", p=128), w=[wud])
                    wgu[m] = (wg, wgd, wu, wud)

                def load_d(m):
                    e_, grp = groups[m]
                    ff0 = grp * G * 128
                    wdn, wdd = wd_r.next()
                    kb.dma("pool", wdn, wd_a[e_][ff0:ff0 + G * 128, :].rearrange("(g p) n -> p g n", p=128), w=[wdd])
                    wdw[m] = (wdn, wdd)

                def gu_units(m):
                    wg, wgd, wu, wud = wgu[m]
                    hT, hTd = hT_r.next()
                    hTs[m] = (hT, hTd)
                    us = []
                    for gi in range(G):
                        for tb in range(HT // 512):
                            def u(gi=gi, tb=tb):
                                tok = slice(half * HT + tb * 512, half * HT + (tb + 1) * 512)
                                xd_blk = self.XTd[(half * HT) // 512 + tb]
                                pg, pgd = pg_r.next()
                                pu, pud = pu_r.next()
                                for k in range(8):
                                    kb.op("pe", lambda e, k=k: e.matmul(pg, lhsT=wg[:, k, gi * 128:(gi + 1) * 128],
                                                                        rhs=self.XT[:, k, tok], start=(k == 0), stop=(k == 7)),
                                          r=[wgd, xd_blk], w=[pgd], mark=(k == 7))
                                for k in range(8):
                                    kb.op("pe", lambda e, k=k: e.matmul(pu, lhsT=wu[:, k, gi * 128:(gi + 1) * 128],
                                                                        rhs=self.XT[:, k, tok], start=(k == 0), stop=(k == 7)),
                                          r=[wud, xd_blk], w=[pud], mark=(k == 7))
                                sg, sgd = sg_r.next()
                                kb.op("act", lambda e: e.activation(out=sg, in_=pg, func=AF.Silu), r=[pgd], w=[sgd])
                                kb.op("dve", lambda e: e.tensor_tensor(out=hT[:, gi, tb * 512:(tb + 1) * 512], in0=pu, in1=sg,
                                                                       op=ALU.mult), r=[pud, sgd], w=[hTd])
                            us.append(u)
                    return us

                def dn_units(m):
                    e_, grp = groups[m]
                    wdn, wdd = wdw[m]
                    hT, hTd = hTs[m]
                    first = (m == 0)
                    us = []
                    for tt in range(HT // 128):
                        for hf in range(2):
                            def u(tt=tt, hf=hf):
                                t = half * (HT // 128) + tt
                                po, pod = po_r.next()
                                for gi in range(G):
                                    kb.op("pe", lambda e, gi=gi: e.matmul(po, lhsT=hT[:, gi, tt * 128:(tt + 1) * 128],
                                                                          rhs=wdn[:, gi, hf * 512:(hf + 1) * 512],
                                                                          start=(gi == 0), stop=(gi == G - 1)),
                                          r=[hTd, wdd], w=[pod], mark=(gi == G - 1))
                                a = acc[:, tt, hf * 512:(hf + 1) * 512]
                                if moe:
                                    gsc = self.gates[:, t, e_:e_ + 1]
                                    if first:
                                        kb.op("dve", lambda e: e.tensor_scalar(out=a, in0=po, scalar1=gsc, scalar2=None,
                                                                               op0=ALU.mult), r=[pod, self.gatesd], w=[accd[tt]])
                                    else:
                                        kb.op("dve", lambda e: e.scalar_tensor_tensor(out=a, in0=po, scalar=gsc, in1=a,
                                                                                      op0=ALU.mult, op1=ALU.add),
                                              r=[pod, self.gatesd, accd[tt]], w=[accd[tt]])
                                else:
                                    if first:
                                        kb.op("act", lambda e: e.copy(out=a, in_=po), r=[pod], w=[accd[tt]])
                                    else:
                                        kb.op("dve", lambda e: e.tensor_tensor(out=a, in0=po, in1=a, op=ALU.add),
                                              r=[pod, accd[tt]], w=[accd[tt]])
                            us.append(u)
                    return us

                load_gu(0)
                load_d(0)
                if N > 1:
                    load_gu(1)
                for u in gu_units(0):
                    u()
                for n in range(N):
                    if n + 1 < N:
                        load_d(n + 1)
                    if n + 2 < N:
                        load_gu(n + 2)
                    dn = dn_units(n)
                    gu = gu_units(n + 1) if n + 1 < N else []
                    per = len(dn) // max(len(gu), 1)
                    di = 0
                    for gi_, u in enumerate(gu):
                        u()
                        for _ in range(per):
                            dn[di]()
                            di += 1
                    while di < len(dn):
                        dn[di]()
                        di += 1
                for tt in range(HT // 128):
                    t = half * (HT // 128) + tt
                    x, xd = x_r.next()
                    kb.dma("sp", x, self.xres[t * 128:(t + 1) * 128, :], w=[xd])
                    z = acc[:, tt, :]
                    zd = accd[tt]
                    kb.op("dve", lambda e: e.scalar_tensor_tensor(out=z, in0=x, scalar=float(ALPHA), in1=z,
                                                                  op0=ALU.mult, op1=ALU.add), r=[xd, zd], w=[zd])
                    st, std = st_r.next()
                    self.layer_norm_tile(z, zd, junk[:], jd, st, std, gb, gbd)
                    if last:
                        kb.dma("sp", self.out[t * 128:(t + 1) * 128, :], z, r=[zd])
                    else:
                        kb.dma("sp", self.xres[t * 128:(t + 1) * 128, :], z, r=[zd, xd])
                        self.transpose_to_XT(z, zd, t, tr)


WEIGHT_KEYS = ("w_in", "w_out", "rwkv_mu", "rwkv_w0", "rwkv_w_up", "rwkv_a0", "rwkv_a_up", "rwkv_g_up",
               "rwkv_k_k", "rwkv_k_a", "rwkv_r_k", "rwkv_ln_g", "rwkv_ln_b", "ret_gn_g", "ret_gn_b",
               "ln_g", "ln_b", "ffn_w_gate", "ffn_w_up", "ffn_w_down", "moe_router", "moe_w_gate",
               "moe_w_up", "moe_w_down")


def make_in_map(inputs, b, consts, shared=None):
    m = {}
    m["x"] = np.ascontiguousarray(np.asarray(inputs["x"])[b], dtype=np.float32)
    if shared is None:
        shared = make_shared(inputs, consts)
    m.update(shared)
    return m


def make_shared(inputs, consts):
    sh = {}
    for k in WEIGHT_KEYS:
        sh[k] = np.ascontiguousarray(np.asarray(inputs[k]), dtype=np.float32)
    rb = np.asarray(inputs["rel_bias"], dtype=np.float32)
    idx = bias_bucket_index()
    g = rb[idx]
    sh["biasg"] = np.ascontiguousarray(np.transpose(g, (0, 4, 1, 2, 3)))
    sh["cst"] = consts["cst"]
    sh["cos2"] = consts["cos2"]
    sh["sin2"] = consts["sin2"]
    sh["amask"] = consts["amask"]
    return sh


_PROG = None


def kernel(**inputs):
    global _PROG
    if _PROG is None:
        p = Prog()
        p.build()
        _PROG = p
    p = _PROG
    shared = make_shared(inputs, p.consts)
    in_maps = [make_in_map(inputs, b, p.consts, shared) for b in range(8)]
    res = run_bass_kernel_spmd(p.nc, in_maps, core_ids=list(range(8)))
    out = np.stack([np.asarray(r["out"], dtype=np.float32) for r in res.results], 0)
    return out
```

```python
import math
from contextlib import ExitStack
import numpy as np
import concourse.bass as bass
import concourse.mybir as mybir
from concourse.bass_utils import run_bass_kernel_spmd

F32 = mybir.dt.float32
BF16 = mybir.dt.bfloat16
ALU = mybir.AluOpType
AF = mybir.ActivationFunctionType
AX = mybir.AxisListType

D = 1024
S = 4096
DEPTH = 4
NT = S // 128
RWKV_IN = 1024
ATTN_IN = 1536
RET_IN = 1024
N_IN = 3584
FF_DENSE = 2816
FF_EXPERT = 3584
N_EXPERTS = 8
ALPHA = (2 * DEPTH) ** 0.25
LN_EPS = 1e-5
RWKV_GN_EPS = 64e-5
C0 = math.exp(-0.5)
PATTERNS = ((128, 1), (512, 4), (2048, 16))
NEG = -240000.0


class Dep:
    __slots__ = ("w", "r", "excl")

    def __init__(self, excl=False):
        self.w = None
        self.r = {}
        self.excl = excl


class Engine:
    def __init__(self, e, sem, idx):
        self.e = e
        self.sem = sem
        self.idx = idx
        self.cnt = 0
        self.seen = {}
        self.slots = []
        self.si = 0


class KB:
    NSLOT = 12

    def __init__(self, nc, es):
        self.nc = nc
        self.sems = []
        self.eng = {}
        for name, e in (("pe", nc.tensor), ("dve", nc.vector), ("act", nc.scalar),
                        ("pool", nc.gpsimd), ("sp", nc.sync)):
            sem = es.enter_context(nc.semaphore("s_" + name))
            self.sems.append(sem)
            self.eng[name] = Engine(e, sem, len(self.sems) - 1)
        for q in ("sp", "pool"):
            for i in range(self.NSLOT):
                sem = es.enter_context(nc.semaphore("d_%s%d" % (q, i)))
                self.sems.append(sem)
                self.eng[q].slots.append([len(self.sems) - 1, 0])
        self.nins = 0

    def _need(self, E, ev, raw=True):
        si, val = ev
        if val <= 0:
            return
        if si == E.idx and val > E.cnt:
            return
        if E.seen.get(si, 0) < val:
            E.e.wait_ge(self.sems[si], val)
            E.seen[si] = val

    def _waits(self, E, r, w):
        for d in r:
            if d.excl:
                if d.w is not None:
                    self._need(E, d.w)
                for si, v in d.r.items():
                    self._need(E, (si, v), raw=False)
            elif d.w is not None:
                self._need(E, d.w)
        for d in w:
            if d.w is not None:
                self._need(E, d.w, raw=False)
            for si, v in d.r.items():
                self._need(E, (si, v), raw=False)

    def _post(self, ev, r, w):
        for d in w:
            d.w = ev
            d.r = {}
        for d in r:
            if d.excl:
                d.w = ev
                d.r = {}
            elif d.r.get(ev[0], 0) < ev[1]:
                d.r[ev[0]] = ev[1]

    def op(self, eng, fn, r=(), w=(), mark=True):
        E = self.eng[eng]
        self._waits(E, r, w)
        ins = fn(E.e)
        self.nins += 1
        if mark:
            E.cnt += 1
            ins.then_inc(E.sem, 1)
            ev = (E.idx, E.cnt)
        else:
            ev = (E.idx, E.cnt + 1)
        self._post(ev, r, w)
        return ins

    def dma(self, q, out, in_, r=(), w=(), slow=False):
        E = self.eng[q]
        slot = E.slots[E.si]
        E.si = (E.si + 1) % len(E.slots)
        self._need(E, (slot[0], slot[1]))
        self._waits(E, r, w)
        if slow:
            ins = E.e.dma_start(out=out, in_=in_, allow_slow_non_contiguous=True)
        else:
            ins = E.e.dma_start(out=out, in_=in_)
        self.nins += 1
        slot[1] += 16
        ins.then_inc(self.sems[slot[0]], 16)
        ev = (slot[0], slot[1])
        self._post(ev, r, w)
        return ins

    def barrier(self):
        evs = []
        for E in self.eng.values():
            if E.cnt > 0:
                evs.append((E.idx, E.cnt))
            for s in E.slots:
                if s[1] > 0:
                    evs.append((s[0], s[1]))
        for E in self.eng.values():
            for ev in evs:
                if ev[0] != E.idx:
                    self._need(E, ev)


class Ring:
    def __init__(self, tile, n):
        self.t = tile
        self.n = n
        self.d = [Dep() for _ in range(n)]
        self.i = 0

    def next(self):
        i = self.i
        self.i = (i + 1) % self.n
        return self.t[:, i], self.d[i]


class BRing:
    def __init__(self, items):
        self.items = list(items)
        self.i = 0

    def next(self):
        it = self.items[self.i]
        self.i = (self.i + 1) % len(self.items)
        return it


def run_interleaved(gens):
    gens = list(gens)
    while gens:
        nxt = []
        for g in gens:
            try:
                next(g)
                nxt.append(g)
            except StopIteration:
                pass
        gens = nxt


def t5_bucket(dist):
    nb = 32
    max_exact = nb // 2
    large = max_exact + (np.log(np.maximum(dist, max_exact) / max_exact)
                         / math.log(2048 / max_exact) * (nb - max_exact)).astype(np.int32)
    return np.where(dist < max_exact, dist, np.minimum(large, nb - 1)).astype(np.int32)


CST = {}


def _cst_layout():
    off = 0
    for name, n in (("ident", 128), ("onesblk", 128), ("nSU", 128), ("nSL", 128), ("SU", 128),
                    ("nIU", 128), ("IU", 128), ("sel", 64), ("sel65", 64), ("dmaskT", 512),
                    ("zeta8", 4), ("xi", 256), ("scanm", 1024), ("m4", 512)):
        CST[name] = (off, n)
        off += n
    return off


CST_N = _cst_layout()


def make_consts():
    c = np.zeros((128, CST_N), np.float32)

    def put(name, arr):
        o, n = CST[name]
        c[:arr.shape[0], o:o + n] = arr.reshape(arr.shape[0], n)

    i = np.arange(128)
    put("ident", np.eye(128, dtype=np.float32))
    put("onesblk", (i[:, None] // 64 == i[None, :] // 64).astype(np.float32))
    su = (i[None, :] > i[:, None]).astype(np.float32)
    sl = (i[None, :] < i[:, None]).astype(np.float32)
    iu = (i[None, :] >= i[:, None]).astype(np.float32)
    put("nSU", -su)
    put("nSL", -sl)
    put("SU", su)
    put("nIU", -iu)
    put("IU", iu)
    put("m4", np.concatenate([su, sl, su, iu], 1))
    put("sel", np.concatenate([np.eye(64), np.eye(64)], 0).astype(np.float32))
    s65 = np.zeros((128, 64), np.float32)
    s65[64, :] = 1.0
    put("sel65", s65)
    lg = np.log1p(-np.exp2(-5.0 - np.arange(4, dtype=np.float64)))
    m = np.arange(128)[:, None, None]
    n = np.arange(128)[None, None, :]
    h = lg[None, :, None]
    dm = np.where(n >= m, np.exp(h * np.maximum(n - m, 0)), 0.0) / 8.0
    put("dmaskT", dm.astype(np.float32))
    put("zeta8", (np.exp(lg[None, :] * (127 - np.arange(128)[:, None])) / 8.0).astype(np.float32))
    xi = np.zeros((128, 2, 128))
    for hp in range(2):
        for hh in range(2):
            xi[hh * 64:(hh + 1) * 64, hp, :] = np.exp(lg[2 * hp + hh] * (np.arange(128) + 1))[None, :]
    put("xi", xi.astype(np.float32))
    sm = np.ones((128, 1024), np.float32)
    sm[:, 0::64] = 0.0
    put("scanm", sm)
    half = 32
    inv = (10000.0 ** (-np.arange(half, dtype=np.float32) / half)).astype(np.float32)
    ang = (np.arange(S, dtype=np.float32)[None, :] * inv[:, None]).astype(np.float32)
    cos = np.cos(ang).astype(np.float32)
    sin = np.sin(ang).astype(np.float32)
    cos2 = np.concatenate([cos, cos, cos, cos], 0)
    sin2 = np.concatenate([-sin, sin, -sin, sin], 0)
    jj = np.arange(128)[:, None]
    ii = np.arange(128)[None, :]
    mk = np.zeros((2, 128, 128), np.float32)
    mk[0] = np.where(ii <= jj, 0.0, NEG)
    mk[1] = np.where(ii >= jj, 0.0, NEG)
    gamma_c = np.exp(lg * 128.0)
    return dict(cst=c, cos2=np.ascontiguousarray(cos2), sin2=np.ascontiguousarray(sin2), amask=mk,
                gamma_c=[float(g) for g in gamma_c])


def bias_bucket_index():
    jj = np.arange(128)[:, None]
    ii = np.arange(128)[None, :]
    out = np.zeros((3, 2, 128, 128), np.int64)
    for p, (win, dil) in enumerate(PATTERNS):
        rel_prev = np.clip(ii + 128 - jj, 0, None)
        rel_cur = np.clip(ii - jj, 0, None)
        out[p, 0] = t5_bucket(rel_prev * dil)
        out[p, 1] = t5_bucket(rel_cur * dil)
    return out


class Prog:
    def __init__(self, n_layers=DEPTH, dbg=False, stop=None):
        self.n_layers = n_layers
        self.dbg = dbg
        self.stop = stop
        self.consts = make_consts()
        self.nc = nc = bass.Bass("TRN2", target_bir_lowering=False)

        def din(name, shape, dt=F32):
            return nc.dram_tensor(name, list(shape), dt, kind="ExternalInput").ap()

        self.I = I = {}
        I["x"] = din("x", [S, D])
        I["w_in"] = din("w_in", [DEPTH, D, N_IN])
        I["w_out"] = din("w_out", [DEPTH, D, D])
        I["rwkv_mu"] = din("rwkv_mu", [DEPTH, RWKV_IN])
        for nm in ("rwkv_w0", "rwkv_a0", "rwkv_k_k", "rwkv_k_a", "rwkv_r_k", "rwkv_ln_g", "rwkv_ln_b",
                   "ret_gn_g", "ret_gn_b"):
            I[nm] = din(nm, [DEPTH, 256])
        I["rwkv_w_up"] = din("rwkv_w_up", [DEPTH, 64, 256])
        I["rwkv_a_up"] = din("rwkv_a_up", [DEPTH, 64, 256])
        I["rwkv_g_up"] = din("rwkv_g_up", [DEPTH, 128, 256])
        I["ln_g"] = din("ln_g", [DEPTH, 2, D])
        I["ln_b"] = din("ln_b", [DEPTH, 2, D])
        I["ffn_w_gate"] = din("ffn_w_gate", [2, D, FF_DENSE])
        I["ffn_w_up"] = din("ffn_w_up", [2, D, FF_DENSE])
        I["ffn_w_down"] = din("ffn_w_down", [2, FF_DENSE, D])
        I["moe_router"] = din("moe_router", [2, D, N_EXPERTS])
        I["moe_w_gate"] = din("moe_w_gate", [2, N_EXPERTS, D, FF_EXPERT])
        I["moe_w_up"] = din("moe_w_up", [2, N_EXPERTS, D, FF_EXPERT])
        I["moe_w_down"] = din("moe_w_down", [2, N_EXPERTS, FF_EXPERT, D])
        I["cst"] = din("cst", [128, CST_N])
        I["cos2"] = din("cos2", [128, S])
        I["sin2"] = din("sin2", [128, S])
        I["amask"] = din("amask", [2, 128, 128])
        I["biasg"] = din("biasg", [3, 8, 2, 128, 128])
        self.out = nc.dram_tensor("out", [S, D], F32, kind="ExternalOutput").ap()

        def scr(name, shape, dt):
            kind = "ExternalOutput" if dbg else "Internal"
            return nc.dram_tensor(name, list(shape), dt, kind=kind).ap()

        self.xres = scr("xres", [S, D], F32)
        self.pT = scr("pT", [N_IN, S], F32)
        self.vtok = scr("vtok", [S, 768], BF16)
        self.yT = scr("yT", [D, S], BF16)

    _uid = 0

    def un(self, n):
        self._uid += 1
        return "%s_%d" % (n, self._uid)

    def cst(self, name, rows=128):
        o, n = CST[name]
        return self.cstt[0:rows, o:o + n]

    def build(self):
        nc = self.nc
        with ExitStack() as es:
            self.kb = kb = KB(nc, es)
            self.arena = es.enter_context(nc.sbuf_tensor("arena", [128, 16384], F32))
            self.XT = self.arena[:, :].bitcast(BF16).rearrange("p (k s) -> p k s", k=8)
            self.XTd = [Dep() for _ in range(8)]
            self.pbank_t = es.enter_context(nc.psum_tensor("psum", [128, 8, 512], F32))
            self.bank = [(self.pbank_t[:, i], Dep(excl=True)) for i in range(8)]
            self.cstt = es.enter_context(nc.sbuf_tensor("cstt", [128, CST_N], F32))
            self.cstd = Dep()
            self.identb = es.enter_context(nc.sbuf_tensor("identb", [128, 128], BF16))
            self.gates = es.enter_context(nc.sbuf_tensor("gates", [128, NT, N_EXPERTS], F32))
            self.gatesd = Dep()
            kb.dma("sp", self.cstt[:], self.I["cst"], w=[self.cstd])
            kb.op("dve", lambda e: e.tensor_copy(out=self.identb[:], in_=self.cst("ident")),
                  r=[self.cstd], w=[self.cstd])
            self.load_x0()
            kb.barrier()
            for l in range(self.n_layers):
                self.layer(l)
                if self.stop is not None and self.stop[0] == l and self.stop[1] == "layer":
                    break
            kb.barrier()
        return nc

    def setup_bias(self):
        nc, kb = self.nc, self.kb
        with ExitStack() as es:
            self.bias8d = Dep()
            tg = es.enter_context(nc.sbuf_tensor(self.un("tg"), [128, 2, 16, 128], F32))
            mk = es.enter_context(nc.sbuf_tensor(self.un("mk"), [128, 2, 128], F32))
            mkd = Dep()
            kb.dma("sp", mk[:], self.I["amask"].rearrange("a j i -> j a i"), w=[mkd])
            ring = Ring(tg, 2)
            for p in range(3):
                t, d = ring.next()
                kb.dma("sp", t.rearrange("j (h a) i -> j h a i", a=2),
                       self.I["biasg"][p].rearrange("h a j i -> j h a i"), w=[d])
                for h in range(8):
                    kb.op("dve", lambda e, t=t, h=h, p=p: e.scalar_tensor_tensor(
                        out=self.bias8[:, p, h], in0=t[:, 2 * h:2 * h + 2, :], scalar=8.0, in1=mk[:],
                        op0=ALU.mult, op1=ALU.add), r=[d, mkd], w=[self.bias8d])
            kb.barrier()

    def load_x0(self):
        nc, kb = self.nc, self.kb
        with ExitStack() as es:
            xt = es.enter_context(nc.sbuf_tensor(self.un("x0t"), [128, 2, D], F32))
            xr = Ring(xt, 2)
            pr = self.tr_ring()
            for t in range(NT):
                x, xd = xr.next()
                kb.dma("sp", x, self.I["x"][t * 128:(t + 1) * 128, :], w=[xd])
                kb.dma("sp", self.xres[t * 128:(t + 1) * 128, :], x, r=[xd])
                self.transpose_to_XT(x, xd, t, pr)
            kb.barrier()

    def tr_ring(self, pairs=((0, 1), (2, 3))):
        items = []
        for a, b in pairs:
            ap = self.pbank_t[:, a:b + 1].rearrange("p a (b c) -> p (a b) c", c=128)
            items.append((ap, [self.bank[a][1], self.bank[b][1]]))
        return BRing(items)

    def transpose_to_XT(self, x, xd, t, pr, f32_out=None):
        kb = self.kb
        p, pds = pr.next()
        for k in range(8):
            kb.op("pe", lambda e, k=k: e.transpose(p[:, k, :], x[:, k * 128:(k + 1) * 128], self.cst("ident")),
                  r=[xd, self.cstd], w=pds, mark=(k == 7))
        xd_blk = self.XTd[t // 4]
        kb.op("act", lambda e: e.copy(out=self.XT[:, :, t * 128:(t + 1) * 128], in_=p), r=pds, w=[xd_blk])
        if f32_out is not None:
            o, od = f32_out
            kb.op("dve", lambda e: e.tensor_copy(out=o, in_=p), r=pds, w=[od])

    def layer(self, l):
        kb = self.kb
        self.inproj(l)
        kb.barrier()
        if self.stop == (l, "inproj"):
            return
        self.retention(l)
        kb.barrier()
        if self.stop == (l, "ret"):
            return
        self.attention(l)
        kb.barrier()
        if self.stop == (l, "attn"):
            return
        self.rwkv(l)
        kb.barrier()
        if self.stop == (l, "rwkv"):
            return
        self.outproj_ln1(l)
        kb.barrier()
        if self.stop == (l, "ln1"):
            return
        self.ffn(l)
        kb.barrier()

    def inproj(self, l):
        nc, kb = self.nc, self.kb
        w_in = self.I["w_in"][l]
        with ExitStack() as es:
            wt = es.enter_context(nc.sbuf_tensor(self.un("ip_w"), [128, 2, 8, 512], BF16))
            stg = es.enter_context(nc.sbuf_tensor(self.un("ip_s"), [128, 2, S], F32))
            wv = es.enter_context(nc.sbuf_tensor(self.un("ip_wv"), [128, 8, 768], BF16))
            vt = es.enter_context(nc.sbuf_tensor(self.un("ip_vt"), [128, 2, 768], BF16))
            wr, pr, vr = Ring(wt, 2), BRing(self.bank), Ring(vt, 2)
            sdeps = [[Dep() for _ in range(8)] for _ in range(2)]
            wvd = Dep()
            kb.dma("pool", wv[:, :, 0:512], w_in[:, 2048:2560].rearrange("(k p) n -> p k n", p=128), w=[wvd])
            kb.dma("pool", wv[:, :, 512:768], w_in[:, 3072:3328].rearrange("(k p) n -> p k n", p=128), w=[wvd])
            n_ev = 0
            si = 0
            for cb in range(7):
                w, wd = wr.next()
                kb.dma("pool", w, w_in[:, cb * 512:(cb + 1) * 512].rearrange("(k p) n -> p k n", p=128), w=[wd])
                for fc in range(4):
                    st = stg[:, si]
                    sd = sdeps[si]
                    si ^= 1
                    for tb in range(8):
                        p, pd = pr.next()
                        for k in range(8):
                            kb.op("pe", lambda e, k=k: e.matmul(
                                p, lhsT=w[:, k, fc * 128:(fc + 1) * 128], rhs=self.XT[:, k, tb * 512:(tb + 1) * 512],
                                start=(k == 0), stop=(k == 7)), r=[wd, self.XTd[tb]], w=[pd], mark=(k == 7))
                        o = st[:, tb * 512:(tb + 1) * 512]
                        if n_ev % 2 == 0:
                            kb.op("act", lambda e: e.copy(out=o, in_=p), r=[pd], w=[sd[tb]])
                        else:
                            kb.op("dve", lambda e: e.tensor_copy(out=o, in_=p), r=[pd], w=[sd[tb]])
                        n_ev += 1
                    row = (cb * 4 + fc) * 128
                    kb.dma("sp", self.pT[row:row + 128, :], st, r=sd)
            for t in range(NT):
                v, vd = vr.next()
                p1, pd1 = pr.next()
                p2, pd2 = pr.next()
                for k in range(8):
                    kb.op("pe", lambda e, k=k: e.matmul(p1, lhsT=self.XT[:, k, t * 128:(t + 1) * 128], rhs=wv[:, k, 0:512],
                                                        start=(k == 0), stop=(k == 7)),
                          r=[wvd, self.XTd[t // 4]], w=[pd1], mark=(k == 7))
                for k in range(8):
                    kb.op("pe", lambda e, k=k: e.matmul(p2[:, 0:256], lhsT=self.XT[:, k, t * 128:(t + 1) * 128],
                                                        rhs=wv[:, k, 512:768], start=(k == 0), stop=(k == 7)),
                          r=[wvd, self.XTd[t // 4]], w=[pd2], mark=(k == 7))
                kb.op("act", lambda e: e.copy(out=v[:, 0:512], in_=p1), r=[pd1], w=[vd])
                kb.op("dve", lambda e: e.tensor_copy(out=v[:, 512:768], in_=p2[:, 0:256]), r=[pd2], w=[vd])
                kb.dma("sp", self.vtok[t * 128:(t + 1) * 128, :], v, r=[vd])


    def abf(self, lo, hi, **kw):
        return self.arena[:, lo:hi].bitcast(BF16)

    def retention(self, l):
        nc, kb = self.nc, self.kb
        base = RWKV_IN + ATTN_IN
        pT = self.pT
        gam = self.consts["gamma_c"]
        H0, H1 = slice(0, 64), slice(64, 128)
        with ExitStack() as es:
            sb = lambda n, sh, dt: es.enter_context(nc.sbuf_tensor(self.un(n), sh, dt))
            pp = lambda n, sh, dt: es.enter_context(nc.psum_tensor(n, sh, dt))
            kr = self.abf(0, 2048)
            qm = [self.abf(2048, 4096), self.abf(4096, 6144)]
            qx = [self.abf(6144, 8192), self.abf(8192, 10240)]
            sgb = self.abf(10240, 12288)
            krd, qmd, qxd, sgd = Dep(), Dep(), Dep(), Dep()
            cs_t = sb("rt_cs", [128, 2, 2, 1024], F32)
            tq_t = sb("rt_tq", [128, 2, 1024], F32)
            tw_t = sb("rt_tw", [128, 2, 1024], F32)
            v = sb("rt_v", [128, NT, 256], BF16)
            yo = sb("rt_yo", [128, S], BF16)
            gnp = sb("rt_gnp", [128, 4], F32)
            rst = sb("rt_rst", [128, 64], F32)
            rstb = sb("rt_rstb", [128, 64], BF16)
            vd, yod, gnd, rsd, rsbd = Dep(), Dep(), Dep(), Dep(), Dep()
            kb.dma("sp", v[:], self.vtok[:, 512:768].rearrange("(t p) c -> p t c", p=128), w=[vd])
            kb.dma("sp", gnp[:, 0:2], self.I["ret_gn_g"][l].rearrange("(a p) -> p a", p=128), w=[gnd], slow=True)
            kb.dma("sp", gnp[:, 2:4], self.I["ret_gn_b"][l].rearrange("(a p) -> p a", p=128), w=[gnd], slow=True)
            csr, tqr, twr = Ring(cs_t, 2), Ring(tq_t, 2), Ring(tw_t, 2)
            B = self.bank
            bfv = lambda i: (B[i][0].bitcast(BF16)[:, 0:128], B[i][1])
            ps_s = BRing([(B[0][0][:, 0:256], B[0][1]), (B[1][0][:, 0:256], B[1][1])])
            ps_k = BRing([bfv(2)])
            ps_kv = BRing([(B[3][0][:, 0:128], B[3][1])])
            ps_o = BRing([(B[4][0][:, 0:128], B[4][1]), (B[5][0][:, 0:128], B[5][1])])
            ps_t = BRing([bfv(6)])
            sTm_r = Ring(sb("rt_sTm", [128, 2, 256], BF16), 2)
            kz_r = Ring(sb("rt_kz", [128, 2, 128], BF16), 2)
            osq_r = Ring(sb("rt_osq", [128, 2, 128], F32), 2)
            yn_r = Ring(sb("rt_yn", [128, 2, 128], BF16), 2)
            st_r = Ring(sb("rt_st", [128, 2, 16], F32), 2)
            tf_r = Ring(sb("rt_tf", [128, 2, 128], F32), 2)
            dmask = self.cst("dmaskT")
            zeta = self.cst("zeta8")
            xi = self.cst("xi").rearrange("p (a n) -> p a n", a=2)
            for hp in range(2):
                kb.op("pool", lambda e: e.memset(qm[0][H1, :], 0.0), w=[qmd])
                kb.op("pool", lambda e: e.memset(qm[1][H0, :], 0.0), w=[qmd])
                kb.op("pool", lambda e: e.memset(qx[0][H1, :], 0.0), w=[qxd])
                kb.op("pool", lambda e: e.memset(qx[1][H0, :], 0.0), w=[qxd])
                kb.op("pool", lambda e: e.memset(rst[:], 0.0), w=[rsd])
                for pc in range(4):
                    sl = slice(pc * 1024, (pc + 1) * 1024)
                    cs, csd = csr.next()
                    kb.dma("sp", cs[:, 0], self.I["cos2"][:, sl], w=[csd])
                    kb.dma("sp", cs[:, 1], self.I["sin2"][:, sl], w=[csd])
                    for which in range(2):
                        r0 = base + which * 256 + hp * 128
                        tq, tqd = tqr.next()
                        tw, twd = twr.next()
                        kb.dma("sp", tq, pT[r0:r0 + 128, sl], w=[tqd])
                        for blk in range(4):
                            src = r0 + (blk ^ 1) * 32
                            kb.dma("sp", tw[blk * 32:(blk + 1) * 32], pT[src:src + 32, sl], w=[twd])
                        kb.op("dve", lambda e: e.tensor_tensor(out=tq, in0=tq, in1=cs[:, 0], op=ALU.mult),
                              r=[csd], w=[tqd])
                        kb.op("pool", lambda e: e.tensor_tensor(out=tw, in0=tw, in1=cs[:, 1], op=ALU.mult),
                              r=[csd], w=[twd])
                        if which == 1:
                            kb.op("dve", lambda e: e.tensor_tensor(out=kr[:, sl], in0=tq, in1=tw, op=ALU.add),
                                  r=[tqd, twd], w=[krd])
                        else:
                            for hh, rows in enumerate((H0, H1)):
                                kb.op("dve", lambda e: e.tensor_tensor(out=qm[hh][rows, sl], in0=tq[rows], in1=tw[rows],
                                                                       op=ALU.add), r=[tqd, twd], w=[qmd])
                    r0 = base + 768 + hp * 128
                    tq, tqd = tqr.next()
                    kb.dma("sp", tq, pT[r0:r0 + 128, sl], w=[tqd])
                    kb.op("act", lambda e: e.activation(out=sgb[:, sl], in_=tq, func=AF.Silu), r=[tqd], w=[sgd])
                for hh, rows in enumerate((H0, H1)):
                    kb.op("pool", lambda e: e.tensor_tensor(
                        out=qx[hh][rows, :].rearrange("p (c n) -> p c n", n=128),
                        in0=qm[hh][rows, :].rearrange("p (c n) -> p c n", n=128),
                        in1=xi[rows, hp:hp + 1, :].broadcast_to([64, NT, 128]), op=ALU.mult),
                        r=[qmd, self.cstd], w=[qxd])
                import os
                CUT = int(os.environ.get("RET_CUT", "99"))
                for c in range((int(os.environ.get("RET_NC", "32")) if CUT > 2 else 0)):
                    cs = slice(c * 128, (c + 1) * 128)
                    p_s, p_sd = ps_s.next()
                    for hh in range(2):
                        kb.op("pe", lambda e: e.matmul(p_s[:, hh * 128:(hh + 1) * 128], lhsT=kr[:, cs], rhs=qm[hh][:, cs],
                                                       start=True, stop=True), r=[krd, qmd], w=[p_sd], mark=(hh == 1))
                    sTm, sTmd = sTm_r.next()
                    kb.op("dve", lambda e: e.tensor_tensor(out=sTm, in0=p_s, in1=dmask[:, hp * 256:(hp + 1) * 256],
                                                           op=ALU.mult), r=[p_sd, self.cstd], w=[sTmd])
                    if CUT <= 3:
                        continue
                    p_k, p_kd = ps_k.next()
                    kb.op("pe", lambda e: e.transpose(p_k, kr[:, cs], self.identb[:]), r=[krd, self.cstd], w=[p_kd])
                    kz, kzd = kz_r.next()
                    for hh in range(2):
                        h = 2 * hp + hh
                        kb.op("act", lambda e: e.mul(out=kz[:, hh * 64:(hh + 1) * 64], in_=p_k[:, hh * 64:(hh + 1) * 64],
                                                     mul=zeta[:, h:h + 1]), r=[p_kd, self.cstd], w=[kzd])
                    if CUT <= 4:
                        continue
                    p_o, p_od = ps_o.next()
                    for hh in range(2):
                        h = 2 * hp + hh
                        kb.op("pe", lambda e: e.matmul(p_o[:, hh * 64:(hh + 1) * 64], lhsT=sTm[:, hh * 128:(hh + 1) * 128],
                                                       rhs=v[:, c, h * 64:(h + 1) * 64], start=True, stop=(c == 0)),
                              r=[sTmd, vd], w=[p_od], mark=(c == 0 and hh == 1))
                        if c > 0:
                            kb.op("pe", lambda e: e.matmul(p_o[:, hh * 64:(hh + 1) * 64], lhsT=qx[hh][:, cs], rhs=rstb[:],
                                                           start=False, stop=True),
                                  r=[qxd, rsbd], w=[p_od], mark=(hh == 1))
                    if CUT <= 5:
                        continue
                    if c < NT - 1:
                        p_kv, p_kvd = ps_kv.next()
                        kb.op("pe", lambda e: e.matmul(p_kv, lhsT=kz, rhs=v[:, c, hp * 128:(hp + 1) * 128],
                                                       start=True, stop=True), r=[kzd, vd], w=[p_kvd])
                        for hh, rows in enumerate((H0, H1)):
                            kb.op("dve", lambda e: e.scalar_tensor_tensor(
                                out=rst[rows, :], in0=rst[rows, :], scalar=gam[2 * hp + hh],
                                in1=p_kv[rows, hh * 64:(hh + 1) * 64], op0=ALU.mult, op1=ALU.add),
                                r=[p_kvd, rsbd], w=[rsd])
                        kb.op("dve", lambda e: e.tensor_copy(out=rstb[:], in_=rst[:]), r=[rsd], w=[rsbd])
                    if CUT <= 6:
                        continue
                    st, std = st_r.next()
                    osq, osqd = osq_r.next()
                    kb.op("act", lambda e: e.activation(out=osq, in_=p_o, func=AF.Square), r=[p_od], w=[osqd])
                    kb.op("dve", lambda e: e.reduce_sum(out=st[:, 0:2], in_=p_o.rearrange("p (h e) -> p h e", e=64),
                                                        axis=AX.X), r=[p_od], w=[std])
                    kb.op("dve", lambda e: e.reduce_sum(out=st[:, 2:4], in_=osq.rearrange("p (h e) -> p h e", e=64),
                                                        axis=AX.X), r=[osqd], w=[std])
                    self.norm_stats(st, std, 2, 64, LN_EPS)
                    if CUT <= 7:
                        continue
                    yn, ynd = yn_r.next()
                    for hh in range(2):
                        kb.op("dve", lambda e: e.tensor_scalar(out=yn[:, hh * 64:(hh + 1) * 64], in0=p_o[:, hh * 64:(hh + 1) * 64],
                                                               scalar1=st[:, 12 + hh:13 + hh], scalar2=st[:, 14 + hh:15 + hh],
                                                               op0=ALU.mult, op1=ALU.add), r=[p_od, std], w=[ynd])
                    if CUT <= 8:
                        continue
                    p_t, p_td = ps_t.next()
                    kb.op("pe", lambda e: e.transpose(p_t, yn, self.identb[:]), r=[ynd, self.cstd], w=[p_td])
                    tf, tfd = tf_r.next()
                    kb.op("dve", lambda e: e.tensor_scalar(out=tf, in0=p_t, scalar1=gnp[:, hp:hp + 1],
                                                           scalar2=gnp[:, 2 + hp:3 + hp], op0=ALU.mult, op1=ALU.add),
                          r=[p_td, gnd], w=[tfd])
                    kb.op("pool", lambda e: e.tensor_tensor(out=yo[:, cs], in0=tf, in1=sgb[:, cs], op=ALU.mult),
                          r=[tfd, sgd], w=[yod])
                kb.dma("sp", self.yT[768 + hp * 128:768 + (hp + 1) * 128, :], yo[:], r=[yod])

    def norm_stats(self, st, std, n, cnt, eps):
        kb = self.kb
        c = lambda i: st[:, i * n:(i + 1) * n]
        kb.op("dve", lambda e: e.tensor_scalar(out=c(2), in0=c(0), scalar1=1.0 / cnt, scalar2=None, op0=ALU.mult),
              r=[std], w=[std])
        kb.op("dve", lambda e: e.tensor_tensor(out=c(3), in0=c(2), in1=c(2), op=ALU.mult), r=[std], w=[std])
        kb.op("dve", lambda e: e.scalar_tensor_tensor(out=c(4), in0=c(1), scalar=1.0 / cnt, in1=c(3),
                                                      op0=ALU.mult, op1=ALU.subtract), r=[std], w=[std])
        kb.op("dve", lambda e: e.tensor_scalar(out=c(4), in0=c(4), scalar1=float(eps), scalar2=None, op0=ALU.add),
              r=[std], w=[std])
        kb.op("act", lambda e: e.activation(out=c(5), in_=c(4), func=AF.Sqrt), r=[std], w=[std])
        kb.op("dve", lambda e: e.reciprocal(out=c(6), in_=c(5)), r=[std], w=[std])
        kb.op("dve", lambda e: e.scalar_tensor_tensor(out=c(7), in0=c(2), scalar=-1.0, in1=c(6),
                                                      op0=ALU.mult, op1=ALU.mult), r=[std], w=[std])

    def attention(self, l):
        nc, kb = self.nc, self.kb
        base = RWKV_IN
        pT = self.pT
        H0, H1 = slice(0, 64), slice(64, 128)
        with ExitStack() as es:
            sb = lambda n, sh, dt: es.enter_context(nc.sbuf_tensor(self.un(n), sh, dt))
            self.bias8 = sb("bias8", [128, 3, 8, 2, 128], BF16)
            self.setup_bias()
            acc = self.arena[0:64, :].rearrange("p (a h s) -> p a h s", a=2, h=2)
            accd = Dep()
            kt = sb("at_k", [128, S], BF16)
            qm = [sb("at_q0", [128, S], BF16), sb("at_q1", [128, S], BF16)]
            vp = [sb("at_v%d" % i, [128, NT, 128], BF16) for i in range(3)]
            ones = sb("at_ones", [128, 64], BF16)
            PT_r = Ring(sb("at_PT", [128, 4, 512], BF16), 4)
            rc = sb("at_rc", [64, 2, S], F32)
            yo = sb("at_yo", [64, 2, S], BF16)
            ktd, qmd, vd, od, rcd, yod = Dep(), Dep(), Dep(), Dep(), Dep(), Dep()
            kb.op("pool", lambda e: e.memset(ones[:], 1.0), w=[od])
            B = self.bank
            ps_s = BRing([B[0], B[1], B[2], B[3]])
            ps_o = BRing([B[4], B[5], B[6], B[7]])
            for pair in range(4):
                kb.op("pool", lambda e: e.memset(qm[0][H1, :], 0.0), w=[qmd])
                kb.op("pool", lambda e: e.memset(qm[1][H0, :], 0.0), w=[qmd])
                kb.op("dve", lambda e: e.memset(self.arena[0:64, :], 0.0), w=[accd])
                q0 = base + pair * 128
                k0 = base + 512 + pair * 128
                kb.dma("pool", qm[0][H0, :], pT[q0:q0 + 64, :], w=[qmd])
                kb.dma("pool", qm[1][H1, :], pT[q0 + 64:q0 + 128, :], w=[qmd])
                kb.dma("pool", kt[:], pT[k0:k0 + 128, :], w=[ktd])
                for p, (win, dil) in enumerate(PATTERNS):
                    nb = NT // dil
                    for r in range(dil):
                        src = self.vtok[:, pair * 128:(pair + 1) * 128].rearrange("(b j r) c -> r j b c", r=dil, j=128)[r]
                        kb.dma("sp", vp[p][:, r * nb:(r + 1) * nb, :], src, w=[vd])
                units = [(p, dil, NT // dil, r, b) for p, (win, dil) in enumerate(PATTERNS)
                         for r in range(dil) for b in range(NT // dil)]

                def tsl(dil, r, bb):
                    st = r + dil * 128 * bb
                    return slice(st, st + dil * 127 + 1, dil)

                def emit_scores(u):
                    p, dil, nb, r, b = u
                    qs = tsl(dil, r, b)
                    halves = (0, 1) if b > 0 else (1,)
                    p_s, p_sd = ps_s.next()
                    for hh in range(2):
                        h = 2 * pair + hh
                        for half in halves:
                            ks = tsl(dil, r, b - 1) if half == 0 else qs
                            o = p_s[:, (hh * 2 + half) * 128:(hh * 2 + half + 1) * 128]
                            kb.op("pe", lambda e: e.matmul(o, lhsT=kt[:, ks], rhs=qm[hh][:, qs], start=True, stop=False),
                                  r=[ktd, qmd], w=[p_sd], mark=False)
                            kb.op("pe", lambda e: e.matmul(o, lhsT=self.identb[:], rhs=self.bias8[:, p, h, half, :],
                                                           start=False, stop=True),
                                  r=[self.cstd, self.bias8d], w=[p_sd], mark=(hh == 1 and half == 1))
                    PT, PTd = PT_r.next()
                    if b > 0:
                        kb.op("act", lambda e: e.activation(out=PT, in_=p_s, func=AF.Exp, scale=0.125),
                              r=[p_sd], w=[PTd])
                    else:
                        v4 = lambda t: t.rearrange("p (h a i) -> p h a i", h=2, a=2)[:, :, 1, :]
                        kb.op("act", lambda e: e.activation(out=v4(PT), in_=v4(p_s), func=AF.Exp, scale=0.125),
                              r=[p_sd], w=[PTd])
                    return PT, PTd

                def emit_pv(u, ctx):
                    p, dil, nb, r, b = u
                    PT, PTd = ctx
                    qs = tsl(dil, r, b)
                    halves = (0, 1) if b > 0 else (1,)
                    p_o, p_od = ps_o.next()
                    for nd in range(2):
                        for hh in range(2):
                            o = p_o[0:64, (nd * 2 + hh) * 128:(nd * 2 + hh + 1) * 128]
                            for hi, half in enumerate(halves):
                                ti = r * nb + (b - 1 if half == 0 else b)
                                lw = vp[p][:, ti, hh * 64:(hh + 1) * 64] if nd == 0 else ones[:]
                                kb.op("pe", lambda e: e.matmul(o, lhsT=lw, rhs=PT[:, (hh * 2 + half) * 128:(hh * 2 + half + 1) * 128],
                                                               start=(hi == 0), stop=(hi == len(halves) - 1)),
                                      r=[PTd, vd, od], w=[p_od], mark=(nd == 1 and hh == 1 and hi == len(halves) - 1))
                    kb.op("dve", lambda e: e.tensor_tensor(
                        out=acc[:, :, :, qs], in0=acc[:, :, :, qs],
                        in1=p_o[0:64, :].rearrange("p (a h i) -> p a h i", a=2, h=2), op=ALU.add),
                        r=[p_od, accd], w=[accd])

                LOOK = 2
                ctxs = {}
                for i in range(min(LOOK, len(units))):
                    ctxs[i] = emit_scores(units[i])
                for i, u in enumerate(units):
                    if i + LOOK < len(units):
                        ctxs[i + LOOK] = emit_scores(units[i + LOOK])
                    emit_pv(u, ctxs.pop(i))
                kb.op("dve", lambda e: e.reciprocal(out=rc[:], in_=acc[:, 1]), r=[accd], w=[rcd])
                kb.op("dve", lambda e: e.tensor_tensor(out=yo[:], in0=acc[:, 0], in1=rc[:], op=ALU.mult),
                      r=[accd, rcd], w=[yod])
                for hh in range(2):
                    r0 = 256 + (2 * pair + hh) * 64
                    kb.dma("sp", self.yT[r0:r0 + 64, :], yo[:, hh, :], r=[yod])

    def rwkv(self, l):
        nc, kb = self.nc, self.kb
        pT = self.pT
        I = self.I
        H0, H1 = slice(0, 64), slice(64, 128)
        SEG = 1024
        NCH = SEG // 64
        with ExitStack() as es:
            sb = lambda n, sh, dt: es.enter_context(nc.sbuf_tensor(self.un(n), sh, dt))
            U = [self.arena[:, i * SEG:(i + 1) * SEG] for i in range(16)]
            Ud = [Dep() for _ in range(16)]
            prm = sb("rw_prm", [128, 32], F32)
            prd = Dep()
            pcol = {}
            kb.dma("sp", prm[:, 0:8], I["rwkv_mu"][l].rearrange("(c p) -> p c", p=128), w=[prd], slow=True)
            for i, nm in enumerate(("rwkv_w0", "rwkv_a0", "rwkv_k_k", "rwkv_k_a", "rwkv_r_k", "rwkv_ln_g", "rwkv_ln_b")):
                pcol[nm] = 8 + 2 * i
                kb.dma("sp", prm[:, 8 + 2 * i:10 + 2 * i], I[nm][l].rearrange("(a p) -> p a", p=128), w=[prd], slow=True)
            pcol["omka"] = 22
            kb.op("dve", lambda e: e.tensor_scalar(out=prm[:, 22:24], in0=prm[:, pcol["rwkv_k_a"]:pcol["rwkv_k_a"] + 2],
                                                   scalar1=-1.0, scalar2=1.0, op0=ALU.mult, op1=ALU.add), r=[prd], w=[prd])
            P = lambda nm, hp: prm[:, pcol[nm] + hp:pcol[nm] + hp + 1]
            wlw = sb("rw_wlw", [128, 256], BF16)
            wla = sb("rw_wla", [128, 256], BF16)
            gup = sb("rw_gup", [128, 256], BF16)
            wd = Dep()
            kb.op("pool", lambda e: e.memset(wlw[H1, :], 0.0), w=[wd])
            kb.op("pool", lambda e: e.memset(wla[H0, :], 0.0), w=[wd])
            kb.dma("pool", wlw[H0, :], I["rwkv_w_up"][l], w=[wd])
            kb.dma("pool", wla[H1, :], I["rwkv_a_up"][l], w=[wd])
            kb.dma("pool", gup[:], I["rwkv_g_up"][l], w=[wd])
            pad = {}
            padd = {}
            for nm in ("Rt", "Kk", "Bt", "Kt", "V"):
                pad[nm] = sb("rw_p" + nm, [128, NCH, 128], F32)
                padd[nm] = Dep()
                kb.op("pool", lambda e, nm=nm: e.memset(pad[nm][:], 0.0), w=[padd[nm]])
            T6 = sb("rw_T6", [128, SEG], BF16)
            SG = sb("rw_SG", [128, SEG], BF16)
            T6d, SGd = Dep(), Dep()
            buf_r = Ring(sb("rw_buf", [128, 2, SEG + 1], F32), 2)
            tmp_r = Ring(sb("rw_tmp", [128, 2, SEG], F32), 2)
            yo_r = Ring(sb("rw_yo", [128, 2, SEG], BF16), 2)
            Hs = [sb("rw_H%d" % i, [128, 128], F32) for i in range(2)]
            Hd = [Dep(), Dep()]
            import os
            NW = int(os.environ.get('RW_NW', '4'))
            wk = []
            for g in range(NW):
                t = {}
                for nm in ("PrkT", "Pa", "Pb", "PT1", "PT2", "PT4",
                           "PT8", "PT16", "PT32", "QT", "GT0", "E", "YnP", "sq"):
                    t[nm] = (sb("rw_%s%d" % (nm, g), [128, 128], F32), Dep())
                tk = sb("rw_TK%d" % g, [128, 640], F32)
                tkd = Dep()
                g4 = sb("rw_G4%d" % g, [128, 512], F32)
                g4d = Dep()
                t["nBtPT"] = (tk[:, 0:128], tkd)
                t["KtPT"] = (tk[:, 128:256], tkd)
                t["VPT"] = (tk[:, 256:384], tkd)
                t["X0"] = (tk[:, 384:640], tkd)
                t["TK"] = (tk, tkd)
                t["NT"] = (g4[:, 0:128], g4d)
                t["N"] = (g4[:, 128:256], g4d)
                t["MakT"] = (g4[:, 256:384], g4d)
                t["nPrbT"] = (g4[:, 384:512], g4d)
                t["G4"] = (g4, g4d)
                for nm in ("Xa", "Xb"):
                    t[nm] = (sb("rw_%s%d" % (nm, g), [128, 256], F32), Dep())
                t["st"] = (sb("rw_st%d" % g, [128, 8], F32), Dep())
                kb.op("pool", lambda e, t=t: e.memset(t["YnP"][0][:], 0.0), w=[t["YnP"][1]])
                wk.append(t)
            banks = BRing(self.bank)
            ident = self.cst("ident")
            cd = self.cstd
            evc = [0]

            def copy_ev(out, in_, r, w, scale=None):
                evc[0] += 1
                if evc[0] % 2 == 0:
                    if scale is None:
                        kb.op("act", lambda e: e.copy(out=out, in_=in_), r=r, w=w)
                    else:
                        kb.op("act", lambda e: e.mul(out=out, in_=in_, mul=float(scale)), r=r, w=w)
                else:
                    if scale is None:
                        kb.op("dve", lambda e: e.tensor_copy(out=out, in_=in_), r=r, w=w)
                    else:
                        kb.op("dve", lambda e: e.tensor_scalar(out=out, in0=in_, scalar1=float(scale), scalar2=None,
                                                               op0=ALU.mult), r=r, w=w)

            def load_shift(row0, seg, c8, dst, dstd):
                buf, bd = buf_r.next()
                tmp, td = tmp_r.next()
                t0 = seg * SEG
                if seg == 0:
                    kb.op("pool", lambda e: e.memset(buf[:, 0:1], 0.0), w=[bd])
                    kb.dma("sp", buf[:, 1:SEG + 1], pT[row0:row0 + 128, 0:SEG], w=[bd])
                else:
                    kb.dma("sp", buf[:, 0:SEG + 1], pT[row0:row0 + 128, t0 - 1:t0 + SEG], w=[bd])
                kb.op("pool", lambda e: e.tensor_tensor(out=tmp, in0=buf[:, 0:SEG], in1=buf[:, 1:SEG + 1], op=ALU.subtract),
                      r=[bd], w=[td])
                kb.op("dve", lambda e: e.scalar_tensor_tensor(out=dst, in0=tmp, scalar=prm[:, c8:c8 + 1], in1=buf[:, 1:SEG + 1],
                                                              op0=ALU.mult, op1=ALU.add), r=[td, bd, prd], w=[dstd])

            def mm_to(lhsT, rhs, rd, n=128):
                bk, bd = banks.next()
                o = bk[:, 0:n]
                kb.op("pe", lambda e: e.matmul(o, lhsT=lhsT, rhs=rhs, start=True, stop=True), r=rd, w=[bd])
                return o, bd

            def chunk_gen(hp, seg, c, g):
                t = wk[g]
                gc = seg * NCH + c
                A = lambda nm: t[nm][0] if nm in ("nBtPT", "KtPT", "VPT", "X0", "NT", "N", "MakT", "nPrbT") else t[nm][0][:]
                Dp = lambda nm: t[nm][1]
                Rt, Kk, Bt, Kt, V = (pad[nm][:, c, :] for nm in ("Rt", "Kk", "Bt", "Kt", "V"))
                dR, dKk, dB, dK, dV = (padd[nm] for nm in ("Rt", "Kk", "Bt", "Kt", "V"))
                X = [t["X0"], t["Xa"], t["Xb"]]
                bk, bd = banks.next()
                for i, (src, sd) in enumerate(((Bt, dB), (Kt, dK), (V, dV), (Kk, dKk))):
                    kb.op("pe", lambda e: e.transpose(bk[:, i * 128:(i + 1) * 128], src, ident), r=[sd, cd], w=[bd],
                          mark=(i == 3))
                copy_ev(t["TK"][0][:, 0:512], bk[:, 0:512], [bd], [t["TK"][1]])
                yield
                bk, bd = banks.next()
                for i, (lh, ld, rh, rdd) in enumerate(((Bt, dB, Kk, dKk), (Kk, dKk, Bt, dB), (Kt, dK, Kk, dKk), (Bt, dB, Rt, dR))):
                    kb.op("pe", lambda e: e.matmul(bk[:, i * 128:(i + 1) * 128], lhsT=lh, rhs=rh, start=True, stop=True),
                          r=[ld, rdd], w=[bd], mark=(i == 3))
                kb.op("dve", lambda e: e.tensor_tensor(out=t["G4"][0][:], in0=bk[:, 0:512], in1=self.cst("m4"), op=ALU.mult),
                      r=[bd, cd], w=[t["G4"][1]])
                o, bd = mm_to(Kt, Rt, [dK, dR])
                kb.op("dve", lambda e: e.tensor_tensor(out=A("PrkT"), in0=o, in1=self.cst("IU"), op=ALU.mult),
                      r=[bd, cd], w=[Dp("PrkT")])
                yield
                o, bd = mm_to(A("MakT"), A("VPT"), [Dp("MakT"), Dp("VPT")])
                copy_ev(t["TK"][0][:, 512:640], o, [bd], [t["TK"][1]])
                yield
                Pc, PTc = ("N", "NT")
                lv_names = {2: "PT2", 4: "PT4", 8: "PT8", 16: "PT16", 32: "PT32"}
                pt_list = ["NT"]
                pbuf = ["Pa", "Pb"]
                pi = 0
                for lvl in (2, 4, 8, 16, 32):
                    o2, bd2 = mm_to(A(Pc), A(PTc), [Dp(Pc), Dp(PTc)])
                    if lvl < 32:
                        o1, bd1 = mm_to(A(PTc), A(Pc), [Dp(Pc), Dp(PTc)])
                    copy_ev(A(lv_names[lvl]), o2, [bd2], [Dp(lv_names[lvl])])
                    if lvl < 32:
                        copy_ev(A(pbuf[pi]), o1, [bd1], [Dp(pbuf[pi])])
                        Pc = pbuf[pi]
                        pi ^= 1
                    PTc = lv_names[lvl]
                    pt_list.append(PTc)
                    yield
                Xc = X[0]
                nxt = 1
                for nm in reversed(pt_list):
                    Xn = X[nxt]
                    o, bd = mm_to(A(nm), Xc[0], [Dp(nm), Xc[1]], n=256)
                    kb.op("dve", lambda e: e.tensor_tensor(out=Xn[0][:, :], in0=o, in1=Xc[0], op=ALU.add),
                          r=[bd, Xc[1]], w=[Xn[1]])
                    Xc = (Xn[0][:, :], Xn[1])
                    nxt = 2 if nxt == 1 else 1
                    yield
                W, Wd = Xc
                W1, W2 = W[:, 0:128], W[:, 128:256]
                o, bd = mm_to(W1, A("nPrbT"), [Wd, Dp("nPrbT")])
                kb.op("dve", lambda e: e.tensor_tensor(out=A("QT"), in0=o, in1=Rt, op=ALU.add), r=[bd, dR], w=[Dp("QT")])
                o, bd = mm_to(W1, A("nBtPT"), [Wd, Dp("nBtPT")])
                kb.op("dve", lambda e: e.tensor_tensor(out=A("GT0"), in0=o, in1=ident, op=ALU.add), r=[bd, cd], w=[Dp("GT0")])
                bk, bd = banks.next()
                o = bk[:, 0:128]
                kb.op("pe", lambda e: e.matmul(o, lhsT=A("KtPT"), rhs=A("VPT"), start=True, stop=False),
                      r=[Dp("KtPT"), Dp("VPT")], w=[bd], mark=False)
                kb.op("pe", lambda e: e.matmul(o, lhsT=A("nBtPT"), rhs=W2, start=False, stop=True),
                      r=[Dp("nBtPT"), Wd], w=[bd])
                dc = U[12][:, c * 64 + 63:c * 64 + 64]
                kb.op("dve", lambda e: e.tensor_scalar(out=A("E"), in0=o, scalar1=dc, scalar2=None, op0=ALU.mult),
                      r=[bd, Ud[12]], w=[Dp("E")])
                yield
                Hc, Hcd = Hs[gc % 2], Hd[gc % 2]
                Hn, Hnd = Hs[(gc + 1) % 2], Hd[(gc + 1) % 2]
                bk, ybd = banks.next()
                yps = bk[:, 0:128]
                kb.op("pe", lambda e: e.matmul(yps, lhsT=A("QT"), rhs=Hc[:], start=True, stop=False),
                      r=[Dp("QT"), Hcd], w=[ybd], mark=False)
                kb.op("pe", lambda e: e.matmul(yps, lhsT=A("PrkT"), rhs=A("VPT"), start=False, stop=False),
                      r=[Dp("PrkT"), Dp("VPT")], w=[ybd], mark=False)
                kb.op("pe", lambda e: e.matmul(yps, lhsT=A("nPrbT"), rhs=W2, start=False, stop=True),
                      r=[Dp("nPrbT"), Wd], w=[ybd])
                o, bd = mm_to(A("GT0"), Hc[:], [Dp("GT0"), Hcd])
                kb.op("dve", lambda e: e.scalar_tensor_tensor(out=Hn[:], in0=o, scalar=dc, in1=A("E"), op0=ALU.mult, op1=ALU.add),
                      r=[bd, Dp("E"), Ud[12]], w=[Hnd])
                yield
                st, std = t["st"]
                kb.op("act", lambda e: e.activation(out=A("sq"), in_=yps, func=AF.Square), r=[ybd], w=[Dp("sq")])
                kb.op("dve", lambda e: e.reduce_sum(out=st[:, 0:1], in_=yps, axis=AX.X), r=[ybd], w=[std])
                kb.op("dve", lambda e: e.reduce_sum(out=st[:, 1:2], in_=A("sq"), axis=AX.X), r=[Dp("sq")], w=[std])
                self.norm_stats(st, std, 1, 64, RWKV_GN_EPS)
                for rows in (H0, H1):
                    kb.op("dve", lambda e: e.tensor_scalar(out=t["YnP"][0][rows, rows], in0=yps[rows, rows],
                                                           scalar1=st[rows, 6:7], scalar2=st[rows, 7:8],
                                                           op0=ALU.mult, op1=ALU.add), r=[ybd, std], w=[Dp("YnP")])
                o, bd = mm_to(A("YnP"), self.cst("sel"), [Dp("YnP"), cd], n=64)
                kb.op("dve", lambda e: e.tensor_scalar(out=U[10][:, c * 64:(c + 1) * 64], in0=o,
                                                       scalar1=P("rwkv_ln_g", hp), scalar2=P("rwkv_ln_b", hp),
                                                       op0=ALU.mult, op1=ALU.add), r=[bd, prd], w=[Ud[10]])
                yield

            for hp in range(2):
                for i in range(2):
                    kb.op("pool", lambda e, i=i: e.memset(Hs[i][:], 0.0), w=[Hd[i]])
                for seg in range(S // SEG):
                    blk2 = [slice(0, 512), slice(512, 1024)]
                    load_shift(768, seg, 6, U[8], Ud[8])
                    kb.op("act", lambda e: e.activation(out=T6[H0, :], in_=U[8][H0, :], func=AF.Tanh), r=[Ud[8]], w=[T6d])
                    kb.op("act", lambda e: e.copy(out=T6[H1, :], in_=U[8][H1, :]), r=[Ud[8]], w=[T6d])
                    load_shift(896, seg, 7, U[9], Ud[9])
                    kb.op("act", lambda e: e.activation(out=SG[:], in_=U[9], func=AF.Sigmoid), r=[Ud[9]], w=[SGd])
                    load_shift(hp * 128, seg, hp, U[0], Ud[0])
                    load_shift(256 + hp * 128, seg, 2 + hp, U[1], Ud[1])
                    load_shift(512 + hp * 128, seg, 4 + hp, U[2], Ud[2])
                    hs = slice(hp * 128, (hp + 1) * 128)
                    for b2 in blk2:
                        o, bd = mm_to(wlw[:, hs], T6[:, b2], [wd, T6d], n=512)
                        kb.op("act", lambda e: e.activation(out=U[4][:, b2], in_=o, func=AF.Sigmoid, bias=P("rwkv_w0", hp)),
                              r=[bd, prd], w=[Ud[4]])
                        o, bd = mm_to(wla[:, hs], T6[:, b2], [wd, T6d], n=512)
                        kb.op("act", lambda e: e.activation(out=U[3][:, b2], in_=o, func=AF.Sigmoid, bias=P("rwkv_a0", hp)),
                              r=[bd, prd], w=[Ud[3]])
                        o, bd = mm_to(gup[:, hs], SG[:, b2], [wd, SGd], n=512)
                        kb.op("act", lambda e: e.copy(out=U[5][:, b2], in_=o), r=[bd], w=[Ud[5]])
                    kb.op("dve", lambda e: e.tensor_scalar(out=U[6], in0=U[1], scalar1=P("rwkv_k_k", hp), scalar2=None,
                                                           op0=ALU.mult), r=[Ud[1], prd], w=[Ud[6]])
                    kb.op("pool", lambda e: e.tensor_tensor(out=U[8], in0=U[6], in1=U[6], op=ALU.mult), r=[Ud[6]], w=[Ud[8]])
                    for b2 in blk2:
                        o, bd = mm_to(self.cst("onesblk"), U[8][:, b2], [cd, Ud[8]], n=512)
                        kb.op("dve", lambda e: e.tensor_scalar(out=U[11][:, b2], in0=o, scalar1=1e-24, scalar2=None,
                                                               op0=ALU.max), r=[bd], w=[Ud[11]])
                    kb.op("act", lambda e: e.activation(out=U[11], in_=U[11], func=AF.Sqrt), r=[Ud[11]], w=[Ud[11]])
                    kb.op("dve", lambda e: e.reciprocal(out=U[11], in_=U[11]), r=[Ud[11]], w=[Ud[11]])
                    kb.op("pool", lambda e: e.tensor_tensor(out=U[6], in0=U[6], in1=U[11], op=ALU.mult),
                          r=[Ud[6], Ud[11]], w=[Ud[6]])
                    kb.op("dve", lambda e: e.tensor_scalar(out=U[7], in0=U[3], scalar1=P("rwkv_k_a", hp), scalar2=P("omka", hp),
                                                           op0=ALU.mult, op1=ALU.add), r=[Ud[3], prd], w=[Ud[7]])
                    kb.op("pool", lambda e: e.tensor_tensor(out=U[7], in0=U[7], in1=U[1], op=ALU.mult),
                          r=[Ud[7], Ud[1]], w=[Ud[7]])
                    kb.op("dve", lambda e: e.scalar_tensor_tensor(out=U[8], in0=U[0], scalar=P("rwkv_r_k", hp), in1=U[7],
                                                                  op0=ALU.mult, op1=ALU.mult), r=[Ud[0], Ud[7], prd], w=[Ud[8]])
                    for b2 in blk2:
                        o, bd = mm_to(self.cst("onesblk"), U[8][:, b2], [cd, Ud[8]], n=512)
                        kb.op("dve", lambda e: e.tensor_tensor(out=U[9][:, b2], in0=o, in1=U[2][:, b2], op=ALU.mult),
                              r=[bd, Ud[2]], w=[Ud[9]])
                    kb.op("dve", lambda e: e.scalar_tensor_tensor(out=U[3], in0=U[3], scalar=-1.0, in1=U[6],
                                                                   op0=ALU.mult, op1=ALU.mult), r=[Ud[3], Ud[6]], w=[Ud[3]])
                    kb.op("dve", lambda e: e.tensor_tensor_scan(out=U[11], data0=self.cst("scanm"), data1=U[4], initial=0.0,
                                                                op0=ALU.mult, op1=ALU.add), r=[Ud[4], cd], w=[Ud[11]])
                    kb.op("act", lambda e: e.activation(out=U[12], in_=U[11], func=AF.Exp, scale=-C0), r=[Ud[11]], w=[Ud[12]])
                    kb.op("act", lambda e: e.activation(out=U[13], in_=U[11], func=AF.Exp, scale=C0), r=[Ud[11]], w=[Ud[13]])
                    kb.op("pool", lambda e: e.tensor_tensor(out=U[4], in0=U[11], in1=U[4], op=ALU.subtract),
                          r=[Ud[11], Ud[4]], w=[Ud[4]])
                    kb.op("act", lambda e: e.activation(out=U[4], in_=U[4], func=AF.Exp, scale=-C0), r=[Ud[4]], w=[Ud[4]])
                    kb.op("pool", lambda e: e.tensor_tensor(out=U[0], in0=U[0], in1=U[12], op=ALU.mult), r=[Ud[0], Ud[12]], w=[Ud[0]])
                    kb.op("dve", lambda e: e.tensor_tensor(out=U[6], in0=U[6], in1=U[4], op=ALU.mult), r=[Ud[6], Ud[4]], w=[Ud[6]])
                    kb.op("pool", lambda e: e.tensor_tensor(out=U[3], in0=U[3], in1=U[13], op=ALU.mult), r=[Ud[3], Ud[13]], w=[Ud[3]])
                    kb.op("dve", lambda e: e.tensor_tensor(out=U[7], in0=U[7], in1=U[13], op=ALU.mult), r=[Ud[7], Ud[13]], w=[Ud[7]])
                    ci = 0
                    for nm, ui in (("Rt", 0), ("Kk", 6), ("Bt", 3), ("Kt", 7), ("V", 2)):
                        for rows in (H0, H1):
                            dst = pad[nm][rows, :, rows]
                            src = U[ui][rows, :].rearrange("p (c t) -> p c t", t=64)
                            eng = ("pool", "dve", "act")[ci % 3]
                            ci += 1
                            if eng == "act":
                                kb.op("act", lambda e: e.copy(out=dst, in_=src), r=[Ud[ui]], w=[padd[nm]])
                            else:
                                kb.op(eng, lambda e: e.tensor_copy(out=dst, in_=src), r=[Ud[ui]], w=[padd[nm]])
                    for c0 in range(0, NCH, NW):
                        run_interleaved([chunk_gen(hp, seg, c0 + g, g) for g in range(NW)])
                    yo, yod = yo_r.next()
                    kb.op("pool", lambda e: e.tensor_tensor(out=U[10], in0=U[10], in1=U[9], op=ALU.add),
                          r=[Ud[10], Ud[9]], w=[Ud[10]])
                    kb.op("dve", lambda e: e.tensor_tensor(out=yo, in0=U[10], in1=U[5], op=ALU.mult),
                          r=[Ud[10], Ud[5]], w=[yod])
                    kb.dma("sp", self.yT[hp * 128:(hp + 1) * 128, seg * SEG:(seg + 1) * SEG], yo, r=[yod])

    def layer_norm_tile(self, z, zd, junk, jd, st, std, gb, gbd):
        kb = self.kb
        kb.op("act", lambda e: e.activation(out=junk, in_=z, func=AF.Square), r=[zd], w=[jd])
        kb.op("dve", lambda e: e.reduce_sum(out=st[:, 0:1], in_=z, axis=AX.X), r=[zd], w=[std])
        kb.op("dve", lambda e: e.reduce_sum(out=st[:, 1:2], in_=junk, axis=AX.X), r=[jd], w=[std])
        self.norm_stats(st, std, 1, D, LN_EPS)
        kb.op("dve", lambda e: e.tensor_scalar(out=z, in0=z, scalar1=st[:, 6:7], scalar2=st[:, 7:8],
                                               op0=ALU.mult, op1=ALU.add), r=[zd, std], w=[zd])
        kb.op("pool", lambda e: e.tensor_tensor(out=z, in0=z, in1=gb[:, 0, :], op=ALU.mult), r=[zd, gbd], w=[zd])
        kb.op("pool", lambda e: e.tensor_tensor(out=z, in0=z, in1=gb[:, 1, :], op=ALU.add), r=[zd, gbd], w=[zd])

    def load_ln_params(self, gb, gbd, l, i):
        kb = self.kb
        kb.dma("sp", gb[:, 0, :], self.I["ln_g"][l, i:i + 1, :].broadcast_to([128, D]), w=[gbd])
        kb.dma("sp", gb[:, 1, :], self.I["ln_b"][l, i:i + 1, :].broadcast_to([128, D]), w=[gbd])

    def outproj_ln1(self, l):
        nc, kb = self.nc, self.kb
        moe = (l % 2 == 1)
        j = l // 2
        with ExitStack() as es:
            sb = lambda n, sh, dt: es.enter_context(nc.sbuf_tensor(self.un(n), sh, dt))
            wo = sb("o_w", [128, 8, D], BF16)
            wod = Dep()
            kb.dma("pool", wo[:], self.I["w_out"][l].rearrange("(k p) n -> p k n", p=128), w=[wod])
            yt_r = Ring(sb("o_yt", [128, 2, 8, 512], BF16), 2)
            x_r = Ring(sb("o_x", [128, 2, D], F32), 2)
            z_r = Ring(sb("o_z", [128, 2, D], F32), 2)
            junk = sb("o_junk", [128, D], BF16)
            jd = Dep()
            st_r = Ring(sb("o_st", [128, 2, 8], F32), 2)
            gb = sb("o_gb", [128, 2, D], F32)
            gbd = Dep()
            self.load_ln_params(gb, gbd, l, 0)
            B = self.bank
            hb = BRing([(0, 1), (2, 3)])
            tr = self.tr_ring(((4, 5), (6, 7)))
            if moe:
                rt = sb("o_rt", [128, 8, N_EXPERTS], F32)
                rtd = Dep()
                kb.dma("sp", rt[:], self.I["moe_router"][j].rearrange("(k p) e -> p k e", p=128), w=[rtd], slow=True)
                xTf_r = Ring(sb("o_xTf", [128, 1, 8, 128], F32), 1)
                g_r = Ring(sb("o_g", [128, 2, 64], F32), 2)
            for tb in range(8):
                yt, ytd = yt_r.next()
                kb.dma("sp", yt, self.yT[:, tb * 512:(tb + 1) * 512].rearrange("(k p) t -> p k t", p=128), w=[ytd])
                for tt in range(4):
                    t = tb * 4 + tt
                    x, xd = x_r.next()
                    z, zd = z_r.next()
                    kb.dma("sp", x, self.xres[t * 128:(t + 1) * 128, :], w=[xd])
                    ba, bb_ = hb.next()
                    for half, bi in enumerate((ba, bb_)):
                        bk, bd = B[bi]
                        for k in range(8):
                            kb.op("pe", lambda e, k=k: e.matmul(bk[:, 0:512], lhsT=yt[:, k, tt * 128:(tt + 1) * 128],
                                                                rhs=wo[:, k, half * 512:(half + 1) * 512],
                                                                start=(k == 0), stop=(k == 7)),
                                  r=[ytd, wod], w=[bd], mark=(k == 7))
                        hs = slice(half * 512, (half + 1) * 512)
                        kb.op("dve", lambda e: e.scalar_tensor_tensor(out=z[:, hs], in0=x[:, hs], scalar=float(ALPHA),
                                                                      in1=bk[:, 0:512], op0=ALU.mult, op1=ALU.add),
                              r=[xd, bd], w=[zd])
                    st, std = st_r.next()
                    self.layer_norm_tile(z, zd, junk[:], jd, st, std, gb, gbd)
                    kb.dma("sp", self.xres[t * 128:(t + 1) * 128, :], z, r=[zd, xd])
                    if moe:
                        xTf, xTfd = xTf_r.next()
                        self.transpose_to_XT(z, zd, t, tr, f32_out=(xTf, xTfd))
                        self.router_gates(xTf, xTfd, rt, rtd, t, g_r)
                    else:
                        self.transpose_to_XT(z, zd, t, tr)

    def router_gates(self, xTf, xTfd, rt, rtd, t, g_r):
        kb = self.kb
        bk, bd = self.bank[0]
        lp = bk[:, 0:N_EXPERTS]
        for k in range(8):
            kb.op("pe", lambda e, k=k: e.matmul(lp, lhsT=xTf[:, k, :], rhs=rt[:, k, :], start=(k == 0), stop=(k == 7)),
                  r=[xTfd, rtd], w=[bd], mark=(k == 7))
        g, gd = g_r.next()
        c = lambda i: g[:, i * 8:(i + 1) * 8]
        sc = lambda i: g[:, 56 + i:57 + i]
        op = lambda fn: kb.op("dve", fn, r=[gd], w=[gd])
        kb.op("dve", lambda e: e.tensor_copy(out=c(0), in_=lp), r=[bd], w=[gd])
        op(lambda e: e.reduce_max(out=sc(0), in_=c(0), axis=AX.X))
        op(lambda e: e.tensor_scalar(out=c(1), in0=c(0), scalar1=sc(0), scalar2=None, op0=ALU.is_equal))
        op(lambda e: e.scalar_tensor_tensor(out=c(2), in0=c(1), scalar=-1e30, in1=c(0), op0=ALU.mult, op1=ALU.add))
        op(lambda e: e.reduce_max(out=sc(1), in_=c(2), axis=AX.X))
        op(lambda e: e.tensor_scalar(out=c(3), in0=c(0), scalar1=sc(1), scalar2=None, op0=ALU.is_ge))
        op(lambda e: e.tensor_scalar(out=sc(2), in0=sc(0), scalar1=-1.0, scalar2=None, op0=ALU.mult))
        kb.op("act", lambda e: e.activation(out=c(4), in_=c(0), func=AF.Exp, bias=sc(2), scale=1.0), r=[gd], w=[gd])
        op(lambda e: e.tensor_tensor(out=c(4), in0=c(4), in1=c(3), op=ALU.mult))
        op(lambda e: e.reduce_sum(out=sc(3), in_=c(4), axis=AX.X))
        op(lambda e: e.reciprocal(out=sc(4), in_=sc(3)))
        kb.op("dve", lambda e: e.tensor_scalar(out=self.gates[:, t, :], in0=c(4), scalar1=sc(4), scalar2=None, op0=ALU.mult),
              r=[gd], w=[self.gatesd])

    def ffn(self, l):
        nc, kb = self.nc, self.kb
        moe = (l % 2 == 1)
        j = l // 2
        last = (l == self.n_layers - 1)
        if moe:
            E, FF = N_EXPERTS, FF_EXPERT
            wg_a, wu_a, wd_a = self.I["moe_w_gate"][j], self.I["moe_w_up"][j], self.I["moe_w_down"][j]
        else:
            E, FF = 1, FF_DENSE
            wg_a, wu_a, wd_a = [self.I[k][j:j + 1] for k in ("ffn_w_gate", "ffn_w_up", "ffn_w_down")]
        G = 2
        ngrp = FF // (128 * G)
        HT = 2048
        with ExitStack() as es:
            sb = lambda n, sh, dt: es.enter_context(nc.sbuf_tensor(self.un(n), sh, dt))
            acc = sb("f_acc", [128, 16, D], F32)
            accd = [Dep() for _ in range(16)]
            wg_r = Ring(sb("f_wg", [128, 2, 8, G * 128], BF16), 2)
            wu_r = Ring(sb("f_wu", [128, 2, 8, G * 128], BF16), 2)
            wd_r = Ring(sb("f_wd", [128, 2, G, D], BF16), 2)
            sg_r = Ring(sb("f_sg", [128, 2, 512], F32), 2)
            x_r = Ring(sb("f_x", [128, 1, D], F32), 1)
            junk = sb("f_junk", [128, D], BF16)
            jd = Dep()
            st_r = Ring(sb("f_st", [128, 2, 8], F32), 2)
            gb = sb("f_gb", [128, 2, D], F32)
            gbd = Dep()
            self.load_ln_params(gb, gbd, l, 1)
            B = self.bank
            pg_r = BRing([B[0], B[1]])
            pu_r = BRing([B[2], B[3]])
            po_r = BRing([B[4], B[5], B[6], B[7]])
            tr = self.tr_ring(((6, 7),))
            hT_r = Ring(sb("f_hT2", [128, 2, G, HT], BF16), 2)
            for half in range(S // HT):
                groups = [(e_, grp) for e_ in range(E) for grp in range(ngrp)]
                N = len(groups)
                wgu = {}
                wdw = {}
                hTs = {}

                def load_gu(m):
                    e_, grp = groups[m]
                    ff0 = grp * G * 128
                    wg, wgd = wg_r.next()
                    wu, wud = wu_r.next()
                    kb.dma("pool", wg, wg_a[e_][:, ff0:ff0 + G * 128].rearrange("(k p) n -> p k n", p=128), w=[wgd])
                    kb.dma("pool", wu, wu_a[e_][:, ff0:ff0 + G * 128].rearrange("(k p) n -> p k n", p=128), w=[wud])
                    wgu[m] = (wg, wgd, wu, wud)

                def load_d(m):
                    e_, grp = groups[m]
                    ff0 = grp * G * 128
                    wdn, wdd = wd_r.next()
                    kb.dma("pool", wdn, wd_a[e_][ff0:ff0 + G * 128, :].rearrange("(g p) n -> p g n", p=128), w=[wdd])
                    wdw[m] = (wdn, wdd)

                def gu_units(m):
                    wg, wgd, wu, wud = wgu[m]
                    hT, hTd = hT_r.next()
                    hTs[m] = (hT, hTd)
                    us = []
                    for gi in range(G):
                        for tb in range(HT // 512):
                            def u(gi=gi, tb=tb):
                                tok = slice(half * HT + tb * 512, half * HT + (tb + 1) * 512)
                                xd_blk = self.XTd[(half * HT) // 512 + tb]
                                pg, pgd = pg_r.next()
                                pu, pud = pu_r.next()
                                for k in range(8):
                                    kb.op("pe", lambda e, k=k: e.matmul(pg, lhsT=wg[:, k, gi * 128:(gi + 1) * 128],
                                                                        rhs=self.XT[:, k, tok], start=(k == 0), stop=(k == 7)),
                                          r=[wgd, xd_blk], w=[pgd], mark=(k == 7))
                                for k in range(8):
                                    kb.op("pe", lambda e, k=k: e.matmul(pu, lhsT=wu[:, k, gi * 128:(gi + 1) * 128],
                                                                        rhs=self.XT[:, k, tok], start=(k == 0), stop=(k == 7)),
                                          r=[wud, xd_blk], w=[pud], mark=(k == 7))
                                sg, sgd = sg_r.next()
                                kb.op("act", lambda e: e.activation(out=sg, in_=pg, func=AF.Silu), r=[pgd], w=[sgd])
                                kb.op("dve", lambda e: e.tensor_tensor(out=hT[:, gi, tb * 512:(tb + 1) * 512], in0=pu, in1=sg,
                                                                       op=ALU.mult), r=[pud, sgd], w=[hTd])
                            us.append(u)
                    return us

                def dn_units(m):
                    e_, grp = groups[m]
                    wdn, wdd = wdw[m]
                    hT, hTd = hTs[m]
                    first = (m == 0)
                    us = []
                    for tt in range(HT // 128):
                        for hf in range(2):
                            def u(tt=tt, hf=hf):
                                t = half * (HT // 128) + tt
                                po, pod = po_r.next()
                                for gi in range(G):
                                    kb.op("pe", lambda e, gi=gi: e.matmul(po, lhsT=hT[:, gi, tt * 128:(tt + 1) * 128],
                                                                          rhs=wdn[:, gi, hf * 512:(hf + 1) * 512],
                                                                          start=(gi == 0), stop=(gi == G - 1)),
                                          r=[hTd, wdd], w=[pod], mark=(gi == G - 1))
                                a = acc[:, tt, hf * 512:(hf + 1) * 512]
                                if moe:
                                    gsc = self.gates[:, t, e_:e_ + 1]
                                    if first:
                                        kb.op("dve", lambda e: e.tensor_scalar(out=a, in0=po, scalar1=gsc, scalar2=None,
                                                                               op0=ALU.mult), r=[pod, self.gatesd], w=[accd[tt]])
                                    else:
                                        kb.op("dve", lambda e: e.scalar_tensor_tensor(out=a, in0=po, scalar=gsc, in1=a,
                                                                                      op0=ALU.mult, op1=ALU.add),
                                              r=[pod, self.gatesd, accd[tt]], w=[accd[tt]])
                                else:
                                    if first:
                                        kb.op("act", lambda e: e.copy(out=a, in_=po), r=[pod], w=[accd[tt]])
                                    else:
                                        kb.op("dve", lambda e: e.tensor_tensor(out=a, in0=po, in1=a, op=ALU.add),
                                              r=[pod, accd[tt]], w=[accd[tt]])
                            us.append(u)
                    return us

                load_gu(0)
                load_d(0)
                if N > 1:
                    load_gu(1)
                for u in gu_units(0):
                    u()
                for n in range(N):
                    if n + 1 < N:
                        load_d(n + 1)
                    if n + 2 < N:
                        load_gu(n + 2)
                    dn = dn_units(n)
                    gu = gu_units(n + 1) if n + 1 < N else []
                    per = len(dn) // max(len(gu), 1)
                    di = 0
                    for gi_, u in enumerate(gu):
                        u()
                        for _ in range(per):
                            dn[di]()
                            di += 1
                    while di < len(dn):
                        dn[di]()
                        di += 1
                for tt in range(HT // 128):
                    t = half * (HT // 128) + tt
                    x, xd = x_r.next()
                    kb.dma("sp", x, self.xres[t * 128:(t + 1) * 128, :], w=[xd])
                    z = acc[:, tt, :]
                    zd = accd[tt]
                    kb.op("dve", lambda e: e.scalar_tensor_tensor(out=z, in0=x, scalar=float(ALPHA), in1=z,
                                                                  op0=ALU.mult, op1=ALU.add), r=[xd, zd], w=[zd])
                    st, std = st_r.next()
                    self.layer_norm_tile(z, zd, junk[:], jd, st, std, gb, gbd)
                    if last:
                        kb.dma("sp", self.out[t * 128:(t + 1) * 128, :], z, r=[zd])
                    else:
                        kb.dma("sp", self.xres[t * 128:(t + 1) * 128, :], z, r=[zd, xd])
                        self.transpose_to_XT(z, zd, t, tr)


WEIGHT_KEYS = ("w_in", "w_out", "rwkv_mu", "rwkv_w0", "rwkv_w_up", "rwkv_a0", "rwkv_a_up", "rwkv_g_up",
               "rwkv_k_k", "rwkv_k_a", "rwkv_r_k", "rwkv_ln_g", "rwkv_ln_b", "ret_gn_g", "ret_gn_b",
               "ln_g", "ln_b", "ffn_w_gate", "ffn_w_up", "ffn_w_down", "moe_router", "moe_w_gate",
               "moe_w_up", "moe_w_down")


def make_in_map(inputs, b, consts, shared=None):
    m = {}
    m["x"] = np.ascontiguousarray(np.asarray(inputs["x"])[b], dtype=np.float32)
    if shared is None:
        shared = make_shared(inputs, consts)
    m.update(shared)
    return m


def make_shared(inputs, consts):
    sh = {}
    for k in WEIGHT_KEYS:
        sh[k] = np.ascontiguousarray(np.asarray(inputs[k]), dtype=np.float32)
    rb = np.asarray(inputs["rel_bias"], dtype=np.float32)
    idx = bias_bucket_index()
    g = rb[idx]
    sh["biasg"] = np.ascontiguousarray(np.transpose(g, (0, 4, 1, 2, 3)))
    sh["cst"] = consts["cst"]
    sh["cos2"] = consts["cos2"]
    sh["sin2"] = consts["sin2"]
    sh["amask"] = consts["amask"]
    return sh


_PROG = None


def kernel(**inputs):
    global _PROG
    if _PROG is None:
        p = Prog()
        p.build()
        _PROG = p
    p = _PROG
    shared = make_shared(inputs, p.consts)
    in_maps = [make_in_map(inputs, b, p.consts, shared) for b in range(8)]
    res = run_bass_kernel_spmd(p.nc, in_maps, core_ids=list(range(8)))
    out = np.stack([np.asarray(r["out"], dtype=np.float32) for r in res.results], 0)
    return out
```

```python
import math
from contextlib import ExitStack
import numpy as np
import concourse.bass as bass
import concourse.mybir as mybir
from concourse.bass_utils import run_bass_kernel_spmd

F32 = mybir.dt.float32
BF16 = mybir.dt.bfloat16
ALU = mybir.AluOpType
AF = mybir.ActivationFunctionType
AX = mybir.AxisListType

D = 1024
S = 4096
DEPTH = 4
NT = S // 128
RWKV_IN = 1024
ATTN_IN = 1536
RET_IN = 1024
N_IN = 3584
FF_DENSE = 2816
FF_EXPERT = 3584
N_EXPERTS = 8
ALPHA = (2 * DEPTH) ** 0.25
LN_EPS = 1e-5
RWKV_GN_EPS = 64e-5
C0 = math.exp(-0.5)
PATTERNS = ((128, 1), (512, 4), (2048, 16))
NEG = -240000.0


class Dep:
    __slots__ = ("w", "r", "excl")

    def __init__(self, excl=False):
        self.w = None
        self.r = {}
        self.excl = excl


class Engine:
    def __init__(self, e, sem, idx):
        self.e = e
        self.sem = sem
        self.idx = idx
        self.cnt = 0
        self.seen = {}
        self.slots = []
        self.si = 0


class KB:
    NSLOT = 12

    def __init__(self, nc, es):
        self.nc = nc
        self.sems = []
        self.eng = {}
        for name, e in (("pe", nc.tensor), ("dve", nc.vector), ("act", nc.scalar),
                        ("pool", nc.gpsimd), ("sp", nc.sync)):
            sem = es.enter_context(nc.semaphore("s_" + name))
            self.sems.append(sem)
            self.eng[name] = Engine(e, sem, len(self.sems) - 1)
        for q in ("sp", "pool"):
            for i in range(self.NSLOT):
                sem = es.enter_context(nc.semaphore("d_%s%d" % (q, i)))
                self.sems.append(sem)
                self.eng[q].slots.append([len(self.sems) - 1, 0])
        self.nins = 0

    def _need(self, E, ev, raw=True):
        si, val = ev
        if val <= 0:
            return
        if si == E.idx and val > E.cnt:
            return
        if E.seen.get(si, 0) < val:
            E.e.wait_ge(self.sems[si], val)
            E.seen[si] = val

    def _waits(self, E, r, w):
        for d in r:
            if d.excl:
                if d.w is not None:
                    self._need(E, d.w)
                for si, v in d.r.items():
                    self._need(E, (si, v), raw=False)
            elif d.w is not None:
                self._need(E, d.w)
        for d in w:
            if d.w is not None:
                self._need(E, d.w, raw=False)
            for si, v in d.r.items():
                self._need(E, (si, v), raw=False)

    def _post(self, ev, r, w):
        for d in w:
            d.w = ev
            d.r = {}
        for d in r:
            if d.excl:
                d.w = ev
                d.r = {}
            elif d.r.get(ev[0], 0) < ev[1]:
                d.r[ev[0]] = ev[1]

    def op(self, eng, fn, r=(), w=(), mark=True):
        E = self.eng[eng]
        self._waits(E, r, w)
        ins = fn(E.e)
        self.nins += 1
        if mark:
            E.cnt += 1
            ins.then_inc(E.sem, 1)
            ev = (E.idx, E.cnt)
        else:
            ev = (E.idx, E.cnt + 1)
        self._post(ev, r, w)
        return ins

    def dma(self, q, out, in_, r=(), w=(), slow=False):
        E = self.eng[q]
        slot = E.slots[E.si]
        E.si = (E.si + 1) % len(E.slots)
        self._need(E, (slot[0], slot[1]))
        self._waits(E, r, w)
        if slow:
            ins = E.e.dma_start(out=out, in_=in_, allow_slow_non_contiguous=True)
        else:
            ins = E.e.dma_start(out=out, in_=in_)
        self.nins += 1
        slot[1] += 16
        ins.then_inc(self.sems[slot[0]], 16)
        ev = (slot[0], slot[1])
        self._post(ev, r, w)
        return ins

    def barrier(self):
        evs = []
        for E in self.eng.values():
            if E.cnt > 0:
                evs.append((E.idx, E.cnt))
            for s in E.slots:
                if s[1] > 0:
                    evs.append((s[0], s[1]))
        for E in self.eng.values():
            for ev in evs:
                if ev[0] != E.idx:
                    self._need(E, ev)


class Ring:
    def __init__(self, tile, n):
        self.t = tile
        self.n = n
        self.d = [Dep() for _ in range(n)]
        self.i = 0

    def next(self):
        i = self.i
        self.i = (i + 1) % self.n
        return self.t[:, i], self.d[i]


class BRing:
    def __init__(self, items):
        self.items = list(items)
        self.i = 0

    def next(self):
        it = self.items[self.i]
        self.i = (self.i + 1) % len(self.items)
        return it


def run_interleaved(gens):
    gens = list(gens)
    while gens:
        nxt = []
        for g in gens:
            try:
                next(g)
                nxt.append(g)
            except StopIteration:
                pass
        gens = nxt


def t5_bucket(dist):
    nb = 32
    max_exact = nb // 2
    large = max_exact + (np.log(np.maximum(dist, max_exact) / max_exact)
                         / math.log(2048 / max_exact) * (nb - max_exact)).astype(np.int32)
    return np.where(dist < max_exact, dist, np.minimum(large, nb - 1)).astype(np.int32)


CST = {}


def _cst_layout():
    off = 0
    for name, n in (("ident", 128), ("onesblk", 128), ("nSU", 128), ("nSL", 128), ("SU", 128),
                    ("nIU", 128), ("IU", 128), ("sel", 64), ("sel65", 64), ("dmaskT", 512),
                    ("zeta8", 4), ("xi", 256), ("scanm", 1024)):
        CST[name] = (off, n)
        off += n
    return off


CST_N = _cst_layout()


def make_consts():
    c = np.zeros((128, CST_N), np.float32)

    def put(name, arr):
        o, n = CST[name]
        c[:arr.shape[0], o:o + n] = arr.reshape(arr.shape[0], n)

    i = np.arange(128)
    put("ident", np.eye(128, dtype=np.float32))
    put("onesblk", (i[:, None] // 64 == i[None, :] // 64).astype(np.float32))
    su = (i[None, :] > i[:, None]).astype(np.float32)
    sl = (i[None, :] < i[:, None]).astype(np.float32)
    iu = (i[None, :] >= i[:, None]).astype(np.float32)
    put("nSU", -su)
    put("nSL", -sl)
    put("SU", su)
    put("nIU", -iu)
    put("IU", iu)
    put("sel", np.concatenate([np.eye(64), np.eye(64)], 0).astype(np.float32))
    s65 = np.zeros((128, 64), np.float32)
    s65[64, :] = 1.0
    put("sel65", s65)
    lg = np.log1p(-np.exp2(-5.0 - np.arange(4, dtype=np.float64)))
    m = np.arange(128)[:, None, None]
    n = np.arange(128)[None, None, :]
    h = lg[None, :, None]
    dm = np.where(n >= m, np.exp(h * np.maximum(n - m, 0)), 0.0) / 8.0
    put("dmaskT", dm.astype(np.float32))
    put("zeta8", (np.exp(lg[None, :] * (127 - np.arange(128)[:, None])) / 8.0).astype(np.float32))
    xi = np.zeros((128, 2, 128))
    for hp in range(2):
        for hh in range(2):
            xi[hh * 64:(hh + 1) * 64, hp, :] = np.exp(lg[2 * hp + hh] * (np.arange(128) + 1))[None, :]
    put("xi", xi.astype(np.float32))
    sm = np.ones((128, 1024), np.float32)
    sm[:, 0::64] = 0.0
    put("scanm", sm)
    half = 32
    inv = (10000.0 ** (-np.arange(half, dtype=np.float32) / half)).astype(np.float32)
    ang = (np.arange(S, dtype=np.float32)[None, :] * inv[:, None]).astype(np.float32)
    cos = np.cos(ang).astype(np.float32)
    sin = np.sin(ang).astype(np.float32)
    cos2 = np.concatenate([cos, cos, cos, cos], 0)
    sin2 = np.concatenate([-sin, sin, -sin, sin], 0)
    jj = np.arange(128)[:, None]
    ii = np.arange(128)[None, :]
    mk = np.zeros((2, 128, 128), np.float32)
    mk[0] = np.where(ii <= jj, 0.0, NEG)
    mk[1] = np.where(ii >= jj, 0.0, NEG)
    gamma_c = np.exp(lg * 128.0)
    return dict(cst=c, cos2=np.ascontiguousarray(cos2), sin2=np.ascontiguousarray(sin2), amask=mk,
                gamma_c=[float(g) for g in gamma_c])


def bias_bucket_index():
    jj = np.arange(128)[:, None]
    ii = np.arange(128)[None, :]
    out = np.zeros((3, 2, 128, 128), np.int64)
    for p, (win, dil) in enumerate(PATTERNS):
        rel_prev = np.clip(ii + 128 - jj, 0, None)
        rel_cur = np.clip(ii - jj, 0, None)
        out[p, 0] = t5_bucket(rel_prev * dil)
        out[p, 1] = t5_bucket(rel_cur * dil)
    return out


class Prog:
    def __init__(self, n_layers=DEPTH, dbg=False, stop=None):
        self.n_layers = n_layers
        self.dbg = dbg
        self.stop = stop
        self.consts = make_consts()
        self.nc = nc = bass.Bass("TRN2", target_bir_lowering=False)

        def din(name, shape, dt=F32):
            return nc.dram_tensor(name, list(shape), dt, kind="ExternalInput").ap()

        self.I = I = {}
        I["x"] = din("x", [S, D])
        I["w_in"] = din("w_in", [DEPTH, D, N_IN])
        I["w_out"] = din("w_out", [DEPTH, D, D])
        I["rwkv_mu"] = din("rwkv_mu", [DEPTH, RWKV_IN])
        for nm in ("rwkv_w0", "rwkv_a0", "rwkv_k_k", "rwkv_k_a", "rwkv_r_k", "rwkv_ln_g", "rwkv_ln_b",
                   "ret_gn_g", "ret_gn_b"):
            I[nm] = din(nm, [DEPTH, 256])
        I["rwkv_w_up"] = din("rwkv_w_up", [DEPTH, 64, 256])
        I["rwkv_a_up"] = din("rwkv_a_up", [DEPTH, 64, 256])
        I["rwkv_g_up"] = din("rwkv_g_up", [DEPTH, 128, 256])
        I["ln_g"] = din("ln_g", [DEPTH, 2, D])
        I["ln_b"] = din("ln_b", [DEPTH, 2, D])
        I["ffn_w_gate"] = din("ffn_w_gate", [2, D, FF_DENSE])
        I["ffn_w_up"] = din("ffn_w_up", [2, D, FF_DENSE])
        I["ffn_w_down"] = din("ffn_w_down", [2, FF_DENSE, D])
        I["moe_router"] = din("moe_router", [2, D, N_EXPERTS])
        I["moe_w_gate"] = din("moe_w_gate", [2, N_EXPERTS, D, FF_EXPERT])
        I["moe_w_up"] = din("moe_w_up", [2, N_EXPERTS, D, FF_EXPERT])
        I["moe_w_down"] = din("moe_w_down", [2, N_EXPERTS, FF_EXPERT, D])
        I["cst"] = din("cst", [128, CST_N])
        I["cos2"] = din("cos2", [128, S])
        I["sin2"] = din("sin2", [128, S])
        I["amask"] = din("amask", [2, 128, 128])
        I["biasg"] = din("biasg", [3, 8, 2, 128, 128])
        self.out = nc.dram_tensor("out", [S, D], F32, kind="ExternalOutput").ap()

        def scr(name, shape, dt):
            kind = "ExternalOutput" if dbg else "Internal"
            return nc.dram_tensor(name, list(shape), dt, kind=kind).ap()

        self.xres = scr("xres", [S, D], F32)
        self.pT = scr("pT", [N_IN, S], F32)
        self.vtok = scr("vtok", [S, 768], BF16)
        self.yT = scr("yT", [D, S], BF16)

    _uid = 0

    def un(self, n):
        self._uid += 1
        return "%s_%d" % (n, self._uid)

    def cst(self, name, rows=128):
        o, n = CST[name]
        return self.cstt[0:rows, o:o + n]

    def build(self):
        nc = self.nc
        with ExitStack() as es:
            self.kb = kb = KB(nc, es)
            self.arena = es.enter_context(nc.sbuf_tensor("arena", [128, 16384], F32))
            self.XT = self.arena[:, :].bitcast(BF16).rearrange("p (k s) -> p k s", k=8)
            self.XTd = [Dep() for _ in range(8)]
            self.pbank_t = es.enter_context(nc.psum_tensor("psum", [128, 8, 512], F32))
            self.bank = [(self.pbank_t[:, i], Dep(excl=True)) for i in range(8)]
            self.cstt = es.enter_context(nc.sbuf_tensor("cstt", [128, CST_N], F32))
            self.cstd = Dep()
            self.identb = es.enter_context(nc.sbuf_tensor("identb", [128, 128], BF16))
            self.gates = es.enter_context(nc.sbuf_tensor("gates", [128, NT, N_EXPERTS], F32))
            self.gatesd = Dep()
            kb.dma("sp", self.cstt[:], self.I["cst"], w=[self.cstd])
            kb.op("dve", lambda e: e.tensor_copy(out=self.identb[:], in_=self.cst("ident")),
                  r=[self.cstd], w=[self.cstd])
            self.load_x0()
            kb.barrier()
            for l in range(self.n_layers):
                self.layer(l)
                if self.stop is not None and self.stop[0] == l and self.stop[1] == "layer":
                    break
            kb.barrier()
        return nc

    def setup_bias(self):
        nc, kb = self.nc, self.kb
        with ExitStack() as es:
            self.bias8d = Dep()
            tg = es.enter_context(nc.sbuf_tensor(self.un("tg"), [128, 2, 16, 128], F32))
            mk = es.enter_context(nc.sbuf_tensor(self.un("mk"), [128, 2, 128], F32))
            mkd = Dep()
            kb.dma("sp", mk[:], self.I["amask"].rearrange("a j i -> j a i"), w=[mkd])
            ring = Ring(tg, 2)
            for p in range(3):
                t, d = ring.next()
                kb.dma("sp", t.rearrange("j (h a) i -> j h a i", a=2),
                       self.I["biasg"][p].rearrange("h a j i -> j h a i"), w=[d])
                for h in range(8):
                    kb.op("dve", lambda e, t=t, h=h, p=p: e.scalar_tensor_tensor(
                        out=self.bias8[:, p, h], in0=t[:, 2 * h:2 * h + 2, :], scalar=8.0, in1=mk[:],
                        op0=ALU.mult, op1=ALU.add), r=[d, mkd], w=[self.bias8d])
            kb.barrier()

    def load_x0(self):
        nc, kb = self.nc, self.kb
        with ExitStack() as es:
            xt = es.enter_context(nc.sbuf_tensor(self.un("x0t"), [128, 2, D], F32))
            xr = Ring(xt, 2)
            pr = self.tr_ring()
            for t in range(NT):
                x, xd = xr.next()
                kb.dma("sp", x, self.I["x"][t * 128:(t + 1) * 128, :], w=[xd])
                kb.dma("sp", self.xres[t * 128:(t + 1) * 128, :], x, r=[xd])
                self.transpose_to_XT(x, xd, t, pr)
            kb.barrier()

    def tr_ring(self, pairs=((0, 1), (2, 3))):
        items = []
        for a, b in pairs:
            ap = self.pbank_t[:, a:b + 1].rearrange("p a (b c) -> p (a b) c", c=128)
            items.append((ap, [self.bank[a][1], self.bank[b][1]]))
        return BRing(items)

    def transpose_to_XT(self, x, xd, t, pr, f32_out=None):
        kb = self.kb
        p, pds = pr.next()
        for k in range(8):
            kb.op("pe", lambda e, k=k: e.transpose(p[:, k, :], x[:, k * 128:(k + 1) * 128], self.cst("ident")),
                  r=[xd, self.cstd], w=pds, mark=(k == 7))
        xd_blk = self.XTd[t // 4]
        kb.op("act", lambda e: e.copy(out=self.XT[:, :, t * 128:(t + 1) * 128], in_=p), r=pds, w=[xd_blk])
        if f32_out is not None:
            o, od = f32_out
            kb.op("dve", lambda e: e.tensor_copy(out=o, in_=p), r=pds, w=[od])

    def layer(self, l):
        kb = self.kb
        self.inproj(l)
        kb.barrier()
        if self.stop == (l, "inproj"):
            return
        self.retention(l)
        kb.barrier()
        if self.stop == (l, "ret"):
            return
        self.attention(l)
        kb.barrier()
        if self.stop == (l, "attn"):
            return
        self.rwkv(l)
        kb.barrier()
        if self.stop == (l, "rwkv"):
            return
        self.outproj_ln1(l)
        kb.barrier()
        if self.stop == (l, "ln1"):
            return
        self.ffn(l)
        kb.barrier()

    def inproj(self, l):
        nc, kb = self.nc, self.kb
        w_in = self.I["w_in"][l]
        with ExitStack() as es:
            wt = es.enter_context(nc.sbuf_tensor(self.un("ip_w"), [128, 2, 8, 512], BF16))
            stg = es.enter_context(nc.sbuf_tensor(self.un("ip_s"), [128, 2, S], F32))
            wv = es.enter_context(nc.sbuf_tensor(self.un("ip_wv"), [128, 8, 768], BF16))
            vt = es.enter_context(nc.sbuf_tensor(self.un("ip_vt"), [128, 2, 768], BF16))
            wr, pr, vr = Ring(wt, 2), BRing(self.bank), Ring(vt, 2)
            sdeps = [[Dep() for _ in range(8)] for _ in range(2)]
            wvd = Dep()
            kb.dma("pool", wv[:, :, 0:512], w_in[:, 2048:2560].rearrange("(k p) n -> p k n", p=128), w=[wvd])
            kb.dma("pool", wv[:, :, 512:768], w_in[:, 3072:3328].rearrange("(k p) n -> p k n", p=128), w=[wvd])
            n_ev = 0
            si = 0
            for cb in range(7):
                w, wd = wr.next()
                kb.dma("pool", w, w_in[:, cb * 512:(cb + 1) * 512].rearrange("(k p) n -> p k n", p=128), w=[wd])
                for fc in range(4):
                    st = stg[:, si]
                    sd = sdeps[si]
                    si ^= 1
                    for tb in range(8):
                        p, pd = pr.next()
                        for k in range(8):
                            kb.op("pe", lambda e, k=k: e.matmul(
                                p, lhsT=w[:, k, fc * 128:(fc + 1) * 128], rhs=self.XT[:, k, tb * 512:(tb + 1) * 512],
                                start=(k == 0), stop=(k == 7)), r=[wd, self.XTd[tb]], w=[pd], mark=(k == 7))
                        o = st[:, tb * 512:(tb + 1) * 512]
                        if n_ev % 2 == 0:
                            kb.op("act", lambda e: e.copy(out=o, in_=p), r=[pd], w=[sd[tb]])
                        else:
                            kb.op("dve", lambda e: e.tensor_copy(out=o, in_=p), r=[pd], w=[sd[tb]])
                        n_ev += 1
                    row = (cb * 4 + fc) * 128
                    kb.dma("sp", self.pT[row:row + 128, :], st, r=sd)
            for t in range(NT):
                v, vd = vr.next()
                p1, pd1 = pr.next()
                p2, pd2 = pr.next()
                for k in range(8):
                    kb.op("pe", lambda e, k=k: e.matmul(p1, lhsT=self.XT[:, k, t * 128:(t + 1) * 128], rhs=wv[:, k, 0:512],
                                                        start=(k == 0), stop=(k == 7)),
                          r=[wvd, self.XTd[t // 4]], w=[pd1], mark=(k == 7))
                for k in range(8):
                    kb.op("pe", lambda e, k=k: e.matmul(p2[:, 0:256], lhsT=self.XT[:, k, t * 128:(t + 1) * 128],
                                                        rhs=wv[:, k, 512:768], start=(k == 0), stop=(k == 7)),
                          r=[wvd, self.XTd[t // 4]], w=[pd2], mark=(k == 7))
                kb.op("act", lambda e: e.copy(out=v[:, 0:512], in_=p1), r=[pd1], w=[vd])
                kb.op("dve", lambda e: e.tensor_copy(out=v[:, 512:768], in_=p2[:, 0:256]), r=[pd2], w=[vd])
                kb.dma("sp", self.vtok[t * 128:(t + 1) * 128, :], v, r=[vd])


    def abf(self, lo, hi, **kw):
        return self.arena[:, lo:hi].bitcast(BF16)

    def retention(self, l):
        nc, kb = self.nc, self.kb
        base = RWKV_IN + ATTN_IN
        pT = self.pT
        gam = self.consts["gamma_c"]
        H0, H1 = slice(0, 64), slice(64, 128)
        with ExitStack() as es:
            sb = lambda n, sh, dt: es.enter_context(nc.sbuf_tensor(self.un(n), sh, dt))
            pp = lambda n, sh, dt: es.enter_context(nc.psum_tensor(n, sh, dt))
            kr = self.abf(0, 2048)
            qm = [self.abf(2048, 4096), self.abf(4096, 6144)]
            qx = [self.abf(6144, 8192), self.abf(8192, 10240)]
            sgb = self.abf(10240, 12288)
            krd, qmd, qxd, sgd = Dep(), Dep(), Dep(), Dep()
            cs_t = sb("rt_cs", [128, 2, 2, 1024], F32)
            tq_t = sb("rt_tq", [128, 2, 1024], F32)
            tw_t = sb("rt_tw", [128, 2, 1024], F32)
            v = sb("rt_v", [128, NT, 256], BF16)
            yo = sb("rt_yo", [128, S], BF16)
            gnp = sb("rt_gnp", [128, 4], F32)
            rst = sb("rt_rst", [128, 64], F32)
            rstb = sb("rt_rstb", [128, 64], BF16)
            vd, yod, gnd, rsd, rsbd = Dep(), Dep(), Dep(), Dep(), Dep()
            kb.dma("sp", v[:], self.vtok[:, 512:768].rearrange("(t p) c -> p t c", p=128), w=[vd])
            kb.dma("sp", gnp[:, 0:2], self.I["ret_gn_g"][l].rearrange("(a p) -> p a", p=128), w=[gnd], slow=True)
            kb.dma("sp", gnp[:, 2:4], self.I["ret_gn_b"][l].rearrange("(a p) -> p a", p=128), w=[gnd], slow=True)
            csr, tqr, twr = Ring(cs_t, 2), Ring(tq_t, 2), Ring(tw_t, 2)
            B = self.bank
            bfv = lambda i: (B[i][0].bitcast(BF16)[:, 0:128], B[i][1])
            ps_s = BRing([(B[0][0][:, 0:256], B[0][1]), (B[1][0][:, 0:256], B[1][1])])
            ps_k = BRing([bfv(2)])
            ps_kv = BRing([(B[3][0][:, 0:128], B[3][1])])
            ps_o = BRing([(B[4][0][:, 0:128], B[4][1]), (B[5][0][:, 0:128], B[5][1])])
            ps_t = BRing([bfv(6)])
            sTm_r = Ring(sb("rt_sTm", [128, 2, 256], BF16), 2)
            kz_r = Ring(sb("rt_kz", [128, 2, 128], BF16), 2)
            osq_r = Ring(sb("rt_osq", [128, 2, 128], F32), 2)
            yn_r = Ring(sb("rt_yn", [128, 2, 128], BF16), 2)
            st_r = Ring(sb("rt_st", [128, 2, 16], F32), 2)
            tf_r = Ring(sb("rt_tf", [128, 2, 128], F32), 2)
            dmask = self.cst("dmaskT")
            zeta = self.cst("zeta8")
            xi = self.cst("xi").rearrange("p (a n) -> p a n", a=2)
            for hp in range(2):
                kb.op("pool", lambda e: e.memset(qm[0][H1, :], 0.0), w=[qmd])
                kb.op("pool", lambda e: e.memset(qm[1][H0, :], 0.0), w=[qmd])
                kb.op("pool", lambda e: e.memset(qx[0][H1, :], 0.0), w=[qxd])
                kb.op("pool", lambda e: e.memset(qx[1][H0, :], 0.0), w=[qxd])
                kb.op("pool", lambda e: e.memset(rst[:], 0.0), w=[rsd])
                for pc in range(4):
                    sl = slice(pc * 1024, (pc + 1) * 1024)
                    cs, csd = csr.next()
                    kb.dma("sp", cs[:, 0], self.I["cos2"][:, sl], w=[csd])
                    kb.dma("sp", cs[:, 1], self.I["sin2"][:, sl], w=[csd])
                    for which in range(2):
                        r0 = base + which * 256 + hp * 128
                        tq, tqd = tqr.next()
                        tw, twd = twr.next()
                        kb.dma("sp", tq, pT[r0:r0 + 128, sl], w=[tqd])
                        for blk in range(4):
                            src = r0 + (blk ^ 1) * 32
                            kb.dma("sp", tw[blk * 32:(blk + 1) * 32], pT[src:src + 32, sl], w=[twd])
                        kb.op("dve", lambda e: e.tensor_tensor(out=tq, in0=tq, in1=cs[:, 0], op=ALU.mult),
                              r=[csd], w=[tqd])
                        kb.op("pool", lambda e: e.tensor_tensor(out=tw, in0=tw, in1=cs[:, 1], op=ALU.mult),
                              r=[csd], w=[twd])
                        if which == 1:
                            kb.op("dve", lambda e: e.tensor_tensor(out=kr[:, sl], in0=tq, in1=tw, op=ALU.add),
                                  r=[tqd, twd], w=[krd])
                        else:
                            for hh, rows in enumerate((H0, H1)):
                                kb.op("dve", lambda e: e.tensor_tensor(out=qm[hh][rows, sl], in0=tq[rows], in1=tw[rows],
                                                                       op=ALU.add), r=[tqd, twd], w=[qmd])
                    r0 = base + 768 + hp * 128
                    tq, tqd = tqr.next()
                    kb.dma("sp", tq, pT[r0:r0 + 128, sl], w=[tqd])
                    kb.op("act", lambda e: e.activation(out=sgb[:, sl], in_=tq, func=AF.Silu), r=[tqd], w=[sgd])
                for hh, rows in enumerate((H0, H1)):
                    kb.op("pool", lambda e: e.tensor_tensor(
                        out=qx[hh][rows, :].rearrange("p (c n) -> p c n", n=128),
                        in0=qm[hh][rows, :].rearrange("p (c n) -> p c n", n=128),
                        in1=xi[rows, hp:hp + 1, :].broadcast_to([64, NT, 128]), op=ALU.mult),
                        r=[qmd, self.cstd], w=[qxd])
                import os
                CUT = int(os.environ.get("RET_CUT", "99"))
                for c in range((int(os.environ.get("RET_NC", "32")) if CUT > 2 else 0)):
                    cs = slice(c * 128, (c + 1) * 128)
                    p_s, p_sd = ps_s.next()
                    for hh in range(2):
                        kb.op("pe", lambda e: e.matmul(p_s[:, hh * 128:(hh + 1) * 128], lhsT=kr[:, cs], rhs=qm[hh][:, cs],
                                                       start=True, stop=True), r=[krd, qmd], w=[p_sd], mark=(hh == 1))
                    sTm, sTmd = sTm_r.next()
                    kb.op("dve", lambda e: e.tensor_tensor(out=sTm, in0=p_s, in1=dmask[:, hp * 256:(hp + 1) * 256],
                                                           op=ALU.mult), r=[p_sd, self.cstd], w=[sTmd])
                    if CUT <= 3:
                        continue
                    p_k, p_kd = ps_k.next()
                    kb.op("pe", lambda e: e.transpose(p_k, kr[:, cs], self.identb[:]), r=[krd, self.cstd], w=[p_kd])
                    kz, kzd = kz_r.next()
                    for hh in range(2):
                        h = 2 * hp + hh
                        kb.op("act", lambda e: e.mul(out=kz[:, hh * 64:(hh + 1) * 64], in_=p_k[:, hh * 64:(hh + 1) * 64],
                                                     mul=zeta[:, h:h + 1]), r=[p_kd, self.cstd], w=[kzd])
                    if CUT <= 4:
                        continue
                    p_o, p_od = ps_o.next()
                    for hh in range(2):
                        h = 2 * hp + hh
                        kb.op("pe", lambda e: e.matmul(p_o[:, hh * 64:(hh + 1) * 64], lhsT=sTm[:, hh * 128:(hh + 1) * 128],
                                                       rhs=v[:, c, h * 64:(h + 1) * 64], start=True, stop=(c == 0)),
                              r=[sTmd, vd], w=[p_od], mark=(c == 0 and hh == 1))
                        if c > 0:
                            kb.op("pe", lambda e: e.matmul(p_o[:, hh * 64:(hh + 1) * 64], lhsT=qx[hh][:, cs], rhs=rstb[:],
                                                           start=False, stop=True),
                                  r=[qxd, rsbd], w=[p_od], mark=(hh == 1))
                    if CUT <= 5:
                        continue
                    if c < NT - 1:
                        p_kv, p_kvd = ps_kv.next()
                        kb.op("pe", lambda e: e.matmul(p_kv, lhsT=kz, rhs=v[:, c, hp * 128:(hp + 1) * 128],
                                                       start=True, stop=True), r=[kzd, vd], w=[p_kvd])
                        for hh, rows in enumerate((H0, H1)):
                            kb.op("dve", lambda e: e.scalar_tensor_tensor(
                                out=rst[rows, :], in0=rst[rows, :], scalar=gam[2 * hp + hh],
                                in1=p_kv[rows, hh * 64:(hh + 1) * 64], op0=ALU.mult, op1=ALU.add),
                                r=[p_kvd, rsbd], w=[rsd])
                        kb.op("dve", lambda e: e.tensor_copy(out=rstb[:], in_=rst[:]), r=[rsd], w=[rsbd])
                    if CUT <= 6:
                        continue
                    st, std = st_r.next()
                    osq, osqd = osq_r.next()
                    kb.op("act", lambda e: e.activation(out=osq, in_=p_o, func=AF.Square), r=[p_od], w=[osqd])
                    kb.op("dve", lambda e: e.reduce_sum(out=st[:, 0:2], in_=p_o.rearrange("p (h e) -> p h e", e=64),
                                                        axis=AX.X), r=[p_od], w=[std])
                    kb.op("dve", lambda e: e.reduce_sum(out=st[:, 2:4], in_=osq.rearrange("p (h e) -> p h e", e=64),
                                                        axis=AX.X), r=[osqd], w=[std])
                    self.norm_stats(st, std, 2, 64, LN_EPS)
                    if CUT <= 7:
                        continue
                    yn, ynd = yn_r.next()
                    for hh in range(2):
                        kb.op("dve", lambda e: e.tensor_scalar(out=yn[:, hh * 64:(hh + 1) * 64], in0=p_o[:, hh * 64:(hh + 1) * 64],
                                                               scalar1=st[:, 12 + hh:13 + hh], scalar2=st[:, 14 + hh:15 + hh],
                                                               op0=ALU.mult, op1=ALU.add), r=[p_od, std], w=[ynd])
                    if CUT <= 8:
                        continue
                    p_t, p_td = ps_t.next()
                    kb.op("pe", lambda e: e.transpose(p_t, yn, self.identb[:]), r=[ynd, self.cstd], w=[p_td])
                    tf, tfd = tf_r.next()
                    kb.op("dve", lambda e: e.tensor_scalar(out=tf, in0=p_t, scalar1=gnp[:, hp:hp + 1],
                                                           scalar2=gnp[:, 2 + hp:3 + hp], op0=ALU.mult, op1=ALU.add),
                          r=[p_td, gnd], w=[tfd])
                    kb.op("pool", lambda e: e.tensor_tensor(out=yo[:, cs], in0=tf, in1=sgb[:, cs], op=ALU.mult),
                          r=[tfd, sgd], w=[yod])
                kb.dma("sp", self.yT[768 + hp * 128:768 + (hp + 1) * 128, :], yo[:], r=[yod])

    def norm_stats_gen(self, st, std, n, cnt, eps):
        kb = self.kb
        c = lambda i: st[:, i * n:(i + 1) * n]
        kb.op("dve", lambda e: e.tensor_scalar(out=c(2), in0=c(0), scalar1=1.0 / cnt, scalar2=None, op0=ALU.mult),
              r=[std], w=[std])
        kb.op("dve", lambda e: e.tensor_tensor(out=c(3), in0=c(2), in1=c(2), op=ALU.mult), r=[std], w=[std])
        yield
        kb.op("dve", lambda e: e.scalar_tensor_tensor(out=c(4), in0=c(1), scalar=1.0 / cnt, in1=c(3),
                                                      op0=ALU.mult, op1=ALU.subtract), r=[std], w=[std])
        yield
        kb.op("act", lambda e: e.activation(out=c(5), in_=c(4), func=AF.Sqrt, bias=float(eps)), r=[std], w=[std])
        yield
        kb.op("dve", lambda e: e.reciprocal(out=c(6), in_=c(5)), r=[std], w=[std])
        yield
        kb.op("dve", lambda e: e.scalar_tensor_tensor(out=c(7), in0=c(2), scalar=-1.0, in1=c(6),
                                                      op0=ALU.mult, op1=ALU.mult), r=[std], w=[std])
        yield

    def norm_stats(self, st, std, n, cnt, eps):
        kb = self.kb
        c = lambda i: st[:, i * n:(i + 1) * n]
        kb.op("dve", lambda e: e.tensor_scalar(out=c(2), in0=c(0), scalar1=1.0 / cnt, scalar2=None, op0=ALU.mult),
              r=[std], w=[std])
        kb.op("dve", lambda e: e.tensor_tensor(out=c(3), in0=c(2), in1=c(2), op=ALU.mult), r=[std], w=[std])
        kb.op("dve", lambda e: e.scalar_tensor_tensor(out=c(4), in0=c(1), scalar=1.0 / cnt, in1=c(3),
                                                      op0=ALU.mult, op1=ALU.subtract), r=[std], w=[std])
        kb.op("dve", lambda e: e.tensor_scalar(out=c(4), in0=c(4), scalar1=float(eps), scalar2=None, op0=ALU.add),
              r=[std], w=[std])
        kb.op("act", lambda e: e.activation(out=c(5), in_=c(4), func=AF.Sqrt), r=[std], w=[std])
        kb.op("dve", lambda e: e.reciprocal(out=c(6), in_=c(5)), r=[std], w=[std])
        kb.op("dve", lambda e: e.scalar_tensor_tensor(out=c(7), in0=c(2), scalar=-1.0, in1=c(6),
                                                      op0=ALU.mult, op1=ALU.mult), r=[std], w=[std])

    def attention(self, l):
        nc, kb = self.nc, self.kb
        base = RWKV_IN
        pT = self.pT
        H0, H1 = slice(0, 64), slice(64, 128)
        with ExitStack() as es:
            sb = lambda n, sh, dt: es.enter_context(nc.sbuf_tensor(self.un(n), sh, dt))
            self.bias8 = sb("bias8", [128, 3, 8, 2, 128], BF16)
            self.setup_bias()
            acc = self.arena[0:64, :].rearrange("p (a h s) -> p a h s", a=2, h=2)
            accd = Dep()
            kt = sb("at_k", [128, S], BF16)
            qm = [sb("at_q0", [128, S], BF16), sb("at_q1", [128, S], BF16)]
            vp = [sb("at_v%d" % i, [128, NT, 128], BF16) for i in range(3)]
            ones = sb("at_ones", [128, 64], BF16)
            PT_r = Ring(sb("at_PT", [128, 4, 512], BF16), 4)
            rc = sb("at_rc", [64, 2, S], F32)
            yo = sb("at_yo", [64, 2, S], BF16)
            ktd, qmd, vd, od, rcd, yod = Dep(), Dep(), Dep(), Dep(), Dep(), Dep()
            kb.op("pool", lambda e: e.memset(ones[:], 1.0), w=[od])
            B = self.bank
            ps_s = BRing([B[0], B[1], B[2], B[3]])
            ps_o = BRing([B[4], B[5], B[6], B[7]])
            for pair in range(4):
                kb.op("pool", lambda e: e.memset(qm[0][H1, :], 0.0), w=[qmd])
                kb.op("pool", lambda e: e.memset(qm[1][H0, :], 0.0), w=[qmd])
                kb.op("dve", lambda e: e.memset(self.arena[0:64, :], 0.0), w=[accd])
                q0 = base + pair * 128
                k0 = base + 512 + pair * 128
                kb.dma("pool", qm[0][H0, :], pT[q0:q0 + 64, :], w=[qmd])
                kb.dma("pool", qm[1][H1, :], pT[q0 + 64:q0 + 128, :], w=[qmd])
                kb.dma("pool", kt[:], pT[k0:k0 + 128, :], w=[ktd])
                for p, (win, dil) in enumerate(PATTERNS):
                    nb = NT // dil
                    for r in range(dil):
                        src = self.vtok[:, pair * 128:(pair + 1) * 128].rearrange("(b j r) c -> r j b c", r=dil, j=128)[r]
                        kb.dma("sp", vp[p][:, r * nb:(r + 1) * nb, :], src, w=[vd])
                units = [(p, dil, NT // dil, r, b) for p, (win, dil) in enumerate(PATTERNS)
                         for r in range(dil) for b in range(NT // dil)]

                def tsl(dil, r, bb):
                    st = r + dil * 128 * bb
                    return slice(st, st + dil * 127 + 1, dil)

                def emit_scores(u):
                    p, dil, nb, r, b = u
                    qs = tsl(dil, r, b)
                    halves = (0, 1) if b > 0 else (1,)
                    p_s, p_sd = ps_s.next()
                    for hh in range(2):
                        h = 2 * pair + hh
                        for half in halves:
                            ks = tsl(dil, r, b - 1) if half == 0 else qs
                            o = p_s[:, (hh * 2 + half) * 128:(hh * 2 + half + 1) * 128]
                            kb.op("pe", lambda e: e.matmul(o, lhsT=kt[:, ks], rhs=qm[hh][:, qs], start=True, stop=False),
                                  r=[ktd, qmd], w=[p_sd], mark=False)
                            kb.op("pe", lambda e: e.matmul(o, lhsT=self.identb[:], rhs=self.bias8[:, p, h, half, :],
                                                           start=False, stop=True),
                                  r=[self.cstd, self.bias8d], w=[p_sd], mark=(hh == 1 and half == 1))
                    PT, PTd = PT_r.next()
                    if b > 0:
                        kb.op("act", lambda e: e.activation(out=PT, in_=p_s, func=AF.Exp, scale=0.125),
                              r=[p_sd], w=[PTd])
                    else:
                        v4 = lambda t: t.rearrange("p (h a i) -> p h a i", h=2, a=2)[:, :, 1, :]
                        kb.op("act", lambda e: e.activation(out=v4(PT), in_=v4(p_s), func=AF.Exp, scale=0.125),
                              r=[p_sd], w=[PTd])
                    return PT, PTd

                def emit_pv(u, ctx):
                    p, dil, nb, r, b = u
                    PT, PTd = ctx
                    qs = tsl(dil, r, b)
                    halves = (0, 1) if b > 0 else (1,)
                    p_o, p_od = ps_o.next()
                    for nd in range(2):
                        for hh in range(2):
                            o = p_o[0:64, (nd * 2 + hh) * 128:(nd * 2 + hh + 1) * 128]
                            for hi, half in enumerate(halves):
                                ti = r * nb + (b - 1 if half == 0 else b)
                                lw = vp[p][:, ti, hh * 64:(hh + 1) * 64] if nd == 0 else ones[:]
                                kb.op("pe", lambda e: e.matmul(o, lhsT=lw, rhs=PT[:, (hh * 2 + half) * 128:(hh * 2 + half + 1) * 128],
                                                               start=(hi == 0), stop=(hi == len(halves) - 1)),
                                      r=[PTd, vd, od], w=[p_od], mark=(nd == 1 and hh == 1 and hi == len(halves) - 1))
                    kb.op("dve", lambda e: e.tensor_tensor(
                        out=acc[:, :, :, qs], in0=acc[:, :, :, qs],
                        in1=p_o[0:64, :].rearrange("p (a h i) -> p a h i", a=2, h=2), op=ALU.add),
                        r=[p_od, accd], w=[accd])

                LOOK = 2
                ctxs = {}
                for i in range(min(LOOK, len(units))):
                    ctxs[i] = emit_scores(units[i])
                for i, u in enumerate(units):
                    if i + LOOK < len(units):
                        ctxs[i + LOOK] = emit_scores(units[i + LOOK])
                    emit_pv(u, ctxs.pop(i))
                kb.op("dve", lambda e: e.reciprocal(out=rc[:], in_=acc[:, 1]), r=[accd], w=[rcd])
                kb.op("dve", lambda e: e.tensor_tensor(out=yo[:], in0=acc[:, 0], in1=rc[:], op=ALU.mult),
                      r=[accd, rcd], w=[yod])
                for hh in range(2):
                    r0 = 256 + (2 * pair + hh) * 64
                    kb.dma("sp", self.yT[r0:r0 + 64, :], yo[:, hh, :], r=[yod])

    def rwkv(self, l):
        nc, kb = self.nc, self.kb
        pT = self.pT
        I = self.I
        H0, H1 = slice(0, 64), slice(64, 128)
        SEG = 1024
        NCH = SEG // 64
        with ExitStack() as es:
            sb = lambda n, sh, dt: es.enter_context(nc.sbuf_tensor(self.un(n), sh, dt))
            U = [self.arena[:, i * SEG:(i + 1) * SEG] for i in range(16)]
            Ud = [Dep() for _ in range(16)]
            prm = sb("rw_prm", [128, 32], F32)
            prd = Dep()
            pcol = {}
            kb.dma("sp", prm[:, 0:8], I["rwkv_mu"][l].rearrange("(c p) -> p c", p=128), w=[prd], slow=True)
            for i, nm in enumerate(("rwkv_w0", "rwkv_a0", "rwkv_k_k", "rwkv_k_a", "rwkv_r_k", "rwkv_ln_g", "rwkv_ln_b")):
                pcol[nm] = 8 + 2 * i
                kb.dma("sp", prm[:, 8 + 2 * i:10 + 2 * i], I[nm][l].rearrange("(a p) -> p a", p=128), w=[prd], slow=True)
            pcol["omka"] = 22
            kb.op("dve", lambda e: e.tensor_scalar(out=prm[:, 22:24], in0=prm[:, pcol["rwkv_k_a"]:pcol["rwkv_k_a"] + 2],
                                                   scalar1=-1.0, scalar2=1.0, op0=ALU.mult, op1=ALU.add), r=[prd], w=[prd])
            P = lambda nm, hp: prm[:, pcol[nm] + hp:pcol[nm] + hp + 1]
            wlw = sb("rw_wlw", [128, 256], BF16)
            wla = sb("rw_wla", [128, 256], BF16)
            gup = sb("rw_gup", [128, 256], BF16)
            wd = Dep()
            kb.op("pool", lambda e: e.memset(wlw[H1, :], 0.0), w=[wd])
            kb.op("pool", lambda e: e.memset(wla[H0, :], 0.0), w=[wd])
            kb.dma("pool", wlw[H0, :], I["rwkv_w_up"][l], w=[wd])
            kb.dma("pool", wla[H1, :], I["rwkv_a_up"][l], w=[wd])
            kb.dma("pool", gup[:], I["rwkv_g_up"][l], w=[wd])
            pad = {}
            padd = {}
            for nm in ("Rt", "Kk", "Bt", "Kt", "V"):
                pad[nm] = sb("rw_p" + nm, [128, NCH, 128], F32)
                padd[nm] = Dep()
                kb.op("pool", lambda e, nm=nm: e.memset(pad[nm][:], 0.0), w=[padd[nm]])
            T6 = sb("rw_T6", [128, SEG], BF16)
            SG = sb("rw_SG", [128, SEG], BF16)
            T6d, SGd = Dep(), Dep()
            buf_r = Ring(sb("rw_buf", [128, 2, SEG + 1], F32), 2)
            tmp_r = Ring(sb("rw_tmp", [128, 2, SEG], F32), 2)
            yo_r = Ring(sb("rw_yo", [128, 2, SEG], BF16), 2)
            Hs = [sb("rw_H%d" % i, [128, 128], F32) for i in range(2)]
            Hd = [Dep(), Dep()]
            import os
            NW = int(os.environ.get('RW_NW', '4'))
            wk = []
            for g in range(NW):
                t = {}
                for nm in ("nBtPT", "KtPT", "VPT", "N", "NT", "MakT", "nPrbT", "PrkT", "Pa", "Pb", "PT1", "PT2", "PT4",
                           "PT8", "PT16", "PT32", "QT", "GT0", "E", "YnP", "sq"):
                    t[nm] = (sb("rw_%s%d" % (nm, g), [128, 128], F32), Dep())
                for nm in ("Xa", "Xb"):
                    t[nm] = (sb("rw_%s%d" % (nm, g), [128, 256], F32), Dep())
                t["st"] = (sb("rw_st%d" % g, [128, 8], F32), Dep())
                kb.op("pool", lambda e, t=t: e.memset(t["YnP"][0][:], 0.0), w=[t["YnP"][1]])
                wk.append(t)
            banks = BRing(self.bank)
            ident = self.cst("ident")
            cd = self.cstd
            evc = [0]

            def copy_ev(out, in_, r, w, scale=None):
                evc[0] += 1
                if evc[0] % 2 == 0:
                    if scale is None:
                        kb.op("act", lambda e: e.copy(out=out, in_=in_), r=r, w=w)
                    else:
                        kb.op("act", lambda e: e.mul(out=out, in_=in_, mul=float(scale)), r=r, w=w)
                else:
                    if scale is None:
                        kb.op("dve", lambda e: e.tensor_copy(out=out, in_=in_), r=r, w=w)
                    else:
                        kb.op("dve", lambda e: e.tensor_scalar(out=out, in0=in_, scalar1=float(scale), scalar2=None,
                                                               op0=ALU.mult), r=r, w=w)

            def load_shift(row0, seg, c8, dst, dstd):
                buf, bd = buf_r.next()
                tmp, td = tmp_r.next()
                t0 = seg * SEG
                if seg == 0:
                    kb.op("pool", lambda e: e.memset(buf[:, 0:1], 0.0), w=[bd])
                    kb.dma("sp", buf[:, 1:SEG + 1], pT[row0:row0 + 128, 0:SEG], w=[bd])
                else:
                    kb.dma("sp", buf[:, 0:SEG + 1], pT[row0:row0 + 128, t0 - 1:t0 + SEG], w=[bd])
                kb.op("pool", lambda e: e.tensor_tensor(out=tmp, in0=buf[:, 0:SEG], in1=buf[:, 1:SEG + 1], op=ALU.subtract),
                      r=[bd], w=[td])
                kb.op("dve", lambda e: e.scalar_tensor_tensor(out=dst, in0=tmp, scalar=prm[:, c8:c8 + 1], in1=buf[:, 1:SEG + 1],
                                                              op0=ALU.mult, op1=ALU.add), r=[td, bd, prd], w=[dstd])

            def mm_to(lhsT, rhs, rd, n=128):
                bk, bd = banks.next()
                o = bk[:, 0:n]
                kb.op("pe", lambda e: e.matmul(o, lhsT=lhsT, rhs=rhs, start=True, stop=True), r=rd, w=[bd])
                return o, bd

            def chunk_gen(hp, seg, c, g):
                t = wk[g]
                gc = seg * NCH + c
                A = lambda nm: t[nm][0][:]
                Dp = lambda nm: t[nm][1]
                Rt, Kk, Bt, Kt, V = (pad[nm][:, c, :] for nm in ("Rt", "Kk", "Bt", "Kt", "V"))
                dR, dKk, dB, dK, dV = (padd[nm] for nm in ("Rt", "Kk", "Bt", "Kt", "V"))
                X = [t["Xa"], t["Xb"]]
                for src, sd, dst, dd, sc in ((Bt, dB, A("nBtPT"), Dp("nBtPT"), -1.0), (Kt, dK, A("KtPT"), Dp("KtPT"), None),
                                             (V, dV, A("VPT"), Dp("VPT"), None), (Kk, dKk, X[0][0][:, 0:128], X[0][1], None)):
                    bk, bd = banks.next()
                    kb.op("pe", lambda e: e.transpose(bk[:, 0:128], src, ident), r=[sd, cd], w=[bd])
                    copy_ev(dst, bk[:, 0:128], [bd], [dd], sc)
                yield
                for lh, ld, rh, rdd, dst, msk in ((Bt, dB, Kk, dKk, "NT", "nSU"), (Kk, dKk, Bt, dB, "N", "nSL"),
                                                 (Kt, dK, Kk, dKk, "MakT", "SU"), (Bt, dB, Rt, dR, "nPrbT", "nIU"),
                                                 (Kt, dK, Rt, dR, "PrkT", "IU")):
                    o, bd = mm_to(lh, rh, [ld, rdd])
                    kb.op("dve", lambda e: e.tensor_tensor(out=A(dst), in0=o, in1=self.cst(msk), op=ALU.mult),
                          r=[bd, cd], w=[Dp(dst)])
                yield
                o, bd = mm_to(A("MakT"), A("VPT"), [Dp("MakT"), Dp("VPT")])
                copy_ev(X[0][0][:, 128:256], o, [bd], [X[0][1]])
                yield
                Pc, PTc = ("N", "NT")
                lv_names = {2: "PT2", 4: "PT4", 8: "PT8", 16: "PT16", 32: "PT32"}
                pt_list = ["NT"]
                pbuf = ["Pa", "Pb"]
                pi = 0
                for lvl in (2, 4, 8, 16, 32):
                    o2, bd2 = mm_to(A(Pc), A(PTc), [Dp(Pc), Dp(PTc)])
                    if lvl < 32:
                        o1, bd1 = mm_to(A(PTc), A(Pc), [Dp(Pc), Dp(PTc)])
                    copy_ev(A(lv_names[lvl]), o2, [bd2], [Dp(lv_names[lvl])])
                    if lvl < 32:
                        copy_ev(A(pbuf[pi]), o1, [bd1], [Dp(pbuf[pi])])
                        Pc = pbuf[pi]
                        pi ^= 1
                    PTc = lv_names[lvl]
                    pt_list.append(PTc)
                    yield
                xi_ = 0
                for nm in reversed(pt_list):
                    Xc, Xn = X[xi_], X[xi_ ^ 1]
                    o, bd = mm_to(A(nm), Xc[0][:], [Dp(nm), Xc[1]], n=256)
                    kb.op("dve", lambda e: e.tensor_tensor(out=Xn[0][:], in0=o, in1=Xc[0][:], op=ALU.add),
                          r=[bd, Xc[1]], w=[Xn[1]])
                    xi_ ^= 1
                    yield
                W, Wd = X[xi_]
                W1, W2 = W[:, 0:128], W[:, 128:256]
                o, bd = mm_to(W1, A("nPrbT"), [Wd, Dp("nPrbT")])
                kb.op("dve", lambda e: e.tensor_tensor(out=A("QT"), in0=o, in1=Rt, op=ALU.add), r=[bd, dR], w=[Dp("QT")])
                o, bd = mm_to(W1, A("nBtPT"), [Wd, Dp("nBtPT")])
                kb.op("dve", lambda e: e.tensor_tensor(out=A("GT0"), in0=o, in1=ident, op=ALU.add), r=[bd, cd], w=[Dp("GT0")])
                bk, bd = banks.next()
                o = bk[:, 0:128]
                kb.op("pe", lambda e: e.matmul(o, lhsT=A("KtPT"), rhs=A("VPT"), start=True, stop=False),
                      r=[Dp("KtPT"), Dp("VPT")], w=[bd], mark=False)
                kb.op("pe", lambda e: e.matmul(o, lhsT=A("nBtPT"), rhs=W2, start=False, stop=True),
                      r=[Dp("nBtPT"), Wd], w=[bd])
                dc = U[12][:, c * 64 + 63:c * 64 + 64]
                kb.op("dve", lambda e: e.tensor_scalar(out=A("E"), in0=o, scalar1=dc, scalar2=None, op0=ALU.mult),
                      r=[bd, Ud[12]], w=[Dp("E")])
                yield
                Hc, Hcd = Hs[gc % 2], Hd[gc % 2]
                Hn, Hnd = Hs[(gc + 1) % 2], Hd[(gc + 1) % 2]
                bk, ybd = banks.next()
                yps = bk[:, 0:128]
                kb.op("pe", lambda e: e.matmul(yps, lhsT=A("QT"), rhs=Hc[:], start=True, stop=False),
                      r=[Dp("QT"), Hcd], w=[ybd], mark=False)
                kb.op("pe", lambda e: e.matmul(yps, lhsT=A("PrkT"), rhs=A("VPT"), start=False, stop=False),
                      r=[Dp("PrkT"), Dp("VPT")], w=[ybd], mark=False)
                kb.op("pe", lambda e: e.matmul(yps, lhsT=A("nPrbT"), rhs=W2, start=False, stop=True),
                      r=[Dp("nPrbT"), Wd], w=[ybd])
                o, bd = mm_to(A("GT0"), Hc[:], [Dp("GT0"), Hcd])
                kb.op("dve", lambda e: e.scalar_tensor_tensor(out=Hn[:], in0=o, scalar=dc, in1=A("E"), op0=ALU.mult, op1=ALU.add),
                      r=[bd, Dp("E"), Ud[12]], w=[Hnd])
                yield
                st, std = t["st"]
                kb.op("act", lambda e: e.activation(out=A("sq"), in_=yps, func=AF.Square), r=[ybd], w=[Dp("sq")])
                kb.op("dve", lambda e: e.reduce_sum(out=st[:, 0:1], in_=yps, axis=AX.X), r=[ybd], w=[std])
                yield
                kb.op("dve", lambda e: e.reduce_sum(out=st[:, 1:2], in_=A("sq"), axis=AX.X), r=[Dp("sq")], w=[std])
                yield
                yield from self.norm_stats_gen(st, std, 1, 64, RWKV_GN_EPS)
                for rows in (H0, H1):
                    kb.op("dve", lambda e: e.tensor_scalar(out=t["YnP"][0][rows, rows], in0=yps[rows, rows],
                                                           scalar1=st[rows, 6:7], scalar2=st[rows, 7:8],
                                                           op0=ALU.mult, op1=ALU.add), r=[ybd, std], w=[Dp("YnP")])
                yield
                o, bd = mm_to(A("YnP"), self.cst("sel"), [Dp("YnP"), cd], n=64)
                yield
                kb.op("dve", lambda e: e.tensor_scalar(out=U[10][:, c * 64:(c + 1) * 64], in0=o,
                                                       scalar1=P("rwkv_ln_g", hp), scalar2=P("rwkv_ln_b", hp),
                                                       op0=ALU.mult, op1=ALU.add), r=[bd, prd], w=[Ud[10]])
                yield

            for hp in range(2):
                for i in range(2):
                    kb.op("pool", lambda e, i=i: e.memset(Hs[i][:], 0.0), w=[Hd[i]])
                for seg in range(S // SEG):
                    blk2 = [slice(0, 512), slice(512, 1024)]
                    load_shift(768, seg, 6, U[8], Ud[8])
                    kb.op("act", lambda e: e.activation(out=T6[H0, :], in_=U[8][H0, :], func=AF.Tanh), r=[Ud[8]], w=[T6d])
                    kb.op("act", lambda e: e.copy(out=T6[H1, :], in_=U[8][H1, :]), r=[Ud[8]], w=[T6d])
                    load_shift(896, seg, 7, U[9], Ud[9])
                    kb.op("act", lambda e: e.activation(out=SG[:], in_=U[9], func=AF.Sigmoid), r=[Ud[9]], w=[SGd])
                    load_shift(hp * 128, seg, hp, U[0], Ud[0])
                    load_shift(256 + hp * 128, seg, 2 + hp, U[1], Ud[1])
                    load_shift(512 + hp * 128, seg, 4 + hp, U[2], Ud[2])
                    hs = slice(hp * 128, (hp + 1) * 128)
                    for b2 in blk2:
                        o, bd = mm_to(wlw[:, hs], T6[:, b2], [wd, T6d], n=512)
                        kb.op("act", lambda e: e.activation(out=U[4][:, b2], in_=o, func=AF.Sigmoid, bias=P("rwkv_w0", hp)),
                              r=[bd, prd], w=[Ud[4]])
                        o, bd = mm_to(wla[:, hs], T6[:, b2], [wd, T6d], n=512)
                        kb.op("act", lambda e: e.activation(out=U[3][:, b2], in_=o, func=AF.Sigmoid, bias=P("rwkv_a0", hp)),
                              r=[bd, prd], w=[Ud[3]])
                        o, bd = mm_to(gup[:, hs], SG[:, b2], [wd, SGd], n=512)
                        kb.op("act", lambda e: e.copy(out=U[5][:, b2], in_=o), r=[bd], w=[Ud[5]])
                    kb.op("dve", lambda e: e.tensor_scalar(out=U[6], in0=U[1], scalar1=P("rwkv_k_k", hp), scalar2=None,
                                                           op0=ALU.mult), r=[Ud[1], prd], w=[Ud[6]])
                    kb.op("pool", lambda e: e.tensor_tensor(out=U[8], in0=U[6], in1=U[6], op=ALU.mult), r=[Ud[6]], w=[Ud[8]])
                    for b2 in blk2:
                        o, bd = mm_to(self.cst("onesblk"), U[8][:, b2], [cd, Ud[8]], n=512)
                        kb.op("dve", lambda e: e.tensor_scalar(out=U[11][:, b2], in0=o, scalar1=1e-24, scalar2=None,
                                                               op0=ALU.max), r=[bd], w=[Ud[11]])
                    kb.op("act", lambda e: e.activation(out=U[11], in_=U[11], func=AF.Sqrt), r=[Ud[11]], w=[Ud[11]])
                    kb.op("dve", lambda e: e.reciprocal(out=U[11], in_=U[11]), r=[Ud[11]], w=[Ud[11]])
                    kb.op("pool", lambda e: e.tensor_tensor(out=U[6], in0=U[6], in1=U[11], op=ALU.mult),
                          r=[Ud[6], Ud[11]], w=[Ud[6]])
                    kb.op("dve", lambda e: e.tensor_scalar(out=U[7], in0=U[3], scalar1=P("rwkv_k_a", hp), scalar2=P("omka", hp),
                                                           op0=ALU.mult, op1=ALU.add), r=[Ud[3], prd], w=[Ud[7]])
                    kb.op("pool", lambda e: e.tensor_tensor(out=U[7], in0=U[7], in1=U[1], op=ALU.mult),
                          r=[Ud[7], Ud[1]], w=[Ud[7]])
                    kb.op("dve", lambda e: e.scalar_tensor_tensor(out=U[8], in0=U[0], scalar=P("rwkv_r_k", hp), in1=U[7],
                                                                  op0=ALU.mult, op1=ALU.mult), r=[Ud[0], Ud[7], prd], w=[Ud[8]])
                    for b2 in blk2:
                        o, bd = mm_to(self.cst("onesblk"), U[8][:, b2], [cd, Ud[8]], n=512)
                        kb.op("dve", lambda e: e.tensor_tensor(out=U[9][:, b2], in0=o, in1=U[2][:, b2], op=ALU.mult),
                              r=[bd, Ud[2]], w=[Ud[9]])
                    kb.op("pool", lambda e: e.tensor_tensor(out=U[3], in0=U[3], in1=U[6], op=ALU.mult),
                          r=[Ud[3], Ud[6]], w=[Ud[3]])
                    kb.op("dve", lambda e: e.tensor_tensor_scan(out=U[11], data0=self.cst("scanm"), data1=U[4], initial=0.0,
                                                                op0=ALU.mult, op1=ALU.add), r=[Ud[4], cd], w=[Ud[11]])
                    kb.op("act", lambda e: e.activation(out=U[12], in_=U[11], func=AF.Exp, scale=-C0), r=[Ud[11]], w=[Ud[12]])
                    kb.op("act", lambda e: e.activation(out=U[13], in_=U[11], func=AF.Exp, scale=C0), r=[Ud[11]], w=[Ud[13]])
                    kb.op("pool", lambda e: e.tensor_tensor(out=U[4], in0=U[11], in1=U[4], op=ALU.subtract),
                          r=[Ud[11], Ud[4]], w=[Ud[4]])
                    kb.op("act", lambda e: e.activation(out=U[4], in_=U[4], func=AF.Exp, scale=-C0), r=[Ud[4]], w=[Ud[4]])
                    kb.op("pool", lambda e: e.tensor_tensor(out=U[0], in0=U[0], in1=U[12], op=ALU.mult), r=[Ud[0], Ud[12]], w=[Ud[0]])
                    kb.op("dve", lambda e: e.tensor_tensor(out=U[6], in0=U[6], in1=U[4], op=ALU.mult), r=[Ud[6], Ud[4]], w=[Ud[6]])
                    kb.op("pool", lambda e: e.tensor_tensor(out=U[3], in0=U[3], in1=U[13], op=ALU.mult), r=[Ud[3], Ud[13]], w=[Ud[3]])
                    kb.op("dve", lambda e: e.tensor_tensor(out=U[7], in0=U[7], in1=U[13], op=ALU.mult), r=[Ud[7], Ud[13]], w=[Ud[7]])
                    ci = 0
                    for nm, ui in (("Rt", 0), ("Kk", 6), ("Bt", 3), ("Kt", 7), ("V", 2)):
                        for rows in (H0, H1):
                            dst = pad[nm][rows, :, rows]
                            src = U[ui][rows, :].rearrange("p (c t) -> p c t", t=64)
                            eng = ("pool", "dve", "act")[ci % 3]
                            ci += 1
                            if eng == "act":
                                kb.op("act", lambda e: e.copy(out=dst, in_=src), r=[Ud[ui]], w=[padd[nm]])
                            else:
                                kb.op(eng, lambda e: e.tensor_copy(out=dst, in_=src), r=[Ud[ui]], w=[padd[nm]])
                    for c0 in range(0, NCH, NW):
                        run_interleaved([chunk_gen(hp, seg, c0 + g, g) for g in range(NW)])
                    yo, yod = yo_r.next()
                    kb.op("pool", lambda e: e.tensor_tensor(out=U[10], in0=U[10], in1=U[9], op=ALU.add),
                          r=[Ud[10], Ud[9]], w=[Ud[10]])
                    kb.op("dve", lambda e: e.tensor_tensor(out=yo, in0=U[10], in1=U[5], op=ALU.mult),
                          r=[Ud[10], Ud[5]], w=[yod])
                    kb.dma("sp", self.yT[hp * 128:(hp + 1) * 128, seg * SEG:(seg + 1) * SEG], yo, r=[yod])

    def layer_norm_tile(self, z, zd, junk, jd, st, std, gb, gbd):
        kb = self.kb
        kb.op("act", lambda e: e.activation(out=junk, in_=z, func=AF.Square), r=[zd], w=[jd])
        kb.op("dve", lambda e: e.reduce_sum(out=st[:, 0:1], in_=z, axis=AX.X), r=[zd], w=[std])
        kb.op("dve", lambda e: e.reduce_sum(out=st[:, 1:2], in_=junk, axis=AX.X), r=[jd], w=[std])
        self.norm_stats(st, std, 1, D, LN_EPS)
        kb.op("dve", lambda e: e.tensor_scalar(out=z, in0=z, scalar1=st[:, 6:7], scalar2=st[:, 7:8],
                                               op0=ALU.mult, op1=ALU.add), r=[zd, std], w=[zd])
        kb.op("pool", lambda e: e.tensor_tensor(out=z, in0=z, in1=gb[:, 0, :], op=ALU.mult), r=[zd, gbd], w=[zd])
        kb.op("pool", lambda e: e.tensor_tensor(out=z, in0=z, in1=gb[:, 1, :], op=ALU.add), r=[zd, gbd], w=[zd])

    def layer_norm_tile_gen(self, z, zd, junk, jd, st, std, gb, gbd):
        kb = self.kb
        kb.op("act", lambda e: e.activation(out=junk, in_=z, func=AF.Square), r=[zd], w=[jd])
        kb.op("dve", lambda e: e.reduce_sum(out=st[:, 0:1], in_=z, axis=AX.X), r=[zd], w=[std])
        yield
        kb.op("dve", lambda e: e.reduce_sum(out=st[:, 1:2], in_=junk, axis=AX.X), r=[jd], w=[std])
        yield
        yield from self.norm_stats_gen(st, std, 1, D, LN_EPS)
        kb.op("dve", lambda e: e.tensor_scalar(out=z, in0=z, scalar1=st[:, 6:7], scalar2=st[:, 7:8],
                                               op0=ALU.mult, op1=ALU.add), r=[zd, std], w=[zd])
        yield
        kb.op("pool", lambda e: e.tensor_tensor(out=z, in0=z, in1=gb[:, 0, :], op=ALU.mult), r=[zd, gbd], w=[zd])
        yield
        kb.op("pool", lambda e: e.tensor_tensor(out=z, in0=z, in1=gb[:, 1, :], op=ALU.add), r=[zd, gbd], w=[zd])
        yield

    def load_ln_params(self, gb, gbd, l, i):
        kb = self.kb
        kb.dma("sp", gb[:, 0, :], self.I["ln_g"][l, i:i + 1, :].broadcast_to([128, D]), w=[gbd])
        kb.dma("sp", gb[:, 1, :], self.I["ln_b"][l, i:i + 1, :].broadcast_to([128, D]), w=[gbd])

    def outproj_ln1(self, l):
        nc, kb = self.nc, self.kb
        moe = (l % 2 == 1)
        j = l // 2
        with ExitStack() as es:
            sb = lambda n, sh, dt: es.enter_context(nc.sbuf_tensor(self.un(n), sh, dt))
            wo = sb("o_w", [128, 8, D], BF16)
            wod = Dep()
            kb.dma("pool", wo[:], self.I["w_out"][l].rearrange("(k p) n -> p k n", p=128), w=[wod])
            yt_r = Ring(sb("o_yt", [128, 2, 8, 512], BF16), 2)
            x_r = Ring(sb("o_x", [128, 2, D], F32), 2)
            z_r = Ring(sb("o_z", [128, 2, D], F32), 2)
            junk = sb("o_junk", [128, D], BF16)
            jd = Dep()
            st_r = Ring(sb("o_st", [128, 2, 8], F32), 2)
            gb = sb("o_gb", [128, 2, D], F32)
            gbd = Dep()
            self.load_ln_params(gb, gbd, l, 0)
            B = self.bank
            hb = BRing([(0, 1), (2, 3)])
            tr = self.tr_ring(((4, 5), (6, 7)))
            if moe:
                rt = sb("o_rt", [128, 8, N_EXPERTS], F32)
                rtd = Dep()
                kb.dma("sp", rt[:], self.I["moe_router"][j].rearrange("(k p) e -> p k e", p=128), w=[rtd], slow=True)
                xTf_r = Ring(sb("o_xTf", [128, 2, 8, 128], F32), 2)
                g_r = Ring(sb("o_g", [128, 2, 64], F32), 2)
            junk_r = Ring(sb("o_junk2", [128, 2, D], BF16), 2)

            def tile_gen(tb, tt, yt, ytd):
                t = tb * 4 + tt
                x, xd = x_r.next()
                z, zd = z_r.next()
                jk, jkd = junk_r.next()
                kb.dma("sp", x, self.xres[t * 128:(t + 1) * 128, :], w=[xd])
                ba, bb_ = hb.next()
                for half, bi in enumerate((ba, bb_)):
                    bk, bd = B[bi]
                    for k in range(8):
                        kb.op("pe", lambda e, k=k: e.matmul(bk[:, 0:512], lhsT=yt[:, k, tt * 128:(tt + 1) * 128],
                                                            rhs=wo[:, k, half * 512:(half + 1) * 512],
                                                            start=(k == 0), stop=(k == 7)),
                              r=[ytd, wod], w=[bd], mark=(k == 7))
                    hs = slice(half * 512, (half + 1) * 512)
                    kb.op("dve", lambda e: e.scalar_tensor_tensor(out=z[:, hs], in0=x[:, hs], scalar=float(ALPHA),
                                                                  in1=bk[:, 0:512], op0=ALU.mult, op1=ALU.add),
                          r=[xd, bd], w=[zd])
                yield
                st, std = st_r.next()
                yield from self.layer_norm_tile_gen(z, zd, jk, jkd, st, std, gb, gbd)
                kb.dma("sp", self.xres[t * 128:(t + 1) * 128, :], z, r=[zd, xd])
                if moe:
                    xTf, xTfd = xTf_r.next()
                    self.transpose_to_XT(z, zd, t, tr, f32_out=(xTf, xTfd))
                    yield
                    yield from self.router_gates_gen(xTf, xTfd, rt, rtd, t, g_r)
                else:
                    self.transpose_to_XT(z, zd, t, tr)
                    yield

            for tb in range(8):
                yt, ytd = yt_r.next()
                kb.dma("sp", yt, self.yT[:, tb * 512:(tb + 1) * 512].rearrange("(k p) t -> p k t", p=128), w=[ytd])
                for t0 in (0, 2):
                    run_interleaved([tile_gen(tb, t0 + g, yt, ytd) for g in range(2)])

    def router_gates_gen(self, xTf, xTfd, rt, rtd, t, g_r):
        kb = self.kb
        bk, bd = self.bank[0]
        lp = bk[:, 0:N_EXPERTS]
        for k in range(8):
            kb.op("pe", lambda e, k=k: e.matmul(lp, lhsT=xTf[:, k, :], rhs=rt[:, k, :], start=(k == 0), stop=(k == 7)),
                  r=[xTfd, rtd], w=[bd], mark=(k == 7))
        g, gd = g_r.next()
        c = lambda i: g[:, i * 8:(i + 1) * 8]
        sc = lambda i: g[:, 56 + i:57 + i]
        op = lambda fn: kb.op("dve", fn, r=[gd], w=[gd])
        kb.op("dve", lambda e: e.tensor_copy(out=c(0), in_=lp), r=[bd], w=[gd])
        yield
        op(lambda e: e.reduce_max(out=sc(0), in_=c(0), axis=AX.X))
        yield
        op(lambda e: e.tensor_scalar(out=c(1), in0=c(0), scalar1=sc(0), scalar2=None, op0=ALU.is_equal))
        yield
        op(lambda e: e.scalar_tensor_tensor(out=c(2), in0=c(1), scalar=-1e30, in1=c(0), op0=ALU.mult, op1=ALU.add))
        yield
        op(lambda e: e.reduce_max(out=sc(1), in_=c(2), axis=AX.X))
        yield
        op(lambda e: e.tensor_scalar(out=c(3), in0=c(0), scalar1=sc(1), scalar2=None, op0=ALU.is_ge))
        op(lambda e: e.tensor_scalar(out=sc(2), in0=sc(0), scalar1=-1.0, scalar2=None, op0=ALU.mult))
        yield
        kb.op("act", lambda e: e.activation(out=c(4), in_=c(0), func=AF.Exp, bias=sc(2), scale=1.0), r=[gd], w=[gd])
        yield
        op(lambda e: e.tensor_tensor(out=c(4), in0=c(4), in1=c(3), op=ALU.mult))
        yield
        op(lambda e: e.reduce_sum(out=sc(3), in_=c(4), axis=AX.X))
        yield
        op(lambda e: e.reciprocal(out=sc(4), in_=sc(3)))
        yield
        kb.op("dve", lambda e: e.tensor_scalar(out=self.gates[:, t, :], in0=c(4), scalar1=sc(4), scalar2=None, op0=ALU.mult),
              r=[gd], w=[self.gatesd])
        yield

    def ffn(self, l):
        nc, kb = self.nc, self.kb
        moe = (l % 2 == 1)
        j = l // 2
        last = (l == self.n_layers - 1)
        if moe:
            E, FF = N_EXPERTS, FF_EXPERT
            wg_a, wu_a, wd_a = self.I["moe_w_gate"][j], self.I["moe_w_up"][j], self.I["moe_w_down"][j]
        else:
            E, FF = 1, FF_DENSE
            wg_a, wu_a, wd_a = [self.I[k][j:j + 1] for k in ("ffn_w_gate", "ffn_w_up", "ffn_w_down")]
        G = 2
        ngrp = FF // (128 * G)
        HT = 2048
        with ExitStack() as es:
            sb = lambda n, sh, dt: es.enter_context(nc.sbuf_tensor(self.un(n), sh, dt))
            acc = sb("f_acc", [128, 16, D], F32)
            accd = [Dep() for _ in range(16)]
            wg_r = Ring(sb("f_wg", [128, 2, 8, G * 128], BF16), 2)
            wu_r = Ring(sb("f_wu", [128, 2, 8, G * 128], BF16), 2)
            wd_r = Ring(sb("f_wd", [128, 2, G, D], BF16), 2)
            sg_r = Ring(sb("f_sg", [128, 2, 512], F32), 2)
            x_r = Ring(sb("f_x", [128, 2, D], F32), 2)
            junk_r = Ring(sb("f_junk", [128, 2, D], BF16), 2)
            st_r = Ring(sb("f_st", [128, 2, 8], F32), 2)
            gb = sb("f_gb", [128, 2, D], F32)
            gbd = Dep()
            self.load_ln_params(gb, gbd, l, 1)
            B = self.bank
            pg_r = BRing([B[0], B[1]])
            pu_r = BRing([B[2], B[3]])
            po_r = BRing([B[4], B[5], B[6], B[7]])
            tr = self.tr_ring(((4, 5), (6, 7)))
            hT_r = Ring(sb("f_hT2", [128, 2, G, HT], BF16), 2)
            for half in range(S // HT):
                groups = [(e_, grp) for e_ in range(E) for grp in range(ngrp)]
                N = len(groups)
                wgu = {}
                wdw = {}
                hTs = {}

                def load_gu(m):
                    e_, grp = groups[m]
                    ff0 = grp * G * 128
                    wg, wgd = wg_r.next()
                    wu, wud = wu_r.next()
                    kb.dma("pool", wg, wg_a[e_][:, ff0:ff0 + G * 128].rearrange("(k p) n -> p k n", p=128), w=[wgd])
                    kb.dma("pool", wu, wu_a[e_][:, ff0:ff0 + G * 128].rearrange("(k p) n -> p k n", p=128), w=[wud])
                    wgu[m] = (wg, wgd, wu, wud)

                def load_d(m):
                    e_, grp = groups[m]
                    ff0 = grp * G * 128
                    wdn, wdd = wd_r.next()
                    kb.dma("pool", wdn, wd_a[e_][ff0:ff0 + G * 128, :].rearrange("(g p) n -> p g n", p=128), w=[wdd])
                    wdw[m] = (wdn, wdd)

                def gu_units(m):
                    wg, wgd, wu, wud = wgu[m]
                    hT, hTd = hT_r.next()
                    hTs[m] = (hT, hTd)
                    us = []
                    for gi in range(G):
                        for tb in range(HT // 512):
                            def u(gi=gi, tb=tb):
                                tok = slice(half * HT + tb * 512, half * HT + (tb + 1) * 512)
                                xd_blk = self.XTd[(half * HT) // 512 + tb]
                                pg, pgd = pg_r.next()
                                pu, pud = pu_r.next()
                                for k in range(8):
                                    kb.op("pe", lambda e, k=k: e.matmul(pg, lhsT=wg[:, k, gi * 128:(gi + 1) * 128],
                                                                        rhs=self.XT[:, k, tok], start=(k == 0), stop=(k == 7)),
                                          r=[wgd, xd_blk], w=[pgd], mark=(k == 7))
                                for k in range(8):
                                    kb.op("pe", lambda e, k=k: e.matmul(pu, lhsT=wu[:, k, gi * 128:(gi + 1) * 128],
                                                                        rhs=self.XT[:, k, tok], start=(k == 0), stop=(k == 7)),
                                          r=[wud, xd_blk], w=[pud], mark=(k == 7))
                                sg, sgd = sg_r.next()
                                kb.op("act", lambda e: e.activation(out=sg, in_=pg, func=AF.Silu), r=[pgd], w=[sgd])
                                kb.op("dve", lambda e: e.tensor_tensor(out=hT[:, gi, tb * 512:(tb + 1) * 512], in0=pu, in1=sg,
                                                                       op=ALU.mult), r=[pud, sgd], w=[hTd])
                            us.append(u)
                    return us

                def dn_units(m):
                    e_, grp = groups[m]
                    wdn, wdd = wdw[m]
                    hT, hTd = hTs[m]
                    first = (m == 0)
                    us = []
                    for tt in range(HT // 128):
                        for hf in range(2):
                            def u(tt=tt, hf=hf):
                                t = half * (HT // 128) + tt
                                po, pod = po_r.next()
                                for gi in range(G):
                                    kb.op("pe", lambda e, gi=gi: e.matmul(po, lhsT=hT[:, gi, tt * 128:(tt + 1) * 128],
                                                                          rhs=wdn[:, gi, hf * 512:(hf + 1) * 512],
                                                                          start=(gi == 0), stop=(gi == G - 1)),
                                          r=[hTd, wdd], w=[pod], mark=(gi == G - 1))
                                a = acc[:, tt, hf * 512:(hf + 1) * 512]
                                if moe:
                                    gsc = self.gates[:, t, e_:e_ + 1]
                                    if first:
                                        kb.op("dve", lambda e: e.tensor_scalar(out=a, in0=po, scalar1=gsc, scalar2=None,
                                                                               op0=ALU.mult), r=[pod, self.gatesd], w=[accd[tt]])
                                    else:
                                        kb.op("dve", lambda e: e.scalar_tensor_tensor(out=a, in0=po, scalar=gsc, in1=a,
                                                                                      op0=ALU.mult, op1=ALU.add),
                                              r=[pod, self.gatesd, accd[tt]], w=[accd[tt]])
                                else:
                                    if first:
                                        kb.op("act", lambda e: e.copy(out=a, in_=po), r=[pod], w=[accd[tt]])
                                    else:
                                        kb.op("dve", lambda e: e.tensor_tensor(out=a, in0=po, in1=a, op=ALU.add),
                                              r=[pod, accd[tt]], w=[accd[tt]])
                            us.append(u)
                    return us

                load_gu(0)
                load_d(0)
                if N > 1:
                    load_gu(1)
                for u in gu_units(0):
                    u()
                for n in range(N):
                    if n + 1 < N:
                        load_d(n + 1)
                    if n + 2 < N:
                        load_gu(n + 2)
                    dn = dn_units(n)
                    gu = gu_units(n + 1) if n + 1 < N else []
                    per = len(dn) // max(len(gu), 1)
                    di = 0
                    for gi_, u in enumerate(gu):
                        u()
                        for _ in range(per):
                            dn[di]()
                            di += 1
                    while di < len(dn):
                        dn[di]()
                        di += 1
                def ep_gen(tt):
                    t = half * (HT // 128) + tt
                    x, xd = x_r.next()
                    jk, jkd = junk_r.next()
                    kb.dma("sp", x, self.xres[t * 128:(t + 1) * 128, :], w=[xd])
                    z = acc[:, tt, :]
                    zd = accd[tt]
                    kb.op("dve", lambda e: e.scalar_tensor_tensor(out=z, in0=x, scalar=float(ALPHA), in1=z,
                                                                  op0=ALU.mult, op1=ALU.add), r=[xd, zd], w=[zd])
                    yield
                    st, std = st_r.next()
                    yield from self.layer_norm_tile_gen(z, zd, jk, jkd, st, std, gb, gbd)
                    if last:
                        kb.dma("sp", self.out[t * 128:(t + 1) * 128, :], z, r=[zd])
                    else:
                        kb.dma("sp", self.xres[t * 128:(t + 1) * 128, :], z, r=[zd, xd])
                        self.transpose_to_XT(z, zd, t, tr)
                    yield

                for t0 in range(0, HT // 128, 2):
                    run_interleaved([ep_gen(t0 + g) for g in range(2)])


WEIGHT_KEYS = ("w_in", "w_out", "rwkv_mu", "rwkv_w0", "rwkv_w_up", "rwkv_a0", "rwkv_a_up", "rwkv_g_up",
               "rwkv_k_k", "rwkv_k_a", "rwkv_r_k", "rwkv_ln_g", "rwkv_ln_b", "ret_gn_g", "ret_gn_b",
               "ln_g", "ln_b", "ffn_w_gate", "ffn_w_up", "ffn_w_down", "moe_router", "moe_w_gate",
               "moe_w_up", "moe_w_down")


def make_in_map(inputs, b, consts, shared=None):
    m = {}
    m["x"] = np.ascontiguousarray(np.asarray(inputs["x"])[b], dtype=np.float32)
    if shared is None:
        shared = make_shared(inputs, consts)
    m.update(shared)
    return m


def make_shared(inputs, consts):
    sh = {}
    for k in WEIGHT_KEYS:
        sh[k] = np.ascontiguousarray(np.asarray(inputs[k]), dtype=np.float32)
    rb = np.asarray(inputs["rel_bias"], dtype=np.float32)
    idx = bias_bucket_index()
    g = rb[idx]
    sh["biasg"] = np.ascontiguousarray(np.transpose(g, (0, 4, 1, 2, 3)))
    sh["cst"] = consts["cst"]
    sh["cos2"] = consts["cos2"]
    sh["sin2"] = consts["sin2"]
    sh["amask"] = consts["amask"]
    return sh


_PROG = None


def kernel(**inputs):
    global _PROG
    if _PROG is None:
        p = Prog()
        p.build()
        _PROG = p
    p = _PROG
    shared = make_shared(inputs, p.consts)
    in_maps = [make_in_map(inputs, b, p.consts, shared) for b in range(8)]
    res = run_bass_kernel_spmd(p.nc, in_maps, core_ids=list(range(8)))
    out = np.stack([np.asarray(r["out"], dtype=np.float32) for r in res.results], 0)
    return out
```

```python
import math
from contextlib import ExitStack
import numpy as np
import concourse.bass as bass
import concourse.mybir as mybir
from concourse.bass_utils import run_bass_kernel_spmd

F32 = mybir.dt.float32
BF16 = mybir.dt.bfloat16
ALU = mybir.AluOpType
AF = mybir.ActivationFunctionType
AX = mybir.AxisListType

D = 1024
S = 4096
DEPTH = 4
NT = S // 128
RWKV_IN = 1024
ATTN_IN = 1536
RET_IN = 1024
N_IN = 3584
FF_DENSE = 2816
FF_EXPERT = 3584
N_EXPERTS = 8
ALPHA = (2 * DEPTH) ** 0.25
LN_EPS = 1e-5
RWKV_GN_EPS = 64e-5
C0 = math.exp(-0.5)
PATTERNS = ((128, 1), (512, 4), (2048, 16))
NEG = -240000.0


class Dep:
    __slots__ = ("w", "r", "excl")

    def __init__(self, excl=False):
        self.w = None
        self.r = {}
        self.excl = excl


class Engine:
    def __init__(self, e, sem, idx):
        self.e = e
        self.sem = sem
        self.idx = idx
        self.cnt = 0
        self.seen = {}
        self.slots = []
        self.si = 0


class KB:
    NSLOT = 12

    def __init__(self, nc, es):
        self.nc = nc
        self.sems = []
        self.eng = {}
        for name, e in (("pe", nc.tensor), ("dve", nc.vector), ("act", nc.scalar),
                        ("pool", nc.gpsimd), ("sp", nc.sync)):
            sem = es.enter_context(nc.semaphore("s_" + name))
            self.sems.append(sem)
            self.eng[name] = Engine(e, sem, len(self.sems) - 1)
        for q in ("sp", "pool"):
            for i in range(self.NSLOT):
                sem = es.enter_context(nc.semaphore("d_%s%d" % (q, i)))
                self.sems.append(sem)
                self.eng[q].slots.append([len(self.sems) - 1, 0])
        self.nins = 0

    def _need(self, E, ev, raw=True):
        si, val = ev
        if val <= 0:
            return
        if si == E.idx and val > E.cnt:
            return
        if E.seen.get(si, 0) < val:
            E.e.wait_ge(self.sems[si], val)
            E.seen[si] = val

    def _waits(self, E, r, w):
        for d in r:
            if d.excl:
                if d.w is not None:
                    self._need(E, d.w)
                for si, v in d.r.items():
                    self._need(E, (si, v), raw=False)
            elif d.w is not None:
                self._need(E, d.w)
        for d in w:
            if d.w is not None:
                self._need(E, d.w, raw=False)
            for si, v in d.r.items():
                self._need(E, (si, v), raw=False)

    def _post(self, ev, r, w):
        for d in w:
            d.w = ev
            d.r = {}
        for d in r:
            if d.excl:
                d.w = ev
                d.r = {}
            elif d.r.get(ev[0], 0) < ev[1]:
                d.r[ev[0]] = ev[1]

    def op(self, eng, fn, r=(), w=(), mark=True):
        E = self.eng[eng]
        self._waits(E, r, w)
        ins = fn(E.e)
        self.nins += 1
        if mark:
            E.cnt += 1
            ins.then_inc(E.sem, 1)
            ev = (E.idx, E.cnt)
        else:
            ev = (E.idx, E.cnt + 1)
        self._post(ev, r, w)
        return ins

    def dma(self, q, out, in_, r=(), w=(), slow=False):
        E = self.eng[q]
        slot = E.slots[E.si]
        E.si = (E.si + 1) % len(E.slots)
        self._need(E, (slot[0], slot[1]))
        self._waits(E, r, w)
        if slow:
            ins = E.e.dma_start(out=out, in_=in_, allow_slow_non_contiguous=True)
        else:
            ins = E.e.dma_start(out=out, in_=in_)
        self.nins += 1
        slot[1] += 16
        ins.then_inc(self.sems[slot[0]], 16)
        ev = (slot[0], slot[1])
        self._post(ev, r, w)
        return ins

    def barrier(self):
        evs = []
        for E in self.eng.values():
            if E.cnt > 0:
                evs.append((E.idx, E.cnt))
            for s in E.slots:
                if s[1] > 0:
                    evs.append((s[0], s[1]))
        for E in self.eng.values():
            for ev in evs:
                if ev[0] != E.idx:
                    self._need(E, ev)


class Ring:
    def __init__(self, tile, n):
        self.t = tile
        self.n = n
        self.d = [Dep() for _ in range(n)]
        self.i = 0

    def next(self):
        i = self.i
        self.i = (i + 1) % self.n
        return self.t[:, i], self.d[i]


class BRing:
    def __init__(self, items):
        self.items = list(items)
        self.i = 0

    def next(self):
        it = self.items[self.i]
        self.i = (self.i + 1) % len(self.items)
        return it


def run_interleaved(gens):
    gens = list(gens)
    while gens:
        nxt = []
        for g in gens:
            try:
                next(g)
                nxt.append(g)
            except StopIteration:
                pass
        gens = nxt


def t5_bucket(dist):
    nb = 32
    max_exact = nb // 2
    large = max_exact + (np.log(np.maximum(dist, max_exact) / max_exact)
                         / math.log(2048 / max_exact) * (nb - max_exact)).astype(np.int32)
    return np.where(dist < max_exact, dist, np.minimum(large, nb - 1)).astype(np.int32)


CST = {}


def _cst_layout():
    off = 0
    for name, n in (("ident", 128), ("onesblk", 128), ("nSU", 128), ("nSL", 128), ("SU", 128),
                    ("nIU", 128), ("IU", 128), ("sel", 64), ("sel65", 64), ("dmaskT", 512),
                    ("zeta8", 4), ("xi", 256), ("scanm", 1024)):
        CST[name] = (off, n)
        off += n
    return off


CST_N = _cst_layout()


def make_consts():
    c = np.zeros((128, CST_N), np.float32)

    def put(name, arr):
        o, n = CST[name]
        c[:arr.shape[0], o:o + n] = arr.reshape(arr.shape[0], n)

    i = np.arange(128)
    put("ident", np.eye(128, dtype=np.float32))
    put("onesblk", (i[:, None] // 64 == i[None, :] // 64).astype(np.float32))
    su = (i[None, :] > i[:, None]).astype(np.float32)
    sl = (i[None, :] < i[:, None]).astype(np.float32)
    iu = (i[None, :] >= i[:, None]).astype(np.float32)
    put("nSU", -su)
    put("nSL", -sl)
    put("SU", su)
    put("nIU", -iu)
    put("IU", iu)
    put("sel", np.concatenate([np.eye(64), np.eye(64)], 0).astype(np.float32))
    s65 = np.zeros((128, 64), np.float32)
    s65[64, :] = 1.0
    put("sel65", s65)
    lg = np.log1p(-np.exp2(-5.0 - np.arange(4, dtype=np.float64)))
    m = np.arange(128)[:, None, None]
    n = np.arange(128)[None, None, :]
    h = lg[None, :, None]
    dm = np.where(n >= m, np.exp(h * np.maximum(n - m, 0)), 0.0) / 8.0
    put("dmaskT", dm.astype(np.float32))
    put("zeta8", (np.exp(lg[None, :] * (127 - np.arange(128)[:, None])) / 8.0).astype(np.float32))
    xi = np.zeros((128, 2, 128))
    for hp in range(2):
        for hh in range(2):
            xi[hh * 64:(hh + 1) * 64, hp, :] = np.exp(lg[2 * hp + hh] * (np.arange(128) + 1))[None, :]
    put("xi", xi.astype(np.float32))
    sm = np.ones((128, 1024), np.float32)
    sm[:, 0::64] = 0.0
    put("scanm", sm)
    half = 32
    inv = (10000.0 ** (-np.arange(half, dtype=np.float32) / half)).astype(np.float32)
    ang = (np.arange(S, dtype=np.float32)[None, :] * inv[:, None]).astype(np.float32)
    cos = np.cos(ang).astype(np.float32)
    sin = np.sin(ang).astype(np.float32)
    cos2 = np.concatenate([cos, cos, cos, cos], 0)
    sin2 = np.concatenate([-sin, sin, -sin, sin], 0)
    jj = np.arange(128)[:, None]
    ii = np.arange(128)[None, :]
    mk = np.zeros((2, 128, 128), np.float32)
    mk[0] = np.where(ii <= jj, 0.0, NEG)
    mk[1] = np.where(ii >= jj, 0.0, NEG)
    gamma_c = np.exp(lg * 128.0)
    return dict(cst=c, cos2=np.ascontiguousarray(cos2), sin2=np.ascontiguousarray(sin2), amask=mk,
                gamma_c=[float(g) for g in gamma_c])


def bias_bucket_index():
    jj = np.arange(128)[:, None]
    ii = np.arange(128)[None, :]
    out = np.zeros((3, 2, 128, 128), np.int64)
    for p, (win, dil) in enumerate(PATTERNS):
        rel_prev = np.clip(ii + 128 - jj, 0, None)
        rel_cur = np.clip(ii - jj, 0, None)
        out[p, 0] = t5_bucket(rel_prev * dil)
        out[p, 1] = t5_bucket(rel_cur * dil)
    return out


class Prog:
    def __init__(self, n_layers=DEPTH, dbg=False, stop=None):
        self.n_layers = n_layers
        self.dbg = dbg
        self.stop = stop
        self.consts = make_consts()
        self.nc = nc = bass.Bass("TRN2", target_bir_lowering=False)

        def din(name, shape, dt=F32):
            return nc.dram_tensor(name, list(shape), dt, kind="ExternalInput").ap()

        self.I = I = {}
        I["x"] = din("x", [S, D])
        I["w_in"] = din("w_in", [DEPTH, D, N_IN])
        I["w_out"] = din("w_out", [DEPTH, D, D])
        I["rwkv_mu"] = din("rwkv_mu", [DEPTH, RWKV_IN])
        for nm in ("rwkv_w0", "rwkv_a0", "rwkv_k_k", "rwkv_k_a", "rwkv_r_k", "rwkv_ln_g", "rwkv_ln_b",
                   "ret_gn_g", "ret_gn_b"):
            I[nm] = din(nm, [DEPTH, 256])
        I["rwkv_w_up"] = din("rwkv_w_up", [DEPTH, 64, 256])
        I["rwkv_a_up"] = din("rwkv_a_up", [DEPTH, 64, 256])
        I["rwkv_g_up"] = din("rwkv_g_up", [DEPTH, 128, 256])
        I["ln_g"] = din("ln_g", [DEPTH, 2, D])
        I["ln_b"] = din("ln_b", [DEPTH, 2, D])
        I["ffn_w_gate"] = din("ffn_w_gate", [2, D, FF_DENSE])
        I["ffn_w_up"] = din("ffn_w_up", [2, D, FF_DENSE])
        I["ffn_w_down"] = din("ffn_w_down", [2, FF_DENSE, D])
        I["moe_router"] = din("moe_router", [2, D, N_EXPERTS])
        I["moe_w_gate"] = din("moe_w_gate", [2, N_EXPERTS, D, FF_EXPERT])
        I["moe_w_up"] = din("moe_w_up", [2, N_EXPERTS, D, FF_EXPERT])
        I["moe_w_down"] = din("moe_w_down", [2, N_EXPERTS, FF_EXPERT, D])
        I["cst"] = din("cst", [128, CST_N])
        I["cos2"] = din("cos2", [128, S])
        I["sin2"] = din("sin2", [128, S])
        I["amask"] = din("amask", [2, 128, 128])
        I["biasg"] = din("biasg", [3, 8, 2, 128, 128])
        self.out = nc.dram_tensor("out", [S, D], F32, kind="ExternalOutput").ap()

        def scr(name, shape, dt):
            kind = "ExternalOutput" if dbg else "Internal"
            return nc.dram_tensor(name, list(shape), dt, kind=kind).ap()

        self.xres = scr("xres", [S, D], F32)
        self.pT = scr("pT", [N_IN, S], F32)
        self.vtok = scr("vtok", [S, 768], BF16)
        self.yT = scr("yT", [D, S], BF16)

    _uid = 0

    def un(self, n):
        self._uid += 1
        return "%s_%d" % (n, self._uid)

    def cst(self, name, rows=128):
        o, n = CST[name]
        return self.cstt[0:rows, o:o + n]

    def build(self):
        nc = self.nc
        with ExitStack() as es:
            self.kb = kb = KB(nc, es)
            self.arena = es.enter_context(nc.sbuf_tensor("arena", [128, 16384], F32))
            self.XT = self.arena[:, :].bitcast(BF16).rearrange("p (k s) -> p k s", k=8)
            self.XTd = [Dep() for _ in range(8)]
            self.pbank_t = es.enter_context(nc.psum_tensor("psum", [128, 8, 512], F32))
            self.bank = [(self.pbank_t[:, i], Dep(excl=True)) for i in range(8)]
            self.cstt = es.enter_context(nc.sbuf_tensor("cstt", [128, CST_N], F32))
            self.cstd = Dep()
            self.identb = es.enter_context(nc.sbuf_tensor("identb", [128, 128], BF16))
            self.gates = es.enter_context(nc.sbuf_tensor("gates", [128, NT, N_EXPERTS], F32))
            self.gatesd = Dep()
            kb.dma("sp", self.cstt[:], self.I["cst"], w=[self.cstd])
            kb.op("dve", lambda e: e.tensor_copy(out=self.identb[:], in_=self.cst("ident")),
                  r=[self.cstd], w=[self.cstd])
            self.load_x0()
            kb.barrier()
            for l in range(self.n_layers):
                self.layer(l)
                if self.stop is not None and self.stop[0] == l and self.stop[1] == "layer":
                    break
            kb.barrier()
        return nc

    def setup_bias(self):
        nc, kb = self.nc, self.kb
        with ExitStack() as es:
            self.bias8d = Dep()
            tg = es.enter_context(nc.sbuf_tensor(self.un("tg"), [128, 2, 16, 128], F32))
            mk = es.enter_context(nc.sbuf_tensor(self.un("mk"), [128, 2, 128], F32))
            mkd = Dep()
            kb.dma("sp", mk[:], self.I["amask"].rearrange("a j i -> j a i"), w=[mkd])
            ring = Ring(tg, 2)
            for p in range(3):
                t, d = ring.next()
                kb.dma("sp", t.rearrange("j (h a) i -> j h a i", a=2),
                       self.I["biasg"][p].rearrange("h a j i -> j h a i"), w=[d])
                for h in range(8):
                    kb.op("dve", lambda e, t=t, h=h, p=p: e.scalar_tensor_tensor(
                        out=self.bias8[:, p, h], in0=t[:, 2 * h:2 * h + 2, :], scalar=8.0, in1=mk[:],
                        op0=ALU.mult, op1=ALU.add), r=[d, mkd], w=[self.bias8d])
            kb.barrier()

    def load_x0(self):
        nc, kb = self.nc, self.kb
        with ExitStack() as es:
            xt = es.enter_context(nc.sbuf_tensor(self.un("x0t"), [128, 2, D], F32))
            xr = Ring(xt, 2)
            pr = self.tr_ring()
            for t in range(NT):
                x, xd = xr.next()
                kb.dma("sp", x, self.I["x"][t * 128:(t + 1) * 128, :], w=[xd])
                kb.dma("sp", self.xres[t * 128:(t + 1) * 128, :], x, r=[xd])
                self.transpose_to_XT(x, xd, t, pr)
            kb.barrier()

    def tr_ring(self, pairs=((0, 1), (2, 3))):
        items = []
        for a, b in pairs:
            ap = self.pbank_t[:, a:b + 1].rearrange("p a (b c) -> p (a b) c", c=128)
            items.append((ap, [self.bank[a][1], self.bank[b][1]]))
        return BRing(items)

    def transpose_to_XT(self, x, xd, t, pr, f32_out=None):
        kb = self.kb
        p, pds = pr.next()
        for k in range(8):
            kb.op("pe", lambda e, k=k: e.transpose(p[:, k, :], x[:, k * 128:(k + 1) * 128], self.cst("ident")),
                  r=[xd, self.cstd], w=pds, mark=(k == 7))
        xd_blk = self.XTd[t // 4]
        kb.op("act", lambda e: e.copy(out=self.XT[:, :, t * 128:(t + 1) * 128], in_=p), r=pds, w=[xd_blk])
        if f32_out is not None:
            o, od = f32_out
            kb.op("dve", lambda e: e.tensor_copy(out=o, in_=p), r=pds, w=[od])

    def layer(self, l):
        kb = self.kb
        self.inproj(l)
        kb.barrier()
        if self.stop == (l, "inproj"):
            return
        self.retention(l)
        kb.barrier()
        if self.stop == (l, "ret"):
            return
        self.attention(l)
        kb.barrier()
        if self.stop == (l, "attn"):
            return
        self.rwkv(l)
        kb.barrier()
        if self.stop == (l, "rwkv"):
            return
        self.outproj_ln1(l)
        kb.barrier()
        if self.stop == (l, "ln1"):
            return
        self.ffn(l)
        kb.barrier()

    def inproj(self, l):
        nc, kb = self.nc, self.kb
        w_in = self.I["w_in"][l]
        with ExitStack() as es:
            wt = es.enter_context(nc.sbuf_tensor(self.un("ip_w"), [128, 2, 8, 512], BF16))
            stg = es.enter_context(nc.sbuf_tensor(self.un("ip_s"), [128, 2, S], F32))
            wv = es.enter_context(nc.sbuf_tensor(self.un("ip_wv"), [128, 8, 768], BF16))
            vt = es.enter_context(nc.sbuf_tensor(self.un("ip_vt"), [128, 2, 768], BF16))
            wr, pr, vr = Ring(wt, 2), BRing(self.bank), Ring(vt, 2)
            sdeps = [[Dep() for _ in range(8)] for _ in range(2)]
            wvd = Dep()
            kb.dma("pool", wv[:, :, 0:512], w_in[:, 2048:2560].rearrange("(k p) n -> p k n", p=128), w=[wvd])
            kb.dma("pool", wv[:, :, 512:768], w_in[:, 3072:3328].rearrange("(k p) n -> p k n", p=128), w=[wvd])
            n_ev = 0
            si = 0
            for cb in range(7):
                w, wd = wr.next()
                kb.dma("pool", w, w_in[:, cb * 512:(cb + 1) * 512].rearrange("(k p) n -> p k n", p=128), w=[wd])
                for fc in range(4):
                    st = stg[:, si]
                    sd = sdeps[si]
                    si ^= 1
                    for tb in range(8):
                        p, pd = pr.next()
                        for k in range(8):
                            kb.op("pe", lambda e, k=k: e.matmul(
                                p, lhsT=w[:, k, fc * 128:(fc + 1) * 128], rhs=self.XT[:, k, tb * 512:(tb + 1) * 512],
                                start=(k == 0), stop=(k == 7)), r=[wd, self.XTd[tb]], w=[pd], mark=(k == 7))
                        o = st[:, tb * 512:(tb + 1) * 512]
                        if n_ev % 2 == 0:
                            kb.op("act", lambda e: e.copy(out=o, in_=p), r=[pd], w=[sd[tb]])
                        else:
                            kb.op("dve", lambda e: e.tensor_copy(out=o, in_=p), r=[pd], w=[sd[tb]])
                        n_ev += 1
                    row = (cb * 4 + fc) * 128
                    kb.dma("sp", self.pT[row:row + 128, :], st, r=sd)
            for t in range(NT):
                v, vd = vr.next()
                p1, pd1 = pr.next()
                p2, pd2 = pr.next()
                for k in range(8):
                    kb.op("pe", lambda e, k=k: e.matmul(p1, lhsT=self.XT[:, k, t * 128:(t + 1) * 128], rhs=wv[:, k, 0:512],
                                                        start=(k == 0), stop=(k == 7)),
                          r=[wvd, self.XTd[t // 4]], w=[pd1], mark=(k == 7))
                for k in range(8):
                    kb.op("pe", lambda e, k=k: e.matmul(p2[:, 0:256], lhsT=self.XT[:, k, t * 128:(t + 1) * 128],
                                                        rhs=wv[:, k, 512:768], start=(k == 0), stop=(k == 7)),
                          r=[wvd, self.XTd[t // 4]], w=[pd2], mark=(k == 7))
                kb.op("act", lambda e: e.copy(out=v[:, 0:512], in_=p1), r=[pd1], w=[vd])
                kb.op("dve", lambda e: e.tensor_copy(out=v[:, 512:768], in_=p2[:, 0:256]), r=[pd2], w=[vd])
                kb.dma("sp", self.vtok[t * 128:(t + 1) * 128, :], v, r=[vd])


    def abf(self, lo, hi, **kw):
        return self.arena[:, lo:hi].bitcast(BF16)

    def retention(self, l):
        nc, kb = self.nc, self.kb
        base = RWKV_IN + ATTN_IN
        pT = self.pT
        gam = self.consts["gamma_c"]
        H0, H1 = slice(0, 64), slice(64, 128)
        with ExitStack() as es:
            sb = lambda n, sh, dt: es.enter_context(nc.sbuf_tensor(self.un(n), sh, dt))
            pp = lambda n, sh, dt: es.enter_context(nc.psum_tensor(n, sh, dt))
            kr = self.abf(0, 2048)
            qm = [self.abf(2048, 4096), self.abf(4096, 6144)]
            qx = [self.abf(6144, 8192), self.abf(8192, 10240)]
            sgb = self.abf(10240, 12288)
            krd, qmd, qxd, sgd = Dep(), Dep(), Dep(), Dep()
            cs_t = sb("rt_cs", [128, 2, 2, 1024], F32)
            tq_t = sb("rt_tq", [128, 2, 1024], F32)
            tw_t = sb("rt_tw", [128, 2, 1024], F32)
            v = sb("rt_v", [128, NT, 256], BF16)
            yo = sb("rt_yo", [128, S], BF16)
            gnp = sb("rt_gnp", [128, 4], F32)
            rst = sb("rt_rst", [128, 64], F32)
            rstb = sb("rt_rstb", [128, 64], BF16)
            vd, yod, gnd, rsd, rsbd = Dep(), Dep(), Dep(), Dep(), Dep()
            kb.dma("sp", v[:], self.vtok[:, 512:768].rearrange("(t p) c -> p t c", p=128), w=[vd])
            kb.dma("sp", gnp[:, 0:2], self.I["ret_gn_g"][l].rearrange("(a p) -> p a", p=128), w=[gnd], slow=True)
            kb.dma("sp", gnp[:, 2:4], self.I["ret_gn_b"][l].rearrange("(a p) -> p a", p=128), w=[gnd], slow=True)
            csr, tqr, twr = Ring(cs_t, 2), Ring(tq_t, 2), Ring(tw_t, 2)
            B = self.bank
            bfv = lambda i: (B[i][0].bitcast(BF16)[:, 0:128], B[i][1])
            ps_s = BRing([(B[0][0][:, 0:256], B[0][1]), (B[1][0][:, 0:256], B[1][1])])
            ps_k = BRing([bfv(2)])
            ps_kv = BRing([(B[3][0][:, 0:128], B[3][1])])
            ps_o = BRing([(B[4][0][:, 0:128], B[4][1]), (B[5][0][:, 0:128], B[5][1])])
            ps_t = BRing([bfv(6)])
            sTm_r = Ring(sb("rt_sTm", [128, 2, 256], BF16), 2)
            kz_r = Ring(sb("rt_kz", [128, 2, 128], BF16), 2)
            osq_r = Ring(sb("rt_osq", [128, 2, 128], F32), 2)
            yn_r = Ring(sb("rt_yn", [128, 2, 128], BF16), 2)
            st_r = Ring(sb("rt_st", [128, 2, 16], F32), 2)
            tf_r = Ring(sb("rt_tf", [128, 2, 128], F32), 2)
            dmask = self.cst("dmaskT")
            zeta = self.cst("zeta8")
            xi = self.cst("xi").rearrange("p (a n) -> p a n", a=2)
            for hp in range(2):
                kb.op("pool", lambda e: e.memset(qm[0][H1, :], 0.0), w=[qmd])
                kb.op("pool", lambda e: e.memset(qm[1][H0, :], 0.0), w=[qmd])
                kb.op("pool", lambda e: e.memset(qx[0][H1, :], 0.0), w=[qxd])
                kb.op("pool", lambda e: e.memset(qx[1][H0, :], 0.0), w=[qxd])
                kb.op("pool", lambda e: e.memset(rst[:], 0.0), w=[rsd])
                for pc in range(4):
                    sl = slice(pc * 1024, (pc + 1) * 1024)
                    cs, csd = csr.next()
                    kb.dma("sp", cs[:, 0], self.I["cos2"][:, sl], w=[csd])
                    kb.dma("sp", cs[:, 1], self.I["sin2"][:, sl], w=[csd])
                    for which in range(2):
                        r0 = base + which * 256 + hp * 128
                        tq, tqd = tqr.next()
                        tw, twd = twr.next()
                        kb.dma("sp", tq, pT[r0:r0 + 128, sl], w=[tqd])
                        for blk in range(4):
                            src = r0 + (blk ^ 1) * 32
                            kb.dma("sp", tw[blk * 32:(blk + 1) * 32], pT[src:src + 32, sl], w=[twd])
                        kb.op("dve", lambda e: e.tensor_tensor(out=tq, in0=tq, in1=cs[:, 0], op=ALU.mult),
                              r=[csd], w=[tqd])
                        kb.op("pool", lambda e: e.tensor_tensor(out=tw, in0=tw, in1=cs[:, 1], op=ALU.mult),
                              r=[csd], w=[twd])
                        if which == 1:
                            kb.op("dve", lambda e: e.tensor_tensor(out=kr[:, sl], in0=tq, in1=tw, op=ALU.add),
                                  r=[tqd, twd], w=[krd])
                        else:
                            for hh, rows in enumerate((H0, H1)):
                                kb.op("dve", lambda e: e.tensor_tensor(out=qm[hh][rows, sl], in0=tq[rows], in1=tw[rows],
                                                                       op=ALU.add), r=[tqd, twd], w=[qmd])
                    r0 = base + 768 + hp * 128
                    tq, tqd = tqr.next()
                    kb.dma("sp", tq, pT[r0:r0 + 128, sl], w=[tqd])
                    kb.op("act", lambda e: e.activation(out=sgb[:, sl], in_=tq, func=AF.Silu), r=[tqd], w=[sgd])
                for hh, rows in enumerate((H0, H1)):
                    kb.op("pool", lambda e: e.tensor_tensor(
                        out=qx[hh][rows, :].rearrange("p (c n) -> p c n", n=128),
                        in0=qm[hh][rows, :].rearrange("p (c n) -> p c n", n=128),
                        in1=xi[rows, hp:hp + 1, :].broadcast_to([64, NT, 128]), op=ALU.mult),
                        r=[qmd, self.cstd], w=[qxd])
                def ret_gen(c):
                    cs = slice(c * 128, (c + 1) * 128)
                    p_s, p_sd = ps_s.next()
                    for hh in range(2):
                        kb.op("pe", lambda e: e.matmul(p_s[:, hh * 128:(hh + 1) * 128], lhsT=kr[:, cs], rhs=qm[hh][:, cs],
                                                       start=True, stop=True), r=[krd, qmd], w=[p_sd], mark=(hh == 1))
                    sTm, sTmd = sTm_r.next()
                    kb.op("dve", lambda e: e.tensor_tensor(out=sTm, in0=p_s, in1=dmask[:, hp * 256:(hp + 1) * 256],
                                                           op=ALU.mult), r=[p_sd, self.cstd], w=[sTmd])
                    yield
                    p_k, p_kd = ps_k.next()
                    kb.op("pe", lambda e: e.transpose(p_k, kr[:, cs], self.identb[:]), r=[krd, self.cstd], w=[p_kd])
                    kz, kzd = kz_r.next()
                    for hh in range(2):
                        h = 2 * hp + hh
                        kb.op("act", lambda e: e.mul(out=kz[:, hh * 64:(hh + 1) * 64], in_=p_k[:, hh * 64:(hh + 1) * 64],
                                                     mul=zeta[:, h:h + 1]), r=[p_kd, self.cstd], w=[kzd])
                    yield
                    p_o, p_od = ps_o.next()
                    for hh in range(2):
                        h = 2 * hp + hh
                        kb.op("pe", lambda e: e.matmul(p_o[:, hh * 64:(hh + 1) * 64], lhsT=sTm[:, hh * 128:(hh + 1) * 128],
                                                       rhs=v[:, c, h * 64:(h + 1) * 64], start=True, stop=(c == 0)),
                              r=[sTmd, vd], w=[p_od], mark=(c == 0 and hh == 1))
                        if c > 0:
                            kb.op("pe", lambda e: e.matmul(p_o[:, hh * 64:(hh + 1) * 64], lhsT=qx[hh][:, cs], rhs=rstb[:],
                                                           start=False, stop=True),
                                  r=[qxd, rsbd], w=[p_od], mark=(hh == 1))
                    if c < NT - 1:
                        p_kv, p_kvd = ps_kv.next()
                        kb.op("pe", lambda e: e.matmul(p_kv, lhsT=kz, rhs=v[:, c, hp * 128:(hp + 1) * 128],
                                                       start=True, stop=True), r=[kzd, vd], w=[p_kvd])
                        for hh, rows in enumerate((H0, H1)):
                            kb.op("dve", lambda e: e.scalar_tensor_tensor(
                                out=rst[rows, :], in0=rst[rows, :], scalar=gam[2 * hp + hh],
                                in1=p_kv[rows, hh * 64:(hh + 1) * 64], op0=ALU.mult, op1=ALU.add),
                                r=[p_kvd, rsbd], w=[rsd])
                        kb.op("dve", lambda e: e.tensor_copy(out=rstb[:], in_=rst[:]), r=[rsd], w=[rsbd])
                    yield
                    st, std = st_r.next()
                    osq, osqd = osq_r.next()
                    kb.op("act", lambda e: e.activation(out=osq, in_=p_o, func=AF.Square), r=[p_od], w=[osqd])
                    kb.op("dve", lambda e: e.reduce_sum(out=st[:, 0:2], in_=p_o.rearrange("p (h e) -> p h e", e=64),
                                                        axis=AX.X), r=[p_od], w=[std])
                    yield
                    kb.op("dve", lambda e: e.reduce_sum(out=st[:, 2:4], in_=osq.rearrange("p (h e) -> p h e", e=64),
                                                        axis=AX.X), r=[osqd], w=[std])
                    yield
                    yield from self.norm_stats_gen(st, std, 2, 64, LN_EPS)
                    yn, ynd = yn_r.next()
                    for hh in range(2):
                        kb.op("dve", lambda e: e.tensor_scalar(out=yn[:, hh * 64:(hh + 1) * 64], in0=p_o[:, hh * 64:(hh + 1) * 64],
                                                               scalar1=st[:, 12 + hh:13 + hh], scalar2=st[:, 14 + hh:15 + hh],
                                                               op0=ALU.mult, op1=ALU.add), r=[p_od, std], w=[ynd])
                    yield
                    p_t, p_td = ps_t.next()
                    kb.op("pe", lambda e: e.transpose(p_t, yn, self.identb[:]), r=[ynd, self.cstd], w=[p_td])
                    tf, tfd = tf_r.next()
                    kb.op("dve", lambda e: e.tensor_scalar(out=tf, in0=p_t, scalar1=gnp[:, hp:hp + 1],
                                                           scalar2=gnp[:, 2 + hp:3 + hp], op0=ALU.mult, op1=ALU.add),
                          r=[p_td, gnd], w=[tfd])
                    yield
                    kb.op("pool", lambda e: e.tensor_tensor(out=yo[:, cs], in0=tf, in1=sgb[:, cs], op=ALU.mult),
                          r=[tfd, sgd], w=[yod])
                    yield

                for c0 in range(0, NT, 2):
                    run_interleaved([ret_gen(c0 + g) for g in range(2)])
                kb.dma("sp", self.yT[768 + hp * 128:768 + (hp + 1) * 128, :], yo[:], r=[yod])

    def norm_stats_gen(self, st, std, n, cnt, eps):
        kb = self.kb
        c = lambda i: st[:, i * n:(i + 1) * n]
        kb.op("dve", lambda e: e.tensor_scalar(out=c(2), in0=c(0), scalar1=1.0 / cnt, scalar2=None, op0=ALU.mult),
              r=[std], w=[std])
        kb.op("dve", lambda e: e.tensor_tensor(out=c(3), in0=c(2), in1=c(2), op=ALU.mult), r=[std], w=[std])
        yield
        kb.op("dve", lambda e: e.scalar_tensor_tensor(out=c(4), in0=c(1), scalar=1.0 / cnt, in1=c(3),
                                                      op0=ALU.mult, op1=ALU.subtract), r=[std], w=[std])
        yield
        kb.op("act", lambda e: e.activation(out=c(5), in_=c(4), func=AF.Sqrt, bias=float(eps)), r=[std], w=[std])
        yield
        kb.op("dve", lambda e: e.reciprocal(out=c(6), in_=c(5)), r=[std], w=[std])
        yield
        kb.op("dve", lambda e: e.scalar_tensor_tensor(out=c(7), in0=c(2), scalar=-1.0, in1=c(6),
                                                      op0=ALU.mult, op1=ALU.mult), r=[std], w=[std])
        yield

    def norm_stats(self, st, std, n, cnt, eps):
        kb = self.kb
        c = lambda i: st[:, i * n:(i + 1) * n]
        kb.op("dve", lambda e: e.tensor_scalar(out=c(2), in0=c(0), scalar1=1.0 / cnt, scalar2=None, op0=ALU.mult),
              r=[std], w=[std])
        kb.op("dve", lambda e: e.tensor_tensor(out=c(3), in0=c(2), in1=c(2), op=ALU.mult), r=[std], w=[std])
        kb.op("dve", lambda e: e.scalar_tensor_tensor(out=c(4), in0=c(1), scalar=1.0 / cnt, in1=c(3),
                                                      op0=ALU.mult, op1=ALU.subtract), r=[std], w=[std])
        kb.op("dve", lambda e: e.tensor_scalar(out=c(4), in0=c(4), scalar1=float(eps), scalar2=None, op0=ALU.add),
              r=[std], w=[std])
        kb.op("act", lambda e: e.activation(out=c(5), in_=c(4), func=AF.Sqrt), r=[std], w=[std])
        kb.op("dve", lambda e: e.reciprocal(out=c(6), in_=c(5)), r=[std], w=[std])
        kb.op("dve", lambda e: e.scalar_tensor_tensor(out=c(7), in0=c(2), scalar=-1.0, in1=c(6),
                                                      op0=ALU.mult, op1=ALU.mult), r=[std], w=[std])

    def attention(self, l):
        nc, kb = self.nc, self.kb
        base = RWKV_IN
        pT = self.pT
        H0, H1 = slice(0, 64), slice(64, 128)
        with ExitStack() as es:
            sb = lambda n, sh, dt: es.enter_context(nc.sbuf_tensor(self.un(n), sh, dt))
            self.bias8 = sb("bias8", [128, 3, 8, 2, 128], BF16)
            self.setup_bias()
            acc = self.arena[0:64, :].rearrange("p (a h s) -> p a h s", a=2, h=2)
            accd = Dep()
            kt = sb("at_k", [128, S], BF16)
            qm = [sb("at_q0", [128, S], BF16), sb("at_q1", [128, S], BF16)]
            vp = [sb("at_v%d" % i, [128, NT, 128], BF16) for i in range(3)]
            ones = sb("at_ones", [128, 64], BF16)
            PT_r = Ring(sb("at_PT", [128, 4, 512], BF16), 4)
            rc = sb("at_rc", [64, 2, S], F32)
            yo = sb("at_yo", [64, 2, S], BF16)
            ktd, qmd, vd, od, rcd, yod = Dep(), Dep(), Dep(), Dep(), Dep(), Dep()
            kb.op("pool", lambda e: e.memset(ones[:], 1.0), w=[od])
            B = self.bank
            ps_s = BRing([B[0], B[1], B[2], B[3]])
            ps_o = BRing([B[4], B[5], B[6], B[7]])
            for pair in range(4):
                kb.op("pool", lambda e: e.memset(qm[0][H1, :], 0.0), w=[qmd])
                kb.op("pool", lambda e: e.memset(qm[1][H0, :], 0.0), w=[qmd])
                kb.op("dve", lambda e: e.memset(self.arena[0:64, :], 0.0), w=[accd])
                q0 = base + pair * 128
                k0 = base + 512 + pair * 128
                kb.dma("pool", qm[0][H0, :], pT[q0:q0 + 64, :], w=[qmd])
                kb.dma("pool", qm[1][H1, :], pT[q0 + 64:q0 + 128, :], w=[qmd])
                kb.dma("pool", kt[:], pT[k0:k0 + 128, :], w=[ktd])
                for p, (win, dil) in enumerate(PATTERNS):
                    nb = NT // dil
                    for r in range(dil):
                        src = self.vtok[:, pair * 128:(pair + 1) * 128].rearrange("(b j r) c -> r j b c", r=dil, j=128)[r]
                        kb.dma("sp", vp[p][:, r * nb:(r + 1) * nb, :], src, w=[vd])
                units = [(p, dil, NT // dil, r, b) for p, (win, dil) in enumerate(PATTERNS)
                         for r in range(dil) for b in range(NT // dil)]

                def tsl(dil, r, bb):
                    st = r + dil * 128 * bb
                    return slice(st, st + dil * 127 + 1, dil)

                def emit_scores(u):
                    p, dil, nb, r, b = u
                    qs = tsl(dil, r, b)
                    halves = (0, 1) if b > 0 else (1,)
                    p_s, p_sd = ps_s.next()
                    for hh in range(2):
                        h = 2 * pair + hh
                        for half in halves:
                            ks = tsl(dil, r, b - 1) if half == 0 else qs
                            o = p_s[:, (hh * 2 + half) * 128:(hh * 2 + half + 1) * 128]
                            kb.op("pe", lambda e: e.matmul(o, lhsT=kt[:, ks], rhs=qm[hh][:, qs], start=True, stop=False),
                                  r=[ktd, qmd], w=[p_sd], mark=False)
                            kb.op("pe", lambda e: e.matmul(o, lhsT=self.identb[:], rhs=self.bias8[:, p, h, half, :],
                                                           start=False, stop=True),
                                  r=[self.cstd, self.bias8d], w=[p_sd], mark=(hh == 1 and half == 1))
                    PT, PTd = PT_r.next()
                    if b > 0:
                        kb.op("act", lambda e: e.activation(out=PT, in_=p_s, func=AF.Exp, scale=0.125),
                              r=[p_sd], w=[PTd])
                    else:
                        v4 = lambda t: t.rearrange("p (h a i) -> p h a i", h=2, a=2)[:, :, 1, :]
                        kb.op("act", lambda e: e.activation(out=v4(PT), in_=v4(p_s), func=AF.Exp, scale=0.125),
                              r=[p_sd], w=[PTd])
                    return PT, PTd

                def emit_pv(u, ctx):
                    p, dil, nb, r, b = u
                    PT, PTd = ctx
                    qs = tsl(dil, r, b)
                    halves = (0, 1) if b > 0 else (1,)
                    p_o, p_od = ps_o.next()
                    for nd in range(2):
                        for hh in range(2):
                            o = p_o[0:64, (nd * 2 + hh) * 128:(nd * 2 + hh + 1) * 128]
                            for hi, half in enumerate(halves):
                                ti = r * nb + (b - 1 if half == 0 else b)
                                lw = vp[p][:, ti, hh * 64:(hh + 1) * 64] if nd == 0 else ones[:]
                                kb.op("pe", lambda e: e.matmul(o, lhsT=lw, rhs=PT[:, (hh * 2 + half) * 128:(hh * 2 + half + 1) * 128],
                                                               start=(hi == 0), stop=(hi == len(halves) - 1)),
                                      r=[PTd, vd, od], w=[p_od], mark=(nd == 1 and hh == 1 and hi == len(halves) - 1))
                    kb.op("dve", lambda e: e.tensor_tensor(
                        out=acc[:, :, :, qs], in0=acc[:, :, :, qs],
                        in1=p_o[0:64, :].rearrange("p (a h i) -> p a h i", a=2, h=2), op=ALU.add),
                        r=[p_od, accd], w=[accd])

                LOOK = 2
                ctxs = {}
                for i in range(min(LOOK, len(units))):
                    ctxs[i] = emit_scores(units[i])
                for i, u in enumerate(units):
                    if i + LOOK < len(units):
                        ctxs[i + LOOK] = emit_scores(units[i + LOOK])
                    emit_pv(u, ctxs.pop(i))
                kb.op("dve", lambda e: e.reciprocal(out=rc[:], in_=acc[:, 1]), r=[accd], w=[rcd])
                kb.op("dve", lambda e: e.tensor_tensor(out=yo[:], in0=acc[:, 0], in1=rc[:], op=ALU.mult),
                      r=[accd, rcd], w=[yod])
                for hh in range(2):
                    r0 = 256 + (2 * pair + hh) * 64
                    kb.dma("sp", self.yT[r0:r0 + 64, :], yo[:, hh, :], r=[yod])

    def rwkv(self, l):
        nc, kb = self.nc, self.kb
        pT = self.pT
        I = self.I
        H0, H1 = slice(0, 64), slice(64, 128)
        SEG = 1024
        NCH = SEG // 64
        with ExitStack() as es:
            sb = lambda n, sh, dt: es.enter_context(nc.sbuf_tensor(self.un(n), sh, dt))
            U = [self.arena[:, i * SEG:(i + 1) * SEG] for i in range(16)]
            Ud = [Dep() for _ in range(16)]
            prm = sb("rw_prm", [128, 32], F32)
            prd = Dep()
            pcol = {}
            kb.dma("sp", prm[:, 0:8], I["rwkv_mu"][l].rearrange("(c p) -> p c", p=128), w=[prd], slow=True)
            for i, nm in enumerate(("rwkv_w0", "rwkv_a0", "rwkv_k_k", "rwkv_k_a", "rwkv_r_k", "rwkv_ln_g", "rwkv_ln_b")):
                pcol[nm] = 8 + 2 * i
                kb.dma("sp", prm[:, 8 + 2 * i:10 + 2 * i], I[nm][l].rearrange("(a p) -> p a", p=128), w=[prd], slow=True)
            pcol["omka"] = 22
            kb.op("dve", lambda e: e.tensor_scalar(out=prm[:, 22:24], in0=prm[:, pcol["rwkv_k_a"]:pcol["rwkv_k_a"] + 2],
                                                   scalar1=-1.0, scalar2=1.0, op0=ALU.mult, op1=ALU.add), r=[prd], w=[prd])
            P = lambda nm, hp: prm[:, pcol[nm] + hp:pcol[nm] + hp + 1]
            wlw = sb("rw_wlw", [128, 256], BF16)
            wla = sb("rw_wla", [128, 256], BF16)
            gup = sb("rw_gup", [128, 256], BF16)
            wd = Dep()
            kb.op("pool", lambda e: e.memset(wlw[H1, :], 0.0), w=[wd])
            kb.op("pool", lambda e: e.memset(wla[H0, :], 0.0), w=[wd])
            kb.dma("pool", wlw[H0, :], I["rwkv_w_up"][l], w=[wd])
            kb.dma("pool", wla[H1, :], I["rwkv_a_up"][l], w=[wd])
            kb.dma("pool", gup[:], I["rwkv_g_up"][l], w=[wd])
            pad = {}
            padd = {}
            for nm in ("Rt", "Kk", "Bt", "Kt", "V"):
                pad[nm] = sb("rw_p" + nm, [128, NCH, 128], F32)
                padd[nm] = Dep()
                kb.op("pool", lambda e, nm=nm: e.memset(pad[nm][:], 0.0), w=[padd[nm]])
            T6 = sb("rw_T6", [128, SEG], BF16)
            SG = sb("rw_SG", [128, SEG], BF16)
            T6d, SGd = Dep(), Dep()
            buf_r = Ring(sb("rw_buf", [128, 2, SEG + 1], F32), 2)
            tmp_r = Ring(sb("rw_tmp", [128, 2, SEG], F32), 2)
            yo_r = Ring(sb("rw_yo", [128, 2, SEG], BF16), 2)
            Hs = [sb("rw_H%d" % i, [128, 128], F32) for i in range(2)]
            Hd = [Dep(), Dep()]
            import os
            NW = int(os.environ.get('RW_NW', '4'))
            wk = []
            for g in range(NW):
                t = {}
                for nm in ("nBtPT", "KtPT", "VPT", "N", "NT", "MakT", "nPrbT", "PrkT", "Pa", "Pb", "PT1", "PT2", "PT4",
                           "PT8", "PT16", "PT32", "QT", "GT0", "E", "YnP", "sq"):
                    t[nm] = (sb("rw_%s%d" % (nm, g), [128, 128], F32), Dep())
                for nm in ("Xa", "Xb"):
                    t[nm] = (sb("rw_%s%d" % (nm, g), [128, 256], F32), Dep())
                t["st"] = (sb("rw_st%d" % g, [128, 8], F32), Dep())
                kb.op("pool", lambda e, t=t: e.memset(t["YnP"][0][:], 0.0), w=[t["YnP"][1]])
                wk.append(t)
            banks = BRing(self.bank)
            ident = self.cst("ident")
            cd = self.cstd
            evc = [0]

            def copy_ev(out, in_, r, w, scale=None):
                evc[0] += 1
                if evc[0] % 2 == 0:
                    if scale is None:
                        kb.op("act", lambda e: e.copy(out=out, in_=in_), r=r, w=w)
                    else:
                        kb.op("act", lambda e: e.mul(out=out, in_=in_, mul=float(scale)), r=r, w=w)
                else:
                    if scale is None:
                        kb.op("dve", lambda e: e.tensor_copy(out=out, in_=in_), r=r, w=w)
                    else:
                        kb.op("dve", lambda e: e.tensor_scalar(out=out, in0=in_, scalar1=float(scale), scalar2=None,
                                                               op0=ALU.mult), r=r, w=w)

            def load_shift(row0, seg, c8, dst, dstd):
                buf, bd = buf_r.next()
                tmp, td = tmp_r.next()
                t0 = seg * SEG
                if seg == 0:
                    kb.op("pool", lambda e: e.memset(buf[:, 0:1], 0.0), w=[bd])
                    kb.dma("sp", buf[:, 1:SEG + 1], pT[row0:row0 + 128, 0:SEG], w=[bd])
                else:
                    kb.dma("sp", buf[:, 0:SEG + 1], pT[row0:row0 + 128, t0 - 1:t0 + SEG], w=[bd])
                kb.op("pool", lambda e: e.tensor_tensor(out=tmp, in0=buf[:, 0:SEG], in1=buf[:, 1:SEG + 1], op=ALU.subtract),
                      r=[bd], w=[td])
                kb.op("dve", lambda e: e.scalar_tensor_tensor(out=dst, in0=tmp, scalar=prm[:, c8:c8 + 1], in1=buf[:, 1:SEG + 1],
                                                              op0=ALU.mult, op1=ALU.add), r=[td, bd, prd], w=[dstd])

            def mm_to(lhsT, rhs, rd, n=128):
                bk, bd = banks.next()
                o = bk[:, 0:n]
                kb.op("pe", lambda e: e.matmul(o, lhsT=lhsT, rhs=rhs, start=True, stop=True), r=rd, w=[bd])
                return o, bd

            def chunk_gen(hp, seg, c, g):
                t = wk[g]
                gc = seg * NCH + c
                A = lambda nm: t[nm][0][:]
                Dp = lambda nm: t[nm][1]
                Rt, Kk, Bt, Kt, V = (pad[nm][:, c, :] for nm in ("Rt", "Kk", "Bt", "Kt", "V"))
                dR, dKk, dB, dK, dV = (padd[nm] for nm in ("Rt", "Kk", "Bt", "Kt", "V"))
                X = [t["Xa"], t["Xb"]]
                for src, sd, dst, dd, sc in ((Bt, dB, A("nBtPT"), Dp("nBtPT"), -1.0), (Kt, dK, A("KtPT"), Dp("KtPT"), None),
                                             (V, dV, A("VPT"), Dp("VPT"), None), (Kk, dKk, X[0][0][:, 0:128], X[0][1], None)):
                    bk, bd = banks.next()
                    kb.op("pe", lambda e: e.transpose(bk[:, 0:128], src, ident), r=[sd, cd], w=[bd])
                    copy_ev(dst, bk[:, 0:128], [bd], [dd], sc)
                yield
                for lh, ld, rh, rdd, dst, msk in ((Bt, dB, Kk, dKk, "NT", "nSU"), (Kk, dKk, Bt, dB, "N", "nSL"),
                                                 (Kt, dK, Kk, dKk, "MakT", "SU"), (Bt, dB, Rt, dR, "nPrbT", "nIU"),
                                                 (Kt, dK, Rt, dR, "PrkT", "IU")):
                    o, bd = mm_to(lh, rh, [ld, rdd])
                    kb.op("dve", lambda e: e.tensor_tensor(out=A(dst), in0=o, in1=self.cst(msk), op=ALU.mult),
                          r=[bd, cd], w=[Dp(dst)])
                yield
                o, bd = mm_to(A("MakT"), A("VPT"), [Dp("MakT"), Dp("VPT")])
                copy_ev(X[0][0][:, 128:256], o, [bd], [X[0][1]])
                yield
                Pc, PTc = ("N", "NT")
                lv_names = {2: "PT2", 4: "PT4", 8: "PT8", 16: "PT16", 32: "PT32"}
                pt_list = ["NT"]
                pbuf = ["Pa", "Pb"]
                pi = 0
                for lvl in (2, 4, 8, 16, 32):
                    o2, bd2 = mm_to(A(Pc), A(PTc), [Dp(Pc), Dp(PTc)])
                    if lvl < 32:
                        o1, bd1 = mm_to(A(PTc), A(Pc), [Dp(Pc), Dp(PTc)])
                    copy_ev(A(lv_names[lvl]), o2, [bd2], [Dp(lv_names[lvl])])
                    if lvl < 32:
                        copy_ev(A(pbuf[pi]), o1, [bd1], [Dp(pbuf[pi])])
                        Pc = pbuf[pi]
                        pi ^= 1
                    PTc = lv_names[lvl]
                    pt_list.append(PTc)
                    yield
                xi_ = 0
                for nm in reversed(pt_list):
                    Xc, Xn = X[xi_], X[xi_ ^ 1]
                    o, bd = mm_to(A(nm), Xc[0][:], [Dp(nm), Xc[1]], n=256)
                    kb.op("dve", lambda e: e.tensor_tensor(out=Xn[0][:], in0=o, in1=Xc[0][:], op=ALU.add),
                          r=[bd, Xc[1]], w=[Xn[1]])
                    xi_ ^= 1
                    yield
                W, Wd = X[xi_]
                W1, W2 = W[:, 0:128], W[:, 128:256]
                o, bd = mm_to(W1, A("nPrbT"), [Wd, Dp("nPrbT")])
                kb.op("dve", lambda e: e.tensor_tensor(out=A("QT"), in0=o, in1=Rt, op=ALU.add), r=[bd, dR], w=[Dp("QT")])
                yield
                o, bd = mm_to(W1, A("nBtPT"), [Wd, Dp("nBtPT")])
                kb.op("dve", lambda e: e.tensor_tensor(out=A("GT0"), in0=o, in1=ident, op=ALU.add), r=[bd, cd], w=[Dp("GT0")])
                bk, bd = banks.next()
                o = bk[:, 0:128]
                kb.op("pe", lambda e: e.matmul(o, lhsT=A("KtPT"), rhs=A("VPT"), start=True, stop=False),
                      r=[Dp("KtPT"), Dp("VPT")], w=[bd], mark=False)
                kb.op("pe", lambda e: e.matmul(o, lhsT=A("nBtPT"), rhs=W2, start=False, stop=True),
                      r=[Dp("nBtPT"), Wd], w=[bd])
                dc = U[12][:, c * 64 + 63:c * 64 + 64]
                kb.op("dve", lambda e: e.tensor_scalar(out=A("E"), in0=o, scalar1=dc, scalar2=None, op0=ALU.mult),
                      r=[bd, Ud[12]], w=[Dp("E")])
                yield
                Hc, Hcd = Hs[gc % 2], Hd[gc % 2]
                Hn, Hnd = Hs[(gc + 1) % 2], Hd[(gc + 1) % 2]
                bk, ybd = banks.next()
                yps = bk[:, 0:128]
                kb.op("pe", lambda e: e.matmul(yps, lhsT=A("QT"), rhs=Hc[:], start=True, stop=False),
                      r=[Dp("QT"), Hcd], w=[ybd], mark=False)
                kb.op("pe", lambda e: e.matmul(yps, lhsT=A("PrkT"), rhs=A("VPT"), start=False, stop=False),
                      r=[Dp("PrkT"), Dp("VPT")], w=[ybd], mark=False)
                kb.op("pe", lambda e: e.matmul(yps, lhsT=A("nPrbT"), rhs=W2, start=False, stop=True),
                      r=[Dp("nPrbT"), Wd], w=[ybd])
                o, bd = mm_to(A("GT0"), Hc[:], [Dp("GT0"), Hcd])
                kb.op("dve", lambda e: e.scalar_tensor_tensor(out=Hn[:], in0=o, scalar=dc, in1=A("E"), op0=ALU.mult, op1=ALU.add),
                      r=[bd, Dp("E"), Ud[12]], w=[Hnd])
                yield
                st, std = t["st"]
                kb.op("act", lambda e: e.activation(out=A("sq"), in_=yps, func=AF.Square), r=[ybd], w=[Dp("sq")])
                kb.op("dve", lambda e: e.reduce_sum(out=st[:, 0:1], in_=yps, axis=AX.X), r=[ybd], w=[std])
                yield
                kb.op("dve", lambda e: e.reduce_sum(out=st[:, 1:2], in_=A("sq"), axis=AX.X), r=[Dp("sq")], w=[std])
                yield
                yield from self.norm_stats_gen(st, std, 1, 64, RWKV_GN_EPS)
                for rows in (H0, H1):
                    kb.op("dve", lambda e: e.tensor_scalar(out=t["YnP"][0][rows, rows], in0=yps[rows, rows],
                                                           scalar1=st[rows, 6:7], scalar2=st[rows, 7:8],
                                                           op0=ALU.mult, op1=ALU.add), r=[ybd, std], w=[Dp("YnP")])
                yield
                o, bd = mm_to(A("YnP"), self.cst("sel"), [Dp("YnP"), cd], n=64)
                yield
                kb.op("dve", lambda e: e.tensor_scalar(out=U[10][:, c * 64:(c + 1) * 64], in0=o,
                                                       scalar1=P("rwkv_ln_g", hp), scalar2=P("rwkv_ln_b", hp),
                                                       op0=ALU.mult, op1=ALU.add), r=[bd, prd], w=[Ud[10]])
                yield

            for hp in range(2):
                for i in range(2):
                    kb.op("pool", lambda e, i=i: e.memset(Hs[i][:], 0.0), w=[Hd[i]])
                for seg in range(S // SEG):
                    blk2 = [slice(0, 512), slice(512, 1024)]
                    load_shift(768, seg, 6, U[8], Ud[8])
                    kb.op("act", lambda e: e.activation(out=T6[H0, :], in_=U[8][H0, :], func=AF.Tanh), r=[Ud[8]], w=[T6d])
                    kb.op("act", lambda e: e.copy(out=T6[H1, :], in_=U[8][H1, :]), r=[Ud[8]], w=[T6d])
                    load_shift(896, seg, 7, U[9], Ud[9])
                    kb.op("act", lambda e: e.activation(out=SG[:], in_=U[9], func=AF.Sigmoid), r=[Ud[9]], w=[SGd])
                    load_shift(hp * 128, seg, hp, U[0], Ud[0])
                    load_shift(256 + hp * 128, seg, 2 + hp, U[1], Ud[1])
                    load_shift(512 + hp * 128, seg, 4 + hp, U[2], Ud[2])
                    hs = slice(hp * 128, (hp + 1) * 128)
                    for b2 in blk2:
                        o, bd = mm_to(wlw[:, hs], T6[:, b2], [wd, T6d], n=512)
                        kb.op("act", lambda e: e.activation(out=U[4][:, b2], in_=o, func=AF.Sigmoid, bias=P("rwkv_w0", hp)),
                              r=[bd, prd], w=[Ud[4]])
                        o, bd = mm_to(wla[:, hs], T6[:, b2], [wd, T6d], n=512)
                        kb.op("act", lambda e: e.activation(out=U[3][:, b2], in_=o, func=AF.Sigmoid, bias=P("rwkv_a0", hp)),
                              r=[bd, prd], w=[Ud[3]])
                        o, bd = mm_to(gup[:, hs], SG[:, b2], [wd, SGd], n=512)
                        kb.op("act", lambda e: e.copy(out=U[5][:, b2], in_=o), r=[bd], w=[Ud[5]])
                    kb.op("dve", lambda e: e.tensor_scalar(out=U[6], in0=U[1], scalar1=P("rwkv_k_k", hp), scalar2=None,
                                                           op0=ALU.mult), r=[Ud[1], prd], w=[Ud[6]])
                    kb.op("pool", lambda e: e.tensor_tensor(out=U[8], in0=U[6], in1=U[6], op=ALU.mult), r=[Ud[6]], w=[Ud[8]])
                    for b2 in blk2:
                        o, bd = mm_to(self.cst("onesblk"), U[8][:, b2], [cd, Ud[8]], n=512)
                        kb.op("dve", lambda e: e.tensor_scalar(out=U[11][:, b2], in0=o, scalar1=1e-24, scalar2=None,
                                                               op0=ALU.max), r=[bd], w=[Ud[11]])
                    kb.op("act", lambda e: e.activation(out=U[11], in_=U[11], func=AF.Sqrt), r=[Ud[11]], w=[Ud[11]])
                    kb.op("dve", lambda e: e.reciprocal(out=U[11], in_=U[11]), r=[Ud[11]], w=[Ud[11]])
                    kb.op("pool", lambda e: e.tensor_tensor(out=U[6], in0=U[6], in1=U[11], op=ALU.mult),
                          r=[Ud[6], Ud[11]], w=[Ud[6]])
                    kb.op("dve", lambda e: e.tensor_scalar(out=U[7], in0=U[3], scalar1=P("rwkv_k_a", hp), scalar2=P("omka", hp),
                                                           op0=ALU.mult, op1=ALU.add), r=[Ud[3], prd], w=[Ud[7]])
                    kb.op("pool", lambda e: e.tensor_tensor(out=U[7], in0=U[7], in1=U[1], op=ALU.mult),
                          r=[Ud[7], Ud[1]], w=[Ud[7]])
                    kb.op("dve", lambda e: e.scalar_tensor_tensor(out=U[8], in0=U[0], scalar=P("rwkv_r_k", hp), in1=U[7],
                                                                  op0=ALU.mult, op1=ALU.mult), r=[Ud[0], Ud[7], prd], w=[Ud[8]])
                    for b2 in blk2:
                        o, bd = mm_to(self.cst("onesblk"), U[8][:, b2], [cd, Ud[8]], n=512)
                        kb.op("dve", lambda e: e.tensor_tensor(out=U[9][:, b2], in0=o, in1=U[2][:, b2], op=ALU.mult),
                              r=[bd, Ud[2]], w=[Ud[9]])
                    kb.op("pool", lambda e: e.tensor_tensor(out=U[3], in0=U[3], in1=U[6], op=ALU.mult),
                          r=[Ud[3], Ud[6]], w=[Ud[3]])
                    kb.op("dve", lambda e: e.tensor_tensor_scan(out=U[11], data0=self.cst("scanm"), data1=U[4], initial=0.0,
                                                                op0=ALU.mult, op1=ALU.add), r=[Ud[4], cd], w=[Ud[11]])
                    kb.op("act", lambda e: e.activation(out=U[12], in_=U[11], func=AF.Exp, scale=-C0), r=[Ud[11]], w=[Ud[12]])
                    kb.op("act", lambda e: e.activation(out=U[13], in_=U[11], func=AF.Exp, scale=C0), r=[Ud[11]], w=[Ud[13]])
                    kb.op("pool", lambda e: e.tensor_tensor(out=U[4], in0=U[11], in1=U[4], op=ALU.subtract),
                          r=[Ud[11], Ud[4]], w=[Ud[4]])
                    kb.op("act", lambda e: e.activation(out=U[4], in_=U[4], func=AF.Exp, scale=-C0), r=[Ud[4]], w=[Ud[4]])
                    kb.op("pool", lambda e: e.tensor_tensor(out=U[0], in0=U[0], in1=U[12], op=ALU.mult), r=[Ud[0], Ud[12]], w=[Ud[0]])
                    kb.op("dve", lambda e: e.tensor_tensor(out=U[6], in0=U[6], in1=U[4], op=ALU.mult), r=[Ud[6], Ud[4]], w=[Ud[6]])
                    kb.op("pool", lambda e: e.tensor_tensor(out=U[3], in0=U[3], in1=U[13], op=ALU.mult), r=[Ud[3], Ud[13]], w=[Ud[3]])
                    kb.op("dve", lambda e: e.tensor_tensor(out=U[7], in0=U[7], in1=U[13], op=ALU.mult), r=[Ud[7], Ud[13]], w=[Ud[7]])
                    ci = 0
                    for nm, ui in (("Rt", 0), ("Kk", 6), ("Bt", 3), ("Kt", 7), ("V", 2)):
                        for rows in (H0, H1):
                            dst = pad[nm][rows, :, rows]
                            src = U[ui][rows, :].rearrange("p (c t) -> p c t", t=64)
                            eng = ("pool", "dve", "act")[ci % 3]
                            ci += 1
                            if eng == "act":
                                kb.op("act", lambda e: e.copy(out=dst, in_=src), r=[Ud[ui]], w=[padd[nm]])
                            else:
                                kb.op(eng, lambda e: e.tensor_copy(out=dst, in_=src), r=[Ud[ui]], w=[padd[nm]])
                    for c0 in range(0, NCH, NW):
                        run_interleaved([chunk_gen(hp, seg, c0 + g, g) for g in range(NW)])
                    yo, yod = yo_r.next()
                    kb.op("pool", lambda e: e.tensor_tensor(out=U[10], in0=U[10], in1=U[9], op=ALU.add),
                          r=[Ud[10], Ud[9]], w=[Ud[10]])
                    kb.op("dve", lambda e: e.tensor_tensor(out=yo, in0=U[10], in1=U[5], op=ALU.mult),
                          r=[Ud[10], Ud[5]], w=[yod])
                    kb.dma("sp", self.yT[hp * 128:(hp + 1) * 128, seg * SEG:(seg + 1) * SEG], yo, r=[yod])

    def layer_norm_tile(self, z, zd, junk, jd, st, std, gb, gbd):
        kb = self.kb
        kb.op("act", lambda e: e.activation(out=junk, in_=z, func=AF.Square), r=[zd], w=[jd])
        kb.op("dve", lambda e: e.reduce_sum(out=st[:, 0:1], in_=z, axis=AX.X), r=[zd], w=[std])
        kb.op("dve", lambda e: e.reduce_sum(out=st[:, 1:2], in_=junk, axis=AX.X), r=[jd], w=[std])
        self.norm_stats(st, std, 1, D, LN_EPS)
        kb.op("dve", lambda e: e.tensor_scalar(out=z, in0=z, scalar1=st[:, 6:7], scalar2=st[:, 7:8],
                                               op0=ALU.mult, op1=ALU.add), r=[zd, std], w=[zd])
        kb.op("pool", lambda e: e.tensor_tensor(out=z, in0=z, in1=gb[:, 0, :], op=ALU.mult), r=[zd, gbd], w=[zd])
        kb.op("pool", lambda e: e.tensor_tensor(out=z, in0=z, in1=gb[:, 1, :], op=ALU.add), r=[zd, gbd], w=[zd])

    def layer_norm_tile_gen(self, z, zd, junk, jd, st, std, gb, gbd):
        kb = self.kb
        kb.op("act", lambda e: e.activation(out=junk, in_=z, func=AF.Square), r=[zd], w=[jd])
        kb.op("dve", lambda e: e.reduce_sum(out=st[:, 0:1], in_=z, axis=AX.X), r=[zd], w=[std])
        yield
        kb.op("dve", lambda e: e.reduce_sum(out=st[:, 1:2], in_=junk, axis=AX.X), r=[jd], w=[std])
        yield
        yield from self.norm_stats_gen(st, std, 1, D, LN_EPS)
        kb.op("dve", lambda e: e.tensor_scalar(out=z, in0=z, scalar1=st[:, 6:7], scalar2=st[:, 7:8],
                                               op0=ALU.mult, op1=ALU.add), r=[zd, std], w=[zd])
        yield
        kb.op("pool", lambda e: e.tensor_tensor(out=z, in0=z, in1=gb[:, 0, :], op=ALU.mult), r=[zd, gbd], w=[zd])
        yield
        kb.op("pool", lambda e: e.tensor_tensor(out=z, in0=z, in1=gb[:, 1, :], op=ALU.add), r=[zd, gbd], w=[zd])
        yield

    def load_ln_params(self, gb, gbd, l, i):
        kb = self.kb
        kb.dma("sp", gb[:, 0, :], self.I["ln_g"][l, i:i + 1, :].broadcast_to([128, D]), w=[gbd])
        kb.dma("sp", gb[:, 1, :], self.I["ln_b"][l, i:i + 1, :].broadcast_to([128, D]), w=[gbd])

    def outproj_ln1(self, l):
        nc, kb = self.nc, self.kb
        moe = (l % 2 == 1)
        j = l // 2
        with ExitStack() as es:
            sb = lambda n, sh, dt: es.enter_context(nc.sbuf_tensor(self.un(n), sh, dt))
            wo = sb("o_w", [128, 8, D], BF16)
            wod = Dep()
            kb.dma("pool", wo[:], self.I["w_out"][l].rearrange("(k p) n -> p k n", p=128), w=[wod])
            yt_r = Ring(sb("o_yt", [128, 2, 8, 512], BF16), 2)
            x_r = Ring(sb("o_x", [128, 2, D], F32), 2)
            z_r = Ring(sb("o_z", [128, 2, D], F32), 2)
            junk = sb("o_junk", [128, D], BF16)
            jd = Dep()
            st_r = Ring(sb("o_st", [128, 2, 8], F32), 2)
            gb = sb("o_gb", [128, 2, D], F32)
            gbd = Dep()
            self.load_ln_params(gb, gbd, l, 0)
            B = self.bank
            hb = BRing([(0, 1), (2, 3)])
            tr = self.tr_ring(((4, 5), (6, 7)))
            if moe:
                rt = sb("o_rt", [128, 8, N_EXPERTS], F32)
                rtd = Dep()
                kb.dma("sp", rt[:], self.I["moe_router"][j].rearrange("(k p) e -> p k e", p=128), w=[rtd], slow=True)
                xTf_r = Ring(sb("o_xTf", [128, 2, 8, 128], F32), 2)
                g_r = Ring(sb("o_g", [128, 2, 64], F32), 2)
            junk_r = Ring(sb("o_junk2", [128, 2, D], BF16), 2)

            def tile_gen(tb, tt, yt, ytd):
                t = tb * 4 + tt
                x, xd = x_r.next()
                z, zd = z_r.next()
                jk, jkd = junk_r.next()
                kb.dma("sp", x, self.xres[t * 128:(t + 1) * 128, :], w=[xd])
                ba, bb_ = hb.next()
                for half, bi in enumerate((ba, bb_)):
                    bk, bd = B[bi]
                    for k in range(8):
                        kb.op("pe", lambda e, k=k: e.matmul(bk[:, 0:512], lhsT=yt[:, k, tt * 128:(tt + 1) * 128],
                                                            rhs=wo[:, k, half * 512:(half + 1) * 512],
                                                            start=(k == 0), stop=(k == 7)),
                              r=[ytd, wod], w=[bd], mark=(k == 7))
                    hs = slice(half * 512, (half + 1) * 512)
                    kb.op("dve", lambda e: e.scalar_tensor_tensor(out=z[:, hs], in0=x[:, hs], scalar=float(ALPHA),
                                                                  in1=bk[:, 0:512], op0=ALU.mult, op1=ALU.add),
                          r=[xd, bd], w=[zd])
                yield
                st, std = st_r.next()
                yield from self.layer_norm_tile_gen(z, zd, jk, jkd, st, std, gb, gbd)
                kb.dma("sp", self.xres[t * 128:(t + 1) * 128, :], z, r=[zd, xd])
                if moe:
                    xTf, xTfd = xTf_r.next()
                    self.transpose_to_XT(z, zd, t, tr, f32_out=(xTf, xTfd))
                    yield
                    yield from self.router_gates_gen(xTf, xTfd, rt, rtd, t, g_r)
                else:
                    self.transpose_to_XT(z, zd, t, tr)
                    yield

            for tb in range(8):
                yt, ytd = yt_r.next()
                kb.dma("sp", yt, self.yT[:, tb * 512:(tb + 1) * 512].rearrange("(k p) t -> p k t", p=128), w=[ytd])
                for t0 in (0, 2):
                    run_interleaved([tile_gen(tb, t0 + g, yt, ytd) for g in range(2)])

    def router_gates_gen(self, xTf, xTfd, rt, rtd, t, g_r):
        kb = self.kb
        bk, bd = self.bank[0]
        lp = bk[:, 0:N_EXPERTS]
        for k in range(8):
            kb.op("pe", lambda e, k=k: e.matmul(lp, lhsT=xTf[:, k, :], rhs=rt[:, k, :], start=(k == 0), stop=(k == 7)),
                  r=[xTfd, rtd], w=[bd], mark=(k == 7))
        g, gd = g_r.next()
        c = lambda i: g[:, i * 8:(i + 1) * 8]
        sc = lambda i: g[:, 56 + i:57 + i]
        op = lambda fn: kb.op("dve", fn, r=[gd], w=[gd])
        kb.op("dve", lambda e: e.tensor_copy(out=c(0), in_=lp), r=[bd], w=[gd])
        yield
        op(lambda e: e.reduce_max(out=sc(0), in_=c(0), axis=AX.X))
        yield
        op(lambda e: e.tensor_scalar(out=c(1), in0=c(0), scalar1=sc(0), scalar2=None, op0=ALU.is_equal))
        yield
        op(lambda e: e.scalar_tensor_tensor(out=c(2), in0=c(1), scalar=-1e30, in1=c(0), op0=ALU.mult, op1=ALU.add))
        yield
        op(lambda e: e.reduce_max(out=sc(1), in_=c(2), axis=AX.X))
        yield
        op(lambda e: e.tensor_scalar(out=c(3), in0=c(0), scalar1=sc(1), scalar2=None, op0=ALU.is_ge))
        op(lambda e: e.tensor_scalar(out=sc(2), in0=sc(0), scalar1=-1.0, scalar2=None, op0=ALU.mult))
        yield
        kb.op("act", lambda e: e.activation(out=c(4), in_=c(0), func=AF.Exp, bias=sc(2), scale=1.0), r=[gd], w=[gd])
        yield
        op(lambda e: e.tensor_tensor(out=c(4), in0=c(4), in1=c(3), op=ALU.mult))
        yield
        op(lambda e: e.reduce_sum(out=sc(3), in_=c(4), axis=AX.X))
        yield
        op(lambda e: e.reciprocal(out=sc(4), in_=sc(3)))
        yield
        kb.op("dve", lambda e: e.tensor_scalar(out=self.gates[:, t, :], in0=c(4), scalar1=sc(4), scalar2=None, op0=ALU.mult),
              r=[gd], w=[self.gatesd])
        yield

    def ffn(self, l):
        nc, kb = self.nc, self.kb
        moe = (l % 2 == 1)
        j = l // 2
        last = (l == self.n_layers - 1)
        if moe:
            E, FF = N_EXPERTS, FF_EXPERT
            wg_a, wu_a, wd_a = self.I["moe_w_gate"][j], self.I["moe_w_up"][j], self.I["moe_w_down"][j]
        else:
            E, FF = 1, FF_DENSE
            wg_a, wu_a, wd_a = [self.I[k][j:j + 1] for k in ("ffn_w_gate", "ffn_w_up", "ffn_w_down")]
        G = 2
        ngrp = FF // (128 * G)
        HT = 2048
        with ExitStack() as es:
            sb = lambda n, sh, dt: es.enter_context(nc.sbuf_tensor(self.un(n), sh, dt))
            acc = sb("f_acc", [128, 16, D], F32)
            accd = [Dep() for _ in range(16)]
            wg_r = Ring(sb("f_wg", [128, 2, 8, G * 128], BF16), 2)
            wu_r = Ring(sb("f_wu", [128, 2, 8, G * 128], BF16), 2)
            wd_r = Ring(sb("f_wd", [128, 2, G, D], BF16), 2)
            sg_r = Ring(sb("f_sg", [128, 2, 512], F32), 2)
            x_r = Ring(sb("f_x", [128, 2, D], F32), 2)
            junk_r = Ring(sb("f_junk", [128, 2, D], BF16), 2)
            st_r = Ring(sb("f_st", [128, 2, 8], F32), 2)
            gb = sb("f_gb", [128, 2, D], F32)
            gbd = Dep()
            self.load_ln_params(gb, gbd, l, 1)
            B = self.bank
            pg_r = BRing([B[0], B[1]])
            pu_r = BRing([B[2], B[3]])
            po_r = BRing([B[4], B[5], B[6], B[7]])
            tr = self.tr_ring(((4, 5), (6, 7)))
            hT_r = Ring(sb("f_hT2", [128, 2, G, HT], BF16), 2)
            for half in range(S // HT):
                groups = [(e_, grp) for e_ in range(E) for grp in range(ngrp)]
                N = len(groups)
                wgu = {}
                wdw = {}
                hTs = {}

                def load_gu(m):
                    e_, grp = groups[m]
                    ff0 = grp * G * 128
                    wg, wgd = wg_r.next()
                    wu, wud = wu_r.next()
                    kb.dma("pool", wg, wg_a[e_][:, ff0:ff0 + G * 128].rearrange("(k p) n -> p k n", p=128), w=[wgd])
                    kb.dma("pool", wu, wu_a[e_][:, ff0:ff0 + G * 128].rearrange("(k p) n -> p k n", p=128), w=[wud])
                    wgu[m] = (wg, wgd, wu, wud)

                def load_d(m):
                    e_, grp = groups[m]
                    ff0 = grp * G * 128
                    wdn, wdd = wd_r.next()
                    kb.dma("pool", wdn, wd_a[e_][ff0:ff0 + G * 128, :].rearrange("(g p) n -> p g n", p=128), w=[wdd])
                    wdw[m] = (wdn, wdd)

                def gu_units(m):
                    wg, wgd, wu, wud = wgu[m]
                    hT, hTd = hT_r.next()
                    hTs[m] = (hT, hTd)
                    us = []
                    for gi in range(G):
                        for tb in range(HT // 512):
                            def u(gi=gi, tb=tb):
                                tok = slice(half * HT + tb * 512, half * HT + (tb + 1) * 512)
                                xd_blk = self.XTd[(half * HT) // 512 + tb]
                                pg, pgd = pg_r.next()
                                pu, pud = pu_r.next()
                                for k in range(8):
                                    kb.op("pe", lambda e, k=k: e.matmul(pg, lhsT=wg[:, k, gi * 128:(gi + 1) * 128],
                                                                        rhs=self.XT[:, k, tok], start=(k == 0), stop=(k == 7)),
                                          r=[wgd, xd_blk], w=[pgd], mark=(k == 7))
                                for k in range(8):
                                    kb.op("pe", lambda e, k=k: e.matmul(pu, lhsT=wu[:, k, gi * 128:(gi + 1) * 128],
                                                                        rhs=self.XT[:, k, tok], start=(k == 0), stop=(k == 7)),
                                          r=[wud, xd_blk], w=[pud], mark=(k == 7))
                                sg, sgd = sg_r.next()
                                kb.op("act", lambda e: e.activation(out=sg, in_=pg, func=AF.Silu), r=[pgd], w=[sgd])
                                kb.op("dve", lambda e: e.tensor_tensor(out=hT[:, gi, tb * 512:(tb + 1) * 512], in0=pu, in1=sg,
                                                                       op=ALU.mult), r=[pud, sgd], w=[hTd])
                            us.append(u)
                    return us

                def dn_units(m):
                    e_, grp = groups[m]
                    wdn, wdd = wdw[m]
                    hT, hTd = hTs[m]
                    first = (m == 0)
                    us = []
                    for tt in range(HT // 128):
                        for hf in range(2):
                            def u(tt=tt, hf=hf):
                                t = half * (HT // 128) + tt
                                po, pod = po_r.next()
                                for gi in range(G):
                                    kb.op("pe", lambda e, gi=gi: e.matmul(po, lhsT=hT[:, gi, tt * 128:(tt + 1) * 128],
                                                                          rhs=wdn[:, gi, hf * 512:(hf + 1) * 512],
                                                                          start=(gi == 0), stop=(gi == G - 1)),
                                          r=[hTd, wdd], w=[pod], mark=(gi == G - 1))
                                a = acc[:, tt, hf * 512:(hf + 1) * 512]
                                if moe:
                                    gsc = self.gates[:, t, e_:e_ + 1]
                                    if first:
                                        kb.op("dve", lambda e: e.tensor_scalar(out=a, in0=po, scalar1=gsc, scalar2=None,
                                                                               op0=ALU.mult), r=[pod, self.gatesd], w=[accd[tt]])
                                    else:
                                        kb.op("dve", lambda e: e.scalar_tensor_tensor(out=a, in0=po, scalar=gsc, in1=a,
                                                                                      op0=ALU.mult, op1=ALU.add),
                                              r=[pod, self.gatesd, accd[tt]], w=[accd[tt]])
                                else:
                                    if first:
                                        kb.op("act", lambda e: e.copy(out=a, in_=po), r=[pod], w=[accd[tt]])
                                    else:
                                        kb.op("dve", lambda e: e.tensor_tensor(out=a, in0=po, in1=a, op=ALU.add),
                                              r=[pod, accd[tt]], w=[accd[tt]])
                            us.append(u)
                    return us

                load_gu(0)
                load_d(0)
                if N > 1:
                    load_gu(1)
                for u in gu_units(0):
                    u()
                for n in range(N):
                    if n + 1 < N:
                        load_d(n + 1)
                    if n + 2 < N:
                        load_gu(n + 2)
                    dn = dn_units(n)
                    gu = gu_units(n + 1) if n + 1 < N else []
                    per = len(dn) // max(len(gu), 1)
                    di = 0
                    for gi_, u in enumerate(gu):
                        u()
                        for _ in range(per):
                            dn[di]()
                            di += 1
                    while di < len(dn):
                        dn[di]()
                        di += 1
                def ep_gen(tt):
                    t = half * (HT // 128) + tt
                    x, xd = x_r.next()
                    jk, jkd = junk_r.next()
                    kb.dma("sp", x, self.xres[t * 128:(t + 1) * 128, :], w=[xd])
                    z = acc[:, tt, :]
                    zd = accd[tt]
                    kb.op("dve", lambda e: e.scalar_tensor_tensor(out=z, in0=x, scalar=float(ALPHA), in1=z,
                                                                  op0=ALU.mult, op1=ALU.add), r=[xd, zd], w=[zd])
                    yield
                    st, std = st_r.next()
                    yield from self.layer_norm_tile_gen(z, zd, jk, jkd, st, std, gb, gbd)
                    if last:
                        kb.dma("sp", self.out[t * 128:(t + 1) * 128, :], z, r=[zd])
                    else:
                        kb.dma("sp", self.xres[t * 128:(t + 1) * 128, :], z, r=[zd, xd])
                        self.transpose_to_XT(z, zd, t, tr)
                    yield

                for t0 in range(0, HT // 128, 2):
                    run_interleaved([ep_gen(t0 + g) for g in range(2)])


WEIGHT_KEYS = ("w_in", "w_out", "rwkv_mu", "rwkv_w0", "rwkv_w_up", "rwkv_a0", "rwkv_a_up", "rwkv_g_up",
               "rwkv_k_k", "rwkv_k_a", "rwkv_r_k", "rwkv_ln_g", "rwkv_ln_b", "ret_gn_g", "ret_gn_b",
               "ln_g", "ln_b", "ffn_w_gate", "ffn_w_up", "ffn_w_down", "moe_router", "moe_w_gate",
               "moe_w_up", "moe_w_down")


def make_in_map(inputs, b, consts, shared=None):
    m = {}
    m["x"] = np.ascontiguousarray(np.asarray(inputs["x"])[b], dtype=np.float32)
    if shared is None:
        shared = make_shared(inputs, consts)
    m.update(shared)
    return m


def make_shared(inputs, consts):
    sh = {}
    for k in WEIGHT_KEYS:
        sh[k] = np.ascontiguousarray(np.asarray(inputs[k]), dtype=np.float32)
    rb = np.asarray(inputs["rel_bias"], dtype=np.float32)
    idx = bias_bucket_index()
    g = rb[idx]
    sh["biasg"] = np.ascontiguousarray(np.transpose(g, (0, 4, 1, 2, 3)))
    sh["cst"] = consts["cst"]
    sh["cos2"] = consts["cos2"]
    sh["sin2"] = consts["sin2"]
    sh["amask"] = consts["amask"]
    return sh


_PROG = None


def kernel(**inputs):
    global _PROG
    if _PROG is None:
        p = Prog()
        p.build()
        _PROG = p
    p = _PROG
    shared = make_shared(inputs, p.consts)
    in_maps = [make_in_map(inputs, b, p.consts, shared) for b in range(8)]
    res = run_bass_kernel_spmd(p.nc, in_maps, core_ids=list(range(8)))
    out = np.stack([np.asarray(r["out"], dtype=np.float32) for r in res.results], 0)
    return out
```
